# Optimizing a Trainium2 kernel written in Bass

```python
import math
import jax, jax.numpy as jnp
from jax import lax
import numpy as np

D_MODEL = 1024
BATCH = 8
SEQ = 4096
DEPTH = 4

N_MIXERS = 3
N_A = (DEPTH + 2) // 3
N_B = (DEPTH + 1) // 3
N_C = DEPTH // 3

DN_ALPHA = (2.0 * DEPTH) ** 0.25
DN_BETA = (8.0 * DEPTH) ** -0.25
LN_EPS = 1e-5

A_HEADS = 16
A_HEAD_DIM = D_MODEL // A_HEADS
A_BRANCHES = ((128, 1), (512, 4), (2048, 16))
A_BLOCK = 128
A_ROT_DIM = A_HEAD_DIM // 4
ROPE_THETA = 500000.0

B_HEADS = 8
B_QK_DIM = D_MODEL // (2 * B_HEADS)
B_V_DIM = D_MODEL // B_HEADS
B_QK_W = 2 * B_HEADS * B_QK_DIM
B_CONV = 4
B_CHUNK = 64
B_IN = B_QK_W + 2 * D_MODEL + 2 * B_HEADS

C_HEADS = 4
C_QK_DIM = D_MODEL // C_HEADS
C_V_DIM = 2 * D_MODEL // C_HEADS
C_CHUNK = 64
C_THETA = 10000.0
C_IN = 2 * D_MODEL + 4 * D_MODEL

MOE_GROUPS = 8
MOE_PER_GROUP = 8
MOE_EXPERTS = MOE_GROUPS * MOE_PER_GROUP
MOE_TOPK = 2
MOE_HIDDEN = D_MODEL // 4
MOE_BLOCK = 128

kernel_name = "hybrid_dilated_mlstm_retention_hmoe"


def layer_norm(x, g, b):
    xf = x.astype(jnp.float32)
    mu = jnp.mean(xf, -1, keepdims=True)
    var = jnp.mean(jnp.square(xf - mu), -1, keepdims=True)
    return ((xf - mu) * lax.rsqrt(var + LN_EPS) * g + b).astype(x.dtype)


def head_norm(h, g):
    mu = jnp.mean(h, -1, keepdims=True)
    var = jnp.mean(jnp.square(h - mu), -1, keepdims=True)
    return (h - mu) * lax.rsqrt(var + LN_EPS) * g


def rope_tables(positions, dim, theta):
    inv = theta ** (-jnp.arange(0, dim, 2, dtype=jnp.float32) / dim)
    ang = positions.astype(jnp.float32)[:, None] * inv[None, :]
    return jnp.cos(ang), jnp.sin(ang)


def apply_rope(t, cos, sin):
    half = t.shape[-1] // 2
    c, s = cos[:, None, :], sin[:, None, :]
    t1, t2 = t[..., :half], t[..., half:]
    return jnp.concatenate([t1 * c - t2 * s, t2 * c + t1 * s], -1)


def dilated_branch(q, k, v, dilation, n_back):
    b, s, h, dh = q.shape
    L = s // dilation
    nb = -(-L // A_BLOCK)
    Lp = nb * A_BLOCK

    def split(t):
        t = t.reshape(b, L, dilation, h, dh).transpose(0, 2, 3, 1, 4)
        t = jnp.pad(t, ((0, 0), (0, 0), (0, 0), (0, Lp - L), (0, 0)))
        return t.reshape(b, dilation, h, nb, A_BLOCK, dh)

    def with_prev(tb):
        prev = jnp.concatenate([jnp.zeros_like(tb[:, :, :, :1]), tb[:, :, :, :-1]], axis=3)
        return jnp.concatenate([prev, tb], axis=4)

    qb = split(q)
    kb = with_prev(split(k))
    vb = with_prev(split(v))
    scores = jnp.einsum('brhnqd,brhnkd->brhnqk', qb, kb)
    qi = jnp.arange(A_BLOCK)[:, None]
    ki = jnp.arange(2 * A_BLOCK)[None, :]
    rel = qi + A_BLOCK - ki
    kpos = jnp.arange(nb)[:, None, None] * A_BLOCK - A_BLOCK + ki[None]
    mask = (rel >= 0) & (rel <= n_back) & (kpos >= 0)
    scores = jnp.where(mask, scores, -jnp.inf)
    m = jnp.max(scores, -1, keepdims=True)
    p = jnp.exp(scores - m)
    den = jnp.sum(p, -1, keepdims=True)
    o = jnp.einsum('brhnqk,brhnkd->brhnqd', p, vb) / den
    lse = (m + jnp.log(den))[..., 0]
    o = o.reshape(b, dilation, h, Lp, dh)[:, :, :, :L].transpose(0, 3, 1, 2, 4).reshape(b, s, h, dh)
    lse = lse.reshape(b, dilation, h, Lp)[..., :L].transpose(0, 3, 1, 2).reshape(b, s, h)
    return o, lse


def mixer_dilated(x, w_in, w_out, cos, sin):
    b, s, _ = x.shape
    qkv = (x @ w_in).astype(jnp.float32).reshape(b, s, 3, A_HEADS, A_HEAD_DIM)
    q, k, v = qkv[:, :, 0], qkv[:, :, 1], qkv[:, :, 2]
    q = jnp.concatenate([apply_rope(q[..., :A_ROT_DIM], cos, sin), q[..., A_ROT_DIM:]], -1)
    k = jnp.concatenate([apply_rope(k[..., :A_ROT_DIM], cos, sin), k[..., A_ROT_DIM:]], -1)
    q = q * (A_HEAD_DIM ** -0.5)
    outs, lses = [], []
    for window, dilation in A_BRANCHES:
        o_g, lse_g = dilated_branch(q, k, v, dilation, window // dilation)
        outs.append(o_g)
        lses.append(lse_g)
    wts = jax.nn.softmax(jnp.stack(lses, 0), axis=0)
    o = jnp.einsum('gbsh,gbshd->bshd', wts, jnp.stack(outs, 0))
    return o.reshape(b, s, D_MODEL).astype(x.dtype) @ w_out


def causal_depthwise_conv(u, w, bias):
    kw, c = w.shape
    y = lax.conv_general_dilated(u, w[:, None, :].astype(u.dtype), window_strides=(1,),
                                 padding=[(kw - 1, 0)], dimension_numbers=('NWC', 'WIO', 'NWC'),
                                 feature_group_count=c)
    return y + bias.astype(u.dtype)


def mlstm_chunkwise(q, k, v, i_pre, logf):
    b, h, s, dk = q.shape
    dv = v.shape[-1]
    L = B_CHUNK
    nc = s // L
    def chunks(t):
        return jnp.moveaxis(t.reshape(b, h, nc, L, *t.shape[3:]), 2, 0)
    causal = jnp.tril(jnp.ones((L, L), dtype=bool))

    def step(carry, inp):
        C, n, m = carry
        qj, kj, vj, ij, fj = inp
        a = jnp.cumsum(fj, axis=-1)
        dmat = jnp.where(causal, a[..., :, None] - a[..., None, :] + ij[..., None, :], -jnp.inf)
        inter = a + m[..., None]
        m_t = jnp.maximum(inter, jnp.max(dmat, -1))
        sc = jnp.einsum('bhtd,bhsd->bhts', qj, kj) * jnp.exp(dmat - m_t[..., None])
        g_inter = jnp.exp(inter - m_t)
        num = jnp.einsum('bhts,bhsv->bhtv', sc, vj) + g_inter[..., None] * jnp.einsum('bhtd,bhdv->bhtv', qj, C)
        den = jnp.sum(sc, -1) + g_inter * jnp.einsum('bhtd,bhd->bht', qj, n)
        h_out = num / jnp.maximum(jnp.abs(den), jnp.exp(-m_t))[..., None]
        a_end = a[..., -1]
        w_s = a_end[..., None] - a + ij
        m_new = jnp.maximum(a_end + m, jnp.max(w_s, -1))
        decay = jnp.exp(a_end + m - m_new)
        ws = jnp.exp(w_s - m_new[..., None])
        C_new = decay[..., None, None] * C + jnp.einsum('bhs,bhsd,bhsv->bhdv', ws, kj, vj)
        n_new = decay[..., None] * n + jnp.einsum('bhs,bhsd->bhd', ws, kj)
        return (C_new, n_new, m_new), h_out

    init = (jnp.zeros((b, h, dk, dv), jnp.float32), jnp.zeros((b, h, dk), jnp.float32),
            jnp.zeros((b, h), jnp.float32))
    _, hs = lax.scan(step, init, (chunks(q), chunks(k), chunks(v), chunks(i_pre), chunks(logf)))
    return hs.transpose(1, 0, 3, 2, 4).reshape(b, s, h, dv)


def mixer_mlstm(x, w_in, gate_bias, conv_w, conv_b, norm_g, w_out):
    b, s, _ = x.shape
    proj = x @ w_in
    qk = proj[..., :B_QK_W]
    v = proj[..., B_QK_W:B_QK_W + D_MODEL]
    o = proj[..., B_QK_W + D_MODEL:B_QK_W + 2 * D_MODEL]
    gates = proj[..., B_QK_W + 2 * D_MODEL:].astype(jnp.float32) + gate_bias
    qk = jax.nn.silu(causal_depthwise_conv(qk, conv_w, conv_b)).astype(jnp.float32)
    half = B_QK_W // 2
    q = qk[..., :half].reshape(b, s, B_HEADS, B_QK_DIM).transpose(0, 2, 1, 3)
    k = qk[..., half:].reshape(b, s, B_HEADS, B_QK_DIM).transpose(0, 2, 1, 3) * (B_QK_DIM ** -0.5)
    v = v.astype(jnp.float32).reshape(b, s, B_HEADS, B_V_DIM).transpose(0, 2, 1, 3)
    i_pre = gates[..., :B_HEADS].transpose(0, 2, 1)
    logf = jax.nn.log_sigmoid(gates[..., B_HEADS:]).transpose(0, 2, 1)
    hh = mlstm_chunkwise(q, k, v, i_pre, logf)
    hh = head_norm(hh, norm_g.reshape(B_HEADS, B_V_DIM)).reshape(b, s, D_MODEL)
    out = hh * jax.nn.sigmoid(o.astype(jnp.float32))
    return out.astype(x.dtype) @ w_out


def retention_chunkwise(q, k, v):
    b, h, s, dk = q.shape
    dv = v.shape[-1]
    L = C_CHUNK
    nc = s // L
    log_gamma = jnp.log(1.0 - 2.0 ** (-5.0 - jnp.arange(h, dtype=jnp.float32)))
    idx = jnp.arange(L, dtype=jnp.float32)
    rel = idx[:, None] - idx[None, :]
    dmask = jnp.where(rel >= 0, jnp.exp(jnp.maximum(rel, 0.0)[None] * log_gamma[:, None, None]), 0.0)
    xi = jnp.exp((idx + 1.0)[None] * log_gamma[:, None])
    zeta = jnp.exp((L - 1.0 - idx)[None] * log_gamma[:, None])
    chunk_decay = jnp.exp(L * log_gamma)
    def chunks(t):
        return jnp.moveaxis(t.reshape(b, h, nc, L, t.shape[-1]), 2, 0)

    def step(R, inp):
        qj, kj, vj = inp
        sc = jnp.einsum('bhtd,bhsd->bhts', qj, kj) * dmask
        out = jnp.einsum('bhts,bhsv->bhtv', sc, vj) + xi[..., None] * jnp.einsum('bhtd,bhdv->bhtv', qj, R)
        R = chunk_decay[:, None, None] * R + jnp.einsum('bhsd,bhsv->bhdv', kj * zeta[..., None], vj)
        return R, out

    _, outs = lax.scan(step, jnp.zeros((b, h, dk, dv), jnp.float32), (chunks(q), chunks(k), chunks(v)))
    return outs.transpose(1, 0, 3, 2, 4).reshape(b, s, h, dv)


def mixer_retention(x, w_in, norm_g, w_out, cos, sin):
    b, s, _ = x.shape
    proj = x @ w_in
    q = proj[..., :D_MODEL].astype(jnp.float32).reshape(b, s, C_HEADS, C_QK_DIM)
    k = proj[..., D_MODEL:2 * D_MODEL].astype(jnp.float32).reshape(b, s, C_HEADS, C_QK_DIM)
    v = proj[..., 2 * D_MODEL:4 * D_MODEL].astype(jnp.float32).reshape(b, s, C_HEADS, C_V_DIM)
    g = proj[..., 4 * D_MODEL:].astype(jnp.float32)
    q = apply_rope(q, cos, sin).transpose(0, 2, 1, 3)
    k = (apply_rope(k, cos, sin) * (C_QK_DIM ** -0.5)).transpose(0, 2, 1, 3)
    v = v.transpose(0, 2, 1, 3)
    o = retention_chunkwise(q, k, v)
    o = head_norm(o, norm_g.reshape(C_HEADS, C_V_DIM)).reshape(b, s, 2 * D_MODEL)
    o = o * jax.nn.silu(g)
    return o.astype(x.dtype) @ w_out


def moe_ffn(x, wg_r, bg_r, we_r, be_r, w_gate, w_up, w_down):
    b, s, d = x.shape
    xt = x.reshape(-1, d)
    n = xt.shape[0]
    g_logits = (xt @ wg_r).astype(jnp.float32) + bg_r
    grp = jnp.argmax(g_logits, -1)
    p_grp = jnp.take_along_axis(jax.nn.softmax(g_logits, -1), grp[:, None], -1)[:, 0]
    e_logits = ((xt @ we_r).astype(jnp.float32) + be_r).reshape(n, MOE_GROUPS, MOE_PER_GROUP)
    e_in = jnp.take_along_axis(e_logits, grp[:, None, None], axis=1)[:, 0]
    top_v, top_i = lax.top_k(e_in, MOE_TOPK)
    gates = jax.nn.softmax(top_v, -1) * p_grp[:, None]
    eid = (grp[:, None] * MOE_PER_GROUP + top_i).reshape(-1).astype(jnp.int32)
    tok = jnp.repeat(jnp.arange(n, dtype=jnp.int32), MOE_TOPK)
    order = jnp.argsort(eid)
    eid_s, tok_s, gate_s = eid[order], tok[order], gates.reshape(-1)[order]
    counts = jnp.bincount(eid, length=MOE_EXPERTS)
    starts = jnp.cumsum(counts) - counts
    padded = (counts + MOE_BLOCK - 1) // MOE_BLOCK * MOE_BLOCK
    pends = jnp.cumsum(padded)
    pstarts = pends - padded
    n_assign = n * MOE_TOPK
    dest = pstarts[eid_s] + (jnp.arange(n_assign) - starts[eid_s])
    P = n_assign + MOE_EXPERTS * MOE_BLOCK
    nb = P // MOE_BLOCK
    buf_tok = jnp.full((P,), n, jnp.int32).at[dest].set(tok_s)
    x_pad = jnp.concatenate([xt, jnp.zeros((1, d), xt.dtype)], 0)
    xb = x_pad[buf_tok].reshape(nb, MOE_BLOCK, d)
    blk_e = jnp.minimum(jnp.searchsorted(pends, jnp.arange(nb) * MOE_BLOCK, side='right'), MOE_EXPERTS - 1)

    def expert_block(args):
        xblk, e = args
        return (jax.nn.silu(xblk @ w_gate[e]) * (xblk @ w_up[e])) @ w_down[e]

    yb = lax.map(expert_block, (xb, blk_e)).reshape(P, d)
    y = yb[dest] * gate_s[:, None].astype(x.dtype)
    out = jnp.zeros((n, d), x.dtype).at[tok_s].add(y)
    return out.reshape(b, s, d)


def setup_inputs(seed: int = 0) -> dict:
    key = jax.random.key(seed)
    ks = jax.random.split(key, 26)
    f32 = jnp.float32
    D = D_MODEL
    def nrm(k, shape, scale):
        return jax.random.normal(k, shape, f32) * scale
    return {
        "x": nrm(ks[0], (BATCH, SEQ, D), 1.0),
        "positions": jnp.arange(SEQ, dtype=jnp.int32),
        "ln1_g": 1.0 + nrm(ks[1], (DEPTH, D), 0.02),
        "ln1_b": nrm(ks[2], (DEPTH, D), 0.02),
        "ln2_g": 1.0 + nrm(ks[3], (DEPTH, D), 0.02),
        "ln2_b": nrm(ks[4], (DEPTH, D), 0.02),
        "a_w_in": nrm(ks[5], (N_A, D, 3 * D), D ** -0.5),
        "a_w_out": nrm(ks[6], (N_A, D, D), DN_BETA * D ** -0.5),
        "b_w_in": nrm(ks[7], (N_B, D, B_IN), D ** -0.5),
        "b_gate_bias": jnp.concatenate([nrm(ks[8], (N_B, B_HEADS), 0.1),
                                        jnp.broadcast_to(jnp.linspace(3.0, 6.0, B_HEADS, dtype=f32), (N_B, B_HEADS))
                                        + nrm(ks[9], (N_B, B_HEADS), 0.1)], -1),
        "b_conv_w": nrm(ks[10], (N_B, B_CONV, B_QK_W), B_CONV ** -0.5),
        "b_conv_b": nrm(ks[11], (N_B, B_QK_W), 0.02),
        "b_norm_g": 1.0 + nrm(ks[12], (N_B, D), 0.02),
        "b_w_out": nrm(ks[13], (N_B, D, D), DN_BETA * D ** -0.5),
        "c_w_in": nrm(ks[14], (N_C, D, C_IN), D ** -0.5),
        "c_norm_g": 1.0 + nrm(ks[15], (N_C, 2 * D), 0.02),
        "c_w_out": nrm(ks[16], (N_C, 2 * D, D), DN_BETA * (2 * D) ** -0.5),
        "r_group_w": nrm(ks[17], (DEPTH, D, MOE_GROUPS), D ** -0.5),
        "r_group_b": nrm(ks[18], (DEPTH, MOE_GROUPS), 0.01),
        "r_expert_w": nrm(ks[19], (DEPTH, D, MOE_EXPERTS), D ** -0.5),
        "r_expert_b": nrm(ks[20], (DEPTH, MOE_EXPERTS), 0.01),
        "e_w_gate": nrm(ks[21], (DEPTH, MOE_EXPERTS, D, MOE_HIDDEN), D ** -0.5),
        "e_w_up": nrm(ks[22], (DEPTH, MOE_EXPERTS, D, MOE_HIDDEN), D ** -0.5),
        "e_w_down": nrm(ks[23], (DEPTH, MOE_EXPERTS, MOE_HIDDEN, D), DN_BETA * MOE_HIDDEN ** -0.5),
    }


def reference(x, positions, ln1_g, ln1_b, ln2_g, ln2_b, a_w_in, a_w_out, b_w_in, b_gate_bias,
              b_conv_w, b_conv_b, b_norm_g, b_w_out, c_w_in, c_norm_g, c_w_out, r_group_w,
              r_group_b, r_expert_w, r_expert_b, e_w_gate, e_w_up, e_w_down):
    cos_a, sin_a = rope_tables(positions, A_ROT_DIM, ROPE_THETA)
    cos_c, sin_c = rope_tables(positions, C_QK_DIM, C_THETA)
    h = x
    for i in range(DEPTH):
        kind, j = i % N_MIXERS, i // N_MIXERS
        if kind == 0:
            y = mixer_dilated(h, a_w_in[j], a_w_out[j], cos_a, sin_a)
        elif kind == 1:
            y = mixer_mlstm(h, b_w_in[j], b_gate_bias[j], b_conv_w[j], b_conv_b[j], b_norm_g[j], b_w_out[j])
        else:
            y = mixer_retention(h, c_w_in[j], c_norm_g[j], c_w_out[j], cos_c, sin_c)
        h = layer_norm(DN_ALPHA * h + y, ln1_g[i], ln1_b[i])
        y = moe_ffn(h, r_group_w[i], r_group_b[i], r_expert_w[i], r_expert_b[i],
                    e_w_gate[i], e_w_up[i], e_w_down[i])
        h = layer_norm(DN_ALPHA * h + y, ln2_g[i], ln2_b[i])
    return h
```

```python
import math
import numpy as np
import concourse.bass as bass
import concourse.mybir as mybir
from concourse.bass_utils import run_bass_kernel_spmd
from contextlib import ExitStack

F32 = mybir.dt.float32
BF16 = mybir.dt.bfloat16
I32 = mybir.dt.int32
ALU = mybir.AluOpType
AF = mybir.ActivationFunctionType
AX = mybir.AxisListType

S = 4096
D = 1024
DEPTH = 4
NT = S // 128
ALPHA = (2.0 * DEPTH) ** 0.25
LN_EPS = 1e-5
TWO_PI = 2.0 * math.pi

SEM_WRAP = 30000
ENGS = ("pe", "act", "dve", "pool", "sp")


class Res:
    __slots__ = ("w", "r", "name", "excl")

    def __init__(self, name="", excl=False):
        self.w = None
        self.r = []
        self.name = name
        self.excl = excl


class T:
    __slots__ = ("t", "res")

    def __init__(self, t, res):
        self.t = t
        self.res = res

    def __getitem__(self, k):
        return self.t[k]


class Sched:
    def __init__(self, nc, es):
        self.nc = nc
        self.es = es
        self.sems = {}
        self.cur = {}
        self.waited = {e: {} for e in ENGS}
        self.pending = {e: {} for e in ENGS}
        self.dma_pool = {}
        self.dma_rr = {}
        self.n_sem = 0
        self.n_tiles = 0
        self.ninst = 0
        self.finals = []
        self.eobj = {"pe": nc.tensor, "act": nc.scalar, "dve": nc.vector, "pool": nc.gpsimd, "sp": nc.sync}
        self.phase_es = None

    def _newsem(self, name):
        h = self.es.enter_context(self.nc.semaphore(f"{name}_{self.n_sem}"))
        self.n_sem += 1
        key = self.n_sem
        self.sems[key] = h
        return key

    def sb(self, shape, dtype, name=None):
        self.n_tiles += 1
        name = name or "t"
        es = self.phase_es or self.es
        t = es.enter_context(self.nc.sbuf_tensor(f"{name}_{self.n_tiles}", list(shape), dtype))
        return T(t, Res(name))

    def ps(self, shape, dtype, name=None):
        self.n_tiles += 1
        name = name or "p"
        es = self.phase_es or self.es
        t = es.enter_context(self.nc.psum_tensor(f"{name}_{self.n_tiles}", list(shape), dtype))
        return T(t, Res(name, excl=True))

    def begin_phase(self):
        self.phase_es = ExitStack()
        return self.phase_es

    def end_phase(self):
        snap = {}
        for e, c in self.cur.items():
            snap[c[0]] = c[1]
        for q, slots in self.dma_pool.items():
            for sl in slots:
                if sl[1] > 0:
                    snap[sl[0]] = sl[1]
        for e in ENGS:
            p = self.pending[e]
            for k, v in snap.items():
                if p.get(k, 0) < v:
                    p[k] = v
        self.phase_es.close()
        self.phase_es = None

    def _engine_event(self, eng):
        if eng not in self.cur or self.cur[eng][1] >= SEM_WRAP:
            self.cur[eng] = [self._newsem(eng), 0]
        c = self.cur[eng]
        c[1] += 1
        return (c[0], c[1])

    def _dma_event(self, q):
        if q not in self.dma_pool:
            self.dma_pool[q] = [[self._newsem(f"dma{q}"), 0] for _ in range(8)]
            self.dma_rr[q] = 0
        i = self.dma_rr[q]
        self.dma_rr[q] = (i + 1) % len(self.dma_pool[q])
        slot = self.dma_pool[q][i]
        extra = (slot[0], slot[1]) if slot[1] > 0 else None
        if slot[1] + 16 > SEM_WRAP:
            slot[0] = self._newsem(f"dma{q}")
            slot[1] = 0
        slot[1] += 16
        return (slot[0], slot[1]), extra

    def _gather(self, eng, reads, writes):
        deps = self.pending[eng]
        self.pending[eng] = {}

        def add(ev):
            if ev is None:
                return
            k, v = ev
            if deps.get(k, 0) < v:
                deps[k] = v
        for r in reads:
            r = r.res if isinstance(r, T) else r
            add(r.w)
            if r.excl:
                for ev in r.r:
                    add(ev)
        for w in writes:
            w = w.res if isinstance(w, T) else w
            add(w.w)
            for ev in w.r:
                add(ev)
        return deps

    def _commit(self, ev, reads, writes):
        for r in reads:
            r = r.res if isinstance(r, T) else r
            r.r.append(ev)
            if len(r.r) > 48:
                d = {}
                for k, v in r.r:
                    if d.get(k, 0) < v:
                        d[k] = v
                r.r = list(d.items())
        for w in writes:
            w = w.res if isinstance(w, T) else w
            w.w = ev
            w.r = []

    def _prune(self, eng, deps):
        out = []
        wd = self.waited[eng]
        own = self.cur.get(eng, [None])[0] if eng == "pe" else None
        for k, v in deps.items():
            if k == own:
                continue
            if wd.get(k, 0) < v:
                wd[k] = v
                out.append((k, v))
        return out

    def _emit(self, eng, waits, fn, ev, inc):
        e = self.eobj[eng]
        for k, v in waits:
            e.wait_ge(self.sems[k], v)
        ins = fn(e)
        ins.then_inc(self.sems[ev[0]], inc)
        self.ninst += 1

    def op(self, eng, fn, reads=(), writes=()):
        deps = self._gather(eng, reads, writes)
        waits = self._prune(eng, deps)
        ev = self._engine_event(eng)
        self._commit(ev, reads, writes)
        self._emit(eng, waits, fn, ev, 1)

    def dma(self, q, fn, reads=(), writes=(), final=False):
        deps = self._gather(q, reads, writes)
        ev, extra = self._dma_event(q)
        if extra is not None:
            k, v = extra
            if deps.get(k, 0) < v:
                deps[k] = v
        waits = self._prune(q, deps)
        self._commit(ev, reads, writes)
        self._emit(q, waits, fn, ev, 16)
        if final:
            self.finals.append(ev)

    def finish(self):
        deps = {}
        for k, v in self.finals:
            if deps.get(k, 0) < v:
                deps[k] = v
        for q, slots in self.dma_pool.items():
            for sl in slots:
                if sl[1] > 0 and deps.get(sl[0], 0) < sl[1]:
                    deps[sl[0]] = sl[1]
        for k, v in deps.items():
            self.nc.sync.wait_ge(self.sems[k], v)


def mm(s, out_t, out_ap, lhsT_t, lhsT_ap, rhs_t, rhs_ap, start, stop):
    s.op("pe", lambda e: e.matmul(out_ap, lhsT=lhsT_ap, rhs=rhs_ap, start=start, stop=stop),
         reads=[lhsT_t, rhs_t], writes=[out_t])


def tt(s, eng, out_t, out_ap, a_t, a_ap, b_t, b_ap, op):
    s.op(eng, lambda e: e.tensor_tensor(out=out_ap, in0=a_ap, in1=b_ap, op=op), reads=[a_t, b_t], writes=[out_t])


def ts(s, eng, out_t, out_ap, a_t, a_ap, s1, s2, op0, op1=None, extra_reads=()):
    if op1 is None:
        s.op(eng, lambda e: e.tensor_scalar(out=out_ap, in0=a_ap, scalar1=s1, scalar2=None, op0=op0),
             reads=[a_t, *extra_reads], writes=[out_t])
    else:
        s.op(eng, lambda e: e.tensor_scalar(out=out_ap, in0=a_ap, scalar1=s1, scalar2=s2, op0=op0, op1=op1),
             reads=[a_t, *extra_reads], writes=[out_t])


def stt(s, eng, out_t, out_ap, a_t, a_ap, scalar, b_t, b_ap, op0, op1, extra_reads=()):
    s.op(eng, lambda e: e.scalar_tensor_tensor(out=out_ap, in0=a_ap, scalar=scalar, in1=b_ap, op0=op0, op1=op1),
         reads=[a_t, b_t, *extra_reads], writes=[out_t])


def cp(s, eng, out_t, out_ap, a_t, a_ap):
    if eng == "act":
        s.op("act", lambda e: e.activation(out=out_ap, in_=a_ap, func=AF.Copy), reads=[a_t], writes=[out_t])
    else:
        s.op(eng, lambda e: e.tensor_copy(out=out_ap, in_=a_ap), reads=[a_t], writes=[out_t])


def act(s, out_t, out_ap, a_t, a_ap, func, scale=1.0, bias=0.0, extra_reads=()):
    s.op("act", lambda e: e.activation(out=out_ap, in_=a_ap, func=func, bias=bias, scale=scale),
         reads=[a_t, *extra_reads], writes=[out_t])


def ld(s, q, out_t, out_ap, src_res, src_ap):
    s.dma(q, lambda e: e.dma_start(out=out_ap, in_=src_ap), reads=[src_res], writes=[out_t])


def st(s, q, dst_res, dst_ap, in_t, in_ap, final=False):
    s.dma(q, lambda e: e.dma_start(out=dst_ap, in_=in_ap), reads=[in_t], writes=[dst_res], final=final)


class Ctx:
    pass


def wload(s, cx, dst_t, dst_ap, wres, src_ap, shape):
    stg = cx.stg[cx.stg_i % 2]
    cx.stg_i += 1
    n = int(np.prod(shape))
    if len(shape) == 1:
        v = stg[:, 0:n]
    else:
        v = stg[:, 0:n].rearrange("p (a b) -> p a b", a=shape[0])
    ld(s, "sp", stg, v, wres, src_ap)
    cp(s, "pool", dst_t, dst_ap, stg, v)


def make_consts(s, cx):
    nc = s.nc
    iot = s.sb([128, 128], I32, "iot")
    iof = s.sb([128, 128], F32, "iof")
    cx.iof = iof
    cx.ident_f = s.sb([128, 128], F32, "identf")
    cx.ident_b = s.sb([128, 128], BF16, "identb")
    cx.maskA = s.sb([128, 2, 2, 128], BF16, "maskA")
    cx.perm = s.sb([128, 128], BF16, "perm")
    cx.ones_b = s.sb([128, 128], BF16, "onesb")
    cx.ones_f = s.sb([128, 128], F32, "onesf")
    cx.tri_le = s.sb([128, 128], F32, "trile")
    cx.tri_b = s.sb([128, 128], BF16, "trib")
    cx.stg = [s.sb([128, 2048], F32, "stg") for _ in range(2)]
    cx.stg_i = 0
    s.op("pool", lambda e: e.iota(iot[:, :], pattern=[[1, 128]], base=0, channel_multiplier=-1), writes=[iot])
    cp(s, "dve", iof, iof[:, :], iot, iot[:, :])
    ts(s, "dve", cx.ident_f, cx.ident_f[:, :], iof, iof[:, :], 0.0, None, ALU.is_equal)
    cp(s, "dve", cx.ident_b, cx.ident_b[:, :], cx.ident_f, cx.ident_f[:, :])
    ts(s, "dve", cx.tri_le, cx.tri_le[:, :], iof, iof[:, :], 0.0, None, ALU.is_ge)
    cp(s, "dve", cx.tri_b, cx.tri_b[:, :], cx.tri_le, cx.tri_le[:, :])
    for hh in range(2):
        ts(s, "dve", cx.maskA, cx.maskA[:, hh, 0, :], iof, iof[:, :], 0.0, None, ALU.is_ge)
        ts(s, "dve", cx.maskA, cx.maskA[:, hh, 1, :], iof, iof[:, :], 0.0, None, ALU.is_le)
    s.op("pool", lambda e: e.memset(cx.perm[:, :], 0.0), writes=[cx.perm])
    s.op("pool", lambda e: e.memset(cx.ones_b[:, :], 1.0), writes=[cx.ones_b])
    s.op("pool", lambda e: e.memset(cx.ones_f[:, :], 1.0), writes=[cx.ones_f])
    for hh in range(2):
        c0 = hh * 64
        ts(s, "dve", cx.perm, cx.perm[:, c0:c0 + 8], iof, iof[:, c0:c0 + 8], -8.0, None, ALU.is_equal)
        ts(s, "dve", cx.perm, cx.perm[:, c0 + 8:c0 + 16], iof, iof[:, c0 + 8:c0 + 16], 8.0, None, ALU.is_equal)


def sin_table(s, out_t, out_ap, x_t, x_ap, shape, sign_ap, sign_t, scratch):
    ki, kf, r = scratch
    C1 = 6.28125
    C2 = TWO_PI - C1
    ts(s, "dve", ki, ki[:, :], x_t, x_ap, 1.0 / TWO_PI, None, ALU.mult)
    cp(s, "dve", kf, kf[:, :], ki, ki[:, :])
    stt(s, "dve", r, r[:, :], kf, kf[:, :], -C1, x_t, x_ap, ALU.mult, ALU.add)
    stt(s, "dve", r, r[:, :], kf, kf[:, :], -C2, r, r[:, :], ALU.mult, ALU.add)
    ts(s, "dve", kf, kf[:, :], r, r[:, :], math.pi, None, ALU.is_gt)
    stt(s, "dve", r, r[:, :], kf, kf[:, :], -TWO_PI, r, r[:, :], ALU.mult, ALU.add)
    ts(s, "dve", kf, kf[:, :], r, r[:, :], -math.pi, None, ALU.is_lt)
    stt(s, "dve", r, r[:, :], kf, kf[:, :], TWO_PI, r, r[:, :], ALU.mult, ALU.add)
    ts(s, "dve", r, r[:, :], r, r[:, :], 3.14159, -3.14159, ALU.min, ALU.max)
    act(s, out_t, out_ap, r, r[:, :], AF.Sin)
    if sign_ap is not None:
        ts(s, "dve", out_t, out_ap, out_t, out_ap, sign_ap, None, ALU.mult, extra_reads=[sign_t])


def load_xT(s, cx, h_res, h_ap, xT):
    old = s.phase_es
    s.phase_es = ExitStack()
    hfs = [s.sb([128, 1024], F32, "hf") for _ in range(3)]
    pts = [s.ps([128, 512], F32, "ptr") for _ in range(2)]
    for i in range(NT):
        hf = hfs[i % 3]
        ld(s, "sp", hf, hf[:, :], h_res, h_ap[i * 128:(i + 1) * 128, :])
        for half in range(2):
            pt = pts[half]
            for c4 in range(4):
                c = half * 4 + c4
                s.op("pe", lambda e, pt=pt, c4=c4, c=c, hf=hf: e.transpose(
                    out=pt[:, c4 * 128:(c4 + 1) * 128], in_=hf[:, c * 128:(c + 1) * 128], identity=cx.ident_f[:, :]),
                    reads=[hf, cx.ident_f], writes=[pt])
            dst = xT[:, half * 4:(half + 1) * 4, i * 128:(i + 1) * 128]
            src = pt[:, :].rearrange("p (c t) -> p c t", c=4)
            cp(s, "act" if half else "dve", xT, dst, pt, src)
    s.end_phase()
    s.phase_es = old


def ln_epilogue(s, cx, y_ps, hf, g_bc, b_bc, wk, out_t):
    r, stats, mv, sd, rstd, nmr, xn = wk
    for half in range(2):
        sl = slice(half * 512, (half + 1) * 512)
        yt, yap = y_ps[half] if isinstance(y_ps[half], tuple) else (y_ps[half], y_ps[half][:, :])
        stt(s, "dve", r, r[:, sl], hf, hf[:, sl], ALPHA, yt, yap, ALU.mult, ALU.add)
        s.op("dve", lambda e, half=half, sl=sl: e.bn_stats(out=stats[:, half, :], in_=r[:, sl]), reads=[r], writes=[stats])
    s.op("dve", lambda e: e.bn_aggr(out=mv[:, :], in_=stats[:, :, :].rearrange("p a b -> p (a b)")), reads=[stats], writes=[mv])
    ts(s, "dve", sd, sd[:, :], mv, mv[:, 1:2], LN_EPS, None, ALU.add)
    act(s, sd, sd[:, :], sd, sd[:, :], AF.Sqrt)
    s.op("dve", lambda e: e.reciprocal(out=rstd[:, :], in_=sd[:, :]), reads=[sd], writes=[rstd])
    stt(s, "dve", nmr, nmr[:, :], mv, mv[:, 0:1], -1.0, rstd, rstd[:, :], ALU.mult, ALU.mult)
    act(s, xn, xn[:, :], r, r[:, :], AF.Identity, scale=rstd[:, 0:1], bias=nmr[:, 0:1], extra_reads=[rstd, nmr])
    tt(s, "pool", xn, xn[:, :], xn, xn[:, :], g_bc, g_bc[:, :], ALU.mult)
    tt(s, "pool", out_t, out_t[:, :], xn, xn[:, :], b_bc, b_bc[:, :], ALU.add)


def ln_work(s):
    return (s.sb([128, 1024], F32, "lnr"), s.sb([128, 2, 6], F32, "lnst"), s.sb([128, 2], F32, "lnmv"),
            s.sb([128, 1], F32, "lnsd"), s.sb([128, 1], F32, "lnrs"), s.sb([128, 1], F32, "lnnm"),
            s.sb([128, 1024], F32, "lnxn"))


def bcast_row(ap_row, n):
    return ap_row.partition_broadcast(128)


def phase_A(s, cx, dr, l, j, h_in, h_out):
    nc = s.nc
    h_in_res, h_in_ap = h_in[0], h_in[1]
    w_in = dr["a_w_in"][j]
    w_out = dr["a_w_out"][j]
    w_in_v = w_in.rearrange("(c p) n -> p c n", p=128)
    qkT_res, qkT = dr["qkT"]
    attT_res, attT = dr["attT"]
    wres = dr["wres"]

    s.begin_phase()
    xT = s.sb([128, 8, S], BF16, "xT")
    load_xT(s, cx, h_in_res, h_in_ap, xT)

    import os
    stop = int(os.environ.get("KSTOP", "9"))
    if stop < 1:
        s.end_phase()
        return
    with ExitStack() as es1:
        old = s.phase_es
        s.phase_es = es1
        wqk = s.sb([128, 8, 2048], BF16, "wqk")
        for c in range(8):
            wload(s, cx, wqk, wqk[:, c, :], wres, w_in[c * 128:(c + 1) * 128, 0:2048], [2048])
        rc = s.sb([128, 4], F32, "ropec")
        ld(s, "sp", rc, rc[:, :], wres, dr["rope_consts"])
        posi = s.sb([128, 512], I32, "posi")
        posf = s.sb([128, 512], F32, "posf")
        ang = s.sb([128, 512], F32, "ang")
        ang2 = s.sb([128, 512], F32, "ang2")
        Ct = s.sb([128, 512], F32, "Ct")
        St = s.sb([128, 512], F32, "St")
        scr = (s.sb([128, 512], I32, "ki"), s.sb([128, 512], F32, "kf"), s.sb([128, 512], F32, "rr"))
        psq = [s.ps([128, 512], F32, "psq") for _ in range(2)]
        psp = [s.ps([128, 512], F32, "psp") for _ in range(2)]
        qbs = [s.sb([128, 512], BF16, "qb") for _ in range(2)]
        t1s = [s.sb([128, 512], F32, "t1") for _ in range(2)]
        t2s = [s.sb([128, 512], F32, "t2") for _ in range(2)]
        obs = [s.sb([128, 512], BF16, "ob") for _ in range(3)]
        it = 0
        ksub = int(os.environ.get("KSUB", "99"))
        for tg in range(int(os.environ.get('KTG', '8')) if ksub > 0 else 0):
            tsl = slice(tg * 512, (tg + 1) * 512)
            ld(s, "sp", posi, posi[:, :], wres, bcast_row(dr["positions"][tg * 512:(tg + 1) * 512], 512))
            cp(s, "dve", posf, posf[:, :], posi, posi[:, :])
            ts(s, "dve", ang, ang[:, :], posf, posf[:, :], rc[:, 0:1], None, ALU.mult, extra_reads=[rc])
            ts(s, "dve", ang2, ang2[:, :], ang, ang[:, :], math.pi / 2, None, ALU.add)
            sin_table(s, St, St[:, :], ang, ang[:, :], None, rc[:, 1:2], rc, scr)
            sin_table(s, Ct, Ct[:, :], ang2, ang2[:, :], None, None, None, scr)
            for hp in range(int(os.environ.get('KHP', '8')) if ksub > 1 else 0):
                for w in range(2):
                    pq = psq[it % 2]
                    pp = psp[it % 2]
                    qb = qbs[it % 2]
                    t1 = t1s[it % 2]
                    t2 = t2s[it % 2]
                    ob = obs[it % 3]
                    it += 1
                    col = w * 1024 + hp * 128
                    for c in range(8):
                        mm(s, pq, pq[:, :], wqk, wqk[:, c, col:col + 128], xT, xT[:, c, tsl], c == 0, c == 7)
                    cp(s, "act", qb, qb[:, :], pq, pq[:, :])
                    if ksub < 3:
                        continue
                    mm(s, pp, pp[:, :], cx.perm, cx.perm[:, :], qb, qb[:, :], True, True)
                    tt(s, "dve", t1, t1[:, :], pq, pq[:, :], Ct, Ct[:, :], ALU.mult)
                    tt(s, "dve", t2, t2[:, :], pp, pp[:, :], St, St[:, :], ALU.mult)
                    tt(s, "pool", ob, ob[:, :], t1, t1[:, :], t2, t2[:, :], ALU.add)
                    if ksub < 4:
                        continue
                    st(s, "pool", qkT_res, qkT[w, hp * 128:(hp + 1) * 128, tsl], ob, ob[:, :])
        s.phase_es = old
    s_sub_barrier(s)
    if stop < 2:
        s.end_phase()
        return

    with ExitStack() as es2:
        old = s.phase_es
        s.phase_es = es2
        QTs = [s.sb([128, S], BF16, "QT") for _ in range(2)]
        KTs = [s.sb([128, S], BF16, "KT") for _ in range(2)]
        wvs = [s.sb([128, 8, 128], BF16, "wv") for _ in range(2)]
        Vds = [s.sb([128, 32, 128], BF16, "Vd") for _ in range(2)]
        acc = s.sb([128, 2, S], F32, "acc")
        obuf = s.sb([128, S], BF16, "obuf")
        psV = [s.ps([128, 512], F32, "psV") for _ in range(2)]
        psS = [s.ps([128, 512], F32, "psS") for _ in range(4)]
        psO_ = [s.ps([128, 512], F32, "psO") for _ in range(2)]
        psO = [T(p[:, 0:256].rearrange("p (a q) -> p a q", a=2), p.res) for p in psO_]
        pes = [s.sb([128, 512], BF16, "pe") for _ in range(4)]
        PTs = [s.sb([128, 2, 2, 128], BF16, "PT") for _ in range(4)]
        vi = 0
        bi_ctr = 0
        k2 = int(os.environ.get("KSUB2", "99"))
        for hp in range(int(os.environ.get("KHP2", "8"))):
            QT = QTs[hp % 2]
            KT = KTs[hp % 2]
            wv = wvs[hp % 2]
            ld(s, "sp", QT, QT[:, :], qkT_res, qkT[0, hp * 128:(hp + 1) * 128, :])
            ld(s, "sp", KT, KT[:, :], qkT_res, qkT[1, hp * 128:(hp + 1) * 128, :])
            wload(s, cx, wv, wv[:, :, :], wres, w_in_v[:, :, 2048 + hp * 128:2048 + (hp + 1) * 128], [8, 128])
            for di, d in enumerate((1, 4, 16)[:int(os.environ.get("KBR", "3"))] if k2 > 0 else ()):
                nb = 32 // d
                Vd = Vds[vi % 2]
                vi += 1

                def tok(r, n, d=d):
                    return slice(r + d * 128 * n, r + d * 128 * n + 127 * d + 1, d)
                for b in range(32):
                    r, n = divmod(b, nb)
                    pv = psV[(b // 4) % 2]
                    for c in range(8):
                        mm(s, pv, pv[:, (b % 4) * 128:(b % 4 + 1) * 128], xT, xT[:, c, tok(r, n)], wv, wv[:, c, :], c == 0, c == 7)
                    if b % 4 == 3:
                        cp(s, "act" if (b // 4) % 2 else "dve", Vd, Vd[:, b - 3:b + 1, :], pv,
                           pv[:, :].rearrange("p (b f) -> p b f", b=4))
                for b2 in range(int(os.environ.get("KB", "16")) if k2 > 1 else 0):
                    blocks = (2 * b2, 2 * b2 + 1)
                    n0 = blocks[0] % nb
                    PTh = []
                    for hh in range(2):
                        hs = slice(hh * 64, (hh + 1) * 64)
                        pS = psS[(2 * bi_ctr + hh) % 4]
                        pe_ = pes[(2 * bi_ctr + hh) % 4]
                        PT = PTs[(2 * bi_ctr + hh) % 4]
                        PTh.append(PT)
                        pS4 = pS[:, :].rearrange("p (b c q) -> p b c q", b=2, c=2)
                        pe4 = pe_[:, :].rearrange("p (b c q) -> p b c q", b=2, c=2)
                        for bi, b in enumerate(blocks):
                            r, n = divmod(b, nb)
                            mm(s, pS, pS4[:, bi, 0, :], KT, KT[hs, tok(r, n)], QT, QT[hs, tok(r, n)], True, True)
                            if n > 0:
                                mm(s, pS, pS4[:, bi, 1, :], KT, KT[hs, tok(r, n - 1)], QT, QT[hs, tok(r, n)], True, True)
                        if n0 > 0:
                            act(s, pe_, pe_[:, :], pS, pS[:, :], AF.Exp, scale=0.125)
                            tt(s, "pool", PT, PT[:, :, :, :], pe_, pe4, cx.maskA, cx.maskA[:, :, :, :], ALU.mult)
                        else:
                            act(s, pe_, pe4[:, 0, 0, :], pS, pS4[:, 0, 0, :], AF.Exp, scale=0.125)
                            act(s, pe_, pe4[:, 1, :, :], pS, pS4[:, 1, :, :], AF.Exp, scale=0.125)
                            tt(s, "pool", PT, PT[:, 0, 0, :], pe_, pe4[:, 0, 0, :], cx.maskA, cx.maskA[:, 0, 0, :], ALU.mult)
                            tt(s, "pool", PT, PT[:, 1, :, :], pe_, pe4[:, 1, :, :], cx.maskA, cx.maskA[:, 1, :, :], ALU.mult)
                    bi_ctr += 1
                    if k2 < 3:
                        continue
                    for bi, b in enumerate(blocks):
                        r, n = divmod(b, nb)
                        pO = psO[b % 2]
                        for hh in range(2):
                            hs = slice(hh * 64, (hh + 1) * 64)
                            PT = PTh[hh]
                            mm(s, pO, pO[hs, 0, :], Vd, Vd[:, b, hs], PT, PT[:, bi, 0, :], True, n == 0)
                            if n > 0:
                                mm(s, pO, pO[hs, 0, :], Vd, Vd[:, b - 1, hs], PT, PT[:, bi, 1, :], False, True)
                            mm(s, pO, pO[hs, 1, :], cx.ones_b, cx.ones_b[:, 0:64], PT, PT[:, bi, 0, :], True, n == 0)
                            if n > 0:
                                mm(s, pO, pO[hs, 1, :], cx.ones_b, cx.ones_b[:, 0:64], PT, PT[:, bi, 1, :], False, True)
                        if k2 < 4:
                            continue
                        if di == 0:
                            cp(s, "dve", acc, acc[:, :, tok(r, n)], pO, pO[:, :, :])
                        else:
                            tt(s, "dve", acc, acc[:, :, tok(r, n)], acc, acc[:, :, tok(r, n)], pO, pO[:, :, :], ALU.add)
            if k2 < 5:
                continue
            s.op("dve", lambda e: e.reciprocal(out=acc[:, 1, :], in_=acc[:, 1, :]), reads=[acc], writes=[acc])
            tt(s, "dve", obuf, obuf[:, :], acc, acc[:, 0, :], acc, acc[:, 1, :], ALU.mult)
            st(s, "sp", attT_res, attT[hp * 128:(hp + 1) * 128, :], obuf, obuf[:, :])
        s.phase_es = old
    s.end_phase()

    if stop < 3:
        return
    s.begin_phase()
    proj_ln(s, cx, dr, attT_res, attT, 8, w_out, h_in, h_out, dr["ln1_g"][l], dr["ln1_b"][l])
    s.end_phase()


def s_sub_barrier(s):
    es = s.phase_es
    s.phase_es = ExitStack()
    s.end_phase()
    s.phase_es = es


def proj_ln(s, cx, dr, aT_res, aT, nchunk, w_out, h_in, h_out, g_row, b_row):
    h_in_res, h_in_ap = h_in[0], h_in[1]
    h_out_res, h_out_ap, final = h_out
    wres = dr["wres"]
    wo = s.sb([128, nchunk, 1024], BF16, "wo")
    for c in range(nchunk):
        wload(s, cx, wo, wo[:, c, :], wres, w_out[c * 128:(c + 1) * 128, :], [1024])
    g_bc = s.sb([128, 1024], F32, "gbc")
    b_bc = s.sb([128, 1024], F32, "bbc")
    ld(s, "sp", g_bc, g_bc[:, :], wres, bcast_row(g_row, 1024))
    ld(s, "sp", b_bc, b_bc[:, :], wres, bcast_row(b_row, 1024))
    ats = [s.sb([128, nchunk, 512], BF16, "at") for _ in range(2)]
    hfs = [s.sb([128, 1024], F32, "hf2") for _ in range(2)]
    hos = [s.sb([128, 1024], F32, "ho") for _ in range(2)]
    psY = [[s.ps([128, 512], F32, "psY") for _ in range(2)] for _ in range(2)]
    wk = ln_work(s)
    aTv = aT[0:nchunk * 128, :].rearrange("(c p) t -> p c t", p=128)
    for tg in range(8):
        at = ats[tg % 2]
        ld(s, "sp", at, at[:, :, :], aT_res, aTv[:, :, tg * 512:(tg + 1) * 512])
        for ti in range(4):
            i = tg * 4 + ti
            hf = hfs[i % 2]
            ho = hos[i % 2]
            py = psY[i % 2]
            ld(s, "sp", hf, hf[:, :], h_in_res, h_in_ap[i * 128:(i + 1) * 128, :])
            for half in range(2):
                for c in range(nchunk):
                    mm(s, py[half], py[half][:, :], at, at[:, c, ti * 128:(ti + 1) * 128], wo,
                       wo[:, c, half * 512:(half + 1) * 512], c == 0, c == nchunk - 1)
            ln_epilogue(s, cx, py, hf, g_bc, b_bc, wk, ho)
            st(s, "pool", h_out_res, h_out_ap[i * 128:(i + 1) * 128, :], ho, ho[:, :], final=final)


def decay_tables(s, cx, LF, ig, nh, kscale_ln, EA, EB, ET, psX):
    n = NT * nh
    Fin = s.sb([128, NT, nh], F32, "Fin")
    tot = s.sb([128, NT, nh], F32, "tot")
    inc = s.sb([128, NT, nh], F32, "inc")
    LF2 = LF[:, :, :].rearrange("p a b -> p (a b)")
    mm(s, psX, psX[:, 0:n], cx.tri_le, cx.tri_le[:, :], LF, LF2, True, True)
    cp(s, "dve", Fin, Fin[:, :, :].rearrange("p a b -> p (a b)"), psX, psX[:, 0:n])
    mm(s, psX, psX[:, 0:n], cx.ones_f, cx.ones_f[:, :], LF, LF2, True, True)
    cp(s, "dve", tot, tot[:, :, :].rearrange("p a b -> p (a b)"), psX, psX[:, 0:n])
    for h in range(nh):
        s.op("dve", lambda e, h=h: e.tensor_tensor_scan(out=inc[:, :, h], data0=cx.ones_f[:, 0:NT], data1=tot[:, :, h],
                                                        initial=0.0, op0=ALU.mult, op1=ALU.add),
             reads=[cx.ones_f, tot], writes=[inc])
    tt(s, "dve", EB, EB[:, :, :], inc, inc[:, :, :], tot, tot[:, :, :], ALU.subtract)
    tt(s, "dve", EA, EA[:, :, :], Fin, Fin[:, :, :], EB, EB[:, :, :], ALU.add)
    if ig is not None:
        tt(s, "dve", EA, EA[:, :, :], ig[0], ig[1], EA, EA[:, :, :], ALU.subtract)
        ts(s, "dve", EA, EA[:, :, :], EA, EA[:, :, :], kscale_ln, None, ALU.add)
    else:
        ts(s, "dve", EA, EA[:, :, :], EA, EA[:, :, :], -1.0, kscale_ln, ALU.mult, ALU.add)
    act(s, ET, ET[:, :, :], Fin, Fin[:, :, :], AF.Exp)


def make_E(s, E, EA, EB, h, tmpE):
    tt(s, "pool", tmpE, tmpE[:, :, :], EA, EA[:, :, h].unsqueeze(2).to_broadcast([128, NT, NT]),
       EB, EB[:, :, h].unsqueeze(1).to_broadcast([128, NT, NT]), ALU.add)
    ts(s, "pool", tmpE, tmpE[:, :, :], tmpE, tmpE[:, :, :], 80.0, None, ALU.min)
    act(s, E, E[:, :, :], tmpE, tmpE[:, :, :], AF.Exp)


def decay_main(s, cx, heads, psS, psAcc, PTs, fin_cb, jmax=NT):
    cnt = [0] * len(heads)
    for j in range(jmax):
        for hi, hd in enumerate(heads):
            acc = psAcc[hi]
            dvw = hd["dvw"]
            nk = len(hd["kparts"])
            for i0 in range(0, j + 1, 4):
                i1 = min(i0 + 4, j + 1)
                n = i1 - i0
                pS = psS[hi][cnt[hi] % 2]
                PT = PTs[hi][cnt[hi] % 2]
                cnt[hi] += 1
                for ii in range(n):
                    i = i0 + ii
                    for kc, (Kt, Kap, Qt, Qap) in enumerate(hd["kparts"]):
                        mm(s, pS, pS[:, ii * 128:(ii + 1) * 128], Kt, Kap(i), Qt, Qap(j), kc == 0, kc == nk - 1)
                E = hd["E"]
                tt(s, "dve", PT, PT[:, 0:n, :], pS, pS[:, 0:n * 128].rearrange("p (a t) -> p a t", a=n),
                   E, E[:, i0:i1, j].unsqueeze(2).to_broadcast([128, n, 128]), ALU.mult)
                if i1 - 1 == j:
                    tt(s, "pool", PT, PT[:, n - 1, :], PT, PT[:, n - 1, :], cx.tri_b, cx.tri_b[:, :], ALU.mult)
                for ii in range(n):
                    i = i0 + ii
                    mm(s, acc, acc[:, 0:dvw], PT, PT[:, ii, :], hd["V"], hd["Vap"](i), i == 0, i == j)
            fin_cb(hi, hd, j, acc)


def small_norm(s, x_t, x_ap, width, wk, out_t, out_ap, g_t, g_ap):
    stats, mv, sd, rstd, nmr, xn = wk
    s.op("dve", lambda e: e.bn_stats(out=stats[:, :], in_=x_ap), reads=[x_t], writes=[stats])
    s.op("dve", lambda e: e.bn_aggr(out=mv[:, :], in_=stats[:, :]), reads=[stats], writes=[mv])
    ts(s, "dve", sd, sd[:, :], mv, mv[:, 1:2], LN_EPS, None, ALU.add)
    act(s, sd, sd[:, :], sd, sd[:, :], AF.Sqrt)
    s.op("dve", lambda e: e.reciprocal(out=rstd[:, :], in_=sd[:, :]), reads=[sd], writes=[rstd])
    stt(s, "dve", nmr, nmr[:, :], mv, mv[:, 0:1], -1.0, rstd, rstd[:, :], ALU.mult, ALU.mult)
    act(s, xn, xn[:, 0:width], x_t, x_ap, AF.Identity, scale=rstd[:, 0:1], bias=nmr[:, 0:1], extra_reads=[rstd, nmr])
    tt(s, "pool", out_t, out_ap, xn, xn[:, 0:width], g_t, g_ap, ALU.mult)


def small_norm_work(s, width):
    return (s.sb([128, 6], F32, "snst"), s.sb([128, 2], F32, "snmv"), s.sb([128, 1], F32, "snsd"),
            s.sb([128, 1], F32, "snrs"), s.sb([128, 1], F32, "snnm"), s.sb([128, width], F32, "snxn"))


def gate_proj_ln(s, cx, dr, xT, HN_res, HN, F, w_gate_ap, gfunc, w_out, h_in, h_out, g_row, b_row):
    h_in_res, h_in_ap = h_in[0], h_in[1]
    h_out_res, h_out_ap, final = h_out
    wres = dr["wres"]
    nch = F // 128
    nb = F // 512
    wg = s.sb([128, 8, F], BF16, "wgate")
    for c in range(8):
        for q in range(F // 1024):
            wload(s, cx, wg, wg[:, c, q * 1024:(q + 1) * 1024], wres, w_gate_ap[c * 128:(c + 1) * 128, q * 1024:(q + 1) * 1024], [1024])
    wo = s.sb([128, nch, 1024], BF16, "wo")
    for c in range(nch):
        wload(s, cx, wo, wo[:, c, :], wres, w_out[c * 128:(c + 1) * 128, :], [1024])
    g_bc = s.sb([128, 1024], F32, "gbc")
    b_bc = s.sb([128, 1024], F32, "bbc")
    ld(s, "sp", g_bc, g_bc[:, :], wres, bcast_row(g_row, 1024))
    ld(s, "sp", b_bc, b_bc[:, :], wres, bcast_row(b_row, 1024))
    nbuf = 1 if F > 1024 else 2
    hns = [s.sb([128, F], F32, "hn") for _ in range(nbuf)]
    sig = s.sb([128, F], F32, "sig")
    Gb = s.sb([128, F], BF16, "Gb")
    GT_ = s.sb([128, nch, 128], BF16, "GTt")
    hfs = [s.sb([128, 1024], F32, "hf2") for _ in range(nbuf)]
    hos = [s.sb([128, 1024], F32, "ho") for _ in range(nbuf)]
    psg = [s.ps([128, 512], F32, "psg") for _ in range(2)]
    pst = [s.ps([128, 1024], BF16, "pst") for _ in range(2)]
    psY = [s.ps([128, 512], F32, "psY") for _ in range(2)]
    wk = ln_work(s)
    for i in range(NT):
        hn = hns[i % nbuf]
        hf = hfs[i % nbuf]
        ho = hos[i % nbuf]
        ld(s, "sp", hn, hn[:, :], HN_res, HN[i * 128:(i + 1) * 128, 0:F])
        ld(s, "sp", hf, hf[:, :], h_in_res, h_in_ap[i * 128:(i + 1) * 128, :])
        for q in range(nb):
            pg = psg[q % 2]
            for c in range(8):
                mm(s, pg, pg[:, :], xT, xT[:, c, i * 128:(i + 1) * 128], wg, wg[:, c, q * 512:(q + 1) * 512], c == 0, c == 7)
            act(s, sig, sig[:, q * 512:(q + 1) * 512], pg, pg[:, :], gfunc)
        tt(s, "pool", Gb, Gb[:, :], hn, hn[:, :], sig, sig[:, :], ALU.mult)
        for q in range(nch // 8):
            pt = pst[q % 2]
            for c8 in range(8):
                c = q * 8 + c8
                s.op("pe", lambda e, pt=pt, c8=c8, c=c: e.transpose(out=pt[:, c8 * 128:(c8 + 1) * 128], in_=Gb[:, c * 128:(c + 1) * 128],
                                                                   identity=cx.ident_b[:, :]), reads=[Gb, cx.ident_b], writes=[pt])
            cp(s, "act" if q else "dve", GT_, GT_[:, q * 8:(q + 1) * 8, :], pt, pt[:, :].rearrange("p (c t) -> p c t", c=8))
        for half in range(2):
            for c in range(nch):
                mm(s, psY[half], psY[half][:, :], GT_, GT_[:, c, :], wo, wo[:, c, half * 512:(half + 1) * 512], c == 0, c == nch - 1)
        ln_epilogue(s, cx, psY, hf, g_bc, b_bc, wk, ho)
        st(s, "pool", h_out_res, h_out_ap[i * 128:(i + 1) * 128, :], ho, ho[:, :], final=final)


def phase_B(s, cx, dr, l, j_, h_in, h_out):
    import os
    nc = s.nc
    h_in_res, h_in_ap = h_in[0], h_in[1]
    wres = dr["wres"]
    w_in = dr["b_w_in"][j_]
    w_in_v = w_in.rearrange("(c p) n -> p c n", p=128)
    HN_res, HN = dr["hn"]
    bstop = int(os.environ.get("BSTOP", "9"))
    jmax = int(os.environ.get("BJMAX", str(NT)))
    npair = int(os.environ.get("BPAIRS", "4"))

    s.begin_phase()
    xT = s.sb([128, 8, S], BF16, "xT")
    GATES = s.sb([128, NT, 16], F32, "GATES")
    with ExitStack() as es0:
        old = s.phase_es
        s.phase_es = es0
        Wg32 = s.sb([128, 8, 16], F32, "Wg32")
        ld(s, "sp", Wg32, Wg32[:, :, :], wres, w_in_v[:, :, 3072:3088])
        gb = s.sb([128, 16], F32, "gbias")
        ld(s, "sp", gb, gb[:, :], wres, bcast_row(dr["b_gate_bias"][j_], 16))
        hfs = [s.sb([128, 1024], F32, "hf") for _ in range(3)]
        xTts = [s.sb([128, 8, 128], F32, "xTt") for _ in range(2)]
        pts = [s.ps([128, 512], F32, "ptr") for _ in range(2)]
        psg = s.ps([128, 512], F32, "psgate")
        for i in range(NT):
            hf = hfs[i % 3]
            xTt = xTts[i % 2]
            ld(s, "sp", hf, hf[:, :], h_in_res, h_in_ap[i * 128:(i + 1) * 128, :])
            for half in range(2):
                pt = pts[half]
                for c4 in range(4):
                    c = half * 4 + c4
                    s.op("pe", lambda e, pt=pt, c4=c4, c=c, hf=hf: e.transpose(
                        out=pt[:, c4 * 128:(c4 + 1) * 128], in_=hf[:, c * 128:(c + 1) * 128], identity=cx.ident_f[:, :]),
                        reads=[hf, cx.ident_f], writes=[pt])
                src = pt[:, :].rearrange("p (c t) -> p c t", c=4)
                cp(s, "act", xT, xT[:, half * 4:(half + 1) * 4, i * 128:(i + 1) * 128], pt, src)
                cp(s, "dve", xTt, xTt[:, half * 4:(half + 1) * 4, :], pt, src)
            for c in range(8):
                mm(s, psg, psg[:, 0:16], xTt, xTt[:, c, :], Wg32, Wg32[:, c, :], c == 0, c == 7)
            tt(s, "dve", GATES, GATES[:, i, :], psg, psg[:, 0:16], gb, gb[:, :], ALU.add)
        s.phase_es = old
    s_sub_barrier(s)

    EA = s.sb([128, NT, 8], F32, "EA")
    EB = s.sb([128, NT, 8], F32, "EB")
    ET = s.sb([128, NT, 8], F32, "ET")
    with ExitStack() as es1:
        old = s.phase_es
        s.phase_es = es1
        LF = s.sb([128, NT, 8], F32, "LF")
        psX = s.ps([128, 512], F32, "psX")
        act(s, LF, LF[:, :, :], GATES, GATES[:, :, 8:16], AF.Exp, scale=-1.0)
        ts(s, "dve", LF, LF[:, :, :], LF, LF[:, :, :], 1.0, None, ALU.add)
        act(s, LF, LF[:, :, :], LF, LF[:, :, :], AF.Ln)
        ts(s, "dve", LF, LF[:, :, :], LF, LF[:, :, :], -1.0, None, ALU.mult)
        decay_tables(s, cx, LF, (GATES, GATES[:, :, 0:8]), 8, math.log(0.125), EA, EB, ET, psX)
        s.phase_es = old
    s_sub_barrier(s)
    if bstop < 2:
        s.end_phase()
        return

    with ExitStack() as es2:
        old = s.phase_es
        s.phase_es = es2
        ng_bc = s.sb([128, 1024], F32, "ngbc")
        ld(s, "sp", ng_bc, ng_bc[:, :], wres, bcast_row(dr["b_norm_g"][j_], 1024))
        wq = s.sb([128, 8, 128], BF16, "wq")
        wkk = s.sb([128, 8, 128], BF16, "wk")
        wv = s.sb([128, 8, 256], BF16, "wv")
        cw = [s.sb([128, 4], F32, "cw") for _ in range(2)]
        cb = [s.sb([128, 1], F32, "cb") for _ in range(2)]
        UT = s.sb([128, S + 3], F32, "UT")
        s.op("pool", lambda e: e.memset(UT[:, 0:3], 0.0), writes=[UT])
        cacc = s.sb([128, S], F32, "cacc")
        QT = s.sb([128, S], BF16, "QT")
        KT = s.sb([128, S], BF16, "KT")
        Vaug = s.sb([128, NT, 2, 129], BF16, "Vaug")
        s.op("pool", lambda e: e.memset(Vaug[:, :, :, 128:129], 1.0), writes=[Vaug])
        Es = [s.sb([128, NT, NT], F32, "E") for _ in range(2)]
        tmpE = s.sb([128, NT, NT], F32, "tmpE")
        psX = [s.ps([128, 512], F32, "psX") for _ in range(2)]
        psS = [[s.ps([128, 512], F32, "psS") for _ in range(2)] for _ in range(2)]
        psAcc = [s.ps([128, 512], F32, "psAcc") for _ in range(2)]
        PTs = [[s.sb([128, 4, 128], BF16, "PT") for _ in range(2)] for _ in range(2)]
        snw = small_norm_work(s, 128)
        sm = {k: s.sb([128, 1], F32, k) for k in ("ed", "ad", "rec", "fac")}
        hnr = s.sb([128, 128], F32, "hnr")
        HNt = [s.sb([128, 256], F32, "HNt") for _ in range(2)]
        for hp in range(npair):
            for w, (wt, dst) in enumerate(((wq, QT), (wkk, KT))):
                col = w * 512 + hp * 128
                wload(s, cx, wt, wt[:, :, :], wres, w_in_v[:, :, col:col + 128], [8, 128])
                for jj in range(4):
                    ld(s, "sp", cw[w], cw[w][:, jj:jj + 1], wres, dr["b_conv_w"][j_][jj, col:col + 128].rearrange("(c o) -> c o", o=1))
                ld(s, "sp", cb[w], cb[w][:, :], wres, dr["b_conv_b"][j_][col:col + 128].rearrange("(c o) -> c o", o=1))
                for tg in range(8):
                    px = psX[tg % 2]
                    for c in range(8):
                        mm(s, px, px[:, :], wt, wt[:, c, :], xT, xT[:, c, tg * 512:(tg + 1) * 512], c == 0, c == 7)
                    cp(s, "act", UT, UT[:, 3 + tg * 512:3 + (tg + 1) * 512], px, px[:, :])
                ts(s, "dve", cacc, cacc[:, :], UT, UT[:, 0:S], cw[w][:, 0:1], None, ALU.mult, extra_reads=[cw[w]])
                for jj in range(1, 4):
                    stt(s, "dve", cacc, cacc[:, :], UT, UT[:, jj:S + jj], cw[w][:, jj:jj + 1], cacc, cacc[:, :], ALU.mult, ALU.add,
                        extra_reads=[cw[w]])
                act(s, dst, dst[:, :], cacc, cacc[:, :], AF.Silu, scale=1.0, bias=cb[w][:, 0:1], extra_reads=[cb[w]])
            wload(s, cx, wv, wv[:, :, :], wres, w_in_v[:, :, 1024 + hp * 256:1024 + (hp + 1) * 256], [8, 256])
            for i in range(NT):
                px = psX[i % 2]
                for c in range(8):
                    mm(s, px, px[:, 0:256], xT, xT[:, c, i * 128:(i + 1) * 128], wv, wv[:, c, :], c == 0, c == 7)
                cp(s, "act", Vaug, Vaug[:, i, :, 0:128], px, px[:, 0:256].rearrange("p (a d) -> p a d", a=2))
            heads = []
            for hh in range(2):
                h = hp * 2 + hh
                make_E(s, Es[hh], EA, EB, h, tmpE)
                hs = slice(hh * 64, (hh + 1) * 64)
                heads.append(dict(
                    kparts=[(KT, (lambda i, hs=hs: KT[hs, i * 128:(i + 1) * 128]), QT, (lambda j, hs=hs: QT[hs, j * 128:(j + 1) * 128]))],
                    V=Vaug, Vap=(lambda i, hh=hh: Vaug[:, i, hh, :]), dvw=129, E=Es[hh], h=h, hh=hh))

            def fin(hi, hd, j, acc):
                h, hh = hd["h"], hd["hh"]
                tt(s, "dve", sm["ed"], sm["ed"][:, :], acc, acc[:, 128:129], ET, ET[:, j, h:h + 1], ALU.mult)
                stt(s, "dve", sm["ad"], sm["ad"][:, :], sm["ed"], sm["ed"][:, :], -1.0, sm["ed"], sm["ed"][:, :], ALU.mult, ALU.max)
                ts(s, "dve", sm["ad"], sm["ad"][:, :], sm["ad"], sm["ad"][:, :], 1.0, None, ALU.max)
                s.op("dve", lambda e: e.reciprocal(out=sm["rec"][:, :], in_=sm["ad"][:, :]), reads=[sm["ad"]], writes=[sm["rec"]])
                tt(s, "dve", sm["fac"], sm["fac"][:, :], sm["rec"], sm["rec"][:, :], ET, ET[:, j, h:h + 1], ALU.mult)
                act(s, hnr, hnr[:, :], acc, acc[:, 0:128], AF.Copy, scale=sm["fac"][:, 0:1], extra_reads=[sm["fac"]])
                ht = HNt[j % 2]
                small_norm(s, hnr, hnr[:, :], 128, snw, ht, ht[:, hh * 128:(hh + 1) * 128], ng_bc, ng_bc[:, h * 128:(h + 1) * 128])
                if hh == 1:
                    st(s, "pool", HN_res, HN[j * 128:(j + 1) * 128, hp * 256:(hp + 1) * 256], ht, ht[:, :])
            decay_main(s, cx, heads, psS, psAcc, PTs, fin, jmax=jmax)
        s.phase_es = old
    s_sub_barrier(s)
    if bstop < 3:
        s.end_phase()
        return

    with ExitStack() as es3:
        old = s.phase_es
        s.phase_es = es3
        gate_proj_ln(s, cx, dr, xT, HN_res, HN, 1024, w_in[:, 2048:3072], AF.Sigmoid, dr["b_w_out"][j_], h_in, h_out,
                     dr["ln1_g"][l], dr["ln1_b"][l])
        s.phase_es = old
    s.end_phase()


def phase_C(s, cx, dr, l, j_, h_in, h_out):
    import os
    nc = s.nc
    h_in_res, h_in_ap = h_in[0], h_in[1]
    wres = dr["wres"]
    w_in = dr["c_w_in"][j_]
    w_in_v = w_in.rearrange("(c p) n -> p c n", p=128)
    HN_res, HN = dr["hn"]
    qkT_res, qkT = dr["qkT"]
    cstop = int(os.environ.get("CSTOP", "9"))
    jmax = int(os.environ.get("CJMAX", str(NT)))
    nheads = int(os.environ.get("CHEADS", "4"))

    s.begin_phase()
    xT = s.sb([128, 8, S], BF16, "xT")
    load_xT(s, cx, h_in_res, h_in_ap, xT)
    EA = s.sb([128, NT, 4], F32, "EA")
    EB = s.sb([128, NT, 4], F32, "EB")
    ET = s.sb([128, NT, 4], F32, "ET")
    with ExitStack() as es1:
        old = s.phase_es
        s.phase_es = es1
        LF = s.sb([128, NT, 4], F32, "LF")
        psX = s.ps([128, 512], F32, "psX")
        for h in range(4):
            s.op("pool", lambda e, h=h: e.memset(LF[:, :, h:h + 1], math.log(1.0 - 2.0 ** (-5.0 - h))), writes=[LF])
        decay_tables(s, cx, LF, None, 4, math.log(1.0 / 16.0), EA, EB, ET, psX)
        s.phase_es = old
    s_sub_barrier(s)

    with ExitStack() as es1:
        old = s.phase_es
        s.phase_es = es1
        wqk = s.sb([128, 8, 2048], BF16, "wqk")
        for c in range(8):
            wload(s, cx, wqk, wqk[:, c, :], wres, w_in[c * 128:(c + 1) * 128, 0:2048], [2048])
        rc = s.sb([128, 4], F32, "ropec")
        ld(s, "sp", rc, rc[:, :], wres, dr["rope_consts"])
        posi = s.sb([128, 512], I32, "posi")
        posf = s.sb([128, 512], F32, "posf")
        ang = s.sb([128, 512], F32, "ang")
        ang2 = s.sb([128, 512], F32, "ang2")
        Ct = s.sb([128, 512], F32, "Ct")
        St = s.sb([128, 512], F32, "St")
        scr = (s.sb([128, 512], I32, "ki"), s.sb([128, 512], F32, "kf"), s.sb([128, 512], F32, "rr"))
        psa = [s.ps([128, 512], F32, "psa") for _ in range(2)]
        psb = [s.ps([128, 512], F32, "psb") for _ in range(2)]
        t1s = [s.sb([128, 512], F32, "t1") for _ in range(2)]
        t2s = [s.sb([128, 512], F32, "t2") for _ in range(2)]
        oas = [s.sb([128, 512], BF16, "oa") for _ in range(2)]
        obs = [s.sb([128, 512], BF16, "ob") for _ in range(2)]
        it = 0
        for tg in range(8):
            tsl = slice(tg * 512, (tg + 1) * 512)
            ld(s, "sp", posi, posi[:, :], wres, bcast_row(dr["positions"][tg * 512:(tg + 1) * 512], 512))
            cp(s, "dve", posf, posf[:, :], posi, posi[:, :])
            ts(s, "dve", ang, ang[:, :], posf, posf[:, :], rc[:, 2:3], None, ALU.mult, extra_reads=[rc])
            ts(s, "dve", ang2, ang2[:, :], ang, ang[:, :], math.pi / 2, None, ALU.add)
            sin_table(s, St, St[:, :], ang, ang[:, :], None, None, None, scr)
            sin_table(s, Ct, Ct[:, :], ang2, ang2[:, :], None, None, None, scr)
            for h in range(4):
                for w in range(2):
                    pa, pb = psa[it % 2], psb[it % 2]
                    t1, t2, oa, ob = t1s[it % 2], t2s[it % 2], oas[it % 2], obs[it % 2]
                    it += 1
                    col = w * 1024 + h * 256
                    for c in range(8):
                        mm(s, pa, pa[:, :], wqk, wqk[:, c, col:col + 128], xT, xT[:, c, tsl], c == 0, c == 7)
                    for c in range(8):
                        mm(s, pb, pb[:, :], wqk, wqk[:, c, col + 128:col + 256], xT, xT[:, c, tsl], c == 0, c == 7)
                    tt(s, "dve", t1, t1[:, :], pa, pa[:, :], Ct, Ct[:, :], ALU.mult)
                    tt(s, "dve", t2, t2[:, :], pb, pb[:, :], St, St[:, :], ALU.mult)
                    tt(s, "pool", oa, oa[:, :], t1, t1[:, :], t2, t2[:, :], ALU.subtract)
                    st(s, "pool", qkT_res, qkT[w, h * 256:h * 256 + 128, tsl], oa, oa[:, :])
                    tt(s, "dve", t1, t1[:, :], pb, pb[:, :], Ct, Ct[:, :], ALU.mult)
                    tt(s, "dve", t2, t2[:, :], pa, pa[:, :], St, St[:, :], ALU.mult)
                    tt(s, "pool", ob, ob[:, :], t1, t1[:, :], t2, t2[:, :], ALU.add)
                    st(s, "pool", qkT_res, qkT[w, h * 256 + 128:h * 256 + 256, tsl], ob, ob[:, :])
        s.phase_es = old
    s_sub_barrier(s)
    if cstop < 2:
        s.end_phase()
        return

    with ExitStack() as es2:
        old = s.phase_es
        s.phase_es = es2
        ng_bc = s.sb([128, 2048], F32, "ngbc")
        ld(s, "sp", ng_bc, ng_bc[:, :], wres, bcast_row(dr["c_norm_g"][j_], 2048))
        QA = s.sb([128, S], BF16, "QA")
        QB = s.sb([128, S], BF16, "QB")
        KA = s.sb([128, S], BF16, "KA")
        KB = s.sb([128, S], BF16, "KB")
        wv = s.sb([128, 8, 512], BF16, "wv")
        Vh = s.sb([128, NT, 512], BF16, "Vh")
        E = s.sb([128, NT, NT], F32, "E")
        tmpE = s.sb([128, NT, NT], F32, "tmpE")
        psX = [s.ps([128, 512], F32, "psX") for _ in range(2)]
        psS = [[s.ps([128, 512], F32, "psS") for _ in range(2)]]
        psAcc = [s.ps([128, 512], F32, "psAcc")]
        PTs = [[s.sb([128, 4, 128], BF16, "PT") for _ in range(2)]]
        snw = small_norm_work(s, 512)
        onr = s.sb([128, 512], F32, "onr")
        HNt = [s.sb([128, 512], F32, "HNt") for _ in range(2)]
        for h in range(nheads):
            for w, (ta, tb) in enumerate(((QA, QB), (KA, KB))):
                ld(s, "sp", ta, ta[:, :], qkT_res, qkT[w, h * 256:h * 256 + 128, :])
                ld(s, "sp", tb, tb[:, :], qkT_res, qkT[w, h * 256 + 128:h * 256 + 256, :])
            wload(s, cx, wv, wv[:, 0:4, :], wres, w_in_v[:, 0:4, 2048 + h * 512:2048 + (h + 1) * 512], [4, 512])
            wload(s, cx, wv, wv[:, 4:8, :], wres, w_in_v[:, 4:8, 2048 + h * 512:2048 + (h + 1) * 512], [4, 512])
            for i in range(NT):
                px = psX[i % 2]
                for c in range(8):
                    mm(s, px, px[:, :], xT, xT[:, c, i * 128:(i + 1) * 128], wv, wv[:, c, :], c == 0, c == 7)
                cp(s, "act", Vh, Vh[:, i, :], px, px[:, :])
            make_E(s, E, EA, EB, h, tmpE)
            heads = [dict(
                kparts=[(KA, (lambda i: KA[:, i * 128:(i + 1) * 128]), QA, (lambda j: QA[:, j * 128:(j + 1) * 128])),
                        (KB, (lambda i: KB[:, i * 128:(i + 1) * 128]), QB, (lambda j: QB[:, j * 128:(j + 1) * 128]))],
                V=Vh, Vap=(lambda i: Vh[:, i, :]), dvw=512, E=E, h=h)]

            def fin(hi, hd, j, acc, h=h):
                act(s, onr, onr[:, :], acc, acc[:, 0:512], AF.Copy, scale=ET[:, j, h:h + 1], extra_reads=[ET])
                ht = HNt[j % 2]
                small_norm(s, onr, onr[:, :], 512, snw, ht, ht[:, :], ng_bc, ng_bc[:, h * 512:(h + 1) * 512])
                st(s, "pool", HN_res, HN[j * 128:(j + 1) * 128, h * 512:(h + 1) * 512], ht, ht[:, :])
            decay_main(s, cx, heads, psS, psAcc, PTs, fin, jmax=jmax)
        s.phase_es = old
    s_sub_barrier(s)
    if cstop < 3:
        s.end_phase()
        return

    with ExitStack() as es3:
        old = s.phase_es
        s.phase_es = es3
        gate_proj_ln(s, cx, dr, xT, HN_res, HN, 2048, w_in[:, 4096:6144], AF.Silu, dr["c_w_out"][j_], h_in, h_out,
                     dr["ln1_g"][l], dr["ln1_b"][l])
        s.phase_es = old
    s.end_phase()


NSLOT = 128
BIGIDX = 4 * 64 * 128


def phase_F(s, cx, dr, l, h_in, h_out):
    import os
    nc = s.nc
    h_in_res, h_in_ap = h_in[0], h_in[1]
    h_out_res, h_out_ap, final = h_out
    wres = dr["wres"]
    xbf_res, xbf = dr["xbf"]
    bt_res, bt = dr["buftok"]
    yb_res, yb = dr["yb"]
    fstop = int(os.environ.get("FSTOP", "9"))

    s.begin_phase()
    OH1 = s.sb([128, NT, 64], F32, "OH1")
    OH2 = s.sb([128, NT, 64], F32, "OH2")
    RK = s.sb([128, NT, 2], F32, "RK")
    GT = s.sb([128, NT, 2], F32, "GT")
    DESTi = s.sb([128, NT, 2], I32, "DESTi")
    IDXW = s.sb([128, NSLOT], I32, "IDXW")
    breg_x = nc.gpsimd.to_reg(S - 1)
    breg_w = nc.gpsimd.to_reg((l + 1) * 64 * 128 - 1)
    breg_y = nc.gpsimd.to_reg(NSLOT * 128 - 1)
    breg_t = nc.gpsimd.to_reg(NSLOT * 128 - 1)

    with ExitStack() as es1:
        old = s.phase_es
        s.phase_es = es1
        Wr = s.sb([128, 8, 72], F32, "Wr")
        ld(s, "sp", Wr, Wr[:, :, 0:8], wres, dr["r_group_w"][l].rearrange("(c p) n -> p c n", p=128))
        ld(s, "sp", Wr, Wr[:, :, 8:72], wres, dr["r_expert_w"][l].rearrange("(c p) n -> p c n", p=128))
        bias = s.sb([128, 72], F32, "rbias")
        ld(s, "sp", bias, bias[:, 0:8], wres, bcast_row(dr["r_group_b"][l], 8))
        ld(s, "sp", bias, bias[:, 8:72], wres, bcast_row(dr["r_expert_b"][l], 64))
        tri_lt = s.sb([128, 128], BF16, "trilt")
        ts(s, "dve", tri_lt, tri_lt[:, :], cx.iof, cx.iof[:, :], 0.0, None, ALU.is_gt)
        toki = s.sb([128, NT], I32, "toki")
        s.op("pool", lambda e: e.iota(toki[:, :], pattern=[[128, NT]], base=0, channel_multiplier=1), writes=[toki])
        cnt = s.sb([128, 64], F32, "cnt")
        s.op("pool", lambda e: e.memset(cnt[:, :], 0.0), writes=[cnt])
        hfs = [s.sb([128, 1024], F32, "hfr") for _ in range(2)]
        hbs = [s.sb([128, 1024], BF16, "hbr") for _ in range(2)]
        xTts = [s.sb([128, 8, 128], F32, "xTt") for _ in range(2)]
        pts = [s.ps([128, 512], F32, "ptr") for _ in range(2)]
        psL = s.ps([128, 512], F32, "psL")
        psPC = s.ps([128, 512], F32, "psPC")
        lg = s.sb([128, 72], F32, "lg")
        sm = {k: s.sb([128, 1], F32, k) for k in ("gmax", "ngmax", "sumg", "pgrp", "v1", "v2", "dv", "ex", "den", "rec")}
        ohg = s.sb([128, 8], F32, "ohg")
        eg = s.sb([128, 8], F32, "eg")
        pen = s.sb([128, 8], F32, "pen")
        em = s.sb([128, 64], F32, "em")
        em2 = s.sb([128, 64], F32, "em2")
        Mb = s.sb([128, 64], BF16, "Mb")
        pos = s.sb([128, 64], F32, "pos")
        tmp = s.sb([128, 64], F32, "tmp")
        for i in range(NT):
            hf = hfs[i % 2]
            hb = hbs[i % 2]
            xTt = xTts[i % 2]
            ld(s, "sp", hf, hf[:, :], h_in_res, h_in_ap[i * 128:(i + 1) * 128, :])
            cp(s, "pool", hb, hb[:, :], hf, hf[:, :])
            st(s, "pool", xbf_res, xbf[i * 128:(i + 1) * 128, :], hb, hb[:, :])
            for half in range(2):
                pt = pts[half]
                for c4 in range(4):
                    c = half * 4 + c4
                    s.op("pe", lambda e, pt=pt, c4=c4, c=c, hf=hf: e.transpose(
                        out=pt[:, c4 * 128:(c4 + 1) * 128], in_=hf[:, c * 128:(c + 1) * 128], identity=cx.ident_f[:, :]),
                        reads=[hf, cx.ident_f], writes=[pt])
                cp(s, "act" if half else "dve", xTt, xTt[:, half * 4:(half + 1) * 4, :], pt,
                   pt[:, :].rearrange("p (c t) -> p c t", c=4))
            for c in range(8):
                mm(s, psL, psL[:, 0:72], xTt, xTt[:, c, :], Wr, Wr[:, c, :], c == 0, c == 7)
            tt(s, "dve", lg, lg[:, :], psL, psL[:, 0:72], bias, bias[:, :], ALU.add)
            g = sm
            s.op("dve", lambda e: e.reduce_max(out=g["gmax"][:, :], in_=lg[:, 0:8], axis=AX.X), reads=[lg], writes=[g["gmax"]])
            ts(s, "dve", ohg, ohg[:, :], lg, lg[:, 0:8], g["gmax"][:, 0:1], None, ALU.is_equal, extra_reads=[g["gmax"]])
            ts(s, "dve", g["ngmax"], g["ngmax"][:, :], g["gmax"], g["gmax"][:, :], -1.0, None, ALU.mult)
            act(s, eg, eg[:, :], lg, lg[:, 0:8], AF.Exp, scale=1.0, bias=g["ngmax"][:, 0:1], extra_reads=[g["ngmax"]])
            s.op("dve", lambda e: e.reduce_sum(out=g["sumg"][:, :], in_=eg[:, :], axis=AX.X), reads=[eg], writes=[g["sumg"]])
            s.op("dve", lambda e: e.reciprocal(out=g["pgrp"][:, :], in_=g["sumg"][:, :]), reads=[g["sumg"]], writes=[g["pgrp"]])
            ts(s, "dve", pen, pen[:, :], ohg, ohg[:, :], -1.0, 1e30, ALU.add, ALU.mult)
            tt(s, "dve", em, em[:, :].rearrange("p (g j) -> p g j", g=8), lg, lg[:, 8:72].rearrange("p (g j) -> p g j", g=8),
               pen, pen[:, :].unsqueeze(2).to_broadcast([128, 8, 8]), ALU.add)
            s.op("dve", lambda e: e.reduce_max(out=g["v1"][:, :], in_=em[:, :], axis=AX.X), reads=[em], writes=[g["v1"]])
            ts(s, "dve", OH1, OH1[:, i, :], em, em[:, :], g["v1"][:, 0:1], None, ALU.is_equal, extra_reads=[g["v1"]])
            stt(s, "dve", em2, em2[:, :], OH1, OH1[:, i, :], -1e30, em, em[:, :], ALU.mult, ALU.add)
            s.op("dve", lambda e: e.reduce_max(out=g["v2"][:, :], in_=em2[:, :], axis=AX.X), reads=[em2], writes=[g["v2"]])
            ts(s, "dve", OH2, OH2[:, i, :], em2, em2[:, :], g["v2"][:, 0:1], None, ALU.is_equal, extra_reads=[g["v2"]])
            tt(s, "dve", g["dv"], g["dv"][:, :], g["v2"], g["v2"][:, :], g["v1"], g["v1"][:, :], ALU.subtract)
            act(s, g["ex"], g["ex"][:, :], g["dv"], g["dv"][:, :], AF.Exp)
            ts(s, "dve", g["den"], g["den"][:, :], g["ex"], g["ex"][:, :], 1.0, None, ALU.add)
            s.op("dve", lambda e: e.reciprocal(out=g["rec"][:, :], in_=g["den"][:, :]), reads=[g["den"]], writes=[g["rec"]])
            tt(s, "dve", GT, GT[:, i, 0:1], g["pgrp"], g["pgrp"][:, :], g["rec"], g["rec"][:, :], ALU.mult)
            tt(s, "dve", GT, GT[:, i, 1:2], GT, GT[:, i, 0:1], g["ex"], g["ex"][:, :], ALU.mult)
            tt(s, "dve", Mb, Mb[:, :], OH1, OH1[:, i, :], OH2, OH2[:, i, :], ALU.add)
            mm(s, psPC, psPC[:, 0:64], tri_lt, tri_lt[:, :], Mb, Mb[:, :], True, True)
            mm(s, psPC, psPC[:, 64:128], cx.ones_b, cx.ones_b[:, :], Mb, Mb[:, :], True, True)
            tt(s, "dve", pos, pos[:, :], psPC, psPC[:, 0:64], cnt, cnt[:, :], ALU.add)
            tt(s, "dve", cnt, cnt[:, :], psPC, psPC[:, 64:128], cnt, cnt[:, :], ALU.add)
            tt(s, "dve", tmp, tmp[:, :], pos, pos[:, :], OH1, OH1[:, i, :], ALU.mult)
            s.op("dve", lambda e, i=i: e.reduce_sum(out=RK[:, i, 0:1], in_=tmp[:, :], axis=AX.X), reads=[tmp], writes=[RK])
            tt(s, "dve", tmp, tmp[:, :], pos, pos[:, :], OH2, OH2[:, i, :], ALU.mult)
            s.op("dve", lambda e, i=i: e.reduce_sum(out=RK[:, i, 1:2], in_=tmp[:, :], axis=AX.X), reads=[tmp], writes=[RK])
        cnti = s.sb([128, 64], I32, "cnti")
        padf = s.sb([128, 64], F32, "padf")
        pends = s.sb([128, 64], F32, "pends")
        pstart = s.sb([128, 64], F32, "pstart")
        ts(s, "dve", cnti, cnti[:, :], cnt, cnt[:, :], 127.0, None, ALU.add)
        ts(s, "dve", cnti, cnti[:, :], cnti, cnti[:, :], 7, 7, ALU.arith_shift_right, ALU.logical_shift_left)
        cp(s, "dve", padf, padf[:, :], cnti, cnti[:, :])
        s.op("dve", lambda e: e.tensor_tensor_scan(out=pends[:, :], data0=cx.ones_f[:, 0:64], data1=padf[:, :],
                                                   initial=0.0, op0=ALU.mult, op1=ALU.add),
             reads=[cx.ones_f, padf], writes=[pends])
        tt(s, "dve", pstart, pstart[:, :], pends, pends[:, :], padf, padf[:, :], ALU.subtract)
        big = s.sb([128, NT, 64], F32, "big")
        DEST = s.sb([128, NT, 2], F32, "DEST")
        for k, OH in enumerate((OH1, OH2)):
            tt(s, "dve", big, big[:, :, :], OH, OH[:, :, :], pstart, pstart[:, :].unsqueeze(1).to_broadcast([128, NT, 64]), ALU.mult)
            s.op("dve", lambda e, k=k: e.reduce_sum(out=DEST[:, :, k], in_=big[:, :, :], axis=AX.X), reads=[big], writes=[DEST])
        tt(s, "dve", DEST, DEST[:, :, :], DEST, DEST[:, :, :], RK, RK[:, :, :], ALU.add)
        cp(s, "dve", DESTi, DESTi[:, :, :], DEST, DEST[:, :, :])
        fill = s.sb([128, NSLOT], I32, "fill")
        s.op("pool", lambda e: e.iota(fill[:, :], pattern=[[0, NSLOT]], base=S, channel_multiplier=0), writes=[fill])
        st(s, "sp", bt_res, bt.rearrange("(p n) o -> p (n o)", p=128), fill, fill[:, :])
        for i in range(NT):
            for k in range(2):
                s.dma("pool", lambda e, i=i, k=k: e.indirect_dma_start(
                    out=bt, out_offset=bass.IndirectOffsetOnAxis(ap=DESTi[:, i, k:k + 1], axis=0),
                    in_=toki[:, i:i + 1], in_offset=None, bounds_check=breg_t, oob_is_err=False),
                    reads=[DESTi, toki], writes=[bt_res])
        slotoff = s.sb([128, NSLOT], F32, "slotoff")
        sloti = s.sb([128, NSLOT], I32, "sloti")
        s.op("pool", lambda e: e.iota(sloti[:, :], pattern=[[128, NSLOT]], base=0, channel_multiplier=0), writes=[sloti])
        cp(s, "dve", slotoff, slotoff[:, :], sloti, sloti[:, :])
        blk = s.sb([128, NSLOT], F32, "blk")
        cmp_ = s.sb([128, 32, 64], F32, "cmp")
        for q in range(NSLOT // 32):
            tt(s, "dve", cmp_, cmp_[:, :, :], pends, pends[:, :].unsqueeze(1).to_broadcast([128, 32, 64]),
               slotoff, slotoff[:, q * 32:(q + 1) * 32].unsqueeze(2).to_broadcast([128, 32, 64]), ALU.is_le)
            s.op("dve", lambda e, q=q: e.reduce_sum(out=blk[:, q * 32:(q + 1) * 32], in_=cmp_[:, :, :], axis=AX.X),
                 reads=[cmp_], writes=[blk])
        pidx = s.sb([128, 1], F32, "pidx")
        pidi = s.sb([128, 1], I32, "pidi")
        s.op("pool", lambda e: e.iota(pidi[:, :], pattern=[[0, 1]], base=0, channel_multiplier=1), writes=[pidi])
        cp(s, "dve", pidx, pidx[:, :], pidi, pidi[:, :])
        used = s.sb([128, NSLOT], F32, "used")
        ts(s, "dve", used, used[:, :], slotoff, slotoff[:, :], pends[:, 63:64], None, ALU.is_lt, extra_reads=[pends])
        ts(s, "dve", blk, blk[:, :], blk, blk[:, :], 63.0, 128.0, ALU.min, ALU.mult)
        ts(s, "dve", blk, blk[:, :], blk, blk[:, :], pidx[:, 0:1], float(l * 64 * 128 - BIGIDX), ALU.add, ALU.add, extra_reads=[pidx])
        tt(s, "dve", blk, blk[:, :], blk, blk[:, :], used, used[:, :], ALU.mult)
        ts(s, "dve", blk, blk[:, :], blk, blk[:, :], float(BIGIDX), None, ALU.add)
        cp(s, "dve", IDXW, IDXW[:, :], blk, blk[:, :])
        if "dbg" in dr:
            dbg = s.sb([128, 1024], F32, "dbg")
            s.op("dve", lambda e: e.memset(dbg[:, :], 0.0), writes=[dbg])
            cp(s, "dve", dbg, dbg[:, 0:128], IDXW, IDXW[:, :])
            cp(s, "dve", dbg, dbg[:, 128:192], cnt, cnt[:, :])
            cp(s, "dve", dbg, dbg[:, 192:256], pends, pends[:, :])
            cp(s, "dve", dbg, dbg[:, 256:320], DEST, DEST[:, :, :].rearrange("p a b -> p (a b)"))
            cp(s, "dve", dbg, dbg[:, 320:384], GT, GT[:, :, :].rearrange("p a b -> p (a b)"))
            cp(s, "dve", dbg, dbg[:, 384:448], used, used[:, 0:64])
            cp(s, "dve", dbg, dbg[:, 448:512], padf, padf[:, :])
            cp(s, "dve", dbg, dbg[:, 512:576], OH1, OH1[:, 0, :])
            cp(s, "dve", dbg, dbg[:, 576:640], OH2, OH2[:, 0, :])
            cp(s, "dve", dbg, dbg[:, 640:704], RK, RK[:, :, :].rearrange("p a b -> p (a b)"))
            st(s, "sp", wres, dr["dbg"], dbg, dbg[:, :], final=True)
        s.phase_es = old
    s_sub_barrier(s)
    if fstop < 2:
        s.end_phase()
        return

    with ExitStack() as es2:
        old = s.phase_es
        s.phase_es = es2
        wviews = [dr[k].rearrange("l e (p j) n -> (l e p) (j n)", p=128) for k in ("e_w_gate", "e_w_up", "e_w_down")]
        stgs = [[s.sb([128, 2048], F32, f"ws{k}") for k in range(3)] for _ in range(2)]
        wbs = [[s.sb([128, 2048], BF16, f"wb{k}") for k in range(3)] for _ in range(2)]
        idxs = [s.sb([128, 1], I32, "idxb") for _ in range(2)]
        xgs = [s.sb([128, 1024], BF16, "xg") for _ in range(2)]
        for xg in xgs:
            s.op("pool", lambda e, xg=xg: e.memset(xg[:, :], 0.0), writes=[xg])
        xgTs = [s.sb([128, 8, 128], BF16, "xgT") for _ in range(2)]
        sgs = [s.sb([128, 256], F32, "sg") for _ in range(2)]
        aTs = [s.sb([128, 2, 128], BF16, "aT") for _ in range(2)]
        yos = [s.sb([128, 1024], F32, "yo") for _ in range(2)]
        psT = [s.ps([128, 1024], BF16, "psT") for _ in range(2)]
        psG = s.ps([128, 512], F32, "psG")
        psU = s.ps([128, 512], F32, "psU")
        psY = [s.ps([128, 512], F32, "psY") for _ in range(2)]
        nslot = int(os.environ.get("FSLOTS", str(NSLOT)))

        def loads(b):
            ix = idxs[b % 2]
            ld(s, "sp", ix, ix[:, :], bt_res, bt[b * 128:(b + 1) * 128, :])
            xg = xgs[b % 2]
            s.dma("pool", lambda e: e.indirect_dma_start(
                out=xg[:, :], out_offset=None, in_=xbf, in_offset=bass.IndirectOffsetOnAxis(ap=ix[:, 0:1], axis=0),
                bounds_check=breg_x, oob_is_err=False), reads=[ix, xbf_res], writes=[xg])
            for k in range(3):
                stg = stgs[b % 2][k]
                s.dma("pool", lambda e, stg=stg, k=k: e.indirect_dma_start(
                    out=stg[:, :], out_offset=None, in_=wviews[k],
                    in_offset=bass.IndirectOffsetOnAxis(ap=IDXW[:, b:b + 1], axis=0),
                    bounds_check=breg_w, oob_is_err=False), reads=[IDXW, wres], writes=[stg])

        loads(0)
        for b in range(nslot):
            if b + 1 < nslot:
                loads(b + 1)
            xg = xgs[b % 2]
            xgT = xgTs[b % 2]
            wg, wu, wd = wbs[b % 2]
            sg = sgs[b % 2]
            aT = aTs[b % 2]
            yo = yos[b % 2]
            pT = psT[b % 2]
            for k, eng in enumerate(("pool", "act", "dve")):
                cp(s, eng, wbs[b % 2][k], wbs[b % 2][k][:, :], stgs[b % 2][k], stgs[b % 2][k][:, :])
            for j in range(8):
                s.op("pe", lambda e, j=j: e.transpose(out=pT[:, j * 128:(j + 1) * 128], in_=xg[:, j:1024:8],
                                                      identity=cx.ident_b[:, :]), reads=[xg, cx.ident_b], writes=[pT])
            cp(s, "dve", xgT, xgT[:, :, :], pT, pT[:, :].rearrange("p (j t) -> p j t", j=8))
            wgv = wg[:, :].rearrange("p (j n) -> p j n", j=8)
            wuv = wu[:, :].rearrange("p (j n) -> p j n", j=8)
            wdv = wd[:, :].rearrange("p (j n) -> p j n", j=2)
            for pS_, wv_ in ((psG, wgv), (psU, wuv)):
                for jh in range(2):
                    for j in range(8):
                        mm(s, pS_, pS_[:, jh * 128:(jh + 1) * 128], wbs[b % 2][0 if pS_ is psG else 1], wv_[:, j, jh:256:2],
                           xgT, xgT[:, j, :], j == 0, j == 7)
            act(s, sg, sg[:, :], psG, psG[:, 0:256], AF.Silu)
            tt(s, "dve", aT, aT[:, :, :].rearrange("p a t -> p (a t)"), sg, sg[:, :], psU, psU[:, 0:256], ALU.mult)
            for half in range(2):
                for jh in range(2):
                    mm(s, psY[half], psY[half][:, :], aT, aT[:, jh, :], wd, wdv[:, jh, half * 512:(half + 1) * 512], jh == 0, jh == 1)
            cp(s, "act", yo, yo[:, 0:512], psY[0], psY[0][:, :])
            cp(s, "dve", yo, yo[:, 512:1024], psY[1], psY[1][:, :])
            st(s, "sp", yb_res, yb[b * 128:(b + 1) * 128, :], yo, yo[:, :])
        s.phase_es = old
    s_sub_barrier(s)
    if fstop < 3:
        s.end_phase()
        return

    with ExitStack() as es3:
        old = s.phase_es
        s.phase_es = es3
        g_bc = s.sb([128, 1024], F32, "gbc")
        b_bc = s.sb([128, 1024], F32, "bbc")
        ld(s, "sp", g_bc, g_bc[:, :], wres, bcast_row(dr["ln2_g"][l], 1024))
        ld(s, "sp", b_bc, b_bc[:, :], wres, bcast_row(dr["ln2_b"][l], 1024))
        y1s = [s.sb([128, 1024], F32, "y1") for _ in range(2)]
        y2s = [s.sb([128, 1024], F32, "y2") for _ in range(2)]
        hfs = [s.sb([128, 1024], F32, "hf3") for _ in range(2)]
        hos = [s.sb([128, 1024], F32, "ho3") for _ in range(2)]
        wk = ln_work(s)
        for i in range(NT):
            y1, y2, hf, ho = y1s[i % 2], y2s[i % 2], hfs[i % 2], hos[i % 2]
            for k, yk in enumerate((y1, y2)):
                s.dma("pool", lambda e, yk=yk, k=k, i=i: e.indirect_dma_start(
                    out=yk[:, :], out_offset=None, in_=yb, in_offset=bass.IndirectOffsetOnAxis(ap=DESTi[:, i, k:k + 1], axis=0),
                    bounds_check=breg_y, oob_is_err=False), reads=[DESTi, yb_res], writes=[yk])
            ld(s, "sp", hf, hf[:, :], h_in_res, h_in_ap[i * 128:(i + 1) * 128, :])
            act(s, y2, y2[:, :], y2, y2[:, :], AF.Copy, scale=GT[:, i, 1:2], extra_reads=[GT])
            stt(s, "dve", y1, y1[:, :], y1, y1[:, :], GT[:, i, 0:1], y2, y2[:, :], ALU.mult, ALU.add, extra_reads=[GT])
            ln_epilogue(s, cx, [(y1, y1[:, 0:512]), (y1, y1[:, 512:1024])], hf, g_bc, b_bc, wk, ho)
            st(s, "pool", h_out_res, h_out_ap[i * 128:(i + 1) * 128, :], ho, ho[:, :], final=final)
        s.phase_es = old
    s.end_phase()


def rope_consts_np():
    rc = np.zeros((128, 4), np.float32)
    inv_a = 500000.0 ** (-np.arange(0, 16, 2, dtype=np.float32) / 16.0)
    for hh in range(2):
        for jx in range(16):
            rc[hh * 64 + jx, 0] = inv_a[jx % 8]
            rc[hh * 64 + jx, 1] = -1.0 if jx < 8 else 1.0
    inv_c = 10000.0 ** (-np.arange(0, 256, 2, dtype=np.float32) / 256.0)
    rc[:, 2] = inv_c
    return rc


IN_SPECS = [
    ("x", [S, D], F32), ("positions", [S], I32),
    ("ln1_g", [4, D], F32), ("ln1_b", [4, D], F32), ("ln2_g", [4, D], F32), ("ln2_b", [4, D], F32),
    ("a_w_in", [2, D, 3072], F32), ("a_w_out", [2, D, D], F32),
    ("b_w_in", [1, D, 3088], F32), ("b_gate_bias", [1, 16], F32), ("b_conv_w", [1, 4, D], F32),
    ("b_conv_b", [1, D], F32), ("b_norm_g", [1, D], F32), ("b_w_out", [1, D, D], F32),
    ("c_w_in", [1, D, 6144], F32), ("c_norm_g", [1, 2 * D], F32), ("c_w_out", [1, 2 * D, D], F32),
    ("r_group_w", [4, D, 8], F32), ("r_group_b", [4, 8], F32), ("r_expert_w", [4, D, 64], F32),
    ("r_expert_b", [4, 64], F32), ("e_w_gate", [4, 64, D, 256], F32), ("e_w_up", [4, 64, D, 256], F32),
    ("e_w_down", [4, 64, 256, D], F32), ("rope_consts", [128, 4], F32),
]


def build(plan, used_inputs=None):
    nc = bass.Bass("TRN2", target_bir_lowering=False)
    dr = {}
    for name, shape, dt in IN_SPECS:
        if used_inputs is not None and name not in used_inputs:
            continue
        dr[name] = nc.dram_tensor(name, shape, dt, kind="ExternalInput").ap()
    out = nc.dram_tensor("out", [S, D], F32, kind="ExternalOutput").ap()
    import os
    if os.environ.get("KDBG"):
        dr["dbg"] = nc.dram_tensor("dbg", [128, 1024], F32, kind="ExternalOutput").ap()
    dr["wres"] = Res("weights")
    dr["qkT"] = (Res("qkT"), nc.dram_tensor("qkT_s", [2, D, S], BF16).ap())
    dr["attT"] = (Res("attT"), nc.dram_tensor("attT_s", [2 * D, S], BF16).ap())
    dr["hn"] = (Res("hn"), nc.dram_tensor("hn_s", [S, 2 * D], F32).ap())
    dr["xbf"] = (Res("xbf"), nc.dram_tensor("xbf_s", [S, D], BF16).ap())
    dr["buftok"] = (Res("buftok"), nc.dram_tensor("buftok_s", [NSLOT * 128, 1], I32).ap())
    dr["yb"] = (Res("yb"), nc.dram_tensor("yb_s", [NSLOT * 128, D], F32).ap())
    hbuf = [(Res("hA"), nc.dram_tensor("hA_s", [S, D], F32).ap(), False),
            (Res("hB"), nc.dram_tensor("hB_s", [S, D], F32).ap(), False)]
    with ExitStack() as es:
        s = Sched(nc, es)
        cx = Ctx()
        make_consts(s, cx)
        cur = (dr["wres"], dr["x"], False)
        for pi, (ph, l) in enumerate(plan):
            last = pi == len(plan) - 1
            nxt = (Res("out"), out, True) if last else hbuf[pi % 2]
            if ph == "A":
                phase_A(s, cx, dr, l, l // 3, cur, nxt)
            elif ph == "B":
                phase_B(s, cx, dr, l, l // 3, cur, nxt)
            elif ph == "C":
                phase_C(s, cx, dr, l, l // 3, cur, nxt)
            elif ph == "F":
                phase_F(s, cx, dr, l, cur, nxt)
            else:
                raise ValueError(ph)
            cur = nxt
        s.finish()
        print("instructions:", s.ninst, "sems:", s.n_sem)
    return nc


PLAN = [("A", 0), ("F", 0), ("B", 1), ("F", 1), ("C", 2), ("F", 2), ("A", 3), ("F", 3)]
_NC_CACHE = {}


def kernel(**inputs):
    x = np.ascontiguousarray(np.asarray(inputs["x"], dtype=np.float32))
    nb = x.shape[0]
    if "nc" not in _NC_CACHE:
        _NC_CACHE["nc"] = build(PLAN)
    nc = _NC_CACHE["nc"]
    shared = {}
    for name, shape, dt in IN_SPECS:
        if name == "x":
            continue
        if name == "rope_consts":
            shared[name] = rope_consts_np()
        elif name == "positions":
            shared[name] = np.ascontiguousarray(np.asarray(inputs[name]).astype(np.int32))
        else:
            shared[name] = np.ascontiguousarray(np.asarray(inputs[name], dtype=np.float32))
    in_maps = []
    for b in range(nb):
        m = dict(shared)
        m["x"] = x[b]
        in_maps.append(m)
    res = run_bass_kernel_spmd(nc, in_maps, core_ids=list(range(nb)))
    return np.stack([np.asarray(r["out"], dtype=np.float32) for r in res.results], axis=0)
```

```python
import math
import numpy as np
import concourse.bass as bass
import concourse.mybir as mybir
from concourse.bass_utils import run_bass_kernel_spmd
from contextlib import ExitStack

F32 = mybir.dt.float32
BF16 = mybir.dt.bfloat16
I32 = mybir.dt.int32
ALU = mybir.AluOpType
AF = mybir.ActivationFunctionType
AX = mybir.AxisListType

S = 4096
D = 1024
DEPTH = 4
NT = S // 128
ALPHA = (2.0 * DEPTH) ** 0.25
LN_EPS = 1e-5
TWO_PI = 2.0 * math.pi

SEM_WRAP = 30000
ENGS = ("pe", "act", "dve", "pool", "sp")


class Res:
    __slots__ = ("w", "r", "name", "excl")

    def __init__(self, name="", excl=False):
        self.w = None
        self.r = []
        self.name = name
        self.excl = excl


class T:
    __slots__ = ("t", "res")

    def __init__(self, t, res):
        self.t = t
        self.res = res

    def __getitem__(self, k):
        return self.t[k]


class Sched:
    def __init__(self, nc, es):
        self.nc = nc
        self.es = es
        self.sems = {}
        self.cur = {}
        self.waited = {e: {} for e in ENGS}
        self.pending = {e: {} for e in ENGS}
        self.dma_pool = {}
        self.dma_rr = {}
        self.n_sem = 0
        self.n_tiles = 0
        self.ninst = 0
        self.finals = []
        self.eobj = {"pe": nc.tensor, "act": nc.scalar, "dve": nc.vector, "pool": nc.gpsimd, "sp": nc.sync}
        self.phase_es = None
        self.recs = []
        self.ev_of = {}
        self.next_id = 0
        import os
        self.window = int(os.environ.get("KWIN", "48"))

    def _newsem(self, name):
        h = self.es.enter_context(self.nc.semaphore(f"{name}_{self.n_sem}"))
        self.n_sem += 1
        key = self.n_sem
        self.sems[key] = h
        return key

    def sb(self, shape, dtype, name=None):
        self.n_tiles += 1
        name = name or "t"
        es = self.phase_es or self.es
        t = es.enter_context(self.nc.sbuf_tensor(f"{name}_{self.n_tiles}", list(shape), dtype))
        return T(t, Res(name))

    def ps(self, shape, dtype, name=None):
        self.n_tiles += 1
        name = name or "p"
        es = self.phase_es or self.es
        t = es.enter_context(self.nc.psum_tensor(f"{name}_{self.n_tiles}", list(shape), dtype))
        return T(t, Res(name, excl=True))

    def begin_phase(self):
        self.phase_es = ExitStack()
        return self.phase_es

    def end_phase(self):
        self.flush()
        snap = {}
        for e, c in self.cur.items():
            snap[c[0]] = c[1]
        for q, slots in self.dma_pool.items():
            for sl in slots:
                if sl[1] > 0:
                    snap[sl[0]] = sl[1]
        for e in ENGS:
            p = self.pending[e]
            for k, v in snap.items():
                if p.get(k, 0) < v:
                    p[k] = v
        self.phase_es.close()
        self.phase_es = None

    def _deps(self, reads, writes):
        deps = set()
        for r in reads:
            r = r.res if isinstance(r, T) else r
            if r.w is not None:
                deps.add(r.w)
            if r.excl:
                deps.update(r.r)
        for w in writes:
            w = w.res if isinstance(w, T) else w
            if w.w is not None:
                deps.add(w.w)
            deps.update(w.r)
        return deps

    def _commit(self, oid, reads, writes):
        for r in reads:
            r = r.res if isinstance(r, T) else r
            r.r.append(oid)
        for w in writes:
            w = w.res if isinstance(w, T) else w
            w.w = oid
            w.r = []

    def op(self, eng, fn, reads=(), writes=(), cost=150.0):
        oid = self.next_id
        self.next_id += 1
        deps = self._deps(reads, writes)
        self._commit(oid, reads, writes)
        self.recs.append((oid, eng, False, fn, deps, float(cost), 0.0, False))

    def dma(self, q, fn, reads=(), writes=(), final=False, nbytes=65536, issue=None):
        oid = self.next_id
        self.next_id += 1
        deps = self._deps(reads, writes)
        self._commit(oid, reads, writes)
        if issue is None:
            issue = 1200.0 if q == "pool" else 80.0
        self.recs.append((oid, q, True, fn, deps, float(issue), 2000.0 + nbytes / 150.0, final))

    def flush(self):
        recs = self.recs
        self.recs = []
        if not recs:
            return
        fin = {}
        queues = {e: [] for e in ENGS}
        for r in recs:
            queues[r[1]].append(r)
        heads = {e: 0 for e in ENGS}
        placed = set()
        mine = {r[0] for r in recs}
        efree = {e: 0.0 for e in ENGS}
        dma_free = [0.0]
        order = []
        nleft = len(recs)
        W = self.window
        taken = {e: set() for e in ENGS}
        while nleft:
            best = None
            for e in ENGS:
                q = queues[e]
                h = heads[e]
                n = len(q)
                while h < n and q[h][0] in taken[e]:
                    h += 1
                heads[e] = h
                lim = min(n, h + W)
                for k in range(h, lim):
                    r = q[k]
                    if r[0] in taken[e]:
                        continue
                    ok = True
                    st_ = efree[e]
                    for d in r[4]:
                        if d in mine:
                            if d not in placed:
                                ok = False
                                break
                            if fin[d] > st_:
                                st_ = fin[d]
                    if not ok:
                        continue
                    key = (st_, r[0])
                    if best is None or key < best[0]:
                        best = (key, e, r)
                    if st_ <= efree[e]:
                        break
            assert best is not None, "scheduler deadlock"
            (st_, _), e, r = best
            taken[e].add(r[0])
            placed.add(r[0])
            nleft -= 1
            if r[2]:
                iend = st_ + r[5]
                efree[e] = iend
                t0 = max(iend, dma_free[0])
                dma_free[0] = t0 + max(0.0, r[6] - 2000.0)
                fin[r[0]] = dma_free[0] + 2000.0
            else:
                efree[e] = st_ + r[5]
                fin[r[0]] = efree[e] + 60.0
            order.append(r)
        for r in order:
            self._emit_rec(r)

    def _engine_event(self, eng):
        if eng not in self.cur or self.cur[eng][1] >= SEM_WRAP:
            self.cur[eng] = [self._newsem(eng), 0]
        c = self.cur[eng]
        c[1] += 1
        return (c[0], c[1])

    def _dma_event(self, q):
        if q not in self.dma_pool:
            self.dma_pool[q] = [[self._newsem(f"dma{q}"), 0] for _ in range(12)]
            self.dma_rr[q] = 0
        i = self.dma_rr[q]
        self.dma_rr[q] = (i + 1) % len(self.dma_pool[q])
        slot = self.dma_pool[q][i]
        extra = (slot[0], slot[1]) if slot[1] > 0 else None
        if slot[1] + 16 > SEM_WRAP:
            slot[0] = self._newsem(f"dma{q}")
            slot[1] = 0
        slot[1] += 16
        return (slot[0], slot[1]), extra

    def _emit_rec(self, r):
        oid, eng, is_dma, fn, deps, _, _, final = r
        need = self.pending[eng]
        self.pending[eng] = {}
        for d in deps:
            k, v = self.ev_of[d]
            if need.get(k, 0) < v:
                need[k] = v
        if is_dma:
            ev, extra = self._dma_event(eng)
            if extra is not None:
                k, v = extra
                if need.get(k, 0) < v:
                    need[k] = v
            inc = 16
        else:
            ev = self._engine_event(eng)
            inc = 1
        wd = self.waited[eng]
        own = self.cur.get(eng, [None])[0] if eng == "pe" else None
        e = self.eobj[eng]
        for k, v in need.items():
            if k == own:
                continue
            if wd.get(k, 0) < v:
                wd[k] = v
                e.wait_ge(self.sems[k], v)
        ins = fn(e)
        ins.then_inc(self.sems[ev[0]], inc)
        self.ev_of[oid] = ev
        self.ninst += 1
        if final:
            self.finals.append(ev)

    def finish(self):
        self.flush()
        deps = {}
        for k, v in self.finals:
            if deps.get(k, 0) < v:
                deps[k] = v
        for q, slots in self.dma_pool.items():
            for sl in slots:
                if sl[1] > 0 and deps.get(sl[0], 0) < sl[1]:
                    deps[sl[0]] = sl[1]
        for k, v in deps.items():
            self.nc.sync.wait_ge(self.sems[k], v)


def _fsz(ap):
    n = 1
    for d in ap.shape[1:]:
        n *= d
    return n


def mm(s, out_t, out_ap, lhsT_t, lhsT_ap, rhs_t, rhs_ap, start, stop):
    n = _fsz(out_ap)
    c = 30.0 + max(n, 64) * 0.45
    if lhsT_ap.dtype == F32:
        c *= 4
    s.op("pe", lambda e: e.matmul(out_ap, lhsT=lhsT_ap, rhs=rhs_ap, start=start, stop=stop),
         reads=[lhsT_t, rhs_t], writes=[out_t], cost=c)


def _ecost(eng, n):
    if eng == "dve":
        return 70.0 + 0.85 * n
    if eng == "act":
        return 200.0 + 0.65 * n
    return 160.0 + 1.6 * n


def tt(s, eng, out_t, out_ap, a_t, a_ap, b_t, b_ap, op):
    s.op(eng, lambda e: e.tensor_tensor(out=out_ap, in0=a_ap, in1=b_ap, op=op), reads=[a_t, b_t], writes=[out_t],
         cost=_ecost(eng, _fsz(out_ap)))


def ts(s, eng, out_t, out_ap, a_t, a_ap, s1, s2, op0, op1=None, extra_reads=()):
    c = _ecost(eng, _fsz(out_ap))
    if op1 is None:
        s.op(eng, lambda e: e.tensor_scalar(out=out_ap, in0=a_ap, scalar1=s1, scalar2=None, op0=op0),
             reads=[a_t, *extra_reads], writes=[out_t], cost=c)
    else:
        s.op(eng, lambda e: e.tensor_scalar(out=out_ap, in0=a_ap, scalar1=s1, scalar2=s2, op0=op0, op1=op1),
             reads=[a_t, *extra_reads], writes=[out_t], cost=c)


def stt(s, eng, out_t, out_ap, a_t, a_ap, scalar, b_t, b_ap, op0, op1, extra_reads=()):
    s.op(eng, lambda e: e.scalar_tensor_tensor(out=out_ap, in0=a_ap, scalar=scalar, in1=b_ap, op0=op0, op1=op1),
         reads=[a_t, b_t, *extra_reads], writes=[out_t], cost=_ecost(eng, _fsz(out_ap)))


def cp(s, eng, out_t, out_ap, a_t, a_ap):
    n = _fsz(out_ap)
    if eng == "act":
        s.op("act", lambda e: e.activation(out=out_ap, in_=a_ap, func=AF.Copy), reads=[a_t], writes=[out_t], cost=_ecost("act", n))
    else:
        c = _ecost(eng, n) if eng == "dve" else 160.0 + 3.0 * n
        s.op(eng, lambda e: e.tensor_copy(out=out_ap, in_=a_ap), reads=[a_t], writes=[out_t], cost=c)


def act(s, out_t, out_ap, a_t, a_ap, func, scale=1.0, bias=0.0, extra_reads=()):
    s.op("act", lambda e: e.activation(out=out_ap, in_=a_ap, func=func, bias=bias, scale=scale),
         reads=[a_t, *extra_reads], writes=[out_t], cost=_ecost("act", _fsz(out_ap)))


def _nbytes(ap):
    n = 1
    for d in ap.shape:
        n *= d
    return n * (2 if ap.dtype == BF16 else 4)


def ld(s, q, out_t, out_ap, src_res, src_ap):
    s.dma(q, lambda e: e.dma_start(out=out_ap, in_=src_ap), reads=[src_res], writes=[out_t], nbytes=_nbytes(out_ap))


def st(s, q, dst_res, dst_ap, in_t, in_ap, final=False):
    s.dma(q, lambda e: e.dma_start(out=dst_ap, in_=in_ap), reads=[in_t], writes=[dst_res], final=final, nbytes=_nbytes(in_ap))


class Ctx:
    pass


def wload(s, cx, dst_t, dst_ap, wres, src_ap, shape):
    stg = cx.stg[cx.stg_i % 2]
    cx.stg_i += 1
    n = int(np.prod(shape))
    if len(shape) == 1:
        v = stg[:, 0:n]
    else:
        v = stg[:, 0:n].rearrange("p (a b) -> p a b", a=shape[0])
    ld(s, "sp", stg, v, wres, src_ap)
    cp(s, "dve" if cx.stg_i % 2 else "act", dst_t, dst_ap, stg, v)


def make_consts(s, cx):
    nc = s.nc
    iot = s.sb([128, 128], I32, "iot")
    iof = s.sb([128, 128], F32, "iof")
    cx.iof = iof
    cx.ident_f = s.sb([128, 128], F32, "identf")
    cx.ident_b = s.sb([128, 128], BF16, "identb")
    cx.maskA = s.sb([128, 2, 2, 128], BF16, "maskA")
    cx.perm = s.sb([128, 128], BF16, "perm")
    cx.ones_b = s.sb([128, 128], BF16, "onesb")
    cx.ones_f = s.sb([128, 128], F32, "onesf")
    cx.tri_le = s.sb([128, 128], F32, "trile")
    cx.tri_b = s.sb([128, 128], BF16, "trib")
    cx.stg = [s.sb([128, 2048], F32, "stg") for _ in range(2)]
    cx.stg_i = 0
    s.op("pool", lambda e: e.iota(iot[:, :], pattern=[[1, 128]], base=0, channel_multiplier=-1), writes=[iot])
    cp(s, "dve", iof, iof[:, :], iot, iot[:, :])
    ts(s, "dve", cx.ident_f, cx.ident_f[:, :], iof, iof[:, :], 0.0, None, ALU.is_equal)
    cp(s, "dve", cx.ident_b, cx.ident_b[:, :], cx.ident_f, cx.ident_f[:, :])
    ts(s, "dve", cx.tri_le, cx.tri_le[:, :], iof, iof[:, :], 0.0, None, ALU.is_ge)
    cp(s, "dve", cx.tri_b, cx.tri_b[:, :], cx.tri_le, cx.tri_le[:, :])
    for hh in range(2):
        ts(s, "dve", cx.maskA, cx.maskA[:, hh, 0, :], iof, iof[:, :], 0.0, None, ALU.is_ge)
        ts(s, "dve", cx.maskA, cx.maskA[:, hh, 1, :], iof, iof[:, :], 0.0, None, ALU.is_le)
    s.op("pool", lambda e: e.memset(cx.perm[:, :], 0.0), writes=[cx.perm])
    s.op("pool", lambda e: e.memset(cx.ones_b[:, :], 1.0), writes=[cx.ones_b])
    s.op("pool", lambda e: e.memset(cx.ones_f[:, :], 1.0), writes=[cx.ones_f])
    for hh in range(2):
        c0 = hh * 64
        ts(s, "dve", cx.perm, cx.perm[:, c0:c0 + 8], iof, iof[:, c0:c0 + 8], -8.0, None, ALU.is_equal)
        ts(s, "dve", cx.perm, cx.perm[:, c0 + 8:c0 + 16], iof, iof[:, c0 + 8:c0 + 16], 8.0, None, ALU.is_equal)


def sin_table(s, out_t, out_ap, x_t, x_ap, shape, sign_ap, sign_t, scratch):
    ki, kf, r = scratch
    C1 = 6.28125
    C2 = TWO_PI - C1
    ts(s, "dve", ki, ki[:, :], x_t, x_ap, 1.0 / TWO_PI, None, ALU.mult)
    cp(s, "dve", kf, kf[:, :], ki, ki[:, :])
    stt(s, "dve", r, r[:, :], kf, kf[:, :], -C1, x_t, x_ap, ALU.mult, ALU.add)
    stt(s, "dve", r, r[:, :], kf, kf[:, :], -C2, r, r[:, :], ALU.mult, ALU.add)
    ts(s, "dve", kf, kf[:, :], r, r[:, :], math.pi, None, ALU.is_gt)
    stt(s, "dve", r, r[:, :], kf, kf[:, :], -TWO_PI, r, r[:, :], ALU.mult, ALU.add)
    ts(s, "dve", kf, kf[:, :], r, r[:, :], -math.pi, None, ALU.is_lt)
    stt(s, "dve", r, r[:, :], kf, kf[:, :], TWO_PI, r, r[:, :], ALU.mult, ALU.add)
    ts(s, "dve", r, r[:, :], r, r[:, :], 3.14159, -3.14159, ALU.min, ALU.max)
    act(s, out_t, out_ap, r, r[:, :], AF.Sin)
    if sign_ap is not None:
        ts(s, "dve", out_t, out_ap, out_t, out_ap, sign_ap, None, ALU.mult, extra_reads=[sign_t])


def load_xT(s, cx, h_res, h_ap, xT):
    old = s.phase_es
    s.phase_es = ExitStack()
    hfs = [s.sb([128, 1024], F32, "hf") for _ in range(3)]
    pts = [s.ps([128, 512], F32, "ptr") for _ in range(2)]
    for i in range(NT):
        hf = hfs[i % 3]
        ld(s, "sp", hf, hf[:, :], h_res, h_ap[i * 128:(i + 1) * 128, :])
        for half in range(2):
            pt = pts[half]
            for c4 in range(4):
                c = half * 4 + c4
                s.op("pe", lambda e, pt=pt, c4=c4, c=c, hf=hf: e.transpose(
                    out=pt[:, c4 * 128:(c4 + 1) * 128], in_=hf[:, c * 128:(c + 1) * 128], identity=cx.ident_f[:, :]),
                    reads=[hf, cx.ident_f], writes=[pt])
            dst = xT[:, half * 4:(half + 1) * 4, i * 128:(i + 1) * 128]
            src = pt[:, :].rearrange("p (c t) -> p c t", c=4)
            cp(s, "act" if half else "dve", xT, dst, pt, src)
    s.end_phase()
    s.phase_es = old


def ln_epilogue(s, cx, y_ps, hf, g_bc, b_bc, wk, out_t):
    r, stats, mv, sd, rstd, nmr, xn = wk
    for half in range(2):
        sl = slice(half * 512, (half + 1) * 512)
        yt, yap = y_ps[half] if isinstance(y_ps[half], tuple) else (y_ps[half], y_ps[half][:, :])
        stt(s, "dve", r, r[:, sl], hf, hf[:, sl], ALPHA, yt, yap, ALU.mult, ALU.add)
        s.op("dve", lambda e, half=half, sl=sl: e.bn_stats(out=stats[:, half, :], in_=r[:, sl]), reads=[r], writes=[stats])
    s.op("dve", lambda e: e.bn_aggr(out=mv[:, :], in_=stats[:, :, :].rearrange("p a b -> p (a b)")), reads=[stats], writes=[mv])
    ts(s, "dve", sd, sd[:, :], mv, mv[:, 1:2], LN_EPS, None, ALU.add)
    act(s, sd, sd[:, :], sd, sd[:, :], AF.Sqrt)
    s.op("dve", lambda e: e.reciprocal(out=rstd[:, :], in_=sd[:, :]), reads=[sd], writes=[rstd])
    stt(s, "dve", nmr, nmr[:, :], mv, mv[:, 0:1], -1.0, rstd, rstd[:, :], ALU.mult, ALU.mult)
    act(s, xn, xn[:, :], r, r[:, :], AF.Identity, scale=rstd[:, 0:1], bias=nmr[:, 0:1], extra_reads=[rstd, nmr])
    tt(s, "pool", xn, xn[:, :], xn, xn[:, :], g_bc, g_bc[:, :], ALU.mult)
    tt(s, "pool", out_t, out_t[:, :], xn, xn[:, :], b_bc, b_bc[:, :], ALU.add)


def ln_work(s):
    return (s.sb([128, 1024], F32, "lnr"), s.sb([128, 2, 6], F32, "lnst"), s.sb([128, 2], F32, "lnmv"),
            s.sb([128, 1], F32, "lnsd"), s.sb([128, 1], F32, "lnrs"), s.sb([128, 1], F32, "lnnm"),
            s.sb([128, 1024], F32, "lnxn"))


def bcast_row(ap_row, n):
    return ap_row.partition_broadcast(128)


def phase_A(s, cx, dr, l, j, h_in, h_out):
    nc = s.nc
    h_in_res, h_in_ap = h_in[0], h_in[1]
    w_in = dr["a_w_in"][j]
    w_out = dr["a_w_out"][j]
    w_in_v = w_in.rearrange("(c p) n -> p c n", p=128)
    qkT_res, qkT = dr["qkT"]
    attT_res, attT = dr["attT"]
    wres = dr["wres"]

    s.begin_phase()
    xT = s.sb([128, 8, S], BF16, "xT")
    load_xT(s, cx, h_in_res, h_in_ap, xT)

    import os
    stop = int(os.environ.get("KSTOP", "9"))
    if stop < 1:
        s.end_phase()
        return
    with ExitStack() as es1:
        old = s.phase_es
        s.phase_es = es1
        wqk = s.sb([128, 8, 2048], BF16, "wqk")
        for c in range(8):
            wload(s, cx, wqk, wqk[:, c, :], wres, w_in[c * 128:(c + 1) * 128, 0:2048], [2048])
        rc = s.sb([128, 4], F32, "ropec")
        ld(s, "sp", rc, rc[:, :], wres, dr["rope_consts"])
        posi = s.sb([128, 512], I32, "posi")
        posf = s.sb([128, 512], F32, "posf")
        ang = s.sb([128, 512], F32, "ang")
        ang2 = s.sb([128, 512], F32, "ang2")
        Ct = s.sb([128, 512], F32, "Ct")
        St = s.sb([128, 512], F32, "St")
        scr = (s.sb([128, 512], I32, "ki"), s.sb([128, 512], F32, "kf"), s.sb([128, 512], F32, "rr"))
        psq = [s.ps([128, 512], F32, "psq") for _ in range(2)]
        psp = [s.ps([128, 512], F32, "psp") for _ in range(2)]
        qbs = [s.sb([128, 512], BF16, "qb") for _ in range(2)]
        t1s = [s.sb([128, 512], F32, "t1") for _ in range(2)]
        t2s = [s.sb([128, 512], F32, "t2") for _ in range(2)]
        obs = [s.sb([128, 512], BF16, "ob") for _ in range(3)]
        it = 0
        ksub = int(os.environ.get("KSUB", "99"))
        for tg in range(int(os.environ.get('KTG', '8')) if ksub > 0 else 0):
            tsl = slice(tg * 512, (tg + 1) * 512)
            ld(s, "sp", posi, posi[:, :], wres, bcast_row(dr["positions"][tg * 512:(tg + 1) * 512], 512))
            cp(s, "dve", posf, posf[:, :], posi, posi[:, :])
            ts(s, "dve", ang, ang[:, :], posf, posf[:, :], rc[:, 0:1], None, ALU.mult, extra_reads=[rc])
            ts(s, "dve", ang2, ang2[:, :], ang, ang[:, :], math.pi / 2, None, ALU.add)
            sin_table(s, St, St[:, :], ang, ang[:, :], None, rc[:, 1:2], rc, scr)
            sin_table(s, Ct, Ct[:, :], ang2, ang2[:, :], None, None, None, scr)
            for hp in range(int(os.environ.get('KHP', '8')) if ksub > 1 else 0):
                for w in range(2):
                    pq = psq[it % 2]
                    pp = psp[it % 2]
                    qb = qbs[it % 2]
                    t1 = t1s[it % 2]
                    t2 = t2s[it % 2]
                    ob = obs[it % 3]
                    it += 1
                    col = w * 1024 + hp * 128
                    for c in range(8):
                        mm(s, pq, pq[:, :], wqk, wqk[:, c, col:col + 128], xT, xT[:, c, tsl], c == 0, c == 7)
                    cp(s, "act", qb, qb[:, :], pq, pq[:, :])
                    if ksub < 3:
                        continue
                    mm(s, pp, pp[:, :], cx.perm, cx.perm[:, :], qb, qb[:, :], True, True)
                    tt(s, "dve", t1, t1[:, :], pq, pq[:, :], Ct, Ct[:, :], ALU.mult)
                    tt(s, "dve", t2, t2[:, :], pp, pp[:, :], St, St[:, :], ALU.mult)
                    tt(s, "pool", ob, ob[:, :], t1, t1[:, :], t2, t2[:, :], ALU.add)
                    if ksub < 4:
                        continue
                    st(s, "pool", qkT_res, qkT[w, hp * 128:(hp + 1) * 128, tsl], ob, ob[:, :])
        s.phase_es = old
    s_sub_barrier(s)
    if stop < 2:
        s.end_phase()
        return

    with ExitStack() as es2:
        old = s.phase_es
        s.phase_es = es2
        QTs = [s.sb([128, S], BF16, "QT") for _ in range(2)]
        KTs = [s.sb([128, S], BF16, "KT") for _ in range(2)]
        wvs = [s.sb([128, 8, 128], BF16, "wv") for _ in range(2)]
        Vds = [s.sb([128, 32, 128], BF16, "Vd") for _ in range(2)]
        acc = s.sb([128, 2, S], F32, "acc")
        obuf = s.sb([128, S], BF16, "obuf")
        psV = [s.ps([128, 512], F32, "psV") for _ in range(2)]
        psS = [s.ps([128, 512], F32, "psS") for _ in range(4)]
        psO_ = [s.ps([128, 512], F32, "psO") for _ in range(2)]
        psO = [T(p[:, 0:256].rearrange("p (a q) -> p a q", a=2), p.res) for p in psO_]
        pes = [s.sb([128, 512], BF16, "pe") for _ in range(4)]
        PTs = [s.sb([128, 2, 2, 128], BF16, "PT") for _ in range(4)]
        vi = 0
        bi_ctr = 0
        k2 = int(os.environ.get("KSUB2", "99"))
        for hp in range(int(os.environ.get("KHP2", "8"))):
            QT = QTs[hp % 2]
            KT = KTs[hp % 2]
            wv = wvs[hp % 2]
            ld(s, "sp", QT, QT[:, :], qkT_res, qkT[0, hp * 128:(hp + 1) * 128, :])
            ld(s, "sp", KT, KT[:, :], qkT_res, qkT[1, hp * 128:(hp + 1) * 128, :])
            wload(s, cx, wv, wv[:, :, :], wres, w_in_v[:, :, 2048 + hp * 128:2048 + (hp + 1) * 128], [8, 128])
            for di, d in enumerate((1, 4, 16)[:int(os.environ.get("KBR", "3"))] if k2 > 0 else ()):
                nb = 32 // d
                Vd = Vds[vi % 2]
                vi += 1

                def tok(r, n, d=d):
                    return slice(r + d * 128 * n, r + d * 128 * n + 127 * d + 1, d)
                for b in range(32):
                    r, n = divmod(b, nb)
                    pv = psV[(b // 4) % 2]
                    for c in range(8):
                        mm(s, pv, pv[:, (b % 4) * 128:(b % 4 + 1) * 128], xT, xT[:, c, tok(r, n)], wv, wv[:, c, :], c == 0, c == 7)
                    if b % 4 == 3:
                        cp(s, "act" if (b // 4) % 2 else "dve", Vd, Vd[:, b - 3:b + 1, :], pv,
                           pv[:, :].rearrange("p (b f) -> p b f", b=4))
                for b2 in range(int(os.environ.get("KB", "16")) if k2 > 1 else 0):
                    blocks = (2 * b2, 2 * b2 + 1)
                    n0 = blocks[0] % nb
                    PTh = []
                    for hh in range(2):
                        hs = slice(hh * 64, (hh + 1) * 64)
                        pS = psS[(2 * bi_ctr + hh) % 4]
                        pe_ = pes[(2 * bi_ctr + hh) % 4]
                        PT = PTs[(2 * bi_ctr + hh) % 4]
                        PTh.append(PT)
                        pS4 = pS[:, :].rearrange("p (b c q) -> p b c q", b=2, c=2)
                        pe4 = pe_[:, :].rearrange("p (b c q) -> p b c q", b=2, c=2)
                        for bi, b in enumerate(blocks):
                            r, n = divmod(b, nb)
                            mm(s, pS, pS4[:, bi, 0, :], KT, KT[hs, tok(r, n)], QT, QT[hs, tok(r, n)], True, True)
                            if n > 0:
                                mm(s, pS, pS4[:, bi, 1, :], KT, KT[hs, tok(r, n - 1)], QT, QT[hs, tok(r, n)], True, True)
                        if n0 > 0:
                            act(s, pe_, pe_[:, :], pS, pS[:, :], AF.Exp, scale=0.125)
                            tt(s, "pool", PT, PT[:, :, :, :], pe_, pe4, cx.maskA, cx.maskA[:, :, :, :], ALU.mult)
                        else:
                            act(s, pe_, pe4[:, 0, 0, :], pS, pS4[:, 0, 0, :], AF.Exp, scale=0.125)
                            act(s, pe_, pe4[:, 1, :, :], pS, pS4[:, 1, :, :], AF.Exp, scale=0.125)
                            tt(s, "pool", PT, PT[:, 0, 0, :], pe_, pe4[:, 0, 0, :], cx.maskA, cx.maskA[:, 0, 0, :], ALU.mult)
                            tt(s, "pool", PT, PT[:, 1, :, :], pe_, pe4[:, 1, :, :], cx.maskA, cx.maskA[:, 1, :, :], ALU.mult)
                    bi_ctr += 1
                    if k2 < 3:
                        continue
                    for bi, b in enumerate(blocks):
                        r, n = divmod(b, nb)
                        pO = psO[b % 2]
                        for hh in range(2):
                            hs = slice(hh * 64, (hh + 1) * 64)
                            PT = PTh[hh]
                            mm(s, pO, pO[hs, 0, :], Vd, Vd[:, b, hs], PT, PT[:, bi, 0, :], True, n == 0)
                            if n > 0:
                                mm(s, pO, pO[hs, 0, :], Vd, Vd[:, b - 1, hs], PT, PT[:, bi, 1, :], False, True)
                            mm(s, pO, pO[hs, 1, :], cx.ones_b, cx.ones_b[:, 0:64], PT, PT[:, bi, 0, :], True, n == 0)
                            if n > 0:
                                mm(s, pO, pO[hs, 1, :], cx.ones_b, cx.ones_b[:, 0:64], PT, PT[:, bi, 1, :], False, True)
                        if k2 < 4:
                            continue
                        if di == 0:
                            cp(s, "dve", acc, acc[:, :, tok(r, n)], pO, pO[:, :, :])
                        else:
                            tt(s, "dve", acc, acc[:, :, tok(r, n)], acc, acc[:, :, tok(r, n)], pO, pO[:, :, :], ALU.add)
            if k2 < 5:
                continue
            s.op("dve", lambda e: e.reciprocal(out=acc[:, 1, :], in_=acc[:, 1, :]), reads=[acc], writes=[acc])
            tt(s, "dve", obuf, obuf[:, :], acc, acc[:, 0, :], acc, acc[:, 1, :], ALU.mult)
            st(s, "sp", attT_res, attT[hp * 128:(hp + 1) * 128, :], obuf, obuf[:, :])
        s.phase_es = old
    s.end_phase()

    if stop < 3:
        return
    s.begin_phase()
    proj_ln(s, cx, dr, attT_res, attT, 8, w_out, h_in, h_out, dr["ln1_g"][l], dr["ln1_b"][l])
    s.end_phase()


def s_sub_barrier(s):
    es = s.phase_es
    s.phase_es = ExitStack()
    s.end_phase()
    s.phase_es = es


def proj_ln(s, cx, dr, aT_res, aT, nchunk, w_out, h_in, h_out, g_row, b_row):
    h_in_res, h_in_ap = h_in[0], h_in[1]
    h_out_res, h_out_ap, final = h_out
    wres = dr["wres"]
    wo = s.sb([128, nchunk, 1024], BF16, "wo")
    for c in range(nchunk):
        wload(s, cx, wo, wo[:, c, :], wres, w_out[c * 128:(c + 1) * 128, :], [1024])
    g_bc = s.sb([128, 1024], F32, "gbc")
    b_bc = s.sb([128, 1024], F32, "bbc")
    ld(s, "sp", g_bc, g_bc[:, :], wres, bcast_row(g_row, 1024))
    ld(s, "sp", b_bc, b_bc[:, :], wres, bcast_row(b_row, 1024))
    ats = [s.sb([128, nchunk, 512], BF16, "at") for _ in range(2)]
    hfs = [s.sb([128, 1024], F32, "hf2") for _ in range(2)]
    hos = [s.sb([128, 1024], F32, "ho") for _ in range(2)]
    psY = [[s.ps([128, 512], F32, "psY") for _ in range(2)] for _ in range(2)]
    wk = ln_work(s)
    aTv = aT[0:nchunk * 128, :].rearrange("(c p) t -> p c t", p=128)
    for tg in range(8):
        at = ats[tg % 2]
        ld(s, "sp", at, at[:, :, :], aT_res, aTv[:, :, tg * 512:(tg + 1) * 512])
        for ti in range(4):
            i = tg * 4 + ti
            hf = hfs[i % 2]
            ho = hos[i % 2]
            py = psY[i % 2]
            ld(s, "sp", hf, hf[:, :], h_in_res, h_in_ap[i * 128:(i + 1) * 128, :])
            for half in range(2):
                for c in range(nchunk):
                    mm(s, py[half], py[half][:, :], at, at[:, c, ti * 128:(ti + 1) * 128], wo,
                       wo[:, c, half * 512:(half + 1) * 512], c == 0, c == nchunk - 1)
            ln_epilogue(s, cx, py, hf, g_bc, b_bc, wk, ho)
            st(s, "pool", h_out_res, h_out_ap[i * 128:(i + 1) * 128, :], ho, ho[:, :], final=final)


def decay_tables(s, cx, LF, ig, nh, kscale_ln, EA, EB, ET, psX):
    n = NT * nh
    Fin = s.sb([128, NT, nh], F32, "Fin")
    tot = s.sb([128, NT, nh], F32, "tot")
    inc = s.sb([128, NT, nh], F32, "inc")
    LF2 = LF[:, :, :].rearrange("p a b -> p (a b)")
    mm(s, psX, psX[:, 0:n], cx.tri_le, cx.tri_le[:, :], LF, LF2, True, True)
    cp(s, "dve", Fin, Fin[:, :, :].rearrange("p a b -> p (a b)"), psX, psX[:, 0:n])
    mm(s, psX, psX[:, 0:n], cx.ones_f, cx.ones_f[:, :], LF, LF2, True, True)
    cp(s, "dve", tot, tot[:, :, :].rearrange("p a b -> p (a b)"), psX, psX[:, 0:n])
    for h in range(nh):
        s.op("dve", lambda e, h=h: e.tensor_tensor_scan(out=inc[:, :, h], data0=cx.ones_f[:, 0:NT], data1=tot[:, :, h],
                                                        initial=0.0, op0=ALU.mult, op1=ALU.add),
             reads=[cx.ones_f, tot], writes=[inc])
    tt(s, "dve", EB, EB[:, :, :], inc, inc[:, :, :], tot, tot[:, :, :], ALU.subtract)
    tt(s, "dve", EA, EA[:, :, :], Fin, Fin[:, :, :], EB, EB[:, :, :], ALU.add)
    if ig is not None:
        tt(s, "dve", EA, EA[:, :, :], ig[0], ig[1], EA, EA[:, :, :], ALU.subtract)
        ts(s, "dve", EA, EA[:, :, :], EA, EA[:, :, :], kscale_ln, None, ALU.add)
    else:
        ts(s, "dve", EA, EA[:, :, :], EA, EA[:, :, :], -1.0, kscale_ln, ALU.mult, ALU.add)
    act(s, ET, ET[:, :, :], Fin, Fin[:, :, :], AF.Exp)


def make_E(s, E, EA, EB, h, tmpE):
    tt(s, "pool", tmpE, tmpE[:, :, :], EA, EA[:, :, h].unsqueeze(2).to_broadcast([128, NT, NT]),
       EB, EB[:, :, h].unsqueeze(1).to_broadcast([128, NT, NT]), ALU.add)
    ts(s, "pool", tmpE, tmpE[:, :, :], tmpE, tmpE[:, :, :], 80.0, None, ALU.min)
    act(s, E, E[:, :, :], tmpE, tmpE[:, :, :], AF.Exp)


def decay_main(s, cx, heads, psS, psAcc, PTs, fin_cb, jmax=NT):
    cnt = [0] * len(heads)
    for j in range(jmax):
        for hi, hd in enumerate(heads):
            acc = psAcc[hi]
            dvw = hd["dvw"]
            nk = len(hd["kparts"])
            for i0 in range(0, j + 1, 4):
                i1 = min(i0 + 4, j + 1)
                n = i1 - i0
                pS = psS[hi][cnt[hi] % 2]
                PT = PTs[hi][cnt[hi] % 2]
                cnt[hi] += 1
                for ii in range(n):
                    i = i0 + ii
                    for kc, (Kt, Kap, Qt, Qap) in enumerate(hd["kparts"]):
                        mm(s, pS, pS[:, ii * 128:(ii + 1) * 128], Kt, Kap(i), Qt, Qap(j), kc == 0, kc == nk - 1)
                E = hd["E"]
                tt(s, "dve", PT, PT[:, 0:n, :], pS, pS[:, 0:n * 128].rearrange("p (a t) -> p a t", a=n),
                   E, E[:, i0:i1, j].unsqueeze(2).to_broadcast([128, n, 128]), ALU.mult)
                if i1 - 1 == j:
                    tt(s, "pool", PT, PT[:, n - 1, :], PT, PT[:, n - 1, :], cx.tri_b, cx.tri_b[:, :], ALU.mult)
                for ii in range(n):
                    i = i0 + ii
                    mm(s, acc, acc[:, 0:dvw], PT, PT[:, ii, :], hd["V"], hd["Vap"](i), i == 0, i == j)
            fin_cb(hi, hd, j, acc)


def small_norm(s, x_t, x_ap, width, wk, out_t, out_ap, g_t, g_ap):
    stats, mv, sd, rstd, nmr, xn = wk
    s.op("dve", lambda e: e.bn_stats(out=stats[:, :], in_=x_ap), reads=[x_t], writes=[stats])
    s.op("dve", lambda e: e.bn_aggr(out=mv[:, :], in_=stats[:, :]), reads=[stats], writes=[mv])
    ts(s, "dve", sd, sd[:, :], mv, mv[:, 1:2], LN_EPS, None, ALU.add)
    act(s, sd, sd[:, :], sd, sd[:, :], AF.Sqrt)
    s.op("dve", lambda e: e.reciprocal(out=rstd[:, :], in_=sd[:, :]), reads=[sd], writes=[rstd])
    stt(s, "dve", nmr, nmr[:, :], mv, mv[:, 0:1], -1.0, rstd, rstd[:, :], ALU.mult, ALU.mult)
    act(s, xn, xn[:, 0:width], x_t, x_ap, AF.Identity, scale=rstd[:, 0:1], bias=nmr[:, 0:1], extra_reads=[rstd, nmr])
    tt(s, "pool", out_t, out_ap, xn, xn[:, 0:width], g_t, g_ap, ALU.mult)


def small_norm_work(s, width):
    return (s.sb([128, 6], F32, "snst"), s.sb([128, 2], F32, "snmv"), s.sb([128, 1], F32, "snsd"),
            s.sb([128, 1], F32, "snrs"), s.sb([128, 1], F32, "snnm"), s.sb([128, width], F32, "snxn"))


def gate_proj_ln(s, cx, dr, xT, HN_res, HN, F, w_gate_ap, gfunc, w_out, h_in, h_out, g_row, b_row):
    h_in_res, h_in_ap = h_in[0], h_in[1]
    h_out_res, h_out_ap, final = h_out
    wres = dr["wres"]
    nch = F // 128
    nb = F // 512
    wg = s.sb([128, 8, F], BF16, "wgate")
    for c in range(8):
        for q in range(F // 1024):
            wload(s, cx, wg, wg[:, c, q * 1024:(q + 1) * 1024], wres, w_gate_ap[c * 128:(c + 1) * 128, q * 1024:(q + 1) * 1024], [1024])
    wo = s.sb([128, nch, 1024], BF16, "wo")
    for c in range(nch):
        wload(s, cx, wo, wo[:, c, :], wres, w_out[c * 128:(c + 1) * 128, :], [1024])
    g_bc = s.sb([128, 1024], F32, "gbc")
    b_bc = s.sb([128, 1024], F32, "bbc")
    ld(s, "sp", g_bc, g_bc[:, :], wres, bcast_row(g_row, 1024))
    ld(s, "sp", b_bc, b_bc[:, :], wres, bcast_row(b_row, 1024))
    nbuf = 1 if F > 1024 else 2
    hns = [s.sb([128, F], F32, "hn") for _ in range(nbuf)]
    sig = s.sb([128, F], F32, "sig")
    Gb = s.sb([128, F], BF16, "Gb")
    GT_ = s.sb([128, nch, 128], BF16, "GTt")
    hfs = [s.sb([128, 1024], F32, "hf2") for _ in range(nbuf)]
    hos = [s.sb([128, 1024], F32, "ho") for _ in range(nbuf)]
    psg = [s.ps([128, 512], F32, "psg") for _ in range(2)]
    pst = [s.ps([128, 1024], BF16, "pst") for _ in range(2)]
    psY = [s.ps([128, 512], F32, "psY") for _ in range(2)]
    wk = ln_work(s)
    for i in range(NT):
        hn = hns[i % nbuf]
        hf = hfs[i % nbuf]
        ho = hos[i % nbuf]
        ld(s, "sp", hn, hn[:, :], HN_res, HN[i * 128:(i + 1) * 128, 0:F])
        ld(s, "sp", hf, hf[:, :], h_in_res, h_in_ap[i * 128:(i + 1) * 128, :])
        for q in range(nb):
            pg = psg[q % 2]
            for c in range(8):
                mm(s, pg, pg[:, :], xT, xT[:, c, i * 128:(i + 1) * 128], wg, wg[:, c, q * 512:(q + 1) * 512], c == 0, c == 7)
            act(s, sig, sig[:, q * 512:(q + 1) * 512], pg, pg[:, :], gfunc)
        tt(s, "pool", Gb, Gb[:, :], hn, hn[:, :], sig, sig[:, :], ALU.mult)
        for q in range(nch // 8):
            pt = pst[q % 2]
            for c8 in range(8):
                c = q * 8 + c8
                s.op("pe", lambda e, pt=pt, c8=c8, c=c: e.transpose(out=pt[:, c8 * 128:(c8 + 1) * 128], in_=Gb[:, c * 128:(c + 1) * 128],
                                                                   identity=cx.ident_b[:, :]), reads=[Gb, cx.ident_b], writes=[pt])
            cp(s, "act" if q else "dve", GT_, GT_[:, q * 8:(q + 1) * 8, :], pt, pt[:, :].rearrange("p (c t) -> p c t", c=8))
        for half in range(2):
            for c in range(nch):
                mm(s, psY[half], psY[half][:, :], GT_, GT_[:, c, :], wo, wo[:, c, half * 512:(half + 1) * 512], c == 0, c == nch - 1)
        ln_epilogue(s, cx, psY, hf, g_bc, b_bc, wk, ho)
        st(s, "pool", h_out_res, h_out_ap[i * 128:(i + 1) * 128, :], ho, ho[:, :], final=final)


def phase_B(s, cx, dr, l, j_, h_in, h_out):
    import os
    nc = s.nc
    h_in_res, h_in_ap = h_in[0], h_in[1]
    wres = dr["wres"]
    w_in = dr["b_w_in"][j_]
    w_in_v = w_in.rearrange("(c p) n -> p c n", p=128)
    HN_res, HN = dr["hn"]
    bstop = int(os.environ.get("BSTOP", "9"))
    jmax = int(os.environ.get("BJMAX", str(NT)))
    npair = int(os.environ.get("BPAIRS", "4"))

    s.begin_phase()
    xT = s.sb([128, 8, S], BF16, "xT")
    GATES = s.sb([128, NT, 16], F32, "GATES")
    with ExitStack() as es0:
        old = s.phase_es
        s.phase_es = es0
        Wg32 = s.sb([128, 8, 16], F32, "Wg32")
        ld(s, "sp", Wg32, Wg32[:, :, :], wres, w_in_v[:, :, 3072:3088])
        gb = s.sb([128, 16], F32, "gbias")
        ld(s, "sp", gb, gb[:, :], wres, bcast_row(dr["b_gate_bias"][j_], 16))
        hfs = [s.sb([128, 1024], F32, "hf") for _ in range(3)]
        xTts = [s.sb([128, 8, 128], F32, "xTt") for _ in range(2)]
        pts = [s.ps([128, 512], F32, "ptr") for _ in range(2)]
        psg = s.ps([128, 512], F32, "psgate")
        for i in range(NT):
            hf = hfs[i % 3]
            xTt = xTts[i % 2]
            ld(s, "sp", hf, hf[:, :], h_in_res, h_in_ap[i * 128:(i + 1) * 128, :])
            for half in range(2):
                pt = pts[half]
                for c4 in range(4):
                    c = half * 4 + c4
                    s.op("pe", lambda e, pt=pt, c4=c4, c=c, hf=hf: e.transpose(
                        out=pt[:, c4 * 128:(c4 + 1) * 128], in_=hf[:, c * 128:(c + 1) * 128], identity=cx.ident_f[:, :]),
                        reads=[hf, cx.ident_f], writes=[pt])
                src = pt[:, :].rearrange("p (c t) -> p c t", c=4)
                cp(s, "act", xT, xT[:, half * 4:(half + 1) * 4, i * 128:(i + 1) * 128], pt, src)
                cp(s, "dve", xTt, xTt[:, half * 4:(half + 1) * 4, :], pt, src)
            for c in range(8):
                mm(s, psg, psg[:, 0:16], xTt, xTt[:, c, :], Wg32, Wg32[:, c, :], c == 0, c == 7)
            tt(s, "dve", GATES, GATES[:, i, :], psg, psg[:, 0:16], gb, gb[:, :], ALU.add)
        s.phase_es = old
    s_sub_barrier(s)

    EA = s.sb([128, NT, 8], F32, "EA")
    EB = s.sb([128, NT, 8], F32, "EB")
    ET = s.sb([128, NT, 8], F32, "ET")
    with ExitStack() as es1:
        old = s.phase_es
        s.phase_es = es1
        LF = s.sb([128, NT, 8], F32, "LF")
        psX = s.ps([128, 512], F32, "psX")
        act(s, LF, LF[:, :, :], GATES, GATES[:, :, 8:16], AF.Exp, scale=-1.0)
        ts(s, "dve", LF, LF[:, :, :], LF, LF[:, :, :], 1.0, None, ALU.add)
        act(s, LF, LF[:, :, :], LF, LF[:, :, :], AF.Ln)
        ts(s, "dve", LF, LF[:, :, :], LF, LF[:, :, :], -1.0, None, ALU.mult)
        decay_tables(s, cx, LF, (GATES, GATES[:, :, 0:8]), 8, math.log(0.125), EA, EB, ET, psX)
        s.phase_es = old
    s_sub_barrier(s)
    if bstop < 2:
        s.end_phase()
        return

    with ExitStack() as es2:
        old = s.phase_es
        s.phase_es = es2
        ng_bc = s.sb([128, 1024], F32, "ngbc")
        ld(s, "sp", ng_bc, ng_bc[:, :], wres, bcast_row(dr["b_norm_g"][j_], 1024))
        wq = s.sb([128, 8, 128], BF16, "wq")
        wkk = s.sb([128, 8, 128], BF16, "wk")
        wv = s.sb([128, 8, 256], BF16, "wv")
        cw = [s.sb([128, 4], F32, "cw") for _ in range(2)]
        cb = [s.sb([128, 1], F32, "cb") for _ in range(2)]
        UT = s.sb([128, S + 3], F32, "UT")
        s.op("pool", lambda e: e.memset(UT[:, 0:3], 0.0), writes=[UT])
        cacc = s.sb([128, S], F32, "cacc")
        QT = s.sb([128, S], BF16, "QT")
        KT = s.sb([128, S], BF16, "KT")
        Vaug = s.sb([128, NT, 2, 129], BF16, "Vaug")
        s.op("pool", lambda e: e.memset(Vaug[:, :, :, 128:129], 1.0), writes=[Vaug])
        Es = [s.sb([128, NT, NT], F32, "E") for _ in range(2)]
        tmpE = s.sb([128, NT, NT], F32, "tmpE")
        psX = [s.ps([128, 512], F32, "psX") for _ in range(2)]
        psS = [[s.ps([128, 512], F32, "psS") for _ in range(2)] for _ in range(2)]
        psAcc = [s.ps([128, 512], F32, "psAcc") for _ in range(2)]
        PTs = [[s.sb([128, 4, 128], BF16, "PT") for _ in range(2)] for _ in range(2)]
        snw = small_norm_work(s, 128)
        sm = {k: s.sb([128, 1], F32, k) for k in ("ed", "ad", "rec", "fac")}
        hnr = s.sb([128, 128], F32, "hnr")
        HNt = [s.sb([128, 256], F32, "HNt") for _ in range(2)]
        for hp in range(npair):
            for w, (wt, dst) in enumerate(((wq, QT), (wkk, KT))):
                col = w * 512 + hp * 128
                wload(s, cx, wt, wt[:, :, :], wres, w_in_v[:, :, col:col + 128], [8, 128])
                for jj in range(4):
                    ld(s, "sp", cw[w], cw[w][:, jj:jj + 1], wres, dr["b_conv_w"][j_][jj, col:col + 128].rearrange("(c o) -> c o", o=1))
                ld(s, "sp", cb[w], cb[w][:, :], wres, dr["b_conv_b"][j_][col:col + 128].rearrange("(c o) -> c o", o=1))
                for tg in range(8):
                    px = psX[tg % 2]
                    for c in range(8):
                        mm(s, px, px[:, :], wt, wt[:, c, :], xT, xT[:, c, tg * 512:(tg + 1) * 512], c == 0, c == 7)
                    cp(s, "act", UT, UT[:, 3 + tg * 512:3 + (tg + 1) * 512], px, px[:, :])
                ts(s, "dve", cacc, cacc[:, :], UT, UT[:, 0:S], cw[w][:, 0:1], None, ALU.mult, extra_reads=[cw[w]])
                for jj in range(1, 4):
                    stt(s, "dve", cacc, cacc[:, :], UT, UT[:, jj:S + jj], cw[w][:, jj:jj + 1], cacc, cacc[:, :], ALU.mult, ALU.add,
                        extra_reads=[cw[w]])
                act(s, dst, dst[:, :], cacc, cacc[:, :], AF.Silu, scale=1.0, bias=cb[w][:, 0:1], extra_reads=[cb[w]])
            wload(s, cx, wv, wv[:, :, :], wres, w_in_v[:, :, 1024 + hp * 256:1024 + (hp + 1) * 256], [8, 256])
            for i in range(NT):
                px = psX[i % 2]
                for c in range(8):
                    mm(s, px, px[:, 0:256], xT, xT[:, c, i * 128:(i + 1) * 128], wv, wv[:, c, :], c == 0, c == 7)
                cp(s, "act", Vaug, Vaug[:, i, :, 0:128], px, px[:, 0:256].rearrange("p (a d) -> p a d", a=2))
            heads = []
            for hh in range(2):
                h = hp * 2 + hh
                make_E(s, Es[hh], EA, EB, h, tmpE)
                hs = slice(hh * 64, (hh + 1) * 64)
                heads.append(dict(
                    kparts=[(KT, (lambda i, hs=hs: KT[hs, i * 128:(i + 1) * 128]), QT, (lambda j, hs=hs: QT[hs, j * 128:(j + 1) * 128]))],
                    V=Vaug, Vap=(lambda i, hh=hh: Vaug[:, i, hh, :]), dvw=129, E=Es[hh], h=h, hh=hh))

            def fin(hi, hd, j, acc):
                h, hh = hd["h"], hd["hh"]
                tt(s, "dve", sm["ed"], sm["ed"][:, :], acc, acc[:, 128:129], ET, ET[:, j, h:h + 1], ALU.mult)
                stt(s, "dve", sm["ad"], sm["ad"][:, :], sm["ed"], sm["ed"][:, :], -1.0, sm["ed"], sm["ed"][:, :], ALU.mult, ALU.max)
                ts(s, "dve", sm["ad"], sm["ad"][:, :], sm["ad"], sm["ad"][:, :], 1.0, None, ALU.max)
                s.op("dve", lambda e: e.reciprocal(out=sm["rec"][:, :], in_=sm["ad"][:, :]), reads=[sm["ad"]], writes=[sm["rec"]])
                tt(s, "dve", sm["fac"], sm["fac"][:, :], sm["rec"], sm["rec"][:, :], ET, ET[:, j, h:h + 1], ALU.mult)
                act(s, hnr, hnr[:, :], acc, acc[:, 0:128], AF.Copy, scale=sm["fac"][:, 0:1], extra_reads=[sm["fac"]])
                ht = HNt[j % 2]
                small_norm(s, hnr, hnr[:, :], 128, snw, ht, ht[:, hh * 128:(hh + 1) * 128], ng_bc, ng_bc[:, h * 128:(h + 1) * 128])
                if hh == 1:
                    st(s, "pool", HN_res, HN[j * 128:(j + 1) * 128, hp * 256:(hp + 1) * 256], ht, ht[:, :])
            decay_main(s, cx, heads, psS, psAcc, PTs, fin, jmax=jmax)
        s.phase_es = old
    s_sub_barrier(s)
    if bstop < 3:
        s.end_phase()
        return

    with ExitStack() as es3:
        old = s.phase_es
        s.phase_es = es3
        gate_proj_ln(s, cx, dr, xT, HN_res, HN, 1024, w_in[:, 2048:3072], AF.Sigmoid, dr["b_w_out"][j_], h_in, h_out,
                     dr["ln1_g"][l], dr["ln1_b"][l])
        s.phase_es = old
    s.end_phase()


def phase_C(s, cx, dr, l, j_, h_in, h_out):
    import os
    nc = s.nc
    h_in_res, h_in_ap = h_in[0], h_in[1]
    wres = dr["wres"]
    w_in = dr["c_w_in"][j_]
    w_in_v = w_in.rearrange("(c p) n -> p c n", p=128)
    HN_res, HN = dr["hn"]
    qkT_res, qkT = dr["qkT"]
    cstop = int(os.environ.get("CSTOP", "9"))
    jmax = int(os.environ.get("CJMAX", str(NT)))
    nheads = int(os.environ.get("CHEADS", "4"))

    s.begin_phase()
    xT = s.sb([128, 8, S], BF16, "xT")
    load_xT(s, cx, h_in_res, h_in_ap, xT)
    EA = s.sb([128, NT, 4], F32, "EA")
    EB = s.sb([128, NT, 4], F32, "EB")
    ET = s.sb([128, NT, 4], F32, "ET")
    with ExitStack() as es1:
        old = s.phase_es
        s.phase_es = es1
        LF = s.sb([128, NT, 4], F32, "LF")
        psX = s.ps([128, 512], F32, "psX")
        for h in range(4):
            s.op("pool", lambda e, h=h: e.memset(LF[:, :, h:h + 1], math.log(1.0 - 2.0 ** (-5.0 - h))), writes=[LF])
        decay_tables(s, cx, LF, None, 4, math.log(1.0 / 16.0), EA, EB, ET, psX)
        s.phase_es = old
    s_sub_barrier(s)

    with ExitStack() as es1:
        old = s.phase_es
        s.phase_es = es1
        wqk = s.sb([128, 8, 2048], BF16, "wqk")
        for c in range(8):
            wload(s, cx, wqk, wqk[:, c, :], wres, w_in[c * 128:(c + 1) * 128, 0:2048], [2048])
        rc = s.sb([128, 4], F32, "ropec")
        ld(s, "sp", rc, rc[:, :], wres, dr["rope_consts"])
        posi = s.sb([128, 512], I32, "posi")
        posf = s.sb([128, 512], F32, "posf")
        ang = s.sb([128, 512], F32, "ang")
        ang2 = s.sb([128, 512], F32, "ang2")
        Ct = s.sb([128, 512], F32, "Ct")
        St = s.sb([128, 512], F32, "St")
        scr = (s.sb([128, 512], I32, "ki"), s.sb([128, 512], F32, "kf"), s.sb([128, 512], F32, "rr"))
        psa = [s.ps([128, 512], F32, "psa") for _ in range(2)]
        psb = [s.ps([128, 512], F32, "psb") for _ in range(2)]
        t1s = [s.sb([128, 512], F32, "t1") for _ in range(2)]
        t2s = [s.sb([128, 512], F32, "t2") for _ in range(2)]
        oas = [s.sb([128, 512], BF16, "oa") for _ in range(2)]
        obs = [s.sb([128, 512], BF16, "ob") for _ in range(2)]
        it = 0
        for tg in range(8):
            tsl = slice(tg * 512, (tg + 1) * 512)
            ld(s, "sp", posi, posi[:, :], wres, bcast_row(dr["positions"][tg * 512:(tg + 1) * 512], 512))
            cp(s, "dve", posf, posf[:, :], posi, posi[:, :])
            ts(s, "dve", ang, ang[:, :], posf, posf[:, :], rc[:, 2:3], None, ALU.mult, extra_reads=[rc])
            ts(s, "dve", ang2, ang2[:, :], ang, ang[:, :], math.pi / 2, None, ALU.add)
            sin_table(s, St, St[:, :], ang, ang[:, :], None, None, None, scr)
            sin_table(s, Ct, Ct[:, :], ang2, ang2[:, :], None, None, None, scr)
            for h in range(4):
                for w in range(2):
                    pa, pb = psa[it % 2], psb[it % 2]
                    t1, t2, oa, ob = t1s[it % 2], t2s[it % 2], oas[it % 2], obs[it % 2]
                    it += 1
                    col = w * 1024 + h * 256
                    for c in range(8):
                        mm(s, pa, pa[:, :], wqk, wqk[:, c, col:col + 128], xT, xT[:, c, tsl], c == 0, c == 7)
                    for c in range(8):
                        mm(s, pb, pb[:, :], wqk, wqk[:, c, col + 128:col + 256], xT, xT[:, c, tsl], c == 0, c == 7)
                    tt(s, "dve", t1, t1[:, :], pa, pa[:, :], Ct, Ct[:, :], ALU.mult)
                    tt(s, "dve", t2, t2[:, :], pb, pb[:, :], St, St[:, :], ALU.mult)
                    tt(s, "pool", oa, oa[:, :], t1, t1[:, :], t2, t2[:, :], ALU.subtract)
                    st(s, "pool", qkT_res, qkT[w, h * 256:h * 256 + 128, tsl], oa, oa[:, :])
                    tt(s, "dve", t1, t1[:, :], pb, pb[:, :], Ct, Ct[:, :], ALU.mult)
                    tt(s, "dve", t2, t2[:, :], pa, pa[:, :], St, St[:, :], ALU.mult)
                    tt(s, "pool", ob, ob[:, :], t1, t1[:, :], t2, t2[:, :], ALU.add)
                    st(s, "pool", qkT_res, qkT[w, h * 256 + 128:h * 256 + 256, tsl], ob, ob[:, :])
        s.phase_es = old
    s_sub_barrier(s)
    if cstop < 2:
        s.end_phase()
        return

    with ExitStack() as es2:
        old = s.phase_es
        s.phase_es = es2
        ng_bc = s.sb([128, 2048], F32, "ngbc")
        ld(s, "sp", ng_bc, ng_bc[:, :], wres, bcast_row(dr["c_norm_g"][j_], 2048))
        QA = s.sb([128, S], BF16, "QA")
        QB = s.sb([128, S], BF16, "QB")
        KA = s.sb([128, S], BF16, "KA")
        KB = s.sb([128, S], BF16, "KB")
        wv = s.sb([128, 8, 512], BF16, "wv")
        Vh = s.sb([128, NT, 512], BF16, "Vh")
        E = s.sb([128, NT, NT], F32, "E")
        tmpE = s.sb([128, NT, NT], F32, "tmpE")
        psX = [s.ps([128, 512], F32, "psX") for _ in range(2)]
        psS = [[s.ps([128, 512], F32, "psS") for _ in range(2)]]
        psAcc = [s.ps([128, 512], F32, "psAcc")]
        PTs = [[s.sb([128, 4, 128], BF16, "PT") for _ in range(2)]]
        snw = small_norm_work(s, 512)
        onr = s.sb([128, 512], F32, "onr")
        HNt = [s.sb([128, 512], F32, "HNt") for _ in range(2)]
        for h in range(nheads):
            for w, (ta, tb) in enumerate(((QA, QB), (KA, KB))):
                ld(s, "sp", ta, ta[:, :], qkT_res, qkT[w, h * 256:h * 256 + 128, :])
                ld(s, "sp", tb, tb[:, :], qkT_res, qkT[w, h * 256 + 128:h * 256 + 256, :])
            wload(s, cx, wv, wv[:, 0:4, :], wres, w_in_v[:, 0:4, 2048 + h * 512:2048 + (h + 1) * 512], [4, 512])
            wload(s, cx, wv, wv[:, 4:8, :], wres, w_in_v[:, 4:8, 2048 + h * 512:2048 + (h + 1) * 512], [4, 512])
            for i in range(NT):
                px = psX[i % 2]
                for c in range(8):
                    mm(s, px, px[:, :], xT, xT[:, c, i * 128:(i + 1) * 128], wv, wv[:, c, :], c == 0, c == 7)
                cp(s, "act", Vh, Vh[:, i, :], px, px[:, :])
            make_E(s, E, EA, EB, h, tmpE)
            heads = [dict(
                kparts=[(KA, (lambda i: KA[:, i * 128:(i + 1) * 128]), QA, (lambda j: QA[:, j * 128:(j + 1) * 128])),
                        (KB, (lambda i: KB[:, i * 128:(i + 1) * 128]), QB, (lambda j: QB[:, j * 128:(j + 1) * 128]))],
                V=Vh, Vap=(lambda i: Vh[:, i, :]), dvw=512, E=E, h=h)]

            def fin(hi, hd, j, acc, h=h):
                act(s, onr, onr[:, :], acc, acc[:, 0:512], AF.Copy, scale=ET[:, j, h:h + 1], extra_reads=[ET])
                ht = HNt[j % 2]
                small_norm(s, onr, onr[:, :], 512, snw, ht, ht[:, :], ng_bc, ng_bc[:, h * 512:(h + 1) * 512])
                st(s, "pool", HN_res, HN[j * 128:(j + 1) * 128, h * 512:(h + 1) * 512], ht, ht[:, :])
            decay_main(s, cx, heads, psS, psAcc, PTs, fin, jmax=jmax)
        s.phase_es = old
    s_sub_barrier(s)
    if cstop < 3:
        s.end_phase()
        return

    with ExitStack() as es3:
        old = s.phase_es
        s.phase_es = es3
        gate_proj_ln(s, cx, dr, xT, HN_res, HN, 2048, w_in[:, 4096:6144], AF.Silu, dr["c_w_out"][j_], h_in, h_out,
                     dr["ln1_g"][l], dr["ln1_b"][l])
        s.phase_es = old
    s.end_phase()


NSLOT = 128
BIGIDX = 4 * 64 * 128


def phase_F(s, cx, dr, l, h_in, h_out):
    import os
    nc = s.nc
    h_in_res, h_in_ap = h_in[0], h_in[1]
    h_out_res, h_out_ap, final = h_out
    wres = dr["wres"]
    xbf_res, xbf = dr["xbf"]
    bt_res, bt = dr["buftok"]
    yb_res, yb = dr["yb"]
    fstop = int(os.environ.get("FSTOP", "9"))

    s.begin_phase()
    OH1 = s.sb([128, NT, 64], F32, "OH1")
    OH2 = s.sb([128, NT, 64], F32, "OH2")
    RK = s.sb([128, NT, 2], F32, "RK")
    GT = s.sb([128, NT, 2], F32, "GT")
    DESTi = s.sb([128, NT, 2], I32, "DESTi")
    IDXW = s.sb([128, NSLOT], I32, "IDXW")
    breg_x = nc.gpsimd.to_reg(S - 1)
    breg_w = nc.gpsimd.to_reg((l + 1) * 64 * 128 - 1)
    breg_y = nc.gpsimd.to_reg(NSLOT * 128 - 1)
    breg_t = nc.gpsimd.to_reg(NSLOT * 128 - 1)

    with ExitStack() as es1:
        old = s.phase_es
        s.phase_es = es1
        Wr = s.sb([128, 8, 72], F32, "Wr")
        ld(s, "sp", Wr, Wr[:, :, 0:8], wres, dr["r_group_w"][l].rearrange("(c p) n -> p c n", p=128))
        ld(s, "sp", Wr, Wr[:, :, 8:72], wres, dr["r_expert_w"][l].rearrange("(c p) n -> p c n", p=128))
        bias = s.sb([128, 72], F32, "rbias")
        ld(s, "sp", bias, bias[:, 0:8], wres, bcast_row(dr["r_group_b"][l], 8))
        ld(s, "sp", bias, bias[:, 8:72], wres, bcast_row(dr["r_expert_b"][l], 64))
        tri_lt = s.sb([128, 128], BF16, "trilt")
        ts(s, "dve", tri_lt, tri_lt[:, :], cx.iof, cx.iof[:, :], 0.0, None, ALU.is_gt)
        toki = s.sb([128, NT], I32, "toki")
        s.op("pool", lambda e: e.iota(toki[:, :], pattern=[[128, NT]], base=0, channel_multiplier=1), writes=[toki])
        cnt = s.sb([128, 64], F32, "cnt")
        s.op("pool", lambda e: e.memset(cnt[:, :], 0.0), writes=[cnt])
        hfs = [s.sb([128, 1024], F32, "hfr") for _ in range(2)]
        hbs = [s.sb([128, 1024], BF16, "hbr") for _ in range(2)]
        xTts = [s.sb([128, 8, 128], F32, "xTt") for _ in range(2)]
        pts = [s.ps([128, 512], F32, "ptr") for _ in range(2)]
        psL = s.ps([128, 512], F32, "psL")
        psPC = s.ps([128, 512], F32, "psPC")
        lg = s.sb([128, 72], F32, "lg")
        sm = {k: s.sb([128, 1], F32, k) for k in ("gmax", "ngmax", "sumg", "pgrp", "v1", "v2", "dv", "ex", "den", "rec")}
        ohg = s.sb([128, 8], F32, "ohg")
        eg = s.sb([128, 8], F32, "eg")
        pen = s.sb([128, 8], F32, "pen")
        em = s.sb([128, 64], F32, "em")
        em2 = s.sb([128, 64], F32, "em2")
        Mb = s.sb([128, 64], BF16, "Mb")
        pos = s.sb([128, 64], F32, "pos")
        tmp = s.sb([128, 64], F32, "tmp")
        for i in range(NT):
            hf = hfs[i % 2]
            hb = hbs[i % 2]
            xTt = xTts[i % 2]
            ld(s, "sp", hf, hf[:, :], h_in_res, h_in_ap[i * 128:(i + 1) * 128, :])
            cp(s, "pool", hb, hb[:, :], hf, hf[:, :])
            st(s, "pool", xbf_res, xbf[i * 128:(i + 1) * 128, :], hb, hb[:, :])
            for half in range(2):
                pt = pts[half]
                for c4 in range(4):
                    c = half * 4 + c4
                    s.op("pe", lambda e, pt=pt, c4=c4, c=c, hf=hf: e.transpose(
                        out=pt[:, c4 * 128:(c4 + 1) * 128], in_=hf[:, c * 128:(c + 1) * 128], identity=cx.ident_f[:, :]),
                        reads=[hf, cx.ident_f], writes=[pt])
                cp(s, "act" if half else "dve", xTt, xTt[:, half * 4:(half + 1) * 4, :], pt,
                   pt[:, :].rearrange("p (c t) -> p c t", c=4))
            for c in range(8):
                mm(s, psL, psL[:, 0:72], xTt, xTt[:, c, :], Wr, Wr[:, c, :], c == 0, c == 7)
            tt(s, "dve", lg, lg[:, :], psL, psL[:, 0:72], bias, bias[:, :], ALU.add)
            g = sm
            s.op("dve", lambda e: e.reduce_max(out=g["gmax"][:, :], in_=lg[:, 0:8], axis=AX.X), reads=[lg], writes=[g["gmax"]])
            ts(s, "dve", ohg, ohg[:, :], lg, lg[:, 0:8], g["gmax"][:, 0:1], None, ALU.is_equal, extra_reads=[g["gmax"]])
            ts(s, "dve", g["ngmax"], g["ngmax"][:, :], g["gmax"], g["gmax"][:, :], -1.0, None, ALU.mult)
            act(s, eg, eg[:, :], lg, lg[:, 0:8], AF.Exp, scale=1.0, bias=g["ngmax"][:, 0:1], extra_reads=[g["ngmax"]])
            s.op("dve", lambda e: e.reduce_sum(out=g["sumg"][:, :], in_=eg[:, :], axis=AX.X), reads=[eg], writes=[g["sumg"]])
            s.op("dve", lambda e: e.reciprocal(out=g["pgrp"][:, :], in_=g["sumg"][:, :]), reads=[g["sumg"]], writes=[g["pgrp"]])
            ts(s, "dve", pen, pen[:, :], ohg, ohg[:, :], -1.0, 1e30, ALU.add, ALU.mult)
            tt(s, "dve", em, em[:, :].rearrange("p (g j) -> p g j", g=8), lg, lg[:, 8:72].rearrange("p (g j) -> p g j", g=8),
               pen, pen[:, :].unsqueeze(2).to_broadcast([128, 8, 8]), ALU.add)
            s.op("dve", lambda e: e.reduce_max(out=g["v1"][:, :], in_=em[:, :], axis=AX.X), reads=[em], writes=[g["v1"]])
            ts(s, "dve", OH1, OH1[:, i, :], em, em[:, :], g["v1"][:, 0:1], None, ALU.is_equal, extra_reads=[g["v1"]])
            stt(s, "dve", em2, em2[:, :], OH1, OH1[:, i, :], -1e30, em, em[:, :], ALU.mult, ALU.add)
            s.op("dve", lambda e: e.reduce_max(out=g["v2"][:, :], in_=em2[:, :], axis=AX.X), reads=[em2], writes=[g["v2"]])
            ts(s, "dve", OH2, OH2[:, i, :], em2, em2[:, :], g["v2"][:, 0:1], None, ALU.is_equal, extra_reads=[g["v2"]])
            tt(s, "dve", g["dv"], g["dv"][:, :], g["v2"], g["v2"][:, :], g["v1"], g["v1"][:, :], ALU.subtract)
            act(s, g["ex"], g["ex"][:, :], g["dv"], g["dv"][:, :], AF.Exp)
            ts(s, "dve", g["den"], g["den"][:, :], g["ex"], g["ex"][:, :], 1.0, None, ALU.add)
            s.op("dve", lambda e: e.reciprocal(out=g["rec"][:, :], in_=g["den"][:, :]), reads=[g["den"]], writes=[g["rec"]])
            tt(s, "dve", GT, GT[:, i, 0:1], g["pgrp"], g["pgrp"][:, :], g["rec"], g["rec"][:, :], ALU.mult)
            tt(s, "dve", GT, GT[:, i, 1:2], GT, GT[:, i, 0:1], g["ex"], g["ex"][:, :], ALU.mult)
            tt(s, "dve", Mb, Mb[:, :], OH1, OH1[:, i, :], OH2, OH2[:, i, :], ALU.add)
            mm(s, psPC, psPC[:, 0:64], tri_lt, tri_lt[:, :], Mb, Mb[:, :], True, True)
            mm(s, psPC, psPC[:, 64:128], cx.ones_b, cx.ones_b[:, :], Mb, Mb[:, :], True, True)
            tt(s, "dve", pos, pos[:, :], psPC, psPC[:, 0:64], cnt, cnt[:, :], ALU.add)
            tt(s, "dve", cnt, cnt[:, :], psPC, psPC[:, 64:128], cnt, cnt[:, :], ALU.add)
            tt(s, "dve", tmp, tmp[:, :], pos, pos[:, :], OH1, OH1[:, i, :], ALU.mult)
            s.op("dve", lambda e, i=i: e.reduce_sum(out=RK[:, i, 0:1], in_=tmp[:, :], axis=AX.X), reads=[tmp], writes=[RK])
            tt(s, "dve", tmp, tmp[:, :], pos, pos[:, :], OH2, OH2[:, i, :], ALU.mult)
            s.op("dve", lambda e, i=i: e.reduce_sum(out=RK[:, i, 1:2], in_=tmp[:, :], axis=AX.X), reads=[tmp], writes=[RK])
        cnti = s.sb([128, 64], I32, "cnti")
        padf = s.sb([128, 64], F32, "padf")
        pends = s.sb([128, 64], F32, "pends")
        pstart = s.sb([128, 64], F32, "pstart")
        ts(s, "dve", cnti, cnti[:, :], cnt, cnt[:, :], 127.0, None, ALU.add)
        ts(s, "dve", cnti, cnti[:, :], cnti, cnti[:, :], 7, 7, ALU.arith_shift_right, ALU.logical_shift_left)
        cp(s, "dve", padf, padf[:, :], cnti, cnti[:, :])
        s.op("dve", lambda e: e.tensor_tensor_scan(out=pends[:, :], data0=cx.ones_f[:, 0:64], data1=padf[:, :],
                                                   initial=0.0, op0=ALU.mult, op1=ALU.add),
             reads=[cx.ones_f, padf], writes=[pends])
        tt(s, "dve", pstart, pstart[:, :], pends, pends[:, :], padf, padf[:, :], ALU.subtract)
        big = s.sb([128, NT, 64], F32, "big")
        DEST = s.sb([128, NT, 2], F32, "DEST")
        for k, OH in enumerate((OH1, OH2)):
            tt(s, "dve", big, big[:, :, :], OH, OH[:, :, :], pstart, pstart[:, :].unsqueeze(1).to_broadcast([128, NT, 64]), ALU.mult)
            s.op("dve", lambda e, k=k: e.reduce_sum(out=DEST[:, :, k], in_=big[:, :, :], axis=AX.X), reads=[big], writes=[DEST])
        tt(s, "dve", DEST, DEST[:, :, :], DEST, DEST[:, :, :], RK, RK[:, :, :], ALU.add)
        cp(s, "dve", DESTi, DESTi[:, :, :], DEST, DEST[:, :, :])
        fill = s.sb([128, NSLOT], I32, "fill")
        s.op("pool", lambda e: e.iota(fill[:, :], pattern=[[0, NSLOT]], base=S, channel_multiplier=0), writes=[fill])
        st(s, "sp", bt_res, bt.rearrange("(p n) o -> p (n o)", p=128), fill, fill[:, :])
        for i in range(NT):
            for k in range(2):
                s.dma("pool", lambda e, i=i, k=k: e.indirect_dma_start(
                    out=bt, out_offset=bass.IndirectOffsetOnAxis(ap=DESTi[:, i, k:k + 1], axis=0),
                    in_=toki[:, i:i + 1], in_offset=None, bounds_check=breg_t, oob_is_err=False),
                    reads=[DESTi, toki], writes=[bt_res])
        slotoff = s.sb([128, NSLOT], F32, "slotoff")
        sloti = s.sb([128, NSLOT], I32, "sloti")
        s.op("pool", lambda e: e.iota(sloti[:, :], pattern=[[128, NSLOT]], base=0, channel_multiplier=0), writes=[sloti])
        cp(s, "dve", slotoff, slotoff[:, :], sloti, sloti[:, :])
        blk = s.sb([128, NSLOT], F32, "blk")
        cmp_ = s.sb([128, 32, 64], F32, "cmp")
        for q in range(NSLOT // 32):
            tt(s, "dve", cmp_, cmp_[:, :, :], pends, pends[:, :].unsqueeze(1).to_broadcast([128, 32, 64]),
               slotoff, slotoff[:, q * 32:(q + 1) * 32].unsqueeze(2).to_broadcast([128, 32, 64]), ALU.is_le)
            s.op("dve", lambda e, q=q: e.reduce_sum(out=blk[:, q * 32:(q + 1) * 32], in_=cmp_[:, :, :], axis=AX.X),
                 reads=[cmp_], writes=[blk])
        pidx = s.sb([128, 1], F32, "pidx")
        pidi = s.sb([128, 1], I32, "pidi")
        s.op("pool", lambda e: e.iota(pidi[:, :], pattern=[[0, 1]], base=0, channel_multiplier=1), writes=[pidi])
        cp(s, "dve", pidx, pidx[:, :], pidi, pidi[:, :])
        used = s.sb([128, NSLOT], F32, "used")
        ts(s, "dve", used, used[:, :], slotoff, slotoff[:, :], pends[:, 63:64], None, ALU.is_lt, extra_reads=[pends])
        ts(s, "dve", blk, blk[:, :], blk, blk[:, :], 63.0, 128.0, ALU.min, ALU.mult)
        ts(s, "dve", blk, blk[:, :], blk, blk[:, :], pidx[:, 0:1], float(l * 64 * 128 - BIGIDX), ALU.add, ALU.add, extra_reads=[pidx])
        tt(s, "dve", blk, blk[:, :], blk, blk[:, :], used, used[:, :], ALU.mult)
        ts(s, "dve", blk, blk[:, :], blk, blk[:, :], float(BIGIDX), None, ALU.add)
        cp(s, "dve", IDXW, IDXW[:, :], blk, blk[:, :])
        if "dbg" in dr:
            dbg = s.sb([128, 1024], F32, "dbg")
            s.op("dve", lambda e: e.memset(dbg[:, :], 0.0), writes=[dbg])
            cp(s, "dve", dbg, dbg[:, 0:128], IDXW, IDXW[:, :])
            cp(s, "dve", dbg, dbg[:, 128:192], cnt, cnt[:, :])
            cp(s, "dve", dbg, dbg[:, 192:256], pends, pends[:, :])
            cp(s, "dve", dbg, dbg[:, 256:320], DEST, DEST[:, :, :].rearrange("p a b -> p (a b)"))
            cp(s, "dve", dbg, dbg[:, 320:384], GT, GT[:, :, :].rearrange("p a b -> p (a b)"))
            cp(s, "dve", dbg, dbg[:, 384:448], used, used[:, 0:64])
            cp(s, "dve", dbg, dbg[:, 448:512], padf, padf[:, :])
            cp(s, "dve", dbg, dbg[:, 512:576], OH1, OH1[:, 0, :])
            cp(s, "dve", dbg, dbg[:, 576:640], OH2, OH2[:, 0, :])
            cp(s, "dve", dbg, dbg[:, 640:704], RK, RK[:, :, :].rearrange("p a b -> p (a b)"))
            st(s, "sp", wres, dr["dbg"], dbg, dbg[:, :], final=True)
        s.phase_es = old
    s_sub_barrier(s)
    if fstop < 2:
        s.end_phase()
        return

    with ExitStack() as es2:
        old = s.phase_es
        s.phase_es = es2
        wviews = [dr[k].rearrange("l e (p j) n -> (l e p) (j n)", p=128) for k in ("e_w_gate", "e_w_up", "e_w_down")]
        stgs = [[s.sb([128, 2048], F32, f"ws{k}") for k in range(3)] for _ in range(2)]
        wbs = [[s.sb([128, 2048], BF16, f"wb{k}") for k in range(3)] for _ in range(2)]
        idxs = [s.sb([128, 1], I32, "idxb") for _ in range(2)]
        xgs = [s.sb([128, 1024], BF16, "xg") for _ in range(2)]
        for xg in xgs:
            s.op("pool", lambda e, xg=xg: e.memset(xg[:, :], 0.0), writes=[xg])
        xgTs = [s.sb([128, 8, 128], BF16, "xgT") for _ in range(2)]
        sgs = [s.sb([128, 256], F32, "sg") for _ in range(2)]
        aTs = [s.sb([128, 2, 128], BF16, "aT") for _ in range(2)]
        yos = [s.sb([128, 1024], F32, "yo") for _ in range(2)]
        psT = [s.ps([128, 1024], BF16, "psT") for _ in range(2)]
        psG = s.ps([128, 512], F32, "psG")
        psU = s.ps([128, 512], F32, "psU")
        psY = [s.ps([128, 512], F32, "psY") for _ in range(2)]
        nslot = int(os.environ.get("FSLOTS", str(NSLOT)))

        def loads(b):
            ix = idxs[b % 2]
            ld(s, "sp", ix, ix[:, :], bt_res, bt[b * 128:(b + 1) * 128, :])
            xg = xgs[b % 2]
            s.dma("pool", lambda e: e.indirect_dma_start(
                out=xg[:, :], out_offset=None, in_=xbf, in_offset=bass.IndirectOffsetOnAxis(ap=ix[:, 0:1], axis=0),
                bounds_check=breg_x, oob_is_err=False), reads=[ix, xbf_res], writes=[xg])
            for k in range(3):
                stg = stgs[b % 2][k]
                s.dma("pool", lambda e, stg=stg, k=k: e.indirect_dma_start(
                    out=stg[:, :], out_offset=None, in_=wviews[k],
                    in_offset=bass.IndirectOffsetOnAxis(ap=IDXW[:, b:b + 1], axis=0),
                    bounds_check=breg_w, oob_is_err=False), reads=[IDXW, wres], writes=[stg])

        loads(0)
        for b in range(nslot):
            if b + 1 < nslot:
                loads(b + 1)
            xg = xgs[b % 2]
            xgT = xgTs[b % 2]
            wg, wu, wd = wbs[b % 2]
            sg = sgs[b % 2]
            aT = aTs[b % 2]
            yo = yos[b % 2]
            pT = psT[b % 2]
            for k, eng in enumerate(("act", "dve", "act")):
                cp(s, eng, wbs[b % 2][k], wbs[b % 2][k][:, :], stgs[b % 2][k], stgs[b % 2][k][:, :])
            for j in range(8):
                s.op("pe", lambda e, j=j, pT=pT, xg=xg: e.transpose(out=pT[:, j * 128:(j + 1) * 128], in_=xg[:, j:1024:8],
                                                      identity=cx.ident_b[:, :]), reads=[xg, cx.ident_b], writes=[pT])
            cp(s, "dve", xgT, xgT[:, :, :], pT, pT[:, :].rearrange("p (j t) -> p j t", j=8))
            wgv = wg[:, :].rearrange("p (j n) -> p j n", j=8)
            wuv = wu[:, :].rearrange("p (j n) -> p j n", j=8)
            wdv = wd[:, :].rearrange("p (j n) -> p j n", j=2)
            for pS_, wv_ in ((psG, wgv), (psU, wuv)):
                for jh in range(2):
                    for j in range(8):
                        mm(s, pS_, pS_[:, jh * 128:(jh + 1) * 128], wbs[b % 2][0 if pS_ is psG else 1], wv_[:, j, jh:256:2],
                           xgT, xgT[:, j, :], j == 0, j == 7)
            act(s, sg, sg[:, :], psG, psG[:, 0:256], AF.Silu)
            tt(s, "dve", aT, aT[:, :, :].rearrange("p a t -> p (a t)"), sg, sg[:, :], psU, psU[:, 0:256], ALU.mult)
            for half in range(2):
                for jh in range(2):
                    mm(s, psY[half], psY[half][:, :], aT, aT[:, jh, :], wd, wdv[:, jh, half * 512:(half + 1) * 512], jh == 0, jh == 1)
            cp(s, "act", yo, yo[:, 0:512], psY[0], psY[0][:, :])
            cp(s, "dve", yo, yo[:, 512:1024], psY[1], psY[1][:, :])
            st(s, "sp", yb_res, yb[b * 128:(b + 1) * 128, :], yo, yo[:, :])
        s.phase_es = old
    s_sub_barrier(s)
    if fstop < 3:
        s.end_phase()
        return

    with ExitStack() as es3:
        old = s.phase_es
        s.phase_es = es3
        g_bc = s.sb([128, 1024], F32, "gbc")
        b_bc = s.sb([128, 1024], F32, "bbc")
        ld(s, "sp", g_bc, g_bc[:, :], wres, bcast_row(dr["ln2_g"][l], 1024))
        ld(s, "sp", b_bc, b_bc[:, :], wres, bcast_row(dr["ln2_b"][l], 1024))
        y1s = [s.sb([128, 1024], F32, "y1") for _ in range(2)]
        y2s = [s.sb([128, 1024], F32, "y2") for _ in range(2)]
        hfs = [s.sb([128, 1024], F32, "hf3") for _ in range(2)]
        hos = [s.sb([128, 1024], F32, "ho3") for _ in range(2)]
        wk = ln_work(s)
        for i in range(NT):
            y1, y2, hf, ho = y1s[i % 2], y2s[i % 2], hfs[i % 2], hos[i % 2]
            for k, yk in enumerate((y1, y2)):
                s.dma("pool", lambda e, yk=yk, k=k, i=i: e.indirect_dma_start(
                    out=yk[:, :], out_offset=None, in_=yb, in_offset=bass.IndirectOffsetOnAxis(ap=DESTi[:, i, k:k + 1], axis=0),
                    bounds_check=breg_y, oob_is_err=False), reads=[DESTi, yb_res], writes=[yk])
            ld(s, "sp", hf, hf[:, :], h_in_res, h_in_ap[i * 128:(i + 1) * 128, :])
            act(s, y2, y2[:, :], y2, y2[:, :], AF.Copy, scale=GT[:, i, 1:2], extra_reads=[GT])
            stt(s, "dve", y1, y1[:, :], y1, y1[:, :], GT[:, i, 0:1], y2, y2[:, :], ALU.mult, ALU.add, extra_reads=[GT])
            ln_epilogue(s, cx, [(y1, y1[:, 0:512]), (y1, y1[:, 512:1024])], hf, g_bc, b_bc, wk, ho)
            st(s, "pool", h_out_res, h_out_ap[i * 128:(i + 1) * 128, :], ho, ho[:, :], final=final)
        s.phase_es = old
    s.end_phase()


def rope_consts_np():
    rc = np.zeros((128, 4), np.float32)
    inv_a = 500000.0 ** (-np.arange(0, 16, 2, dtype=np.float32) / 16.0)
    for hh in range(2):
        for jx in range(16):
            rc[hh * 64 + jx, 0] = inv_a[jx % 8]
            rc[hh * 64 + jx, 1] = -1.0 if jx < 8 else 1.0
    inv_c = 10000.0 ** (-np.arange(0, 256, 2, dtype=np.float32) / 256.0)
    rc[:, 2] = inv_c
    return rc


IN_SPECS = [
    ("x", [S, D], F32), ("positions", [S], I32),
    ("ln1_g", [4, D], F32), ("ln1_b", [4, D], F32), ("ln2_g", [4, D], F32), ("ln2_b", [4, D], F32),
    ("a_w_in", [2, D, 3072], F32), ("a_w_out", [2, D, D], F32),
    ("b_w_in", [1, D, 3088], F32), ("b_gate_bias", [1, 16], F32), ("b_conv_w", [1, 4, D], F32),
    ("b_conv_b", [1, D], F32), ("b_norm_g", [1, D], F32), ("b_w_out", [1, D, D], F32),
    ("c_w_in", [1, D, 6144], F32), ("c_norm_g", [1, 2 * D], F32), ("c_w_out", [1, 2 * D, D], F32),
    ("r_group_w", [4, D, 8], F32), ("r_group_b", [4, 8], F32), ("r_expert_w", [4, D, 64], F32),
    ("r_expert_b", [4, 64], F32), ("e_w_gate", [4, 64, D, 256], F32), ("e_w_up", [4, 64, D, 256], F32),
    ("e_w_down", [4, 64, 256, D], F32), ("rope_consts", [128, 4], F32),
]


def build(plan, used_inputs=None):
    nc = bass.Bass("TRN2", target_bir_lowering=False)
    dr = {}
    for name, shape, dt in IN_SPECS:
        if used_inputs is not None and name not in used_inputs:
            continue
        dr[name] = nc.dram_tensor(name, shape, dt, kind="ExternalInput").ap()
    out = nc.dram_tensor("out", [S, D], F32, kind="ExternalOutput").ap()
    import os
    if os.environ.get("KDBG"):
        dr["dbg"] = nc.dram_tensor("dbg", [128, 1024], F32, kind="ExternalOutput").ap()
    dr["wres"] = Res("weights")
    dr["qkT"] = (Res("qkT"), nc.dram_tensor("qkT_s", [2, D, S], BF16).ap())
    dr["attT"] = (Res("attT"), nc.dram_tensor("attT_s", [2 * D, S], BF16).ap())
    dr["hn"] = (Res("hn"), nc.dram_tensor("hn_s", [S, 2 * D], F32).ap())
    dr["xbf"] = (Res("xbf"), nc.dram_tensor("xbf_s", [S, D], BF16).ap())
    dr["buftok"] = (Res("buftok"), nc.dram_tensor("buftok_s", [NSLOT * 128, 1], I32).ap())
    dr["yb"] = (Res("yb"), nc.dram_tensor("yb_s", [NSLOT * 128, D], F32).ap())
    hbuf = [(Res("hA"), nc.dram_tensor("hA_s", [S, D], F32).ap(), False),
            (Res("hB"), nc.dram_tensor("hB_s", [S, D], F32).ap(), False)]
    with ExitStack() as es:
        s = Sched(nc, es)
        cx = Ctx()
        make_consts(s, cx)
        cur = (dr["wres"], dr["x"], False)
        for pi, (ph, l) in enumerate(plan):
            last = pi == len(plan) - 1
            nxt = (Res("out"), out, True) if last else hbuf[pi % 2]
            if ph == "A":
                phase_A(s, cx, dr, l, l // 3, cur, nxt)
            elif ph == "B":
                phase_B(s, cx, dr, l, l // 3, cur, nxt)
            elif ph == "C":
                phase_C(s, cx, dr, l, l // 3, cur, nxt)
            elif ph == "F":
                phase_F(s, cx, dr, l, cur, nxt)
            else:
                raise ValueError(ph)
            cur = nxt
        s.finish()
        print("instructions:", s.ninst, "sems:", s.n_sem)
    return nc


PLAN = [("A", 0), ("F", 0), ("B", 1), ("F", 1), ("C", 2), ("F", 2), ("A", 3), ("F", 3)]
_NC_CACHE = {}


def kernel(**inputs):
    x = np.ascontiguousarray(np.asarray(inputs["x"], dtype=np.float32))
    nb = x.shape[0]
    if "nc" not in _NC_CACHE:
        _NC_CACHE["nc"] = build(PLAN)
    nc = _NC_CACHE["nc"]
    shared = {}
    for name, shape, dt in IN_SPECS:
        if name == "x":
            continue
        if name == "rope_consts":
            shared[name] = rope_consts_np()
        elif name == "positions":
            shared[name] = np.ascontiguousarray(np.asarray(inputs[name]).astype(np.int32))
        else:
            shared[name] = np.ascontiguousarray(np.asarray(inputs[name], dtype=np.float32))
    in_maps = []
    for b in range(nb):
        m = dict(shared)
        m["x"] = x[b]
        in_maps.append(m)
    res = run_bass_kernel_spmd(nc, in_maps, core_ids=list(range(nb)))
    return np.stack([np.asarray(r["out"], dtype=np.float32) for r in res.results], axis=0)
```

```python
import math
import numpy as np
import concourse.bass as bass
import concourse.mybir as mybir
from concourse.bass_utils import run_bass_kernel_spmd
from contextlib import ExitStack

F32 = mybir.dt.float32
BF16 = mybir.dt.bfloat16
I32 = mybir.dt.int32
ALU = mybir.AluOpType
AF = mybir.ActivationFunctionType
AX = mybir.AxisListType

S = 4096
D = 1024
DEPTH = 4
NT = S // 128
ALPHA = (2.0 * DEPTH) ** 0.25
LN_EPS = 1e-5
TWO_PI = 2.0 * math.pi

SEM_WRAP = 30000
ENGS = ("pe", "act", "dve", "pool", "sp")


class Res:
    __slots__ = ("w", "r", "name", "excl")

    def __init__(self, name="", excl=False):
        self.w = None
        self.r = []
        self.name = name
        self.excl = excl


class T:
    __slots__ = ("t", "res")

    def __init__(self, t, res):
        self.t = t
        self.res = res

    def __getitem__(self, k):
        return self.t[k]


class Sched:
    def __init__(self, nc, es):
        self.nc = nc
        self.es = es
        self.sems = {}
        self.cur = {}
        self.waited = {e: {} for e in ENGS}
        self.pending = {e: {} for e in ENGS}
        self.dma_pool = {}
        self.dma_rr = {}
        self.n_sem = 0
        self.n_tiles = 0
        self.ninst = 0
        self.finals = []
        self.eobj = {"pe": nc.tensor, "act": nc.scalar, "dve": nc.vector, "pool": nc.gpsimd, "sp": nc.sync}
        self.phase_es = None
        self.recs = []
        self.ev_of = {}
        self.next_id = 0
        import os
        self.window = int(os.environ.get("KWIN", "48"))

    def _newsem(self, name):
        h = self.es.enter_context(self.nc.semaphore(f"{name}_{self.n_sem}"))
        self.n_sem += 1
        key = self.n_sem
        self.sems[key] = h
        return key

    def sb(self, shape, dtype, name=None):
        self.n_tiles += 1
        name = name or "t"
        es = self.phase_es or self.es
        t = es.enter_context(self.nc.sbuf_tensor(f"{name}_{self.n_tiles}", list(shape), dtype))
        return T(t, Res(name))

    def ps(self, shape, dtype, name=None):
        self.n_tiles += 1
        name = name or "p"
        es = self.phase_es or self.es
        t = es.enter_context(self.nc.psum_tensor(f"{name}_{self.n_tiles}", list(shape), dtype))
        return T(t, Res(name, excl=True))

    def begin_phase(self):
        self.phase_es = ExitStack()
        return self.phase_es

    def end_phase(self):
        self.flush()
        snap = {}
        for e, c in self.cur.items():
            snap[c[0]] = c[1]
        for q, slots in self.dma_pool.items():
            for sl in slots:
                if sl[1] > 0:
                    snap[sl[0]] = sl[1]
        for e in ENGS:
            p = self.pending[e]
            for k, v in snap.items():
                if p.get(k, 0) < v:
                    p[k] = v
        self.phase_es.close()
        self.phase_es = None

    def _deps(self, reads, writes):
        deps = set()
        for r in reads:
            r = r.res if isinstance(r, T) else r
            if r.w is not None:
                deps.add(r.w)
            if r.excl:
                deps.update(r.r)
        for w in writes:
            w = w.res if isinstance(w, T) else w
            if w.w is not None:
                deps.add(w.w)
            deps.update(w.r)
        return deps

    def _commit(self, oid, reads, writes):
        for r in reads:
            r = r.res if isinstance(r, T) else r
            r.r.append(oid)
        for w in writes:
            w = w.res if isinstance(w, T) else w
            w.w = oid
            w.r = []

    def op(self, eng, fn, reads=(), writes=(), cost=150.0):
        oid = self.next_id
        self.next_id += 1
        deps = self._deps(reads, writes)
        self._commit(oid, reads, writes)
        self.recs.append((oid, eng, False, fn, deps, float(cost), 0.0, False))

    def dma(self, q, fn, reads=(), writes=(), final=False, nbytes=65536, issue=None):
        oid = self.next_id
        self.next_id += 1
        deps = self._deps(reads, writes)
        self._commit(oid, reads, writes)
        if issue is None:
            issue = 1200.0 if q == "pool" else 80.0
        self.recs.append((oid, q, True, fn, deps, float(issue), 2000.0 + nbytes / 150.0, final))

    def flush(self):
        recs = self.recs
        self.recs = []
        if not recs:
            return
        fin = {}
        queues = {e: [] for e in ENGS}
        for r in recs:
            queues[r[1]].append(r)
        heads = {e: 0 for e in ENGS}
        placed = set()
        mine = {r[0] for r in recs}
        efree = {e: 0.0 for e in ENGS}
        dma_free = [0.0]
        order = []
        nleft = len(recs)
        W = self.window
        taken = {e: set() for e in ENGS}
        while nleft:
            best = None
            for e in ENGS:
                q = queues[e]
                h = heads[e]
                n = len(q)
                while h < n and q[h][0] in taken[e]:
                    h += 1
                heads[e] = h
                lim = min(n, h + W)
                for k in range(h, lim):
                    r = q[k]
                    if r[0] in taken[e]:
                        continue
                    ok = True
                    st_ = efree[e]
                    for d in r[4]:
                        if d in mine:
                            if d not in placed:
                                ok = False
                                break
                            if fin[d] > st_:
                                st_ = fin[d]
                    if not ok:
                        continue
                    key = (st_, r[0])
                    if best is None or key < best[0]:
                        best = (key, e, r)
                    if st_ <= efree[e]:
                        break
            assert best is not None, "scheduler deadlock"
            (st_, _), e, r = best
            taken[e].add(r[0])
            placed.add(r[0])
            nleft -= 1
            if r[2]:
                iend = st_ + r[5]
                efree[e] = iend
                t0 = max(iend, dma_free[0])
                dma_free[0] = t0 + max(0.0, r[6] - 2000.0)
                fin[r[0]] = dma_free[0] + 2000.0
            else:
                efree[e] = st_ + r[5]
                fin[r[0]] = efree[e] + 60.0
            order.append(r)
        for r in order:
            self._emit_rec(r)

    def _engine_event(self, eng):
        if eng not in self.cur or self.cur[eng][1] >= SEM_WRAP:
            self.cur[eng] = [self._newsem(eng), 0]
        c = self.cur[eng]
        c[1] += 1
        return (c[0], c[1])

    def _dma_event(self, q):
        if q not in self.dma_pool:
            self.dma_pool[q] = [[self._newsem(f"dma{q}"), 0] for _ in range(12)]
            self.dma_rr[q] = 0
        i = self.dma_rr[q]
        self.dma_rr[q] = (i + 1) % len(self.dma_pool[q])
        slot = self.dma_pool[q][i]
        extra = (slot[0], slot[1]) if slot[1] > 0 else None
        if slot[1] + 16 > SEM_WRAP:
            slot[0] = self._newsem(f"dma{q}")
            slot[1] = 0
        slot[1] += 16
        return (slot[0], slot[1]), extra

    def _emit_rec(self, r):
        oid, eng, is_dma, fn, deps, _, _, final = r
        need = self.pending[eng]
        self.pending[eng] = {}
        for d in deps:
            k, v = self.ev_of[d]
            if need.get(k, 0) < v:
                need[k] = v
        if is_dma:
            ev, extra = self._dma_event(eng)
            if extra is not None:
                k, v = extra
                if need.get(k, 0) < v:
                    need[k] = v
            inc = 16
        else:
            ev = self._engine_event(eng)
            inc = 1
        wd = self.waited[eng]
        own = self.cur.get(eng, [None])[0] if eng == "pe" else None
        e = self.eobj[eng]
        for k, v in need.items():
            if k == own:
                continue
            if wd.get(k, 0) < v:
                wd[k] = v
                e.wait_ge(self.sems[k], v)
        ins = fn(e)
        ins.then_inc(self.sems[ev[0]], inc)
        self.ev_of[oid] = ev
        self.ninst += 1
        if final:
            self.finals.append(ev)

    def finish(self):
        self.flush()
        deps = {}
        for k, v in self.finals:
            if deps.get(k, 0) < v:
                deps[k] = v
        for q, slots in self.dma_pool.items():
            for sl in slots:
                if sl[1] > 0 and deps.get(sl[0], 0) < sl[1]:
                    deps[sl[0]] = sl[1]
        for k, v in deps.items():
            self.nc.sync.wait_ge(self.sems[k], v)


def _fsz(ap):
    n = 1
    for d in ap.shape[1:]:
        n *= d
    return n


def mm(s, out_t, out_ap, lhsT_t, lhsT_ap, rhs_t, rhs_ap, start, stop):
    n = _fsz(out_ap)
    c = 30.0 + max(n, 64) * 0.45
    if lhsT_ap.dtype == F32:
        c *= 4
    s.op("pe", lambda e: e.matmul(out_ap, lhsT=lhsT_ap, rhs=rhs_ap, start=start, stop=stop),
         reads=[lhsT_t, rhs_t], writes=[out_t], cost=c)


def _ecost(eng, n):
    if eng == "dve":
        return 70.0 + 0.85 * n
    if eng == "act":
        return 200.0 + 0.65 * n
    return 160.0 + 1.6 * n


def tt(s, eng, out_t, out_ap, a_t, a_ap, b_t, b_ap, op):
    s.op(eng, lambda e: e.tensor_tensor(out=out_ap, in0=a_ap, in1=b_ap, op=op), reads=[a_t, b_t], writes=[out_t],
         cost=_ecost(eng, _fsz(out_ap)))


def ts(s, eng, out_t, out_ap, a_t, a_ap, s1, s2, op0, op1=None, extra_reads=()):
    c = _ecost(eng, _fsz(out_ap))
    if op1 is None:
        s.op(eng, lambda e: e.tensor_scalar(out=out_ap, in0=a_ap, scalar1=s1, scalar2=None, op0=op0),
             reads=[a_t, *extra_reads], writes=[out_t], cost=c)
    else:
        s.op(eng, lambda e: e.tensor_scalar(out=out_ap, in0=a_ap, scalar1=s1, scalar2=s2, op0=op0, op1=op1),
             reads=[a_t, *extra_reads], writes=[out_t], cost=c)


def stt(s, eng, out_t, out_ap, a_t, a_ap, scalar, b_t, b_ap, op0, op1, extra_reads=()):
    s.op(eng, lambda e: e.scalar_tensor_tensor(out=out_ap, in0=a_ap, scalar=scalar, in1=b_ap, op0=op0, op1=op1),
         reads=[a_t, b_t, *extra_reads], writes=[out_t], cost=_ecost(eng, _fsz(out_ap)))


def cp(s, eng, out_t, out_ap, a_t, a_ap):
    n = _fsz(out_ap)
    if eng == "act":
        s.op("act", lambda e: e.activation(out=out_ap, in_=a_ap, func=AF.Copy), reads=[a_t], writes=[out_t], cost=_ecost("act", n))
    else:
        c = _ecost(eng, n) if eng == "dve" else 160.0 + 3.0 * n
        s.op(eng, lambda e: e.tensor_copy(out=out_ap, in_=a_ap), reads=[a_t], writes=[out_t], cost=c)


def act(s, out_t, out_ap, a_t, a_ap, func, scale=1.0, bias=0.0, extra_reads=()):
    s.op("act", lambda e: e.activation(out=out_ap, in_=a_ap, func=func, bias=bias, scale=scale),
         reads=[a_t, *extra_reads], writes=[out_t], cost=_ecost("act", _fsz(out_ap)))


def _nbytes(ap):
    n = 1
    for d in ap.shape:
        n *= d
    return n * (2 if ap.dtype == BF16 else 4)


def ld(s, q, out_t, out_ap, src_res, src_ap):
    s.dma(q, lambda e: e.dma_start(out=out_ap, in_=src_ap), reads=[src_res], writes=[out_t], nbytes=_nbytes(out_ap))


def st(s, q, dst_res, dst_ap, in_t, in_ap, final=False):
    s.dma(q, lambda e: e.dma_start(out=dst_ap, in_=in_ap), reads=[in_t], writes=[dst_res], final=final, nbytes=_nbytes(in_ap))


class Ctx:
    pass


def wload(s, cx, dst_t, dst_ap, wres, src_ap, shape):
    stg = cx.stg[cx.stg_i % 2]
    cx.stg_i += 1
    n = int(np.prod(shape))
    if len(shape) == 1:
        v = stg[:, 0:n]
    else:
        v = stg[:, 0:n].rearrange("p (a b) -> p a b", a=shape[0])
    ld(s, "sp", stg, v, wres, src_ap)
    cp(s, "dve" if cx.stg_i % 2 else "act", dst_t, dst_ap, stg, v)


def make_consts(s, cx):
    nc = s.nc
    iot = s.sb([128, 128], I32, "iot")
    iof = s.sb([128, 128], F32, "iof")
    cx.iof = iof
    cx.ident_f = s.sb([128, 128], F32, "identf")
    cx.ident_b = s.sb([128, 128], BF16, "identb")
    cx.maskA = s.sb([128, 2, 2, 128], BF16, "maskA")
    cx.perm = s.sb([128, 128], BF16, "perm")
    cx.ones_b = s.sb([128, 128], BF16, "onesb")
    cx.ones_f = s.sb([128, 128], F32, "onesf")
    cx.tri_le = s.sb([128, 128], F32, "trile")
    cx.tri_b = s.sb([128, 128], BF16, "trib")
    cx.swlo = s.sb([128, 128], F32, "swlo")
    cx.swhi = s.sb([128, 128], F32, "swhi")
    cx.stg = [s.sb([128, 2048], F32, "stg") for _ in range(2)]
    cx.stg_i = 0
    s.op("pool", lambda e: e.iota(iot[:, :], pattern=[[1, 128]], base=0, channel_multiplier=-1), writes=[iot])
    cp(s, "dve", iof, iof[:, :], iot, iot[:, :])
    ts(s, "dve", cx.ident_f, cx.ident_f[:, :], iof, iof[:, :], 0.0, None, ALU.is_equal)
    cp(s, "dve", cx.ident_b, cx.ident_b[:, :], cx.ident_f, cx.ident_f[:, :])
    ts(s, "dve", cx.tri_le, cx.tri_le[:, :], iof, iof[:, :], 0.0, None, ALU.is_ge)
    cp(s, "dve", cx.tri_b, cx.tri_b[:, :], cx.tri_le, cx.tri_le[:, :])
    ts(s, "dve", cx.swlo, cx.swlo[:, :], iof, iof[:, :], 64.0, None, ALU.is_equal)
    ts(s, "dve", cx.swhi, cx.swhi[:, :], iof, iof[:, :], -64.0, None, ALU.is_equal)
    for hh in range(2):
        ts(s, "dve", cx.maskA, cx.maskA[:, hh, 0, :], iof, iof[:, :], 0.0, None, ALU.is_ge)
        ts(s, "dve", cx.maskA, cx.maskA[:, hh, 1, :], iof, iof[:, :], 0.0, None, ALU.is_le)
    s.op("pool", lambda e: e.memset(cx.perm[:, :], 0.0), writes=[cx.perm])
    s.op("pool", lambda e: e.memset(cx.ones_b[:, :], 1.0), writes=[cx.ones_b])
    s.op("pool", lambda e: e.memset(cx.ones_f[:, :], 1.0), writes=[cx.ones_f])
    for hh in range(2):
        c0 = hh * 64
        ts(s, "dve", cx.perm, cx.perm[:, c0:c0 + 8], iof, iof[:, c0:c0 + 8], -8.0, None, ALU.is_equal)
        ts(s, "dve", cx.perm, cx.perm[:, c0 + 8:c0 + 16], iof, iof[:, c0 + 8:c0 + 16], 8.0, None, ALU.is_equal)


def sin_table(s, out_t, out_ap, x_t, x_ap, shape, sign_ap, sign_t, scratch):
    ki, kf, r = scratch
    C1 = 6.28125
    C2 = TWO_PI - C1
    ts(s, "dve", ki, ki[:, :], x_t, x_ap, 1.0 / TWO_PI, None, ALU.mult)
    cp(s, "dve", kf, kf[:, :], ki, ki[:, :])
    stt(s, "dve", r, r[:, :], kf, kf[:, :], -C1, x_t, x_ap, ALU.mult, ALU.add)
    stt(s, "dve", r, r[:, :], kf, kf[:, :], -C2, r, r[:, :], ALU.mult, ALU.add)
    ts(s, "dve", kf, kf[:, :], r, r[:, :], math.pi, None, ALU.is_gt)
    stt(s, "dve", r, r[:, :], kf, kf[:, :], -TWO_PI, r, r[:, :], ALU.mult, ALU.add)
    ts(s, "dve", kf, kf[:, :], r, r[:, :], -math.pi, None, ALU.is_lt)
    stt(s, "dve", r, r[:, :], kf, kf[:, :], TWO_PI, r, r[:, :], ALU.mult, ALU.add)
    ts(s, "dve", r, r[:, :], r, r[:, :], 3.14159, -3.14159, ALU.min, ALU.max)
    act(s, out_t, out_ap, r, r[:, :], AF.Sin)
    if sign_ap is not None:
        ts(s, "dve", out_t, out_ap, out_t, out_ap, sign_ap, None, ALU.mult, extra_reads=[sign_t])


def load_xT(s, cx, h_res, h_ap, xT):
    old = s.phase_es
    s.phase_es = ExitStack()
    hfs = [s.sb([128, 1024], F32, "hf") for _ in range(3)]
    pts = [s.ps([128, 512], F32, "ptr") for _ in range(2)]
    for i in range(NT):
        hf = hfs[i % 3]
        ld(s, "sp", hf, hf[:, :], h_res, h_ap[i * 128:(i + 1) * 128, :])
        for half in range(2):
            pt = pts[half]
            for c4 in range(4):
                c = half * 4 + c4
                s.op("pe", lambda e, pt=pt, c4=c4, c=c, hf=hf: e.transpose(
                    out=pt[:, c4 * 128:(c4 + 1) * 128], in_=hf[:, c * 128:(c + 1) * 128], identity=cx.ident_f[:, :]),
                    reads=[hf, cx.ident_f], writes=[pt])
            dst = xT[:, half * 4:(half + 1) * 4, i * 128:(i + 1) * 128]
            src = pt[:, :].rearrange("p (c t) -> p c t", c=4)
            cp(s, "act" if half else "dve", xT, dst, pt, src)
    s.end_phase()
    s.phase_es = old


def ln_epilogue(s, cx, y_ps, hf, g_bc, b_bc, wk, out_t):
    r, stats, mv, sd, rstd, nmr, xn = wk
    for half in range(2):
        sl = slice(half * 512, (half + 1) * 512)
        yt, yap = y_ps[half] if isinstance(y_ps[half], tuple) else (y_ps[half], y_ps[half][:, :])
        stt(s, "dve", r, r[:, sl], hf, hf[:, sl], ALPHA, yt, yap, ALU.mult, ALU.add)
        s.op("dve", lambda e, half=half, sl=sl: e.bn_stats(out=stats[:, half, :], in_=r[:, sl]), reads=[r], writes=[stats])
    s.op("dve", lambda e: e.bn_aggr(out=mv[:, :], in_=stats[:, :, :].rearrange("p a b -> p (a b)")), reads=[stats], writes=[mv])
    ts(s, "dve", sd, sd[:, :], mv, mv[:, 1:2], LN_EPS, None, ALU.add)
    act(s, sd, sd[:, :], sd, sd[:, :], AF.Sqrt)
    s.op("dve", lambda e: e.reciprocal(out=rstd[:, :], in_=sd[:, :]), reads=[sd], writes=[rstd])
    stt(s, "dve", nmr, nmr[:, :], mv, mv[:, 0:1], -1.0, rstd, rstd[:, :], ALU.mult, ALU.mult)
    act(s, xn, xn[:, :], r, r[:, :], AF.Identity, scale=rstd[:, 0:1], bias=nmr[:, 0:1], extra_reads=[rstd, nmr])
    tt(s, "pool", xn, xn[:, :], xn, xn[:, :], g_bc, g_bc[:, :], ALU.mult)
    tt(s, "pool", out_t, out_t[:, :], xn, xn[:, :], b_bc, b_bc[:, :], ALU.add)


def ln_work(s):
    return (s.sb([128, 1024], F32, "lnr"), s.sb([128, 2, 6], F32, "lnst"), s.sb([128, 2], F32, "lnmv"),
            s.sb([128, 1], F32, "lnsd"), s.sb([128, 1], F32, "lnrs"), s.sb([128, 1], F32, "lnnm"),
            s.sb([128, 1024], F32, "lnxn"))


def bcast_row(ap_row, n):
    return ap_row.partition_broadcast(128)


def phase_A(s, cx, dr, l, j, h_in, h_out):
    nc = s.nc
    h_in_res, h_in_ap = h_in[0], h_in[1]
    w_in = dr["a_w_in"][j]
    w_out = dr["a_w_out"][j]
    w_in_v = w_in.rearrange("(c p) n -> p c n", p=128)
    qkT_res, qkT = dr["qkT"]
    attT_res, attT = dr["attT"]
    wres = dr["wres"]

    s.begin_phase()
    xT = s.sb([128, 8, S], BF16, "xT")
    load_xT(s, cx, h_in_res, h_in_ap, xT)

    import os
    stop = int(os.environ.get("KSTOP", "9"))
    if stop < 1:
        s.end_phase()
        return
    with ExitStack() as es1:
        old = s.phase_es
        s.phase_es = es1
        wqk = s.sb([128, 8, 2048], BF16, "wqk")
        for c in range(8):
            wload(s, cx, wqk, wqk[:, c, :], wres, w_in[c * 128:(c + 1) * 128, 0:2048], [2048])
        rc = s.sb([128, 4], F32, "ropec")
        ld(s, "sp", rc, rc[:, :], wres, dr["rope_consts"])
        posi = s.sb([128, 512], I32, "posi")
        posf = s.sb([128, 512], F32, "posf")
        ang = s.sb([128, 512], F32, "ang")
        ang2 = s.sb([128, 512], F32, "ang2")
        Ct = s.sb([128, 512], F32, "Ct")
        St = s.sb([128, 512], F32, "St")
        scr = (s.sb([128, 512], I32, "ki"), s.sb([128, 512], F32, "kf"), s.sb([128, 512], F32, "rr"))
        psq = [s.ps([128, 512], F32, "psq") for _ in range(2)]
        psp = [s.ps([128, 512], F32, "psp") for _ in range(2)]
        qbs = [s.sb([128, 512], BF16, "qb") for _ in range(2)]
        t1s = [s.sb([128, 512], F32, "t1") for _ in range(2)]
        t2s = [s.sb([128, 512], F32, "t2") for _ in range(2)]
        obs = [s.sb([128, 512], BF16, "ob") for _ in range(3)]
        it = 0
        ksub = int(os.environ.get("KSUB", "99"))
        for tg in range(int(os.environ.get('KTG', '8')) if ksub > 0 else 0):
            tsl = slice(tg * 512, (tg + 1) * 512)
            ld(s, "sp", posi, posi[:, :], wres, bcast_row(dr["positions"][tg * 512:(tg + 1) * 512], 512))
            cp(s, "dve", posf, posf[:, :], posi, posi[:, :])
            ts(s, "dve", ang, ang[:, :], posf, posf[:, :], rc[:, 0:1], None, ALU.mult, extra_reads=[rc])
            ts(s, "dve", ang2, ang2[:, :], ang, ang[:, :], math.pi / 2, None, ALU.add)
            sin_table(s, St, St[:, :], ang, ang[:, :], None, rc[:, 1:2], rc, scr)
            sin_table(s, Ct, Ct[:, :], ang2, ang2[:, :], None, None, None, scr)
            for hp in range(int(os.environ.get('KHP', '8')) if ksub > 1 else 0):
                for w in range(2):
                    pq = psq[it % 2]
                    pp = psp[it % 2]
                    qb = qbs[it % 2]
                    t1 = t1s[it % 2]
                    t2 = t2s[it % 2]
                    ob = obs[it % 3]
                    it += 1
                    col = w * 1024 + hp * 128
                    for c in range(8):
                        mm(s, pq, pq[:, :], wqk, wqk[:, c, col:col + 128], xT, xT[:, c, tsl], c == 0, c == 7)
                    cp(s, "act", qb, qb[:, :], pq, pq[:, :])
                    if ksub < 3:
                        continue
                    mm(s, pp, pp[:, :], cx.perm, cx.perm[:, :], qb, qb[:, :], True, True)
                    tt(s, "dve", t1, t1[:, :], pq, pq[:, :], Ct, Ct[:, :], ALU.mult)
                    tt(s, "dve", t2, t2[:, :], pp, pp[:, :], St, St[:, :], ALU.mult)
                    tt(s, "pool", ob, ob[:, :], t1, t1[:, :], t2, t2[:, :], ALU.add)
                    if ksub < 4:
                        continue
                    st(s, "pool", qkT_res, qkT[w, hp * 128:(hp + 1) * 128, tsl], ob, ob[:, :])
        s.phase_es = old
    s_sub_barrier(s)
    if stop < 2:
        s.end_phase()
        return

    vsc_res, vsc = dr["vsc"]
    with ExitStack() as es2:
        old = s.phase_es
        s.phase_es = es2
        QT = s.sb([128, S], BF16, "QT")
        KT = s.sb([128, S], BF16, "KT")
        wv = s.sb([128, 8, 128], BF16, "wv")
        Vn = s.sb([128, 32, 128], BF16, "Vn")
        Vaugs = [s.sb([128, 32, 2, 128], BF16, "Vaug") for _ in range(2)]
        for Va in Vaugs:
            s.op("pool", lambda e, Va=Va: e.memset(Va[:, :, 0, 64:128], 1.0), writes=[Va], cost=3000)
            s.op("pool", lambda e, Va=Va: e.memset(Va[:, :, 1, 0:64], 1.0), writes=[Va], cost=3000)
        acc = s.sb([128, 2, S], F32, "acc")
        obuf = s.sb([128, S], BF16, "obuf")
        recs = [s.sb([128, 512], F32, "rec") for _ in range(2)]
        psV = [s.ps([128, 512], F32, "psV") for _ in range(2)]
        psS = [s.ps([128, 512], F32, "psS") for _ in range(4)]
        psO_ = [s.ps([128, 512], F32, "psO") for _ in range(2)]
        psO = [T(p[:, 0:256].rearrange("p (a q) -> p a q", a=2), p.res) for p in psO_]
        pes = [s.sb([128, 512], BF16, "pe") for _ in range(4)]
        PTs = [s.sb([128, 2, 2, 128], BF16, "PT") for _ in range(4)]
        vi = 0
        bi_ctr = 0
        k2 = int(os.environ.get("KSUB2", "99"))
        for hp in range(int(os.environ.get("KHP2", "8"))):
            ld(s, "sp", QT, QT[:, :], qkT_res, qkT[0, hp * 128:(hp + 1) * 128, :])
            ld(s, "sp", KT, KT[:, :], qkT_res, qkT[1, hp * 128:(hp + 1) * 128, :])
            wload(s, cx, wv, wv[:, :, :], wres, w_in_v[:, :, 2048 + hp * 128:2048 + (hp + 1) * 128], [8, 128])
            for b in range(32):
                pv = psV[(b // 4) % 2]
                for c in range(8):
                    mm(s, pv, pv[:, (b % 4) * 128:(b % 4 + 1) * 128], xT, xT[:, c, b * 128:(b + 1) * 128], wv, wv[:, c, :], c == 0, c == 7)
                if b % 4 == 3:
                    cp(s, "act" if (b // 4) % 2 else "dve", Vn, Vn[:, b - 3:b + 1, :], pv,
                       pv[:, :].rearrange("p (b f) -> p b f", b=4))
            vs = vsc[hp % 2]
            st(s, "sp", vsc_res, vs.rearrange("(i p) c -> p i c", p=128), Vn, Vn[:, :, :])
            for di, d in enumerate((1, 4, 16)[:int(os.environ.get("KBR", "3"))] if k2 > 0 else ()):
                nb = 32 // d
                Vaug = Vaugs[vi % 2]
                vi += 1

                def tok(r, n, d=d):
                    return slice(r + d * 128 * n, r + d * 128 * n + 127 * d + 1, d)
                if d > 1:
                    view = vs.rearrange("(n p r) c -> p r n c", p=128, r=d)
                    for r in range(d):
                        s.dma("sp" if r % 2 else "act", lambda e, r=r, nb=nb, view=view: e.dma_start(
                            out=Vn[:, r * nb:(r + 1) * nb, :], in_=view[:, r, :, :]),
                            reads=[vsc_res], writes=[Vn], nbytes=4 * nb * 32768)
                cp(s, "act", Vaug, Vaug[:, :, 0, 0:64], Vn, Vn[:, :, 0:64])
                cp(s, "act", Vaug, Vaug[:, :, 1, 64:128], Vn, Vn[:, :, 64:128])
                for b2 in range(int(os.environ.get("KB", "16")) if k2 > 1 else 0):
                    blocks = (2 * b2, 2 * b2 + 1)
                    n0 = blocks[0] % nb
                    PTh = []
                    for hh in range(2):
                        hs = slice(hh * 64, (hh + 1) * 64)
                        pS = psS[(2 * bi_ctr + hh) % 4]
                        pe_ = pes[(2 * bi_ctr + hh) % 4]
                        PT = PTs[(2 * bi_ctr + hh) % 4]
                        PTh.append(PT)
                        pS4 = pS[:, :].rearrange("p (b c q) -> p b c q", b=2, c=2)
                        pe4 = pe_[:, :].rearrange("p (b c q) -> p b c q", b=2, c=2)
                        for bi, b in enumerate(blocks):
                            r, n = divmod(b, nb)
                            mm(s, pS, pS4[:, bi, 0, :], KT, KT[hs, tok(r, n)], QT, QT[hs, tok(r, n)], True, True)
                            if n > 0:
                                mm(s, pS, pS4[:, bi, 1, :], KT, KT[hs, tok(r, n - 1)], QT, QT[hs, tok(r, n)], True, True)
                        if n0 > 0:
                            act(s, pe_, pe_[:, :], pS, pS[:, :], AF.Exp, scale=0.125)
                            tt(s, "pool", PT, PT[:, :, :, :], pe_, pe4, cx.maskA, cx.maskA[:, :, :, :], ALU.mult)
                        else:
                            act(s, pe_, pe4[:, 0, 0, :], pS, pS4[:, 0, 0, :], AF.Exp, scale=0.125)
                            act(s, pe_, pe4[:, 1, :, :], pS, pS4[:, 1, :, :], AF.Exp, scale=0.125)
                            tt(s, "pool", PT, PT[:, 0, 0, :], pe_, pe4[:, 0, 0, :], cx.maskA, cx.maskA[:, 0, 0, :], ALU.mult)
                            tt(s, "pool", PT, PT[:, 1, :, :], pe_, pe4[:, 1, :, :], cx.maskA, cx.maskA[:, 1, :, :], ALU.mult)
                    bi_ctr += 1
                    if k2 < 3:
                        continue
                    for bi, b in enumerate(blocks):
                        r, n = divmod(b, nb)
                        pO = psO[b % 2]
                        for hh in range(2):
                            PT = PTh[hh]
                            mm(s, pO, pO[:, hh, :], Vaug, Vaug[:, b, hh, :], PT, PT[:, bi, 0, :], True, n == 0)
                            if n > 0:
                                mm(s, pO, pO[:, hh, :], Vaug, Vaug[:, b - 1, hh, :], PT, PT[:, bi, 1, :], False, True)
                        if k2 < 4:
                            continue
                        if di == 0:
                            cp(s, "dve", acc, acc[:, :, tok(r, n)], pO, pO[:, :, :])
                        else:
                            tt(s, "dve", acc, acc[:, :, tok(r, n)], acc, acc[:, :, tok(r, n)], pO, pO[:, :, :], ALU.add)
            if k2 < 5:
                continue
            for tg in range(8):
                tsl = slice(tg * 512, (tg + 1) * 512)
                pd = psV[tg % 2]
                rec = recs[tg % 2]
                mm(s, pd, pd[:, :], cx.swlo, cx.swlo[:, :], acc, acc[:, 1, tsl], True, False)
                mm(s, pd, pd[:, :], cx.swhi, cx.swhi[:, :], acc, acc[:, 0, tsl], False, True)
                s.op("dve", lambda e, rec=rec, pd=pd: e.reciprocal(out=rec[:, :], in_=pd[:, :]), reads=[pd], writes=[rec], cost=500)
                tt(s, "pool", obuf, obuf[0:64, tsl], acc, acc[0:64, 0, tsl], rec, rec[0:64, :], ALU.mult)
                tt(s, "pool", obuf, obuf[64:128, tsl], acc, acc[64:128, 1, tsl], rec, rec[64:128, :], ALU.mult)
            st(s, "sp", attT_res, attT[hp * 128:(hp + 1) * 128, :], obuf, obuf[:, :])
        s.phase_es = old
    s.end_phase()

    if stop < 3:
        return
    s.begin_phase()
    proj_ln(s, cx, dr, attT_res, attT, 8, w_out, h_in, h_out, dr["ln1_g"][l], dr["ln1_b"][l])
    s.end_phase()


def s_sub_barrier(s):
    es = s.phase_es
    s.phase_es = ExitStack()
    s.end_phase()
    s.phase_es = es


def proj_ln(s, cx, dr, aT_res, aT, nchunk, w_out, h_in, h_out, g_row, b_row):
    h_in_res, h_in_ap = h_in[0], h_in[1]
    h_out_res, h_out_ap, final = h_out
    wres = dr["wres"]
    wo = s.sb([128, nchunk, 1024], BF16, "wo")
    for c in range(nchunk):
        wload(s, cx, wo, wo[:, c, :], wres, w_out[c * 128:(c + 1) * 128, :], [1024])
    g_bc = s.sb([128, 1024], F32, "gbc")
    b_bc = s.sb([128, 1024], F32, "bbc")
    ld(s, "sp", g_bc, g_bc[:, :], wres, bcast_row(g_row, 1024))
    ld(s, "sp", b_bc, b_bc[:, :], wres, bcast_row(b_row, 1024))
    ats = [s.sb([128, nchunk, 512], BF16, "at") for _ in range(2)]
    hfs = [s.sb([128, 1024], F32, "hf2") for _ in range(2)]
    hos = [s.sb([128, 1024], F32, "ho") for _ in range(2)]
    psY = [[s.ps([128, 512], F32, "psY") for _ in range(2)] for _ in range(2)]
    wk = ln_work(s)
    aTv = aT[0:nchunk * 128, :].rearrange("(c p) t -> p c t", p=128)
    for tg in range(8):
        at = ats[tg % 2]
        ld(s, "sp", at, at[:, :, :], aT_res, aTv[:, :, tg * 512:(tg + 1) * 512])
        for ti in range(4):
            i = tg * 4 + ti
            hf = hfs[i % 2]
            ho = hos[i % 2]
            py = psY[i % 2]
            ld(s, "sp", hf, hf[:, :], h_in_res, h_in_ap[i * 128:(i + 1) * 128, :])
            for half in range(2):
                for c in range(nchunk):
                    mm(s, py[half], py[half][:, :], at, at[:, c, ti * 128:(ti + 1) * 128], wo,
                       wo[:, c, half * 512:(half + 1) * 512], c == 0, c == nchunk - 1)
            ln_epilogue(s, cx, py, hf, g_bc, b_bc, wk, ho)
            st(s, "pool", h_out_res, h_out_ap[i * 128:(i + 1) * 128, :], ho, ho[:, :], final=final)


def decay_tables(s, cx, LF, ig, nh, kscale_ln, EA, EB, ET, psX):
    n = NT * nh
    Fin = s.sb([128, NT, nh], F32, "Fin")
    tot = s.sb([128, NT, nh], F32, "tot")
    inc = s.sb([128, NT, nh], F32, "inc")
    LF2 = LF[:, :, :].rearrange("p a b -> p (a b)")
    mm(s, psX, psX[:, 0:n], cx.tri_le, cx.tri_le[:, :], LF, LF2, True, True)
    cp(s, "dve", Fin, Fin[:, :, :].rearrange("p a b -> p (a b)"), psX, psX[:, 0:n])
    mm(s, psX, psX[:, 0:n], cx.ones_f, cx.ones_f[:, :], LF, LF2, True, True)
    cp(s, "dve", tot, tot[:, :, :].rearrange("p a b -> p (a b)"), psX, psX[:, 0:n])
    for h in range(nh):
        s.op("dve", lambda e, h=h: e.tensor_tensor_scan(out=inc[:, :, h], data0=cx.ones_f[:, 0:NT], data1=tot[:, :, h],
                                                        initial=0.0, op0=ALU.mult, op1=ALU.add),
             reads=[cx.ones_f, tot], writes=[inc])
    tt(s, "dve", EB, EB[:, :, :], inc, inc[:, :, :], tot, tot[:, :, :], ALU.subtract)
    tt(s, "dve", EA, EA[:, :, :], Fin, Fin[:, :, :], EB, EB[:, :, :], ALU.add)
    if ig is not None:
        tt(s, "dve", EA, EA[:, :, :], ig[0], ig[1], EA, EA[:, :, :], ALU.subtract)
        ts(s, "dve", EA, EA[:, :, :], EA, EA[:, :, :], kscale_ln, None, ALU.add)
    else:
        ts(s, "dve", EA, EA[:, :, :], EA, EA[:, :, :], -1.0, kscale_ln, ALU.mult, ALU.add)
    act(s, ET, ET[:, :, :], Fin, Fin[:, :, :], AF.Exp)


def make_E(s, E, EA, EB, h, tmpE):
    tt(s, "pool", tmpE, tmpE[:, :, :], EA, EA[:, :, h].unsqueeze(2).to_broadcast([128, NT, NT]),
       EB, EB[:, :, h].unsqueeze(1).to_broadcast([128, NT, NT]), ALU.add)
    ts(s, "pool", tmpE, tmpE[:, :, :], tmpE, tmpE[:, :, :], 80.0, None, ALU.min)
    act(s, E, E[:, :, :], tmpE, tmpE[:, :, :], AF.Exp)


def decay_main(s, cx, heads, psS, psAcc, PTs, fin_cb, jmax=NT):
    cnt = [0] * len(heads)
    for j in range(jmax):
        for hi, hd in enumerate(heads):
            acc = psAcc[hi]
            dvw = hd["dvw"]
            nk = len(hd["kparts"])
            for i0 in range(0, j + 1, 4):
                i1 = min(i0 + 4, j + 1)
                n = i1 - i0
                pS = psS[hi][cnt[hi] % 2]
                PT = PTs[hi][cnt[hi] % 2]
                cnt[hi] += 1
                for ii in range(n):
                    i = i0 + ii
                    for kc, (Kt, Kap, Qt, Qap) in enumerate(hd["kparts"]):
                        mm(s, pS, pS[:, ii * 128:(ii + 1) * 128], Kt, Kap(i), Qt, Qap(j), kc == 0, kc == nk - 1)
                E = hd["E"]
                tt(s, "dve", PT, PT[:, 0:n, :], pS, pS[:, 0:n * 128].rearrange("p (a t) -> p a t", a=n),
                   E, E[:, i0:i1, j].unsqueeze(2).to_broadcast([128, n, 128]), ALU.mult)
                if i1 - 1 == j:
                    tt(s, "pool", PT, PT[:, n - 1, :], PT, PT[:, n - 1, :], cx.tri_b, cx.tri_b[:, :], ALU.mult)
                for ii in range(n):
                    i = i0 + ii
                    mm(s, acc, acc[:, 0:dvw], PT, PT[:, ii, :], hd["V"], hd["Vap"](i), i == 0, i == j)
            fin_cb(hi, hd, j, acc)


def small_norm(s, x_t, x_ap, width, wk, out_t, out_ap, g_t, g_ap):
    stats, mv, sd, rstd, nmr, xn = wk
    s.op("dve", lambda e: e.bn_stats(out=stats[:, :], in_=x_ap), reads=[x_t], writes=[stats])
    s.op("dve", lambda e: e.bn_aggr(out=mv[:, :], in_=stats[:, :]), reads=[stats], writes=[mv])
    ts(s, "dve", sd, sd[:, :], mv, mv[:, 1:2], LN_EPS, None, ALU.add)
    act(s, sd, sd[:, :], sd, sd[:, :], AF.Sqrt)
    s.op("dve", lambda e: e.reciprocal(out=rstd[:, :], in_=sd[:, :]), reads=[sd], writes=[rstd])
    stt(s, "dve", nmr, nmr[:, :], mv, mv[:, 0:1], -1.0, rstd, rstd[:, :], ALU.mult, ALU.mult)
    act(s, xn, xn[:, 0:width], x_t, x_ap, AF.Identity, scale=rstd[:, 0:1], bias=nmr[:, 0:1], extra_reads=[rstd, nmr])
    tt(s, "pool", out_t, out_ap, xn, xn[:, 0:width], g_t, g_ap, ALU.mult)


def small_norm_work(s, width):
    return (s.sb([128, 6], F32, "snst"), s.sb([128, 2], F32, "snmv"), s.sb([128, 1], F32, "snsd"),
            s.sb([128, 1], F32, "snrs"), s.sb([128, 1], F32, "snnm"), s.sb([128, width], F32, "snxn"))


def gate_proj_ln(s, cx, dr, xT, HN_res, HN, F, w_gate_ap, gfunc, w_out, h_in, h_out, g_row, b_row):
    h_in_res, h_in_ap = h_in[0], h_in[1]
    h_out_res, h_out_ap, final = h_out
    wres = dr["wres"]
    nch = F // 128
    nb = F // 512
    wg = s.sb([128, 8, F], BF16, "wgate")
    for c in range(8):
        for q in range(F // 1024):
            wload(s, cx, wg, wg[:, c, q * 1024:(q + 1) * 1024], wres, w_gate_ap[c * 128:(c + 1) * 128, q * 1024:(q + 1) * 1024], [1024])
    wo = s.sb([128, nch, 1024], BF16, "wo")
    for c in range(nch):
        wload(s, cx, wo, wo[:, c, :], wres, w_out[c * 128:(c + 1) * 128, :], [1024])
    g_bc = s.sb([128, 1024], F32, "gbc")
    b_bc = s.sb([128, 1024], F32, "bbc")
    ld(s, "sp", g_bc, g_bc[:, :], wres, bcast_row(g_row, 1024))
    ld(s, "sp", b_bc, b_bc[:, :], wres, bcast_row(b_row, 1024))
    nbuf = 1 if F > 1024 else 2
    hns = [s.sb([128, F], F32, "hn") for _ in range(nbuf)]
    sig = s.sb([128, F], F32, "sig")
    Gb = s.sb([128, F], BF16, "Gb")
    GT_ = s.sb([128, nch, 128], BF16, "GTt")
    hfs = [s.sb([128, 1024], F32, "hf2") for _ in range(nbuf)]
    hos = [s.sb([128, 1024], F32, "ho") for _ in range(nbuf)]
    psg = [s.ps([128, 512], F32, "psg") for _ in range(2)]
    pst = [s.ps([128, 1024], BF16, "pst") for _ in range(2)]
    psY = [s.ps([128, 512], F32, "psY") for _ in range(2)]
    wk = ln_work(s)
    for i in range(NT):
        hn = hns[i % nbuf]
        hf = hfs[i % nbuf]
        ho = hos[i % nbuf]
        ld(s, "sp", hn, hn[:, :], HN_res, HN[i * 128:(i + 1) * 128, 0:F])
        ld(s, "sp", hf, hf[:, :], h_in_res, h_in_ap[i * 128:(i + 1) * 128, :])
        for q in range(nb):
            pg = psg[q % 2]
            for c in range(8):
                mm(s, pg, pg[:, :], xT, xT[:, c, i * 128:(i + 1) * 128], wg, wg[:, c, q * 512:(q + 1) * 512], c == 0, c == 7)
            act(s, sig, sig[:, q * 512:(q + 1) * 512], pg, pg[:, :], gfunc)
        tt(s, "pool", Gb, Gb[:, :], hn, hn[:, :], sig, sig[:, :], ALU.mult)
        for q in range(nch // 8):
            pt = pst[q % 2]
            for c8 in range(8):
                c = q * 8 + c8
                s.op("pe", lambda e, pt=pt, c8=c8, c=c: e.transpose(out=pt[:, c8 * 128:(c8 + 1) * 128], in_=Gb[:, c * 128:(c + 1) * 128],
                                                                   identity=cx.ident_b[:, :]), reads=[Gb, cx.ident_b], writes=[pt])
            cp(s, "act" if q else "dve", GT_, GT_[:, q * 8:(q + 1) * 8, :], pt, pt[:, :].rearrange("p (c t) -> p c t", c=8))
        for half in range(2):
            for c in range(nch):
                mm(s, psY[half], psY[half][:, :], GT_, GT_[:, c, :], wo, wo[:, c, half * 512:(half + 1) * 512], c == 0, c == nch - 1)
        ln_epilogue(s, cx, psY, hf, g_bc, b_bc, wk, ho)
        st(s, "pool", h_out_res, h_out_ap[i * 128:(i + 1) * 128, :], ho, ho[:, :], final=final)


def phase_B(s, cx, dr, l, j_, h_in, h_out):
    import os
    nc = s.nc
    h_in_res, h_in_ap = h_in[0], h_in[1]
    wres = dr["wres"]
    w_in = dr["b_w_in"][j_]
    w_in_v = w_in.rearrange("(c p) n -> p c n", p=128)
    HN_res, HN = dr["hn"]
    bstop = int(os.environ.get("BSTOP", "9"))
    jmax = int(os.environ.get("BJMAX", str(NT)))
    npair = int(os.environ.get("BPAIRS", "4"))

    s.begin_phase()
    xT = s.sb([128, 8, S], BF16, "xT")
    GATES = s.sb([128, NT, 16], F32, "GATES")
    with ExitStack() as es0:
        old = s.phase_es
        s.phase_es = es0
        Wg32 = s.sb([128, 8, 16], F32, "Wg32")
        ld(s, "sp", Wg32, Wg32[:, :, :], wres, w_in_v[:, :, 3072:3088])
        gb = s.sb([128, 16], F32, "gbias")
        ld(s, "sp", gb, gb[:, :], wres, bcast_row(dr["b_gate_bias"][j_], 16))
        hfs = [s.sb([128, 1024], F32, "hf") for _ in range(3)]
        xTts = [s.sb([128, 8, 128], F32, "xTt") for _ in range(2)]
        pts = [s.ps([128, 512], F32, "ptr") for _ in range(2)]
        psg = s.ps([128, 512], F32, "psgate")
        for i in range(NT):
            hf = hfs[i % 3]
            xTt = xTts[i % 2]
            ld(s, "sp", hf, hf[:, :], h_in_res, h_in_ap[i * 128:(i + 1) * 128, :])
            for half in range(2):
                pt = pts[half]
                for c4 in range(4):
                    c = half * 4 + c4
                    s.op("pe", lambda e, pt=pt, c4=c4, c=c, hf=hf: e.transpose(
                        out=pt[:, c4 * 128:(c4 + 1) * 128], in_=hf[:, c * 128:(c + 1) * 128], identity=cx.ident_f[:, :]),
                        reads=[hf, cx.ident_f], writes=[pt])
                src = pt[:, :].rearrange("p (c t) -> p c t", c=4)
                cp(s, "act", xT, xT[:, half * 4:(half + 1) * 4, i * 128:(i + 1) * 128], pt, src)
                cp(s, "dve", xTt, xTt[:, half * 4:(half + 1) * 4, :], pt, src)
            for c in range(8):
                mm(s, psg, psg[:, 0:16], xTt, xTt[:, c, :], Wg32, Wg32[:, c, :], c == 0, c == 7)
            tt(s, "dve", GATES, GATES[:, i, :], psg, psg[:, 0:16], gb, gb[:, :], ALU.add)
        s.phase_es = old
    s_sub_barrier(s)

    EA = s.sb([128, NT, 8], F32, "EA")
    EB = s.sb([128, NT, 8], F32, "EB")
    ET = s.sb([128, NT, 8], F32, "ET")
    with ExitStack() as es1:
        old = s.phase_es
        s.phase_es = es1
        LF = s.sb([128, NT, 8], F32, "LF")
        psX = s.ps([128, 512], F32, "psX")
        act(s, LF, LF[:, :, :], GATES, GATES[:, :, 8:16], AF.Exp, scale=-1.0)
        ts(s, "dve", LF, LF[:, :, :], LF, LF[:, :, :], 1.0, None, ALU.add)
        act(s, LF, LF[:, :, :], LF, LF[:, :, :], AF.Ln)
        ts(s, "dve", LF, LF[:, :, :], LF, LF[:, :, :], -1.0, None, ALU.mult)
        decay_tables(s, cx, LF, (GATES, GATES[:, :, 0:8]), 8, math.log(0.125), EA, EB, ET, psX)
        s.phase_es = old
    s_sub_barrier(s)
    if bstop < 2:
        s.end_phase()
        return

    with ExitStack() as es2:
        old = s.phase_es
        s.phase_es = es2
        ng_bc = s.sb([128, 1024], F32, "ngbc")
        ld(s, "sp", ng_bc, ng_bc[:, :], wres, bcast_row(dr["b_norm_g"][j_], 1024))
        wq = s.sb([128, 8, 128], BF16, "wq")
        wkk = s.sb([128, 8, 128], BF16, "wk")
        wv = s.sb([128, 8, 256], BF16, "wv")
        cw = [s.sb([128, 4], F32, "cw") for _ in range(2)]
        cb = [s.sb([128, 1], F32, "cb") for _ in range(2)]
        UT = s.sb([128, S + 3], F32, "UT")
        s.op("pool", lambda e: e.memset(UT[:, 0:3], 0.0), writes=[UT])
        cacc = s.sb([128, S], F32, "cacc")
        QT = s.sb([128, S], BF16, "QT")
        KT = s.sb([128, S], BF16, "KT")
        Vaug = s.sb([128, NT, 2, 129], BF16, "Vaug")
        s.op("pool", lambda e: e.memset(Vaug[:, :, :, 128:129], 1.0), writes=[Vaug])
        Es = [s.sb([128, NT, NT], F32, "E") for _ in range(2)]
        tmpE = s.sb([128, NT, NT], F32, "tmpE")
        psX = [s.ps([128, 512], F32, "psX") for _ in range(2)]
        psS = [[s.ps([128, 512], F32, "psS") for _ in range(2)] for _ in range(2)]
        psAcc = [s.ps([128, 512], F32, "psAcc") for _ in range(2)]
        PTs = [[s.sb([128, 4, 128], BF16, "PT") for _ in range(2)] for _ in range(2)]
        snw = small_norm_work(s, 128)
        sm = {k: s.sb([128, 1], F32, k) for k in ("ed", "ad", "rec", "fac")}
        hnr = s.sb([128, 128], F32, "hnr")
        HNt = [s.sb([128, 256], F32, "HNt") for _ in range(2)]
        for hp in range(npair):
            for w, (wt, dst) in enumerate(((wq, QT), (wkk, KT))):
                col = w * 512 + hp * 128
                wload(s, cx, wt, wt[:, :, :], wres, w_in_v[:, :, col:col + 128], [8, 128])
                for jj in range(4):
                    ld(s, "sp", cw[w], cw[w][:, jj:jj + 1], wres, dr["b_conv_w"][j_][jj, col:col + 128].rearrange("(c o) -> c o", o=1))
                ld(s, "sp", cb[w], cb[w][:, :], wres, dr["b_conv_b"][j_][col:col + 128].rearrange("(c o) -> c o", o=1))
                for tg in range(8):
                    px = psX[tg % 2]
                    for c in range(8):
                        mm(s, px, px[:, :], wt, wt[:, c, :], xT, xT[:, c, tg * 512:(tg + 1) * 512], c == 0, c == 7)
                    cp(s, "act", UT, UT[:, 3 + tg * 512:3 + (tg + 1) * 512], px, px[:, :])
                ts(s, "dve", cacc, cacc[:, :], UT, UT[:, 0:S], cw[w][:, 0:1], None, ALU.mult, extra_reads=[cw[w]])
                for jj in range(1, 4):
                    stt(s, "dve", cacc, cacc[:, :], UT, UT[:, jj:S + jj], cw[w][:, jj:jj + 1], cacc, cacc[:, :], ALU.mult, ALU.add,
                        extra_reads=[cw[w]])
                act(s, dst, dst[:, :], cacc, cacc[:, :], AF.Silu, scale=1.0, bias=cb[w][:, 0:1], extra_reads=[cb[w]])
            wload(s, cx, wv, wv[:, :, :], wres, w_in_v[:, :, 1024 + hp * 256:1024 + (hp + 1) * 256], [8, 256])
            for i in range(NT):
                px = psX[i % 2]
                for c in range(8):
                    mm(s, px, px[:, 0:256], xT, xT[:, c, i * 128:(i + 1) * 128], wv, wv[:, c, :], c == 0, c == 7)
                cp(s, "act", Vaug, Vaug[:, i, :, 0:128], px, px[:, 0:256].rearrange("p (a d) -> p a d", a=2))
            heads = []
            for hh in range(2):
                h = hp * 2 + hh
                make_E(s, Es[hh], EA, EB, h, tmpE)
                hs = slice(hh * 64, (hh + 1) * 64)
                heads.append(dict(
                    kparts=[(KT, (lambda i, hs=hs: KT[hs, i * 128:(i + 1) * 128]), QT, (lambda j, hs=hs: QT[hs, j * 128:(j + 1) * 128]))],
                    V=Vaug, Vap=(lambda i, hh=hh: Vaug[:, i, hh, :]), dvw=129, E=Es[hh], h=h, hh=hh))

            def fin(hi, hd, j, acc):
                h, hh = hd["h"], hd["hh"]
                tt(s, "dve", sm["ed"], sm["ed"][:, :], acc, acc[:, 128:129], ET, ET[:, j, h:h + 1], ALU.mult)
                stt(s, "dve", sm["ad"], sm["ad"][:, :], sm["ed"], sm["ed"][:, :], -1.0, sm["ed"], sm["ed"][:, :], ALU.mult, ALU.max)
                ts(s, "dve", sm["ad"], sm["ad"][:, :], sm["ad"], sm["ad"][:, :], 1.0, None, ALU.max)
                s.op("dve", lambda e: e.reciprocal(out=sm["rec"][:, :], in_=sm["ad"][:, :]), reads=[sm["ad"]], writes=[sm["rec"]])
                tt(s, "dve", sm["fac"], sm["fac"][:, :], sm["rec"], sm["rec"][:, :], ET, ET[:, j, h:h + 1], ALU.mult)
                act(s, hnr, hnr[:, :], acc, acc[:, 0:128], AF.Copy, scale=sm["fac"][:, 0:1], extra_reads=[sm["fac"]])
                ht = HNt[j % 2]
                small_norm(s, hnr, hnr[:, :], 128, snw, ht, ht[:, hh * 128:(hh + 1) * 128], ng_bc, ng_bc[:, h * 128:(h + 1) * 128])
                if hh == 1:
                    st(s, "pool", HN_res, HN[j * 128:(j + 1) * 128, hp * 256:(hp + 1) * 256], ht, ht[:, :])
            decay_main(s, cx, heads, psS, psAcc, PTs, fin, jmax=jmax)
        s.phase_es = old
    s_sub_barrier(s)
    if bstop < 3:
        s.end_phase()
        return

    with ExitStack() as es3:
        old = s.phase_es
        s.phase_es = es3
        gate_proj_ln(s, cx, dr, xT, HN_res, HN, 1024, w_in[:, 2048:3072], AF.Sigmoid, dr["b_w_out"][j_], h_in, h_out,
                     dr["ln1_g"][l], dr["ln1_b"][l])
        s.phase_es = old
    s.end_phase()


def phase_C(s, cx, dr, l, j_, h_in, h_out):
    import os
    nc = s.nc
    h_in_res, h_in_ap = h_in[0], h_in[1]
    wres = dr["wres"]
    w_in = dr["c_w_in"][j_]
    w_in_v = w_in.rearrange("(c p) n -> p c n", p=128)
    HN_res, HN = dr["hn"]
    qkT_res, qkT = dr["qkT"]
    cstop = int(os.environ.get("CSTOP", "9"))
    jmax = int(os.environ.get("CJMAX", str(NT)))
    nheads = int(os.environ.get("CHEADS", "4"))

    s.begin_phase()
    xT = s.sb([128, 8, S], BF16, "xT")
    load_xT(s, cx, h_in_res, h_in_ap, xT)
    EA = s.sb([128, NT, 4], F32, "EA")
    EB = s.sb([128, NT, 4], F32, "EB")
    ET = s.sb([128, NT, 4], F32, "ET")
    with ExitStack() as es1:
        old = s.phase_es
        s.phase_es = es1
        LF = s.sb([128, NT, 4], F32, "LF")
        psX = s.ps([128, 512], F32, "psX")
        for h in range(4):
            s.op("pool", lambda e, h=h: e.memset(LF[:, :, h:h + 1], math.log(1.0 - 2.0 ** (-5.0 - h))), writes=[LF])
        decay_tables(s, cx, LF, None, 4, math.log(1.0 / 16.0), EA, EB, ET, psX)
        s.phase_es = old
    s_sub_barrier(s)

    with ExitStack() as es1:
        old = s.phase_es
        s.phase_es = es1
        wqk = s.sb([128, 8, 2048], BF16, "wqk")
        for c in range(8):
            wload(s, cx, wqk, wqk[:, c, :], wres, w_in[c * 128:(c + 1) * 128, 0:2048], [2048])
        rc = s.sb([128, 4], F32, "ropec")
        ld(s, "sp", rc, rc[:, :], wres, dr["rope_consts"])
        posi = s.sb([128, 512], I32, "posi")
        posf = s.sb([128, 512], F32, "posf")
        ang = s.sb([128, 512], F32, "ang")
        ang2 = s.sb([128, 512], F32, "ang2")
        Ct = s.sb([128, 512], F32, "Ct")
        St = s.sb([128, 512], F32, "St")
        scr = (s.sb([128, 512], I32, "ki"), s.sb([128, 512], F32, "kf"), s.sb([128, 512], F32, "rr"))
        psa = [s.ps([128, 512], F32, "psa") for _ in range(2)]
        psb = [s.ps([128, 512], F32, "psb") for _ in range(2)]
        t1s = [s.sb([128, 512], F32, "t1") for _ in range(2)]
        t2s = [s.sb([128, 512], F32, "t2") for _ in range(2)]
        oas = [s.sb([128, 512], BF16, "oa") for _ in range(2)]
        obs = [s.sb([128, 512], BF16, "ob") for _ in range(2)]
        it = 0
        for tg in range(8):
            tsl = slice(tg * 512, (tg + 1) * 512)
            ld(s, "sp", posi, posi[:, :], wres, bcast_row(dr["positions"][tg * 512:(tg + 1) * 512], 512))
            cp(s, "dve", posf, posf[:, :], posi, posi[:, :])
            ts(s, "dve", ang, ang[:, :], posf, posf[:, :], rc[:, 2:3], None, ALU.mult, extra_reads=[rc])
            ts(s, "dve", ang2, ang2[:, :], ang, ang[:, :], math.pi / 2, None, ALU.add)
            sin_table(s, St, St[:, :], ang, ang[:, :], None, None, None, scr)
            sin_table(s, Ct, Ct[:, :], ang2, ang2[:, :], None, None, None, scr)
            for h in range(4):
                for w in range(2):
                    pa, pb = psa[it % 2], psb[it % 2]
                    t1, t2, oa, ob = t1s[it % 2], t2s[it % 2], oas[it % 2], obs[it % 2]
                    it += 1
                    col = w * 1024 + h * 256
                    for c in range(8):
                        mm(s, pa, pa[:, :], wqk, wqk[:, c, col:col + 128], xT, xT[:, c, tsl], c == 0, c == 7)
                    for c in range(8):
                        mm(s, pb, pb[:, :], wqk, wqk[:, c, col + 128:col + 256], xT, xT[:, c, tsl], c == 0, c == 7)
                    tt(s, "dve", t1, t1[:, :], pa, pa[:, :], Ct, Ct[:, :], ALU.mult)
                    tt(s, "dve", t2, t2[:, :], pb, pb[:, :], St, St[:, :], ALU.mult)
                    tt(s, "pool", oa, oa[:, :], t1, t1[:, :], t2, t2[:, :], ALU.subtract)
                    st(s, "pool", qkT_res, qkT[w, h * 256:h * 256 + 128, tsl], oa, oa[:, :])
                    tt(s, "dve", t1, t1[:, :], pb, pb[:, :], Ct, Ct[:, :], ALU.mult)
                    tt(s, "dve", t2, t2[:, :], pa, pa[:, :], St, St[:, :], ALU.mult)
                    tt(s, "pool", ob, ob[:, :], t1, t1[:, :], t2, t2[:, :], ALU.add)
                    st(s, "pool", qkT_res, qkT[w, h * 256 + 128:h * 256 + 256, tsl], ob, ob[:, :])
        s.phase_es = old
    s_sub_barrier(s)
    if cstop < 2:
        s.end_phase()
        return

    with ExitStack() as es2:
        old = s.phase_es
        s.phase_es = es2
        ng_bc = s.sb([128, 2048], F32, "ngbc")
        ld(s, "sp", ng_bc, ng_bc[:, :], wres, bcast_row(dr["c_norm_g"][j_], 2048))
        QA = s.sb([128, S], BF16, "QA")
        QB = s.sb([128, S], BF16, "QB")
        KA = s.sb([128, S], BF16, "KA")
        KB = s.sb([128, S], BF16, "KB")
        wv = s.sb([128, 8, 512], BF16, "wv")
        Vh = s.sb([128, NT, 512], BF16, "Vh")
        E = s.sb([128, NT, NT], F32, "E")
        tmpE = s.sb([128, NT, NT], F32, "tmpE")
        psX = [s.ps([128, 512], F32, "psX") for _ in range(2)]
        psS = [[s.ps([128, 512], F32, "psS") for _ in range(2)]]
        psAcc = [s.ps([128, 512], F32, "psAcc")]
        PTs = [[s.sb([128, 4, 128], BF16, "PT") for _ in range(2)]]
        snw = small_norm_work(s, 512)
        onr = s.sb([128, 512], F32, "onr")
        HNt = [s.sb([128, 512], F32, "HNt") for _ in range(2)]
        for h in range(nheads):
            for w, (ta, tb) in enumerate(((QA, QB), (KA, KB))):
                ld(s, "sp", ta, ta[:, :], qkT_res, qkT[w, h * 256:h * 256 + 128, :])
                ld(s, "sp", tb, tb[:, :], qkT_res, qkT[w, h * 256 + 128:h * 256 + 256, :])
            wload(s, cx, wv, wv[:, 0:4, :], wres, w_in_v[:, 0:4, 2048 + h * 512:2048 + (h + 1) * 512], [4, 512])
            wload(s, cx, wv, wv[:, 4:8, :], wres, w_in_v[:, 4:8, 2048 + h * 512:2048 + (h + 1) * 512], [4, 512])
            for i in range(NT):
                px = psX[i % 2]
                for c in range(8):
                    mm(s, px, px[:, :], xT, xT[:, c, i * 128:(i + 1) * 128], wv, wv[:, c, :], c == 0, c == 7)
                cp(s, "act", Vh, Vh[:, i, :], px, px[:, :])
            make_E(s, E, EA, EB, h, tmpE)
            heads = [dict(
                kparts=[(KA, (lambda i: KA[:, i * 128:(i + 1) * 128]), QA, (lambda j: QA[:, j * 128:(j + 1) * 128])),
                        (KB, (lambda i: KB[:, i * 128:(i + 1) * 128]), QB, (lambda j: QB[:, j * 128:(j + 1) * 128]))],
                V=Vh, Vap=(lambda i: Vh[:, i, :]), dvw=512, E=E, h=h)]

            def fin(hi, hd, j, acc, h=h):
                act(s, onr, onr[:, :], acc, acc[:, 0:512], AF.Copy, scale=ET[:, j, h:h + 1], extra_reads=[ET])
                ht = HNt[j % 2]
                small_norm(s, onr, onr[:, :], 512, snw, ht, ht[:, :], ng_bc, ng_bc[:, h * 512:(h + 1) * 512])
                st(s, "pool", HN_res, HN[j * 128:(j + 1) * 128, h * 512:(h + 1) * 512], ht, ht[:, :])
            decay_main(s, cx, heads, psS, psAcc, PTs, fin, jmax=jmax)
        s.phase_es = old
    s_sub_barrier(s)
    if cstop < 3:
        s.end_phase()
        return

    with ExitStack() as es3:
        old = s.phase_es
        s.phase_es = es3
        gate_proj_ln(s, cx, dr, xT, HN_res, HN, 2048, w_in[:, 4096:6144], AF.Silu, dr["c_w_out"][j_], h_in, h_out,
                     dr["ln1_g"][l], dr["ln1_b"][l])
        s.phase_es = old
    s.end_phase()


NSLOT = 128
BIGIDX = 4 * 64 * 128


def phase_F(s, cx, dr, l, h_in, h_out):
    import os
    nc = s.nc
    h_in_res, h_in_ap = h_in[0], h_in[1]
    h_out_res, h_out_ap, final = h_out
    wres = dr["wres"]
    xbf_res, xbf = dr["xbf"]
    bt_res, bt = dr["buftok"]
    yb_res, yb = dr["yb"]
    fstop = int(os.environ.get("FSTOP", "9"))

    s.begin_phase()
    OH1 = s.sb([128, NT, 64], F32, "OH1")
    OH2 = s.sb([128, NT, 64], F32, "OH2")
    RK = s.sb([128, NT, 2], F32, "RK")
    GT = s.sb([128, NT, 2], F32, "GT")
    DESTi = s.sb([128, NT, 2], I32, "DESTi")
    IDXW = s.sb([128, NSLOT], I32, "IDXW")
    breg_x = nc.gpsimd.to_reg(S - 1)
    breg_w = nc.gpsimd.to_reg((l + 1) * 64 * 128 - 1)
    breg_y = nc.gpsimd.to_reg(NSLOT * 128 - 1)
    breg_t = nc.gpsimd.to_reg(NSLOT * 128 - 1)

    with ExitStack() as es1:
        old = s.phase_es
        s.phase_es = es1
        Wr = s.sb([128, 8, 72], F32, "Wr")
        ld(s, "sp", Wr, Wr[:, :, 0:8], wres, dr["r_group_w"][l].rearrange("(c p) n -> p c n", p=128))
        ld(s, "sp", Wr, Wr[:, :, 8:72], wres, dr["r_expert_w"][l].rearrange("(c p) n -> p c n", p=128))
        bias = s.sb([128, 72], F32, "rbias")
        ld(s, "sp", bias, bias[:, 0:8], wres, bcast_row(dr["r_group_b"][l], 8))
        ld(s, "sp", bias, bias[:, 8:72], wres, bcast_row(dr["r_expert_b"][l], 64))
        tri_lt = s.sb([128, 128], BF16, "trilt")
        ts(s, "dve", tri_lt, tri_lt[:, :], cx.iof, cx.iof[:, :], 0.0, None, ALU.is_gt)
        toki = s.sb([128, NT], I32, "toki")
        s.op("pool", lambda e: e.iota(toki[:, :], pattern=[[128, NT]], base=0, channel_multiplier=1), writes=[toki])
        cnt = s.sb([128, 64], F32, "cnt")
        s.op("pool", lambda e: e.memset(cnt[:, :], 0.0), writes=[cnt])
        hfs = [s.sb([128, 1024], F32, "hfr") for _ in range(2)]
        hbs = [s.sb([128, 1024], BF16, "hbr") for _ in range(2)]
        xTts = [s.sb([128, 8, 128], F32, "xTt") for _ in range(2)]
        pts = [s.ps([128, 512], F32, "ptr") for _ in range(2)]
        psL = s.ps([128, 512], F32, "psL")
        psPC = s.ps([128, 512], F32, "psPC")
        lg = s.sb([128, 72], F32, "lg")
        sm = {k: s.sb([128, 1], F32, k) for k in ("gmax", "ngmax", "sumg", "pgrp", "v1", "v2", "dv", "ex", "den", "rec")}
        ohg = s.sb([128, 8], F32, "ohg")
        eg = s.sb([128, 8], F32, "eg")
        pen = s.sb([128, 8], F32, "pen")
        em = s.sb([128, 64], F32, "em")
        em2 = s.sb([128, 64], F32, "em2")
        Mb = s.sb([128, 64], BF16, "Mb")
        pos = s.sb([128, 64], F32, "pos")
        tmp = s.sb([128, 64], F32, "tmp")
        for i in range(NT):
            hf = hfs[i % 2]
            hb = hbs[i % 2]
            xTt = xTts[i % 2]
            ld(s, "sp", hf, hf[:, :], h_in_res, h_in_ap[i * 128:(i + 1) * 128, :])
            cp(s, "pool", hb, hb[:, :], hf, hf[:, :])
            st(s, "pool", xbf_res, xbf[i * 128:(i + 1) * 128, :], hb, hb[:, :])
            for half in range(2):
                pt = pts[half]
                for c4 in range(4):
                    c = half * 4 + c4
                    s.op("pe", lambda e, pt=pt, c4=c4, c=c, hf=hf: e.transpose(
                        out=pt[:, c4 * 128:(c4 + 1) * 128], in_=hf[:, c * 128:(c + 1) * 128], identity=cx.ident_f[:, :]),
                        reads=[hf, cx.ident_f], writes=[pt])
                cp(s, "act" if half else "dve", xTt, xTt[:, half * 4:(half + 1) * 4, :], pt,
                   pt[:, :].rearrange("p (c t) -> p c t", c=4))
            for c in range(8):
                mm(s, psL, psL[:, 0:72], xTt, xTt[:, c, :], Wr, Wr[:, c, :], c == 0, c == 7)
            tt(s, "dve", lg, lg[:, :], psL, psL[:, 0:72], bias, bias[:, :], ALU.add)
            g = sm
            s.op("dve", lambda e: e.reduce_max(out=g["gmax"][:, :], in_=lg[:, 0:8], axis=AX.X), reads=[lg], writes=[g["gmax"]])
            ts(s, "dve", ohg, ohg[:, :], lg, lg[:, 0:8], g["gmax"][:, 0:1], None, ALU.is_equal, extra_reads=[g["gmax"]])
            ts(s, "dve", g["ngmax"], g["ngmax"][:, :], g["gmax"], g["gmax"][:, :], -1.0, None, ALU.mult)
            act(s, eg, eg[:, :], lg, lg[:, 0:8], AF.Exp, scale=1.0, bias=g["ngmax"][:, 0:1], extra_reads=[g["ngmax"]])
            s.op("dve", lambda e: e.reduce_sum(out=g["sumg"][:, :], in_=eg[:, :], axis=AX.X), reads=[eg], writes=[g["sumg"]])
            s.op("dve", lambda e: e.reciprocal(out=g["pgrp"][:, :], in_=g["sumg"][:, :]), reads=[g["sumg"]], writes=[g["pgrp"]])
            ts(s, "dve", pen, pen[:, :], ohg, ohg[:, :], -1.0, 1e30, ALU.add, ALU.mult)
            tt(s, "dve", em, em[:, :].rearrange("p (g j) -> p g j", g=8), lg, lg[:, 8:72].rearrange("p (g j) -> p g j", g=8),
               pen, pen[:, :].unsqueeze(2).to_broadcast([128, 8, 8]), ALU.add)
            s.op("dve", lambda e: e.reduce_max(out=g["v1"][:, :], in_=em[:, :], axis=AX.X), reads=[em], writes=[g["v1"]])
            ts(s, "dve", OH1, OH1[:, i, :], em, em[:, :], g["v1"][:, 0:1], None, ALU.is_equal, extra_reads=[g["v1"]])
            stt(s, "dve", em2, em2[:, :], OH1, OH1[:, i, :], -1e30, em, em[:, :], ALU.mult, ALU.add)
            s.op("dve", lambda e: e.reduce_max(out=g["v2"][:, :], in_=em2[:, :], axis=AX.X), reads=[em2], writes=[g["v2"]])
            ts(s, "dve", OH2, OH2[:, i, :], em2, em2[:, :], g["v2"][:, 0:1], None, ALU.is_equal, extra_reads=[g["v2"]])
            tt(s, "dve", g["dv"], g["dv"][:, :], g["v2"], g["v2"][:, :], g["v1"], g["v1"][:, :], ALU.subtract)
            act(s, g["ex"], g["ex"][:, :], g["dv"], g["dv"][:, :], AF.Exp)
            ts(s, "dve", g["den"], g["den"][:, :], g["ex"], g["ex"][:, :], 1.0, None, ALU.add)
            s.op("dve", lambda e: e.reciprocal(out=g["rec"][:, :], in_=g["den"][:, :]), reads=[g["den"]], writes=[g["rec"]])
            tt(s, "dve", GT, GT[:, i, 0:1], g["pgrp"], g["pgrp"][:, :], g["rec"], g["rec"][:, :], ALU.mult)
            tt(s, "dve", GT, GT[:, i, 1:2], GT, GT[:, i, 0:1], g["ex"], g["ex"][:, :], ALU.mult)
            tt(s, "dve", Mb, Mb[:, :], OH1, OH1[:, i, :], OH2, OH2[:, i, :], ALU.add)
            mm(s, psPC, psPC[:, 0:64], tri_lt, tri_lt[:, :], Mb, Mb[:, :], True, True)
            mm(s, psPC, psPC[:, 64:128], cx.ones_b, cx.ones_b[:, :], Mb, Mb[:, :], True, True)
            tt(s, "dve", pos, pos[:, :], psPC, psPC[:, 0:64], cnt, cnt[:, :], ALU.add)
            tt(s, "dve", cnt, cnt[:, :], psPC, psPC[:, 64:128], cnt, cnt[:, :], ALU.add)
            tt(s, "dve", tmp, tmp[:, :], pos, pos[:, :], OH1, OH1[:, i, :], ALU.mult)
            s.op("dve", lambda e, i=i: e.reduce_sum(out=RK[:, i, 0:1], in_=tmp[:, :], axis=AX.X), reads=[tmp], writes=[RK])
            tt(s, "dve", tmp, tmp[:, :], pos, pos[:, :], OH2, OH2[:, i, :], ALU.mult)
            s.op("dve", lambda e, i=i: e.reduce_sum(out=RK[:, i, 1:2], in_=tmp[:, :], axis=AX.X), reads=[tmp], writes=[RK])
        cnti = s.sb([128, 64], I32, "cnti")
        padf = s.sb([128, 64], F32, "padf")
        pends = s.sb([128, 64], F32, "pends")
        pstart = s.sb([128, 64], F32, "pstart")
        ts(s, "dve", cnti, cnti[:, :], cnt, cnt[:, :], 127.0, None, ALU.add)
        ts(s, "dve", cnti, cnti[:, :], cnti, cnti[:, :], 7, 7, ALU.arith_shift_right, ALU.logical_shift_left)
        cp(s, "dve", padf, padf[:, :], cnti, cnti[:, :])
        s.op("dve", lambda e: e.tensor_tensor_scan(out=pends[:, :], data0=cx.ones_f[:, 0:64], data1=padf[:, :],
                                                   initial=0.0, op0=ALU.mult, op1=ALU.add),
             reads=[cx.ones_f, padf], writes=[pends])
        tt(s, "dve", pstart, pstart[:, :], pends, pends[:, :], padf, padf[:, :], ALU.subtract)
        big = s.sb([128, NT, 64], F32, "big")
        DEST = s.sb([128, NT, 2], F32, "DEST")
        for k, OH in enumerate((OH1, OH2)):
            tt(s, "dve", big, big[:, :, :], OH, OH[:, :, :], pstart, pstart[:, :].unsqueeze(1).to_broadcast([128, NT, 64]), ALU.mult)
            s.op("dve", lambda e, k=k: e.reduce_sum(out=DEST[:, :, k], in_=big[:, :, :], axis=AX.X), reads=[big], writes=[DEST])
        tt(s, "dve", DEST, DEST[:, :, :], DEST, DEST[:, :, :], RK, RK[:, :, :], ALU.add)
        cp(s, "dve", DESTi, DESTi[:, :, :], DEST, DEST[:, :, :])
        fill = s.sb([128, NSLOT], I32, "fill")
        s.op("pool", lambda e: e.iota(fill[:, :], pattern=[[0, NSLOT]], base=S, channel_multiplier=0), writes=[fill])
        st(s, "sp", bt_res, bt.rearrange("(p n) o -> p (n o)", p=128), fill, fill[:, :])
        for i in range(NT):
            for k in range(2):
                s.dma("pool", lambda e, i=i, k=k: e.indirect_dma_start(
                    out=bt, out_offset=bass.IndirectOffsetOnAxis(ap=DESTi[:, i, k:k + 1], axis=0),
                    in_=toki[:, i:i + 1], in_offset=None, bounds_check=breg_t, oob_is_err=False),
                    reads=[DESTi, toki], writes=[bt_res])
        slotoff = s.sb([128, NSLOT], F32, "slotoff")
        sloti = s.sb([128, NSLOT], I32, "sloti")
        s.op("pool", lambda e: e.iota(sloti[:, :], pattern=[[128, NSLOT]], base=0, channel_multiplier=0), writes=[sloti])
        cp(s, "dve", slotoff, slotoff[:, :], sloti, sloti[:, :])
        blk = s.sb([128, NSLOT], F32, "blk")
        cmp_ = s.sb([128, 32, 64], F32, "cmp")
        for q in range(NSLOT // 32):
            tt(s, "dve", cmp_, cmp_[:, :, :], pends, pends[:, :].unsqueeze(1).to_broadcast([128, 32, 64]),
               slotoff, slotoff[:, q * 32:(q + 1) * 32].unsqueeze(2).to_broadcast([128, 32, 64]), ALU.is_le)
            s.op("dve", lambda e, q=q: e.reduce_sum(out=blk[:, q * 32:(q + 1) * 32], in_=cmp_[:, :, :], axis=AX.X),
                 reads=[cmp_], writes=[blk])
        pidx = s.sb([128, 1], F32, "pidx")
        pidi = s.sb([128, 1], I32, "pidi")
        s.op("pool", lambda e: e.iota(pidi[:, :], pattern=[[0, 1]], base=0, channel_multiplier=1), writes=[pidi])
        cp(s, "dve", pidx, pidx[:, :], pidi, pidi[:, :])
        used = s.sb([128, NSLOT], F32, "used")
        ts(s, "dve", used, used[:, :], slotoff, slotoff[:, :], pends[:, 63:64], None, ALU.is_lt, extra_reads=[pends])
        ts(s, "dve", blk, blk[:, :], blk, blk[:, :], 63.0, 128.0, ALU.min, ALU.mult)
        ts(s, "dve", blk, blk[:, :], blk, blk[:, :], pidx[:, 0:1], float(l * 64 * 128 - BIGIDX), ALU.add, ALU.add, extra_reads=[pidx])
        tt(s, "dve", blk, blk[:, :], blk, blk[:, :], used, used[:, :], ALU.mult)
        ts(s, "dve", blk, blk[:, :], blk, blk[:, :], float(BIGIDX), None, ALU.add)
        cp(s, "dve", IDXW, IDXW[:, :], blk, blk[:, :])
        if "dbg" in dr:
            dbg = s.sb([128, 1024], F32, "dbg")
            s.op("dve", lambda e: e.memset(dbg[:, :], 0.0), writes=[dbg])
            cp(s, "dve", dbg, dbg[:, 0:128], IDXW, IDXW[:, :])
            cp(s, "dve", dbg, dbg[:, 128:192], cnt, cnt[:, :])
            cp(s, "dve", dbg, dbg[:, 192:256], pends, pends[:, :])
            cp(s, "dve", dbg, dbg[:, 256:320], DEST, DEST[:, :, :].rearrange("p a b -> p (a b)"))
            cp(s, "dve", dbg, dbg[:, 320:384], GT, GT[:, :, :].rearrange("p a b -> p (a b)"))
            cp(s, "dve", dbg, dbg[:, 384:448], used, used[:, 0:64])
            cp(s, "dve", dbg, dbg[:, 448:512], padf, padf[:, :])
            cp(s, "dve", dbg, dbg[:, 512:576], OH1, OH1[:, 0, :])
            cp(s, "dve", dbg, dbg[:, 576:640], OH2, OH2[:, 0, :])
            cp(s, "dve", dbg, dbg[:, 640:704], RK, RK[:, :, :].rearrange("p a b -> p (a b)"))
            st(s, "sp", wres, dr["dbg"], dbg, dbg[:, :], final=True)
        s.phase_es = old
    s_sub_barrier(s)
    if fstop < 2:
        s.end_phase()
        return

    with ExitStack() as es2:
        old = s.phase_es
        s.phase_es = es2
        wviews = [dr[k].rearrange("l e (p j) n -> (l e p) (j n)", p=128) for k in ("e_w_gate", "e_w_up", "e_w_down")]
        stgs = [[s.sb([128, 2048], F32, f"ws{k}") for k in range(3)] for _ in range(2)]
        wbs = [[s.sb([128, 2048], BF16, f"wb{k}") for k in range(3)] for _ in range(2)]
        idxs = [s.sb([128, 1], I32, "idxb") for _ in range(2)]
        xgs = [s.sb([128, 1024], BF16, "xg") for _ in range(2)]
        for xg in xgs:
            s.op("pool", lambda e, xg=xg: e.memset(xg[:, :], 0.0), writes=[xg])
        xgTs = [s.sb([128, 8, 128], BF16, "xgT") for _ in range(2)]
        sgs = [s.sb([128, 256], F32, "sg") for _ in range(2)]
        aTs = [s.sb([128, 2, 128], BF16, "aT") for _ in range(2)]
        yos = [s.sb([128, 1024], F32, "yo") for _ in range(2)]
        psT = [s.ps([128, 1024], BF16, "psT") for _ in range(2)]
        psG = s.ps([128, 512], F32, "psG")
        psU = s.ps([128, 512], F32, "psU")
        psY = [s.ps([128, 512], F32, "psY") for _ in range(2)]
        nslot = int(os.environ.get("FSLOTS", str(NSLOT)))

        def loads(b):
            ix = idxs[b % 2]
            ld(s, "sp", ix, ix[:, :], bt_res, bt[b * 128:(b + 1) * 128, :])
            xg = xgs[b % 2]
            s.dma("pool", lambda e: e.indirect_dma_start(
                out=xg[:, :], out_offset=None, in_=xbf, in_offset=bass.IndirectOffsetOnAxis(ap=ix[:, 0:1], axis=0),
                bounds_check=breg_x, oob_is_err=False), reads=[ix, xbf_res], writes=[xg])
            for k in range(3):
                stg = stgs[b % 2][k]
                s.dma("pool", lambda e, stg=stg, k=k: e.indirect_dma_start(
                    out=stg[:, :], out_offset=None, in_=wviews[k],
                    in_offset=bass.IndirectOffsetOnAxis(ap=IDXW[:, b:b + 1], axis=0),
                    bounds_check=breg_w, oob_is_err=False), reads=[IDXW, wres], writes=[stg])

        loads(0)
        for b in range(nslot):
            if b + 1 < nslot:
                loads(b + 1)
            xg = xgs[b % 2]
            xgT = xgTs[b % 2]
            wg, wu, wd = wbs[b % 2]
            sg = sgs[b % 2]
            aT = aTs[b % 2]
            yo = yos[b % 2]
            pT = psT[b % 2]
            for k, eng in enumerate(("act", "dve", "act")):
                cp(s, eng, wbs[b % 2][k], wbs[b % 2][k][:, :], stgs[b % 2][k], stgs[b % 2][k][:, :])
            for j in range(8):
                s.op("pe", lambda e, j=j, pT=pT, xg=xg: e.transpose(out=pT[:, j * 128:(j + 1) * 128], in_=xg[:, j:1024:8],
                                                      identity=cx.ident_b[:, :]), reads=[xg, cx.ident_b], writes=[pT])
            cp(s, "dve", xgT, xgT[:, :, :], pT, pT[:, :].rearrange("p (j t) -> p j t", j=8))
            wgv = wg[:, :].rearrange("p (j n) -> p j n", j=8)
            wuv = wu[:, :].rearrange("p (j n) -> p j n", j=8)
            wdv = wd[:, :].rearrange("p (j n) -> p j n", j=2)
            for pS_, wv_ in ((psG, wgv), (psU, wuv)):
                for jh in range(2):
                    for j in range(8):
                        mm(s, pS_, pS_[:, jh * 128:(jh + 1) * 128], wbs[b % 2][0 if pS_ is psG else 1], wv_[:, j, jh:256:2],
                           xgT, xgT[:, j, :], j == 0, j == 7)
            act(s, sg, sg[:, :], psG, psG[:, 0:256], AF.Silu)
            tt(s, "dve", aT, aT[:, :, :].rearrange("p a t -> p (a t)"), sg, sg[:, :], psU, psU[:, 0:256], ALU.mult)
            for half in range(2):
                for jh in range(2):
                    mm(s, psY[half], psY[half][:, :], aT, aT[:, jh, :], wd, wdv[:, jh, half * 512:(half + 1) * 512], jh == 0, jh == 1)
            cp(s, "act", yo, yo[:, 0:512], psY[0], psY[0][:, :])
            cp(s, "dve", yo, yo[:, 512:1024], psY[1], psY[1][:, :])
            st(s, "sp", yb_res, yb[b * 128:(b + 1) * 128, :], yo, yo[:, :])
        s.phase_es = old
    s_sub_barrier(s)
    if fstop < 3:
        s.end_phase()
        return

    with ExitStack() as es3:
        old = s.phase_es
        s.phase_es = es3
        g_bc = s.sb([128, 1024], F32, "gbc")
        b_bc = s.sb([128, 1024], F32, "bbc")
        ld(s, "sp", g_bc, g_bc[:, :], wres, bcast_row(dr["ln2_g"][l], 1024))
        ld(s, "sp", b_bc, b_bc[:, :], wres, bcast_row(dr["ln2_b"][l], 1024))
        y1s = [s.sb([128, 1024], F32, "y1") for _ in range(2)]
        y2s = [s.sb([128, 1024], F32, "y2") for _ in range(2)]
        hfs = [s.sb([128, 1024], F32, "hf3") for _ in range(2)]
        hos = [s.sb([128, 1024], F32, "ho3") for _ in range(2)]
        wk = ln_work(s)
        for i in range(NT):
            y1, y2, hf, ho = y1s[i % 2], y2s[i % 2], hfs[i % 2], hos[i % 2]
            for k, yk in enumerate((y1, y2)):
                s.dma("pool", lambda e, yk=yk, k=k, i=i: e.indirect_dma_start(
                    out=yk[:, :], out_offset=None, in_=yb, in_offset=bass.IndirectOffsetOnAxis(ap=DESTi[:, i, k:k + 1], axis=0),
                    bounds_check=breg_y, oob_is_err=False), reads=[DESTi, yb_res], writes=[yk])
            ld(s, "sp", hf, hf[:, :], h_in_res, h_in_ap[i * 128:(i + 1) * 128, :])
            act(s, y2, y2[:, :], y2, y2[:, :], AF.Copy, scale=GT[:, i, 1:2], extra_reads=[GT])
            stt(s, "dve", y1, y1[:, :], y1, y1[:, :], GT[:, i, 0:1], y2, y2[:, :], ALU.mult, ALU.add, extra_reads=[GT])
            ln_epilogue(s, cx, [(y1, y1[:, 0:512]), (y1, y1[:, 512:1024])], hf, g_bc, b_bc, wk, ho)
            st(s, "pool", h_out_res, h_out_ap[i * 128:(i + 1) * 128, :], ho, ho[:, :], final=final)
        s.phase_es = old
    s.end_phase()


def rope_consts_np():
    rc = np.zeros((128, 4), np.float32)
    inv_a = 500000.0 ** (-np.arange(0, 16, 2, dtype=np.float32) / 16.0)
    for hh in range(2):
        for jx in range(16):
            rc[hh * 64 + jx, 0] = inv_a[jx % 8]
            rc[hh * 64 + jx, 1] = -1.0 if jx < 8 else 1.0
    inv_c = 10000.0 ** (-np.arange(0, 256, 2, dtype=np.float32) / 256.0)
    rc[:, 2] = inv_c
    return rc


IN_SPECS = [
    ("x", [S, D], F32), ("positions", [S], I32),
    ("ln1_g", [4, D], F32), ("ln1_b", [4, D], F32), ("ln2_g", [4, D], F32), ("ln2_b", [4, D], F32),
    ("a_w_in", [2, D, 3072], F32), ("a_w_out", [2, D, D], F32),
    ("b_w_in", [1, D, 3088], F32), ("b_gate_bias", [1, 16], F32), ("b_conv_w", [1, 4, D], F32),
    ("b_conv_b", [1, D], F32), ("b_norm_g", [1, D], F32), ("b_w_out", [1, D, D], F32),
    ("c_w_in", [1, D, 6144], F32), ("c_norm_g", [1, 2 * D], F32), ("c_w_out", [1, 2 * D, D], F32),
    ("r_group_w", [4, D, 8], F32), ("r_group_b", [4, 8], F32), ("r_expert_w", [4, D, 64], F32),
    ("r_expert_b", [4, 64], F32), ("e_w_gate", [4, 64, D, 256], F32), ("e_w_up", [4, 64, D, 256], F32),
    ("e_w_down", [4, 64, 256, D], F32), ("rope_consts", [128, 4], F32),
]


def build(plan, used_inputs=None):
    nc = bass.Bass("TRN2", target_bir_lowering=False)
    dr = {}
    for name, shape, dt in IN_SPECS:
        if used_inputs is not None and name not in used_inputs:
            continue
        dr[name] = nc.dram_tensor(name, shape, dt, kind="ExternalInput").ap()
    out = nc.dram_tensor("out", [S, D], F32, kind="ExternalOutput").ap()
    import os
    if os.environ.get("KDBG"):
        dr["dbg"] = nc.dram_tensor("dbg", [128, 1024], F32, kind="ExternalOutput").ap()
    dr["wres"] = Res("weights")
    dr["qkT"] = (Res("qkT"), nc.dram_tensor("qkT_s", [2, D, S], BF16).ap())
    dr["vsc"] = (Res("vsc"), nc.dram_tensor("vsc_s", [2, S, 128], BF16).ap())
    dr["attT"] = (Res("attT"), nc.dram_tensor("attT_s", [2 * D, S], BF16).ap())
    dr["hn"] = (Res("hn"), nc.dram_tensor("hn_s", [S, 2 * D], F32).ap())
    dr["xbf"] = (Res("xbf"), nc.dram_tensor("xbf_s", [S, D], BF16).ap())
    dr["buftok"] = (Res("buftok"), nc.dram_tensor("buftok_s", [NSLOT * 128, 1], I32).ap())
    dr["yb"] = (Res("yb"), nc.dram_tensor("yb_s", [NSLOT * 128, D], F32).ap())
    hbuf = [(Res("hA"), nc.dram_tensor("hA_s", [S, D], F32).ap(), False),
            (Res("hB"), nc.dram_tensor("hB_s", [S, D], F32).ap(), False)]
    with ExitStack() as es:
        s = Sched(nc, es)
        cx = Ctx()
        make_consts(s, cx)
        cur = (dr["wres"], dr["x"], False)
        for pi, (ph, l) in enumerate(plan):
            last = pi == len(plan) - 1
            nxt = (Res("out"), out, True) if last else hbuf[pi % 2]
            if ph == "A":
                phase_A(s, cx, dr, l, l // 3, cur, nxt)
            elif ph == "B":
                phase_B(s, cx, dr, l, l // 3, cur, nxt)
            elif ph == "C":
                phase_C(s, cx, dr, l, l // 3, cur, nxt)
            elif ph == "F":
                phase_F(s, cx, dr, l, cur, nxt)
            else:
                raise ValueError(ph)
            cur = nxt
        s.finish()
        print("instructions:", s.ninst, "sems:", s.n_sem)
    return nc


PLAN = [("A", 0), ("F", 0), ("B", 1), ("F", 1), ("C", 2), ("F", 2), ("A", 3), ("F", 3)]
_NC_CACHE = {}


def kernel(**inputs):
    x = np.ascontiguousarray(np.asarray(inputs["x"], dtype=np.float32))
    nb = x.shape[0]
    if "nc" not in _NC_CACHE:
        _NC_CACHE["nc"] = build(PLAN)
    nc = _NC_CACHE["nc"]
    shared = {}
    for name, shape, dt in IN_SPECS:
        if name == "x":
            continue
        if name == "rope_consts":
            shared[name] = rope_consts_np()
        elif name == "positions":
            shared[name] = np.ascontiguousarray(np.asarray(inputs[name]).astype(np.int32))
        else:
            shared[name] = np.ascontiguousarray(np.asarray(inputs[name], dtype=np.float32))
    in_maps = []
    for b in range(nb):
        m = dict(shared)
        m["x"] = x[b]
        in_maps.append(m)
    res = run_bass_kernel_spmd(nc, in_maps, core_ids=list(range(nb)))
    return np.stack([np.asarray(r["out"], dtype=np.float32) for r in res.results], axis=0)
```

```python
import math
import numpy as np
import concourse.bass as bass
import concourse.mybir as mybir
from concourse.bass_utils import run_bass_kernel_spmd
from contextlib import ExitStack

F32 = mybir.dt.float32
BF16 = mybir.dt.bfloat16
I32 = mybir.dt.int32
ALU = mybir.AluOpType
AF = mybir.ActivationFunctionType
AX = mybir.AxisListType

S = 4096
D = 1024
DEPTH = 4
NT = S // 128
ALPHA = (2.0 * DEPTH) ** 0.25
LN_EPS = 1e-5
TWO_PI = 2.0 * math.pi

SEM_WRAP = 30000
ENGS = ("pe", "act", "dve", "pool", "sp")


class Res:
    __slots__ = ("w", "r", "name", "excl")

    def __init__(self, name="", excl=False):
        self.w = None
        self.r = []
        self.name = name
        self.excl = excl


class T:
    __slots__ = ("t", "res")

    def __init__(self, t, res):
        self.t = t
        self.res = res

    def __getitem__(self, k):
        return self.t[k]


class Sched:
    def __init__(self, nc, es):
        self.nc = nc
        self.es = es
        self.sems = {}
        self.cur = {}
        self.waited = {e: {} for e in ENGS}
        self.pending = {e: {} for e in ENGS}
        self.dma_pool = {}
        self.dma_rr = {}
        self.n_sem = 0
        self.n_tiles = 0
        self.ninst = 0
        self.finals = []
        self.eobj = {"pe": nc.tensor, "act": nc.scalar, "dve": nc.vector, "pool": nc.gpsimd, "sp": nc.sync}
        self.phase_es = None
        self.recs = []
        self.ev_of = {}
        self.next_id = 0
        import os
        self.window = int(os.environ.get("KWIN", "48"))

    def _newsem(self, name):
        h = self.es.enter_context(self.nc.semaphore(f"{name}_{self.n_sem}"))
        self.n_sem += 1
        key = self.n_sem
        self.sems[key] = h
        return key

    def sb(self, shape, dtype, name=None):
        self.n_tiles += 1
        name = name or "t"
        es = self.phase_es or self.es
        t = es.enter_context(self.nc.sbuf_tensor(f"{name}_{self.n_tiles}", list(shape), dtype))
        return T(t, Res(name))

    def ps(self, shape, dtype, name=None):
        self.n_tiles += 1
        name = name or "p"
        es = self.phase_es or self.es
        t = es.enter_context(self.nc.psum_tensor(f"{name}_{self.n_tiles}", list(shape), dtype))
        return T(t, Res(name, excl=True))

    def begin_phase(self):
        self.phase_es = ExitStack()
        return self.phase_es

    def end_phase(self):
        self.flush()
        snap = {}
        for e, c in self.cur.items():
            snap[c[0]] = c[1]
        for q, slots in self.dma_pool.items():
            for sl in slots:
                if sl[1] > 0:
                    snap[sl[0]] = sl[1]
        for e in ENGS:
            p = self.pending[e]
            for k, v in snap.items():
                if p.get(k, 0) < v:
                    p[k] = v
        self.phase_es.close()
        self.phase_es = None

    def _deps(self, reads, writes):
        deps = set()
        for r in reads:
            r = r.res if isinstance(r, T) else r
            if r.w is not None:
                deps.add(r.w)
            if r.excl:
                deps.update(r.r)
        for w in writes:
            w = w.res if isinstance(w, T) else w
            if w.w is not None:
                deps.add(w.w)
            deps.update(w.r)
        return deps

    def _commit(self, oid, reads, writes):
        for r in reads:
            r = r.res if isinstance(r, T) else r
            r.r.append(oid)
        for w in writes:
            w = w.res if isinstance(w, T) else w
            w.w = oid
            w.r = []

    def op(self, eng, fn, reads=(), writes=(), cost=150.0):
        oid = self.next_id
        self.next_id += 1
        deps = self._deps(reads, writes)
        self._commit(oid, reads, writes)
        self.recs.append((oid, eng, False, fn, deps, float(cost), 0.0, False))

    def dma(self, q, fn, reads=(), writes=(), final=False, nbytes=65536, issue=None):
        oid = self.next_id
        self.next_id += 1
        deps = self._deps(reads, writes)
        self._commit(oid, reads, writes)
        if issue is None:
            issue = 1200.0 if q == "pool" else 80.0
        self.recs.append((oid, q, True, fn, deps, float(issue), 2000.0 + nbytes / 150.0, final))

    def flush(self):
        recs = self.recs
        self.recs = []
        if not recs:
            return
        fin = {}
        queues = {e: [] for e in ENGS}
        for r in recs:
            queues[r[1]].append(r)
        heads = {e: 0 for e in ENGS}
        placed = set()
        mine = {r[0] for r in recs}
        efree = {e: 0.0 for e in ENGS}
        dma_free = [0.0]
        order = []
        nleft = len(recs)
        W = self.window
        taken = {e: set() for e in ENGS}
        while nleft:
            best = None
            for e in ENGS:
                q = queues[e]
                h = heads[e]
                n = len(q)
                while h < n and q[h][0] in taken[e]:
                    h += 1
                heads[e] = h
                lim = min(n, h + W)
                for k in range(h, lim):
                    r = q[k]
                    if r[0] in taken[e]:
                        continue
                    ok = True
                    st_ = efree[e]
                    for d in r[4]:
                        if d in mine:
                            if d not in placed:
                                ok = False
                                break
                            if fin[d] > st_:
                                st_ = fin[d]
                    if not ok:
                        continue
                    key = (st_, r[0])
                    if best is None or key < best[0]:
                        best = (key, e, r)
                    if st_ <= efree[e]:
                        break
            assert best is not None, "scheduler deadlock"
            (st_, _), e, r = best
            taken[e].add(r[0])
            placed.add(r[0])
            nleft -= 1
            if r[2]:
                iend = st_ + r[5]
                efree[e] = iend
                t0 = max(iend, dma_free[0])
                dma_free[0] = t0 + max(0.0, r[6] - 2000.0)
                fin[r[0]] = dma_free[0] + 2000.0
            else:
                efree[e] = st_ + r[5]
                fin[r[0]] = efree[e] + 60.0
            order.append(r)
        for r in order:
            self._emit_rec(r)

    def _engine_event(self, eng):
        if eng not in self.cur or self.cur[eng][1] >= SEM_WRAP:
            self.cur[eng] = [self._newsem(eng), 0]
        c = self.cur[eng]
        c[1] += 1
        return (c[0], c[1])

    def _dma_event(self, q):
        if q not in self.dma_pool:
            self.dma_pool[q] = [[self._newsem(f"dma{q}"), 0] for _ in range(12)]
            self.dma_rr[q] = 0
        i = self.dma_rr[q]
        self.dma_rr[q] = (i + 1) % len(self.dma_pool[q])
        slot = self.dma_pool[q][i]
        extra = (slot[0], slot[1]) if slot[1] > 0 else None
        if slot[1] + 16 > SEM_WRAP:
            slot[0] = self._newsem(f"dma{q}")
            slot[1] = 0
        slot[1] += 16
        return (slot[0], slot[1]), extra

    def _emit_rec(self, r):
        oid, eng, is_dma, fn, deps, _, _, final = r
        need = self.pending[eng]
        self.pending[eng] = {}
        for d in deps:
            k, v = self.ev_of[d]
            if need.get(k, 0) < v:
                need[k] = v
        if is_dma:
            ev, extra = self._dma_event(eng)
            if extra is not None:
                k, v = extra
                if need.get(k, 0) < v:
                    need[k] = v
            inc = 16
        else:
            ev = self._engine_event(eng)
            inc = 1
        wd = self.waited[eng]
        own = self.cur.get(eng, [None])[0] if eng == "pe" else None
        e = self.eobj[eng]
        for k, v in need.items():
            if k == own:
                continue
            if wd.get(k, 0) < v:
                wd[k] = v
                e.wait_ge(self.sems[k], v)
        ins = fn(e)
        ins.then_inc(self.sems[ev[0]], inc)
        self.ev_of[oid] = ev
        self.ninst += 1
        if final:
            self.finals.append(ev)

    def finish(self):
        self.flush()
        deps = {}
        for k, v in self.finals:
            if deps.get(k, 0) < v:
                deps[k] = v
        for q, slots in self.dma_pool.items():
            for sl in slots:
                if sl[1] > 0 and deps.get(sl[0], 0) < sl[1]:
                    deps[sl[0]] = sl[1]
        for k, v in deps.items():
            self.nc.sync.wait_ge(self.sems[k], v)


def _fsz(ap):
    n = 1
    for d in ap.shape[1:]:
        n *= d
    return n


def mm(s, out_t, out_ap, lhsT_t, lhsT_ap, rhs_t, rhs_ap, start, stop):
    n = _fsz(out_ap)
    c = 30.0 + max(n, 64) * 0.45
    if lhsT_ap.dtype == F32:
        c *= 4
    s.op("pe", lambda e: e.matmul(out_ap, lhsT=lhsT_ap, rhs=rhs_ap, start=start, stop=stop),
         reads=[lhsT_t, rhs_t], writes=[out_t], cost=c)


def _ecost(eng, n):
    if eng == "dve":
        return 70.0 + 0.85 * n
    if eng == "act":
        return 200.0 + 0.65 * n
    return 160.0 + 1.6 * n


def tt(s, eng, out_t, out_ap, a_t, a_ap, b_t, b_ap, op):
    s.op(eng, lambda e: e.tensor_tensor(out=out_ap, in0=a_ap, in1=b_ap, op=op), reads=[a_t, b_t], writes=[out_t],
         cost=_ecost(eng, _fsz(out_ap)))


def ts(s, eng, out_t, out_ap, a_t, a_ap, s1, s2, op0, op1=None, extra_reads=()):
    c = _ecost(eng, _fsz(out_ap))
    if op1 is None:
        s.op(eng, lambda e: e.tensor_scalar(out=out_ap, in0=a_ap, scalar1=s1, scalar2=None, op0=op0),
             reads=[a_t, *extra_reads], writes=[out_t], cost=c)
    else:
        s.op(eng, lambda e: e.tensor_scalar(out=out_ap, in0=a_ap, scalar1=s1, scalar2=s2, op0=op0, op1=op1),
             reads=[a_t, *extra_reads], writes=[out_t], cost=c)


def stt(s, eng, out_t, out_ap, a_t, a_ap, scalar, b_t, b_ap, op0, op1, extra_reads=()):
    s.op(eng, lambda e: e.scalar_tensor_tensor(out=out_ap, in0=a_ap, scalar=scalar, in1=b_ap, op0=op0, op1=op1),
         reads=[a_t, b_t, *extra_reads], writes=[out_t], cost=_ecost(eng, _fsz(out_ap)))


def cp(s, eng, out_t, out_ap, a_t, a_ap):
    n = _fsz(out_ap)
    if eng == "act":
        s.op("act", lambda e: e.activation(out=out_ap, in_=a_ap, func=AF.Copy), reads=[a_t], writes=[out_t], cost=_ecost("act", n))
    else:
        c = _ecost(eng, n) if eng == "dve" else 160.0 + 3.0 * n
        s.op(eng, lambda e: e.tensor_copy(out=out_ap, in_=a_ap), reads=[a_t], writes=[out_t], cost=c)


def act(s, out_t, out_ap, a_t, a_ap, func, scale=1.0, bias=0.0, extra_reads=()):
    s.op("act", lambda e: e.activation(out=out_ap, in_=a_ap, func=func, bias=bias, scale=scale),
         reads=[a_t, *extra_reads], writes=[out_t], cost=_ecost("act", _fsz(out_ap)))


def _nbytes(ap):
    n = 1
    for d in ap.shape:
        n *= d
    return n * (2 if ap.dtype == BF16 else 4)


def ld(s, q, out_t, out_ap, src_res, src_ap):
    s.dma(q, lambda e: e.dma_start(out=out_ap, in_=src_ap), reads=[src_res], writes=[out_t], nbytes=_nbytes(out_ap))


def st(s, q, dst_res, dst_ap, in_t, in_ap, final=False):
    s.dma(q, lambda e: e.dma_start(out=dst_ap, in_=in_ap), reads=[in_t], writes=[dst_res], final=final, nbytes=_nbytes(in_ap))


class Ctx:
    pass


def wload(s, cx, dst_t, dst_ap, wres, src_ap, shape):
    stg = cx.stg[cx.stg_i % 2]
    cx.stg_i += 1
    n = int(np.prod(shape))
    if len(shape) == 1:
        v = stg[:, 0:n]
    else:
        v = stg[:, 0:n].rearrange("p (a b) -> p a b", a=shape[0])
    ld(s, "sp", stg, v, wres, src_ap)
    cp(s, "dve" if cx.stg_i % 2 else "act", dst_t, dst_ap, stg, v)


def make_consts(s, cx):
    nc = s.nc
    iot = s.sb([128, 128], I32, "iot")
    iof = s.sb([128, 128], F32, "iof")
    cx.iof = iof
    cx.ident_f = s.sb([128, 128], F32, "identf")
    cx.ident_b = s.sb([128, 128], BF16, "identb")
    cx.maskA = s.sb([128, 2, 2, 128], BF16, "maskA")
    cx.perm = s.sb([128, 128], BF16, "perm")
    cx.ones_b = s.sb([128, 128], BF16, "onesb")
    cx.ones_f = s.sb([128, 128], F32, "onesf")
    cx.tri_le = s.sb([128, 128], F32, "trile")
    cx.tri_b = s.sb([128, 128], BF16, "trib")
    cx.swlo = s.sb([128, 128], F32, "swlo")
    cx.swhi = s.sb([128, 128], F32, "swhi")
    cx.stg = [s.sb([128, 2048], F32, "stg") for _ in range(2)]
    cx.stg_i = 0
    s.op("pool", lambda e: e.iota(iot[:, :], pattern=[[1, 128]], base=0, channel_multiplier=-1), writes=[iot])
    cp(s, "dve", iof, iof[:, :], iot, iot[:, :])
    ts(s, "dve", cx.ident_f, cx.ident_f[:, :], iof, iof[:, :], 0.0, None, ALU.is_equal)
    cp(s, "dve", cx.ident_b, cx.ident_b[:, :], cx.ident_f, cx.ident_f[:, :])
    ts(s, "dve", cx.tri_le, cx.tri_le[:, :], iof, iof[:, :], 0.0, None, ALU.is_ge)
    cp(s, "dve", cx.tri_b, cx.tri_b[:, :], cx.tri_le, cx.tri_le[:, :])
    ts(s, "dve", cx.swlo, cx.swlo[:, :], iof, iof[:, :], 64.0, None, ALU.is_equal)
    ts(s, "dve", cx.swhi, cx.swhi[:, :], iof, iof[:, :], -64.0, None, ALU.is_equal)
    for hh in range(2):
        ts(s, "dve", cx.maskA, cx.maskA[:, hh, 0, :], iof, iof[:, :], 0.0, None, ALU.is_ge)
        ts(s, "dve", cx.maskA, cx.maskA[:, hh, 1, :], iof, iof[:, :], 0.0, None, ALU.is_le)
    s.op("pool", lambda e: e.memset(cx.perm[:, :], 0.0), writes=[cx.perm])
    s.op("pool", lambda e: e.memset(cx.ones_b[:, :], 1.0), writes=[cx.ones_b])
    s.op("pool", lambda e: e.memset(cx.ones_f[:, :], 1.0), writes=[cx.ones_f])
    for hh in range(2):
        c0 = hh * 64
        ts(s, "dve", cx.perm, cx.perm[:, c0:c0 + 8], iof, iof[:, c0:c0 + 8], -8.0, None, ALU.is_equal)
        ts(s, "dve", cx.perm, cx.perm[:, c0 + 8:c0 + 16], iof, iof[:, c0 + 8:c0 + 16], 8.0, None, ALU.is_equal)


def sin_table(s, out_t, out_ap, x_t, x_ap, shape, sign_ap, sign_t, scratch):
    ki, kf, r = scratch
    C1 = 6.28125
    C2 = TWO_PI - C1
    ts(s, "dve", ki, ki[:, :], x_t, x_ap, 1.0 / TWO_PI, None, ALU.mult)
    cp(s, "dve", kf, kf[:, :], ki, ki[:, :])
    stt(s, "dve", r, r[:, :], kf, kf[:, :], -C1, x_t, x_ap, ALU.mult, ALU.add)
    stt(s, "dve", r, r[:, :], kf, kf[:, :], -C2, r, r[:, :], ALU.mult, ALU.add)
    ts(s, "dve", kf, kf[:, :], r, r[:, :], math.pi, None, ALU.is_gt)
    stt(s, "dve", r, r[:, :], kf, kf[:, :], -TWO_PI, r, r[:, :], ALU.mult, ALU.add)
    ts(s, "dve", kf, kf[:, :], r, r[:, :], -math.pi, None, ALU.is_lt)
    stt(s, "dve", r, r[:, :], kf, kf[:, :], TWO_PI, r, r[:, :], ALU.mult, ALU.add)
    ts(s, "dve", r, r[:, :], r, r[:, :], 3.14159, -3.14159, ALU.min, ALU.max)
    act(s, out_t, out_ap, r, r[:, :], AF.Sin)
    if sign_ap is not None:
        ts(s, "dve", out_t, out_ap, out_t, out_ap, sign_ap, None, ALU.mult, extra_reads=[sign_t])


def load_xT(s, cx, h_res, h_ap, xT):
    old = s.phase_es
    s.phase_es = ExitStack()
    hfs = [s.sb([128, 1024], F32, "hf") for _ in range(3)]
    pts = [s.ps([128, 512], F32, "ptr") for _ in range(2)]
    for i in range(NT):
        hf = hfs[i % 3]
        ld(s, "sp", hf, hf[:, :], h_res, h_ap[i * 128:(i + 1) * 128, :])
        for half in range(2):
            pt = pts[half]
            for c4 in range(4):
                c = half * 4 + c4
                s.op("pe", lambda e, pt=pt, c4=c4, c=c, hf=hf: e.transpose(
                    out=pt[:, c4 * 128:(c4 + 1) * 128], in_=hf[:, c * 128:(c + 1) * 128], identity=cx.ident_f[:, :]),
                    reads=[hf, cx.ident_f], writes=[pt])
            dst = xT[:, half * 4:(half + 1) * 4, i * 128:(i + 1) * 128]
            src = pt[:, :].rearrange("p (c t) -> p c t", c=4)
            cp(s, "act" if half else "dve", xT, dst, pt, src)
    s.end_phase()
    s.phase_es = old


def ln_epilogue(s, cx, y_ps, hf, g_bc, b_bc, wk, out_t):
    r, stats, mv, sd, rstd, nmr, xn = wk
    for half in range(2):
        sl = slice(half * 512, (half + 1) * 512)
        yt, yap = y_ps[half] if isinstance(y_ps[half], tuple) else (y_ps[half], y_ps[half][:, :])
        stt(s, "dve", r, r[:, sl], hf, hf[:, sl], ALPHA, yt, yap, ALU.mult, ALU.add)
        s.op("dve", lambda e, half=half, sl=sl: e.bn_stats(out=stats[:, half, :], in_=r[:, sl]), reads=[r], writes=[stats])
    s.op("dve", lambda e: e.bn_aggr(out=mv[:, :], in_=stats[:, :, :].rearrange("p a b -> p (a b)")), reads=[stats], writes=[mv])
    ts(s, "dve", sd, sd[:, :], mv, mv[:, 1:2], LN_EPS, None, ALU.add)
    act(s, sd, sd[:, :], sd, sd[:, :], AF.Sqrt)
    s.op("dve", lambda e: e.reciprocal(out=rstd[:, :], in_=sd[:, :]), reads=[sd], writes=[rstd])
    stt(s, "dve", nmr, nmr[:, :], mv, mv[:, 0:1], -1.0, rstd, rstd[:, :], ALU.mult, ALU.mult)
    act(s, xn, xn[:, :], r, r[:, :], AF.Identity, scale=rstd[:, 0:1], bias=nmr[:, 0:1], extra_reads=[rstd, nmr])
    tt(s, "pool", xn, xn[:, :], xn, xn[:, :], g_bc, g_bc[:, :], ALU.mult)
    tt(s, "pool", out_t, out_t[:, :], xn, xn[:, :], b_bc, b_bc[:, :], ALU.add)


def ln_work(s):
    return (s.sb([128, 1024], F32, "lnr"), s.sb([128, 2, 6], F32, "lnst"), s.sb([128, 2], F32, "lnmv"),
            s.sb([128, 1], F32, "lnsd"), s.sb([128, 1], F32, "lnrs"), s.sb([128, 1], F32, "lnnm"),
            s.sb([128, 1024], F32, "lnxn"))


def bcast_row(ap_row, n):
    return ap_row.partition_broadcast(128)


def phase_A(s, cx, dr, l, j, h_in, h_out):
    nc = s.nc
    h_in_res, h_in_ap = h_in[0], h_in[1]
    w_in = dr["a_w_in"][j]
    w_out = dr["a_w_out"][j]
    w_in_v = w_in.rearrange("(c p) n -> p c n", p=128)
    qkT_res, qkT = dr["qkT"]
    attT_res, attT = dr["attT"]
    wres = dr["wres"]

    s.begin_phase()
    xT = s.sb([128, 8, S], BF16, "xT")
    load_xT(s, cx, h_in_res, h_in_ap, xT)

    import os
    stop = int(os.environ.get("KSTOP", "9"))
    if stop < 1:
        s.end_phase()
        return
    with ExitStack() as es1:
        old = s.phase_es
        s.phase_es = es1
        wqk = s.sb([128, 8, 2048], BF16, "wqk")
        for c in range(8):
            wload(s, cx, wqk, wqk[:, c, :], wres, w_in[c * 128:(c + 1) * 128, 0:2048], [2048])
        rc = s.sb([128, 4], F32, "ropec")
        ld(s, "sp", rc, rc[:, :], wres, dr["rope_consts"])
        posi = s.sb([128, 512], I32, "posi")
        posf = s.sb([128, 512], F32, "posf")
        ang = s.sb([128, 512], F32, "ang")
        ang2 = s.sb([128, 512], F32, "ang2")
        Ct = s.sb([128, 512], F32, "Ct")
        St = s.sb([128, 512], F32, "St")
        scr = (s.sb([128, 512], I32, "ki"), s.sb([128, 512], F32, "kf"), s.sb([128, 512], F32, "rr"))
        psq = [s.ps([128, 512], F32, "psq") for _ in range(2)]
        psp = [s.ps([128, 512], F32, "psp") for _ in range(2)]
        qbs = [s.sb([128, 512], BF16, "qb") for _ in range(2)]
        t1s = [s.sb([128, 512], F32, "t1") for _ in range(2)]
        t2s = [s.sb([128, 512], F32, "t2") for _ in range(2)]
        obs = [s.sb([128, 512], BF16, "ob") for _ in range(3)]
        it = 0
        ksub = int(os.environ.get("KSUB", "99"))
        for tg in range(int(os.environ.get('KTG', '8')) if ksub > 0 else 0):
            tsl = slice(tg * 512, (tg + 1) * 512)
            ld(s, "sp", posi, posi[:, :], wres, bcast_row(dr["positions"][tg * 512:(tg + 1) * 512], 512))
            cp(s, "dve", posf, posf[:, :], posi, posi[:, :])
            ts(s, "dve", ang, ang[:, :], posf, posf[:, :], rc[:, 0:1], None, ALU.mult, extra_reads=[rc])
            ts(s, "dve", ang2, ang2[:, :], ang, ang[:, :], math.pi / 2, None, ALU.add)
            sin_table(s, St, St[:, :], ang, ang[:, :], None, rc[:, 1:2], rc, scr)
            sin_table(s, Ct, Ct[:, :], ang2, ang2[:, :], None, None, None, scr)
            for hp in range(int(os.environ.get('KHP', '8')) if ksub > 1 else 0):
                for w in range(2):
                    pq = psq[it % 2]
                    pp = psp[it % 2]
                    qb = qbs[it % 2]
                    t1 = t1s[it % 2]
                    t2 = t2s[it % 2]
                    ob = obs[it % 3]
                    it += 1
                    col = w * 1024 + hp * 128
                    for c in range(8):
                        mm(s, pq, pq[:, :], wqk, wqk[:, c, col:col + 128], xT, xT[:, c, tsl], c == 0, c == 7)
                    cp(s, "act", qb, qb[:, :], pq, pq[:, :])
                    if ksub < 3:
                        continue
                    mm(s, pp, pp[:, :], cx.perm, cx.perm[:, :], qb, qb[:, :], True, True)
                    tt(s, "dve", t1, t1[:, :], pq, pq[:, :], Ct, Ct[:, :], ALU.mult)
                    tt(s, "dve", t2, t2[:, :], pp, pp[:, :], St, St[:, :], ALU.mult)
                    tt(s, "pool", ob, ob[:, :], t1, t1[:, :], t2, t2[:, :], ALU.add)
                    if ksub < 4:
                        continue
                    st(s, "pool", qkT_res, qkT[w, hp * 128:(hp + 1) * 128, tsl], ob, ob[:, :])
        s.phase_es = old
    s_sub_barrier(s)
    if stop < 2:
        s.end_phase()
        return

    vsc_res, vsc = dr["vsc"]
    with ExitStack() as es2:
        old = s.phase_es
        s.phase_es = es2
        QT = s.sb([128, S], BF16, "QT")
        KT = s.sb([128, S], BF16, "KT")
        wv = s.sb([128, 8, 128], BF16, "wv")
        Vn = s.sb([128, 32, 128], BF16, "Vn")
        Vaugs = [s.sb([128, 32, 2, 128], BF16, "Vaug") for _ in range(2)]
        for Va in Vaugs:
            s.op("pool", lambda e, Va=Va: e.memset(Va[:, :, 0, 64:128], 1.0), writes=[Va], cost=3000)
            s.op("pool", lambda e, Va=Va: e.memset(Va[:, :, 1, 0:64], 1.0), writes=[Va], cost=3000)
        acc = s.sb([128, 2, S], F32, "acc")
        obuf = s.sb([128, S], BF16, "obuf")
        recs = [s.sb([128, 512], F32, "rec") for _ in range(2)]
        psV = [s.ps([128, 512], F32, "psV") for _ in range(2)]
        psS = [s.ps([128, 512], F32, "psS") for _ in range(4)]
        psO_ = [s.ps([128, 512], F32, "psO") for _ in range(2)]
        psO = [T(p[:, 0:256].rearrange("p (a q) -> p a q", a=2), p.res) for p in psO_]
        pes = [s.sb([128, 512], BF16, "pe") for _ in range(4)]
        PTs = [s.sb([128, 2, 2, 128], BF16, "PT") for _ in range(4)]
        vi = 0
        bi_ctr = 0
        k2 = int(os.environ.get("KSUB2", "99"))
        for hp in range(int(os.environ.get("KHP2", "8"))):
            ld(s, "sp", QT, QT[:, :], qkT_res, qkT[0, hp * 128:(hp + 1) * 128, :])
            ld(s, "sp", KT, KT[:, :], qkT_res, qkT[1, hp * 128:(hp + 1) * 128, :])
            wload(s, cx, wv, wv[:, :, :], wres, w_in_v[:, :, 2048 + hp * 128:2048 + (hp + 1) * 128], [8, 128])
            for b in range(32):
                pv = psV[(b // 4) % 2]
                for c in range(8):
                    mm(s, pv, pv[:, (b % 4) * 128:(b % 4 + 1) * 128], xT, xT[:, c, b * 128:(b + 1) * 128], wv, wv[:, c, :], c == 0, c == 7)
                if b % 4 == 3:
                    cp(s, "act" if (b // 4) % 2 else "dve", Vn, Vn[:, b - 3:b + 1, :], pv,
                       pv[:, :].rearrange("p (b f) -> p b f", b=4))
            vs = vsc[hp % 2]
            st(s, "sp", vsc_res, vs.rearrange("(i p) c -> p i c", p=128), Vn, Vn[:, :, :])
            for di, d in enumerate((1, 4, 16)[:int(os.environ.get("KBR", "3"))] if k2 > 0 else ()):
                nb = 32 // d
                Vaug = Vaugs[vi % 2]
                vi += 1

                def tok(r, n, d=d):
                    return slice(r + d * 128 * n, r + d * 128 * n + 127 * d + 1, d)
                if d > 1:
                    view = vs.rearrange("(n p r) c -> p r n c", p=128, r=d)
                    for r in range(d):
                        s.dma("sp" if r % 2 else "act", lambda e, r=r, nb=nb, view=view: e.dma_start(
                            out=Vn[:, r * nb:(r + 1) * nb, :], in_=view[:, r, :, :]),
                            reads=[vsc_res], writes=[Vn], nbytes=4 * nb * 32768)
                cp(s, "act", Vaug, Vaug[:, :, 0, 0:64], Vn, Vn[:, :, 0:64])
                cp(s, "act", Vaug, Vaug[:, :, 1, 64:128], Vn, Vn[:, :, 64:128])
                for b2 in range(int(os.environ.get("KB", "16")) if k2 > 1 else 0):
                    blocks = (2 * b2, 2 * b2 + 1)
                    n0 = blocks[0] % nb
                    PTh = []
                    for hh in range(2):
                        hs = slice(hh * 64, (hh + 1) * 64)
                        pS = psS[(2 * bi_ctr + hh) % 4]
                        pe_ = pes[(2 * bi_ctr + hh) % 4]
                        PT = PTs[(2 * bi_ctr + hh) % 4]
                        PTh.append(PT)
                        pS4 = pS[:, :].rearrange("p (b c q) -> p b c q", b=2, c=2)
                        pe4 = pe_[:, :].rearrange("p (b c q) -> p b c q", b=2, c=2)
                        for bi, b in enumerate(blocks):
                            r, n = divmod(b, nb)
                            mm(s, pS, pS4[:, bi, 0, :], KT, KT[hs, tok(r, n)], QT, QT[hs, tok(r, n)], True, True)
                            if n > 0:
                                mm(s, pS, pS4[:, bi, 1, :], KT, KT[hs, tok(r, n - 1)], QT, QT[hs, tok(r, n)], True, True)
                        if n0 > 0:
                            act(s, pe_, pe_[:, :], pS, pS[:, :], AF.Exp, scale=0.125)
                            tt(s, "pool", PT, PT[:, :, :, :], pe_, pe4, cx.maskA, cx.maskA[:, :, :, :], ALU.mult)
                        else:
                            act(s, pe_, pe4[:, 0, 0, :], pS, pS4[:, 0, 0, :], AF.Exp, scale=0.125)
                            act(s, pe_, pe4[:, 1, :, :], pS, pS4[:, 1, :, :], AF.Exp, scale=0.125)
                            tt(s, "pool", PT, PT[:, 0, 0, :], pe_, pe4[:, 0, 0, :], cx.maskA, cx.maskA[:, 0, 0, :], ALU.mult)
                            tt(s, "pool", PT, PT[:, 1, :, :], pe_, pe4[:, 1, :, :], cx.maskA, cx.maskA[:, 1, :, :], ALU.mult)
                    bi_ctr += 1
                    if k2 < 3:
                        continue
                    for bi, b in enumerate(blocks):
                        r, n = divmod(b, nb)
                        pO = psO[b % 2]
                        for hh in range(2):
                            PT = PTh[hh]
                            mm(s, pO, pO[:, hh, :], Vaug, Vaug[:, b, hh, :], PT, PT[:, bi, 0, :], True, n == 0)
                            if n > 0:
                                mm(s, pO, pO[:, hh, :], Vaug, Vaug[:, b - 1, hh, :], PT, PT[:, bi, 1, :], False, True)
                        if k2 < 4:
                            continue
                        if di == 0:
                            cp(s, "dve", acc, acc[:, :, tok(r, n)], pO, pO[:, :, :])
                        else:
                            tt(s, "dve", acc, acc[:, :, tok(r, n)], acc, acc[:, :, tok(r, n)], pO, pO[:, :, :], ALU.add)
            if k2 < 5:
                continue
            for tg in range(8):
                tsl = slice(tg * 512, (tg + 1) * 512)
                pd = psV[tg % 2]
                rec = recs[tg % 2]
                mm(s, pd, pd[:, :], cx.swlo, cx.swlo[:, :], acc, acc[:, 1, tsl], True, False)
                mm(s, pd, pd[:, :], cx.swhi, cx.swhi[:, :], acc, acc[:, 0, tsl], False, True)
                s.op("dve", lambda e, rec=rec, pd=pd: e.reciprocal(out=rec[:, :], in_=pd[:, :]), reads=[pd], writes=[rec], cost=500)
                tt(s, "pool", obuf, obuf[0:64, tsl], acc, acc[0:64, 0, tsl], rec, rec[0:64, :], ALU.mult)
                tt(s, "pool", obuf, obuf[64:128, tsl], acc, acc[64:128, 1, tsl], rec, rec[64:128, :], ALU.mult)
            st(s, "sp", attT_res, attT[hp * 128:(hp + 1) * 128, :], obuf, obuf[:, :])
        s.phase_es = old
    s.end_phase()

    if stop < 3:
        return
    s.begin_phase()
    proj_ln(s, cx, dr, attT_res, attT, 8, w_out, h_in, h_out, dr["ln1_g"][l], dr["ln1_b"][l])
    s.end_phase()


def s_sub_barrier(s):
    es = s.phase_es
    s.phase_es = ExitStack()
    s.end_phase()
    s.phase_es = es


def proj_ln(s, cx, dr, aT_res, aT, nchunk, w_out, h_in, h_out, g_row, b_row):
    h_in_res, h_in_ap = h_in[0], h_in[1]
    h_out_res, h_out_ap, final = h_out
    wres = dr["wres"]
    wo = s.sb([128, nchunk, 1024], BF16, "wo")
    for c in range(nchunk):
        wload(s, cx, wo, wo[:, c, :], wres, w_out[c * 128:(c + 1) * 128, :], [1024])
    g_bc = s.sb([128, 1024], F32, "gbc")
    b_bc = s.sb([128, 1024], F32, "bbc")
    ld(s, "sp", g_bc, g_bc[:, :], wres, bcast_row(g_row, 1024))
    ld(s, "sp", b_bc, b_bc[:, :], wres, bcast_row(b_row, 1024))
    ats = [s.sb([128, nchunk, 512], BF16, "at") for _ in range(2)]
    hfs = [s.sb([128, 1024], F32, "hf2") for _ in range(2)]
    hos = [s.sb([128, 1024], F32, "ho") for _ in range(2)]
    psY = [[s.ps([128, 512], F32, "psY") for _ in range(2)] for _ in range(2)]
    wk = ln_work(s)
    aTv = aT[0:nchunk * 128, :].rearrange("(c p) t -> p c t", p=128)
    for tg in range(8):
        at = ats[tg % 2]
        ld(s, "sp", at, at[:, :, :], aT_res, aTv[:, :, tg * 512:(tg + 1) * 512])
        for ti in range(4):
            i = tg * 4 + ti
            hf = hfs[i % 2]
            ho = hos[i % 2]
            py = psY[i % 2]
            ld(s, "sp", hf, hf[:, :], h_in_res, h_in_ap[i * 128:(i + 1) * 128, :])
            for half in range(2):
                for c in range(nchunk):
                    mm(s, py[half], py[half][:, :], at, at[:, c, ti * 128:(ti + 1) * 128], wo,
                       wo[:, c, half * 512:(half + 1) * 512], c == 0, c == nchunk - 1)
            ln_epilogue(s, cx, py, hf, g_bc, b_bc, wk, ho)
            st(s, "pool", h_out_res, h_out_ap[i * 128:(i + 1) * 128, :], ho, ho[:, :], final=final)


def decay_tables(s, cx, LF, ig, nh, kscale_ln, EA, EB, ET, psX, DEC=None):
    n = NT * nh
    Fin = s.sb([128, NT, nh], F32, "Fin")
    tot = s.sb([128, NT, nh], F32, "tot")
    inc = s.sb([128, NT, nh], F32, "inc")
    LF2 = LF[:, :, :].rearrange("p a b -> p (a b)")
    mm(s, psX, psX[:, 0:n], cx.tri_le, cx.tri_le[:, :], LF, LF2, True, True)
    cp(s, "dve", Fin, Fin[:, :, :].rearrange("p a b -> p (a b)"), psX, psX[:, 0:n])
    mm(s, psX, psX[:, 0:n], cx.ones_f, cx.ones_f[:, :], LF, LF2, True, True)
    cp(s, "dve", tot, tot[:, :, :].rearrange("p a b -> p (a b)"), psX, psX[:, 0:n])
    for h in range(nh):
        s.op("dve", lambda e, h=h: e.tensor_tensor_scan(out=inc[:, :, h], data0=cx.ones_f[:, 0:NT], data1=tot[:, :, h],
                                                        initial=0.0, op0=ALU.mult, op1=ALU.add),
             reads=[cx.ones_f, tot], writes=[inc])
    tt(s, "dve", EB, EB[:, :, :], inc, inc[:, :, :], tot, tot[:, :, :], ALU.subtract)
    tt(s, "dve", EA, EA[:, :, :], Fin, Fin[:, :, :], EB, EB[:, :, :], ALU.add)
    if ig is not None:
        tt(s, "dve", EA, EA[:, :, :], ig[0], ig[1], EA, EA[:, :, :], ALU.subtract)
        ts(s, "dve", EA, EA[:, :, :], EA, EA[:, :, :], kscale_ln, None, ALU.add)
    else:
        ts(s, "dve", EA, EA[:, :, :], EA, EA[:, :, :], -1.0, kscale_ln, ALU.mult, ALU.add)
    act(s, ET, ET[:, :, :], Fin, Fin[:, :, :], AF.Exp)
    if DEC is not None:
        act(s, DEC, DEC[:, :, :], tot, tot[:, :, :], AF.Exp)


def make_Ediag(s, Ediag, Enext, EA, EB, h, tmp2):
    tt(s, "dve", tmp2, tmp2[:, :], EA, EA[:, :, h], EB, EB[:, :, h], ALU.add)
    ts(s, "dve", tmp2, tmp2[:, :], tmp2, tmp2[:, :], 80.0, None, ALU.min)
    act(s, Ediag, Ediag[:, :], tmp2, tmp2[:, :], AF.Exp)
    tt(s, "dve", tmp2, tmp2[:, 0:NT - 1], EA, EA[:, 0:NT - 1, h], EB, EB[:, 1:NT, h], ALU.add)
    ts(s, "dve", tmp2, tmp2[:, 0:NT - 1], tmp2, tmp2[:, 0:NT - 1], 80.0, None, ALU.min)
    act(s, Enext, Enext[:, 0:NT - 1], tmp2, tmp2[:, 0:NT - 1], AF.Exp)


def make_E(s, E, EA, EB, h, tmpE):
    tt(s, "pool", tmpE, tmpE[:, :, :], EA, EA[:, :, h].unsqueeze(2).to_broadcast([128, NT, NT]),
       EB, EB[:, :, h].unsqueeze(1).to_broadcast([128, NT, NT]), ALU.add)
    ts(s, "pool", tmpE, tmpE[:, :, :], tmpE, tmpE[:, :, :], 80.0, None, ALU.min)
    act(s, E, E[:, :, :], tmpE, tmpE[:, :, :], AF.Exp)


def decay_main(s, cx, heads, psS, psAcc, PTs, fin_cb, jmax=NT):
    cnt = [0] * len(heads)
    for j in range(jmax):
        for hi, hd in enumerate(heads):
            acc = psAcc[hi]
            dvw = hd["dvw"]
            nk = len(hd["kparts"])
            for i0 in range(0, j + 1, 4):
                i1 = min(i0 + 4, j + 1)
                n = i1 - i0
                pS = psS[hi][cnt[hi] % 2]
                PT = PTs[hi][cnt[hi] % 2]
                cnt[hi] += 1
                for ii in range(n):
                    i = i0 + ii
                    for kc, (Kt, Kap, Qt, Qap) in enumerate(hd["kparts"]):
                        mm(s, pS, pS[:, ii * 128:(ii + 1) * 128], Kt, Kap(i), Qt, Qap(j), kc == 0, kc == nk - 1)
                E = hd["E"]
                tt(s, "dve", PT, PT[:, 0:n, :], pS, pS[:, 0:n * 128].rearrange("p (a t) -> p a t", a=n),
                   E, E[:, i0:i1, j].unsqueeze(2).to_broadcast([128, n, 128]), ALU.mult)
                if i1 - 1 == j:
                    tt(s, "pool", PT, PT[:, n - 1, :], PT, PT[:, n - 1, :], cx.tri_b, cx.tri_b[:, :], ALU.mult)
                for ii in range(n):
                    i = i0 + ii
                    mm(s, acc, acc[:, 0:dvw], PT, PT[:, ii, :], hd["V"], hd["Vap"](i), i == 0, i == j)
            fin_cb(hi, hd, j, acc)


def small_norm(s, x_t, x_ap, width, wk, out_t, out_ap, g_t, g_ap):
    stats, mv, sd, rstd, nmr, xn = wk
    s.op("dve", lambda e: e.bn_stats(out=stats[:, :], in_=x_ap), reads=[x_t], writes=[stats])
    s.op("dve", lambda e: e.bn_aggr(out=mv[:, :], in_=stats[:, :]), reads=[stats], writes=[mv])
    ts(s, "dve", sd, sd[:, :], mv, mv[:, 1:2], LN_EPS, None, ALU.add)
    act(s, sd, sd[:, :], sd, sd[:, :], AF.Sqrt)
    s.op("dve", lambda e: e.reciprocal(out=rstd[:, :], in_=sd[:, :]), reads=[sd], writes=[rstd])
    stt(s, "dve", nmr, nmr[:, :], mv, mv[:, 0:1], -1.0, rstd, rstd[:, :], ALU.mult, ALU.mult)
    act(s, xn, xn[:, 0:width], x_t, x_ap, AF.Identity, scale=rstd[:, 0:1], bias=nmr[:, 0:1], extra_reads=[rstd, nmr])
    tt(s, "pool", out_t, out_ap, xn, xn[:, 0:width], g_t, g_ap, ALU.mult)


def small_norm_work(s, width):
    return (s.sb([128, 6], F32, "snst"), s.sb([128, 2], F32, "snmv"), s.sb([128, 1], F32, "snsd"),
            s.sb([128, 1], F32, "snrs"), s.sb([128, 1], F32, "snnm"), s.sb([128, width], F32, "snxn"))


def gate_proj_ln(s, cx, dr, xT, HN_res, HN, F, w_gate_ap, gfunc, w_out, h_in, h_out, g_row, b_row):
    h_in_res, h_in_ap = h_in[0], h_in[1]
    h_out_res, h_out_ap, final = h_out
    wres = dr["wres"]
    nch = F // 128
    nb = F // 512
    wg = s.sb([128, 8, F], BF16, "wgate")
    for c in range(8):
        for q in range(F // 1024):
            wload(s, cx, wg, wg[:, c, q * 1024:(q + 1) * 1024], wres, w_gate_ap[c * 128:(c + 1) * 128, q * 1024:(q + 1) * 1024], [1024])
    wo = s.sb([128, nch, 1024], BF16, "wo")
    for c in range(nch):
        wload(s, cx, wo, wo[:, c, :], wres, w_out[c * 128:(c + 1) * 128, :], [1024])
    g_bc = s.sb([128, 1024], F32, "gbc")
    b_bc = s.sb([128, 1024], F32, "bbc")
    ld(s, "sp", g_bc, g_bc[:, :], wres, bcast_row(g_row, 1024))
    ld(s, "sp", b_bc, b_bc[:, :], wres, bcast_row(b_row, 1024))
    nbuf = 1 if F > 1024 else 2
    hns = [s.sb([128, F], F32, "hn") for _ in range(nbuf)]
    sig = s.sb([128, F], F32, "sig")
    Gb = s.sb([128, F], BF16, "Gb")
    GT_ = s.sb([128, nch, 128], BF16, "GTt")
    hfs = [s.sb([128, 1024], F32, "hf2") for _ in range(nbuf)]
    hos = [s.sb([128, 1024], F32, "ho") for _ in range(nbuf)]
    psg = [s.ps([128, 512], F32, "psg") for _ in range(2)]
    pst = [s.ps([128, 1024], BF16, "pst") for _ in range(2)]
    psY = [s.ps([128, 512], F32, "psY") for _ in range(2)]
    wk = ln_work(s)
    for i in range(NT):
        hn = hns[i % nbuf]
        hf = hfs[i % nbuf]
        ho = hos[i % nbuf]
        ld(s, "sp", hn, hn[:, :], HN_res, HN[i * 128:(i + 1) * 128, 0:F])
        ld(s, "sp", hf, hf[:, :], h_in_res, h_in_ap[i * 128:(i + 1) * 128, :])
        for q in range(nb):
            pg = psg[q % 2]
            for c in range(8):
                mm(s, pg, pg[:, :], xT, xT[:, c, i * 128:(i + 1) * 128], wg, wg[:, c, q * 512:(q + 1) * 512], c == 0, c == 7)
            act(s, sig, sig[:, q * 512:(q + 1) * 512], pg, pg[:, :], gfunc)
        tt(s, "pool", Gb, Gb[:, :], hn, hn[:, :], sig, sig[:, :], ALU.mult)
        for q in range(nch // 8):
            pt = pst[q % 2]
            for c8 in range(8):
                c = q * 8 + c8
                s.op("pe", lambda e, pt=pt, c8=c8, c=c: e.transpose(out=pt[:, c8 * 128:(c8 + 1) * 128], in_=Gb[:, c * 128:(c + 1) * 128],
                                                                   identity=cx.ident_b[:, :]), reads=[Gb, cx.ident_b], writes=[pt])
            cp(s, "act" if q else "dve", GT_, GT_[:, q * 8:(q + 1) * 8, :], pt, pt[:, :].rearrange("p (c t) -> p c t", c=8))
        for half in range(2):
            for c in range(nch):
                mm(s, psY[half], psY[half][:, :], GT_, GT_[:, c, :], wo, wo[:, c, half * 512:(half + 1) * 512], c == 0, c == nch - 1)
        ln_epilogue(s, cx, psY, hf, g_bc, b_bc, wk, ho)
        st(s, "pool", h_out_res, h_out_ap[i * 128:(i + 1) * 128, :], ho, ho[:, :], final=final)


def phase_B(s, cx, dr, l, j_, h_in, h_out):
    import os
    nc = s.nc
    h_in_res, h_in_ap = h_in[0], h_in[1]
    wres = dr["wres"]
    w_in = dr["b_w_in"][j_]
    w_in_v = w_in.rearrange("(c p) n -> p c n", p=128)
    HN_res, HN = dr["hn"]
    bstop = int(os.environ.get("BSTOP", "9"))
    jmax = int(os.environ.get("BJMAX", str(NT)))
    npair = int(os.environ.get("BPAIRS", "4"))

    s.begin_phase()
    xT = s.sb([128, 8, S], BF16, "xT")
    GATES = s.sb([128, NT, 16], F32, "GATES")
    with ExitStack() as es0:
        old = s.phase_es
        s.phase_es = es0
        Wg32 = s.sb([128, 8, 16], F32, "Wg32")
        ld(s, "sp", Wg32, Wg32[:, :, :], wres, w_in_v[:, :, 3072:3088])
        gb = s.sb([128, 16], F32, "gbias")
        ld(s, "sp", gb, gb[:, :], wres, bcast_row(dr["b_gate_bias"][j_], 16))
        hfs = [s.sb([128, 1024], F32, "hf") for _ in range(3)]
        xTts = [s.sb([128, 8, 128], F32, "xTt") for _ in range(2)]
        pts = [s.ps([128, 512], F32, "ptr") for _ in range(2)]
        psg = s.ps([128, 512], F32, "psgate")
        for i in range(NT):
            hf = hfs[i % 3]
            xTt = xTts[i % 2]
            ld(s, "sp", hf, hf[:, :], h_in_res, h_in_ap[i * 128:(i + 1) * 128, :])
            for half in range(2):
                pt = pts[half]
                for c4 in range(4):
                    c = half * 4 + c4
                    s.op("pe", lambda e, pt=pt, c4=c4, c=c, hf=hf: e.transpose(
                        out=pt[:, c4 * 128:(c4 + 1) * 128], in_=hf[:, c * 128:(c + 1) * 128], identity=cx.ident_f[:, :]),
                        reads=[hf, cx.ident_f], writes=[pt])
                src = pt[:, :].rearrange("p (c t) -> p c t", c=4)
                cp(s, "act", xT, xT[:, half * 4:(half + 1) * 4, i * 128:(i + 1) * 128], pt, src)
                cp(s, "dve", xTt, xTt[:, half * 4:(half + 1) * 4, :], pt, src)
            for c in range(8):
                mm(s, psg, psg[:, 0:16], xTt, xTt[:, c, :], Wg32, Wg32[:, c, :], c == 0, c == 7)
            tt(s, "dve", GATES, GATES[:, i, :], psg, psg[:, 0:16], gb, gb[:, :], ALU.add)
        s.phase_es = old
    s_sub_barrier(s)

    EA = s.sb([128, NT, 8], F32, "EA")
    EB = s.sb([128, NT, 8], F32, "EB")
    ET = s.sb([128, NT, 8], F32, "ET")
    DEC = s.sb([128, NT, 8], F32, "DEC")
    with ExitStack() as es1:
        old = s.phase_es
        s.phase_es = es1
        LF = s.sb([128, NT, 8], F32, "LF")
        psX = s.ps([128, 512], F32, "psX")
        act(s, LF, LF[:, :, :], GATES, GATES[:, :, 8:16], AF.Exp, scale=-1.0)
        ts(s, "dve", LF, LF[:, :, :], LF, LF[:, :, :], 1.0, None, ALU.add)
        act(s, LF, LF[:, :, :], LF, LF[:, :, :], AF.Ln)
        ts(s, "dve", LF, LF[:, :, :], LF, LF[:, :, :], -1.0, None, ALU.mult)
        decay_tables(s, cx, LF, (GATES, GATES[:, :, 0:8]), 8, math.log(0.125), EA, EB, ET, psX, DEC)
        s.phase_es = old
    s_sub_barrier(s)
    if bstop < 2:
        s.end_phase()
        return

    with ExitStack() as es2:
        old = s.phase_es
        s.phase_es = es2
        ng_bc = s.sb([128, 1024], F32, "ngbc")
        ld(s, "sp", ng_bc, ng_bc[:, :], wres, bcast_row(dr["b_norm_g"][j_], 1024))
        wq = s.sb([128, 8, 128], BF16, "wq")
        wkk = s.sb([128, 8, 128], BF16, "wk")
        wv = s.sb([128, 8, 256], BF16, "wv")
        cw = [s.sb([128, 4], F32, "cw") for _ in range(2)]
        cb = [s.sb([128, 1], F32, "cb") for _ in range(2)]
        UT = s.sb([128, S + 3], F32, "UT")
        s.op("pool", lambda e: e.memset(UT[:, 0:3], 0.0), writes=[UT])
        cacc = s.sb([128, S], F32, "cacc")
        QT = s.sb([128, S], BF16, "QT")
        KT = s.sb([128, S], BF16, "KT")
        Vaug = s.sb([128, NT, 2, 129], BF16, "Vaug")
        s.op("pool", lambda e: e.memset(Vaug[:, :, :, 128:129], 1.0), writes=[Vaug])
        Edg = [s.sb([128, NT], F32, "Edg") for _ in range(2)]
        Enx = [s.sb([128, NT], F32, "Enx") for _ in range(2)]
        tmp2 = s.sb([128, NT], F32, "tmp2")
        Dp = s.sb([128, NT], F32, "Dp")
        R32 = s.sb([128, 129], F32, "R32")
        Rb = s.sb([128, 129], BF16, "Rb")
        Ktss = [s.sb([128, 128], BF16, "Kts") for _ in range(2)]
        psX = [s.ps([128, 512], F32, "psX") for _ in range(2)]
        psS = [s.ps([128, 512], F32, "psS") for _ in range(2)]
        psAcc = [s.ps([128, 512], F32, "psAcc") for _ in range(2)]
        psT = s.ps([128, 1024], BF16, "psTk")
        psKV = s.ps([128, 512], F32, "psKV")
        PTs = [[s.sb([128, 1, 128], BF16, "PT") for _ in range(2)] for _ in range(2)]
        snw = small_norm_work(s, 128)
        sm = {k: s.sb([128, 1], F32, k) for k in ("ed", "ad", "rec", "fac")}
        hnr = s.sb([128, 128], F32, "hnr")
        HNt = [s.sb([128, 256], F32, "HNt") for _ in range(2)]
        for hp in range(npair):
            for w, (wt, dst) in enumerate(((wq, QT), (wkk, KT))):
                col = w * 512 + hp * 128
                wload(s, cx, wt, wt[:, :, :], wres, w_in_v[:, :, col:col + 128], [8, 128])
                for jj in range(4):
                    ld(s, "sp", cw[w], cw[w][:, jj:jj + 1], wres, dr["b_conv_w"][j_][jj, col:col + 128].rearrange("(c o) -> c o", o=1))
                ld(s, "sp", cb[w], cb[w][:, :], wres, dr["b_conv_b"][j_][col:col + 128].rearrange("(c o) -> c o", o=1))
                for tg in range(8):
                    px = psX[tg % 2]
                    for c in range(8):
                        mm(s, px, px[:, :], wt, wt[:, c, :], xT, xT[:, c, tg * 512:(tg + 1) * 512], c == 0, c == 7)
                    cp(s, "act", UT, UT[:, 3 + tg * 512:3 + (tg + 1) * 512], px, px[:, :])
                ts(s, "dve", cacc, cacc[:, :], UT, UT[:, 0:S], cw[w][:, 0:1], None, ALU.mult, extra_reads=[cw[w]])
                for jj in range(1, 4):
                    stt(s, "dve", cacc, cacc[:, :], UT, UT[:, jj:S + jj], cw[w][:, jj:jj + 1], cacc, cacc[:, :], ALU.mult, ALU.add,
                        extra_reads=[cw[w]])
                act(s, dst, dst[:, :], cacc, cacc[:, :], AF.Silu, scale=1.0, bias=cb[w][:, 0:1], extra_reads=[cb[w]])
            wload(s, cx, wv, wv[:, :, :], wres, w_in_v[:, :, 1024 + hp * 256:1024 + (hp + 1) * 256], [8, 256])
            for i in range(NT):
                px = psX[i % 2]
                for c in range(8):
                    mm(s, px, px[:, 0:256], xT, xT[:, c, i * 128:(i + 1) * 128], wv, wv[:, c, :], c == 0, c == 7)
                cp(s, "act", Vaug, Vaug[:, i, :, 0:128], px, px[:, 0:256].rearrange("p (a d) -> p a d", a=2))
            heads = []
            for hh in range(2):
                h = hp * 2 + hh
                make_Ediag(s, Edg[hh], Enx[hh], EA, EB, h, tmp2)
                hs = slice(hh * 64, (hh + 1) * 64)
                cp(s, "pool", Dp, Dp[hs, :], DEC, DEC[hs, :, h])
                heads.append(dict(h=h, hh=hh))
            s.op("pool", lambda e: e.memset(R32[:, :], 0.0), writes=[R32])
            s.op("pool", lambda e: e.memset(Rb[:, :], 0.0), writes=[Rb])

            def fin(hi, hd, j, acc):
                h, hh = hd["h"], hd["hh"]
                tt(s, "dve", sm["ed"], sm["ed"][:, :], acc, acc[:, 128:129], ET, ET[:, j, h:h + 1], ALU.mult)
                stt(s, "dve", sm["ad"], sm["ad"][:, :], sm["ed"], sm["ed"][:, :], -1.0, sm["ed"], sm["ed"][:, :], ALU.mult, ALU.max)
                ts(s, "dve", sm["ad"], sm["ad"][:, :], sm["ad"], sm["ad"][:, :], 1.0, None, ALU.max)
                s.op("dve", lambda e: e.reciprocal(out=sm["rec"][:, :], in_=sm["ad"][:, :]), reads=[sm["ad"]], writes=[sm["rec"]])
                tt(s, "dve", sm["fac"], sm["fac"][:, :], sm["rec"], sm["rec"][:, :], ET, ET[:, j, h:h + 1], ALU.mult)
                act(s, hnr, hnr[:, :], acc, acc[:, 0:128], AF.Copy, scale=sm["fac"][:, 0:1], extra_reads=[sm["fac"]])
                ht = HNt[j % 2]
                small_norm(s, hnr, hnr[:, :], 128, snw, ht, ht[:, hh * 128:(hh + 1) * 128], ng_bc, ng_bc[:, h * 128:(h + 1) * 128])
                if hh == 1:
                    st(s, "pool", HN_res, HN[j * 128:(j + 1) * 128, hp * 256:(hp + 1) * 256], ht, ht[:, :])
            for j in range(jmax):
                tj = slice(j * 128, (j + 1) * 128)
                for hh in range(2):
                    hs = slice(hh * 64, (hh + 1) * 64)
                    pS = psS[hh]
                    PT = PTs[hh][j % 2]
                    acc = psAcc[hh]
                    mm(s, pS, pS[:, 0:128], KT, KT[hs, tj], QT, QT[hs, tj], True, True)
                    ts(s, "dve", PT, PT[:, 0, :], pS, pS[:, 0:128], Edg[hh][:, j:j + 1], None, ALU.mult, extra_reads=[Edg[hh]])
                    tt(s, "pool", PT, PT[:, 0, :], PT, PT[:, 0, :], cx.tri_b, cx.tri_b[:, :], ALU.mult)
                    mm(s, acc, acc[:, 0:129], PT, PT[:, 0, :], Vaug, Vaug[:, j, hh, :], True, j == 0)
                    if j > 0:
                        mm(s, acc, acc[:, 0:129], QT, QT[hs, tj], Rb, Rb[hs, :], False, True)
                    fin(hh, heads[hh], j, acc)
                if j < jmax - 1:
                    s.op("pe", lambda e, tj=tj: e.transpose(out=psT[:, 0:128], in_=KT[:, tj], identity=cx.ident_b[:, :]),
                         reads=[KT, cx.ident_b], writes=[psT], cost=150)
                    Kts = Ktss[j % 2]
                    for hh in range(2):
                        hs = slice(hh * 64, (hh + 1) * 64)
                        act(s, Kts, Kts[:, hs], psT, psT[:, hs], AF.Copy, scale=Enx[hh][:, j:j + 1], extra_reads=[Enx[hh]])
                    for hh in range(2):
                        hs = slice(hh * 64, (hh + 1) * 64)
                        mm(s, psKV, psKV[hs, 0:129], Kts, Kts[:, hs], Vaug, Vaug[:, j, hh, :], True, True)
                    stt(s, "dve", R32, R32[:, :], R32, R32[:, :], Dp[:, j:j + 1], psKV, psKV[:, 0:129], ALU.mult, ALU.add, extra_reads=[Dp])
                    cp(s, "act", Rb, Rb[:, :], R32, R32[:, :])
        s.phase_es = old
    s_sub_barrier(s)
    if bstop < 3:
        s.end_phase()
        return

    with ExitStack() as es3:
        old = s.phase_es
        s.phase_es = es3
        gate_proj_ln(s, cx, dr, xT, HN_res, HN, 1024, w_in[:, 2048:3072], AF.Sigmoid, dr["b_w_out"][j_], h_in, h_out,
                     dr["ln1_g"][l], dr["ln1_b"][l])
        s.phase_es = old
    s.end_phase()


def phase_C(s, cx, dr, l, j_, h_in, h_out):
    import os
    nc = s.nc
    h_in_res, h_in_ap = h_in[0], h_in[1]
    wres = dr["wres"]
    w_in = dr["c_w_in"][j_]
    w_in_v = w_in.rearrange("(c p) n -> p c n", p=128)
    HN_res, HN = dr["hn"]
    qkT_res, qkT = dr["qkT"]
    cstop = int(os.environ.get("CSTOP", "9"))
    jmax = int(os.environ.get("CJMAX", str(NT)))
    nheads = int(os.environ.get("CHEADS", "4"))

    s.begin_phase()
    xT = s.sb([128, 8, S], BF16, "xT")
    load_xT(s, cx, h_in_res, h_in_ap, xT)
    EA = s.sb([128, NT, 4], F32, "EA")
    EB = s.sb([128, NT, 4], F32, "EB")
    ET = s.sb([128, NT, 4], F32, "ET")
    DEC = s.sb([128, NT, 4], F32, "DEC")
    with ExitStack() as es1:
        old = s.phase_es
        s.phase_es = es1
        LF = s.sb([128, NT, 4], F32, "LF")
        psX = s.ps([128, 512], F32, "psX")
        for h in range(4):
            s.op("pool", lambda e, h=h: e.memset(LF[:, :, h:h + 1], math.log(1.0 - 2.0 ** (-5.0 - h))), writes=[LF])
        decay_tables(s, cx, LF, None, 4, math.log(1.0 / 16.0), EA, EB, ET, psX, DEC)
        s.phase_es = old
    s_sub_barrier(s)

    with ExitStack() as es1:
        old = s.phase_es
        s.phase_es = es1
        wqk = s.sb([128, 8, 2048], BF16, "wqk")
        for c in range(8):
            wload(s, cx, wqk, wqk[:, c, :], wres, w_in[c * 128:(c + 1) * 128, 0:2048], [2048])
        rc = s.sb([128, 4], F32, "ropec")
        ld(s, "sp", rc, rc[:, :], wres, dr["rope_consts"])
        posi = s.sb([128, 512], I32, "posi")
        posf = s.sb([128, 512], F32, "posf")
        ang = s.sb([128, 512], F32, "ang")
        ang2 = s.sb([128, 512], F32, "ang2")
        Ct = s.sb([128, 512], F32, "Ct")
        St = s.sb([128, 512], F32, "St")
        scr = (s.sb([128, 512], I32, "ki"), s.sb([128, 512], F32, "kf"), s.sb([128, 512], F32, "rr"))
        psa = [s.ps([128, 512], F32, "psa") for _ in range(2)]
        psb = [s.ps([128, 512], F32, "psb") for _ in range(2)]
        t1s = [s.sb([128, 512], F32, "t1") for _ in range(2)]
        t2s = [s.sb([128, 512], F32, "t2") for _ in range(2)]
        oas = [s.sb([128, 512], BF16, "oa") for _ in range(2)]
        obs = [s.sb([128, 512], BF16, "ob") for _ in range(2)]
        it = 0
        for tg in range(8):
            tsl = slice(tg * 512, (tg + 1) * 512)
            ld(s, "sp", posi, posi[:, :], wres, bcast_row(dr["positions"][tg * 512:(tg + 1) * 512], 512))
            cp(s, "dve", posf, posf[:, :], posi, posi[:, :])
            ts(s, "dve", ang, ang[:, :], posf, posf[:, :], rc[:, 2:3], None, ALU.mult, extra_reads=[rc])
            ts(s, "dve", ang2, ang2[:, :], ang, ang[:, :], math.pi / 2, None, ALU.add)
            sin_table(s, St, St[:, :], ang, ang[:, :], None, None, None, scr)
            sin_table(s, Ct, Ct[:, :], ang2, ang2[:, :], None, None, None, scr)
            for h in range(4):
                for w in range(2):
                    pa, pb = psa[it % 2], psb[it % 2]
                    t1, t2, oa, ob = t1s[it % 2], t2s[it % 2], oas[it % 2], obs[it % 2]
                    it += 1
                    col = w * 1024 + h * 256
                    for c in range(8):
                        mm(s, pa, pa[:, :], wqk, wqk[:, c, col:col + 128], xT, xT[:, c, tsl], c == 0, c == 7)
                    for c in range(8):
                        mm(s, pb, pb[:, :], wqk, wqk[:, c, col + 128:col + 256], xT, xT[:, c, tsl], c == 0, c == 7)
                    tt(s, "dve", t1, t1[:, :], pa, pa[:, :], Ct, Ct[:, :], ALU.mult)
                    tt(s, "dve", t2, t2[:, :], pb, pb[:, :], St, St[:, :], ALU.mult)
                    tt(s, "pool", oa, oa[:, :], t1, t1[:, :], t2, t2[:, :], ALU.subtract)
                    st(s, "pool", qkT_res, qkT[w, h * 256:h * 256 + 128, tsl], oa, oa[:, :])
                    tt(s, "dve", t1, t1[:, :], pb, pb[:, :], Ct, Ct[:, :], ALU.mult)
                    tt(s, "dve", t2, t2[:, :], pa, pa[:, :], St, St[:, :], ALU.mult)
                    tt(s, "pool", ob, ob[:, :], t1, t1[:, :], t2, t2[:, :], ALU.add)
                    st(s, "pool", qkT_res, qkT[w, h * 256 + 128:h * 256 + 256, tsl], ob, ob[:, :])
        s.phase_es = old
    s_sub_barrier(s)
    if cstop < 2:
        s.end_phase()
        return

    with ExitStack() as es2:
        old = s.phase_es
        s.phase_es = es2
        ng_bc = s.sb([128, 2048], F32, "ngbc")
        ld(s, "sp", ng_bc, ng_bc[:, :], wres, bcast_row(dr["c_norm_g"][j_], 2048))
        QA = s.sb([128, S], BF16, "QA")
        QB = s.sb([128, S], BF16, "QB")
        KA = s.sb([128, S], BF16, "KA")
        KB = s.sb([128, S], BF16, "KB")
        wv = s.sb([128, 8, 512], BF16, "wv")
        Vh = s.sb([128, NT, 512], BF16, "Vh")
        Edg = s.sb([128, NT], F32, "Edg")
        Enx = s.sb([128, NT], F32, "Enx")
        tmp2 = s.sb([128, NT], F32, "tmp2")
        R32 = s.sb([128, 2, 512], F32, "R32")
        Rb = s.sb([128, 2, 512], BF16, "Rb")
        Ktss = [s.sb([128, 256], BF16, "Kts") for _ in range(2)]
        psX = [s.ps([128, 512], F32, "psX") for _ in range(2)]
        psS = [s.ps([128, 512], F32, "psS") for _ in range(2)]
        psAcc = s.ps([128, 512], F32, "psAcc")
        psT = s.ps([128, 1024], BF16, "psTk")
        psKV = [s.ps([128, 512], F32, "psKV") for _ in range(2)]
        PTs = [s.sb([128, 128], BF16, "PT") for _ in range(2)]
        snw = small_norm_work(s, 512)
        onr = s.sb([128, 512], F32, "onr")
        HNt = [s.sb([128, 512], F32, "HNt") for _ in range(2)]
        for h in range(nheads):
            for w, (ta, tb) in enumerate(((QA, QB), (KA, KB))):
                ld(s, "sp", ta, ta[:, :], qkT_res, qkT[w, h * 256:h * 256 + 128, :])
                ld(s, "sp", tb, tb[:, :], qkT_res, qkT[w, h * 256 + 128:h * 256 + 256, :])
            wload(s, cx, wv, wv[:, 0:4, :], wres, w_in_v[:, 0:4, 2048 + h * 512:2048 + (h + 1) * 512], [4, 512])
            wload(s, cx, wv, wv[:, 4:8, :], wres, w_in_v[:, 4:8, 2048 + h * 512:2048 + (h + 1) * 512], [4, 512])
            for i in range(NT):
                px = psX[i % 2]
                for c in range(8):
                    mm(s, px, px[:, :], xT, xT[:, c, i * 128:(i + 1) * 128], wv, wv[:, c, :], c == 0, c == 7)
                cp(s, "act", Vh, Vh[:, i, :], px, px[:, :])
            make_Ediag(s, Edg, Enx, EA, EB, h, tmp2)
            s.op("pool", lambda e: e.memset(R32[:, :, :], 0.0), writes=[R32], cost=2000)
            s.op("pool", lambda e: e.memset(Rb[:, :, :], 0.0), writes=[Rb], cost=2000)

            def fin(hi, hd, j, acc, h=h):
                act(s, onr, onr[:, :], acc, acc[:, 0:512], AF.Copy, scale=ET[:, j, h:h + 1], extra_reads=[ET])
                ht = HNt[j % 2]
                small_norm(s, onr, onr[:, :], 512, snw, ht, ht[:, :], ng_bc, ng_bc[:, h * 512:(h + 1) * 512])
                st(s, "pool", HN_res, HN[j * 128:(j + 1) * 128, h * 512:(h + 1) * 512], ht, ht[:, :])
            for j in range(jmax):
                tj = slice(j * 128, (j + 1) * 128)
                pS = psS[j % 2]
                PT = PTs[j % 2]
                acc = psAcc
                mm(s, pS, pS[:, 0:128], KA, KA[:, tj], QA, QA[:, tj], True, False)
                mm(s, pS, pS[:, 0:128], KB, KB[:, tj], QB, QB[:, tj], False, True)
                ts(s, "dve", PT, PT[:, :], pS, pS[:, 0:128], Edg[:, j:j + 1], None, ALU.mult, extra_reads=[Edg])
                tt(s, "pool", PT, PT[:, :], PT, PT[:, :], cx.tri_b, cx.tri_b[:, :], ALU.mult)
                mm(s, acc, acc[:, :], PT, PT[:, :], Vh, Vh[:, j, :], True, j == 0)
                if j > 0:
                    mm(s, acc, acc[:, :], QA, QA[:, tj], Rb, Rb[:, 0, :], False, False)
                    mm(s, acc, acc[:, :], QB, QB[:, tj], Rb, Rb[:, 1, :], False, True)
                fin(0, None, j, acc)
                if j < jmax - 1:
                    s.op("pe", lambda e, tj=tj: e.transpose(out=psT[:, 0:128], in_=KA[:, tj], identity=cx.ident_b[:, :]),
                         reads=[KA, cx.ident_b], writes=[psT], cost=150)
                    s.op("pe", lambda e, tj=tj: e.transpose(out=psT[:, 128:256], in_=KB[:, tj], identity=cx.ident_b[:, :]),
                         reads=[KB, cx.ident_b], writes=[psT], cost=150)
                    Kts = Ktss[j % 2]
                    act(s, Kts, Kts[:, :], psT, psT[:, 0:256], AF.Copy, scale=Enx[:, j:j + 1], extra_reads=[Enx])
                    for ck in range(2):
                        mm(s, psKV[ck], psKV[ck][:, :], Kts, Kts[:, ck * 128:(ck + 1) * 128], Vh, Vh[:, j, :], True, True)
                        stt(s, "dve", R32, R32[:, ck, :], R32, R32[:, ck, :], DEC[:, j, h:h + 1], psKV[ck], psKV[ck][:, :],
                            ALU.mult, ALU.add, extra_reads=[DEC])
                    cp(s, "act", Rb, Rb[:, 0, :], R32, R32[:, 0, :])
                    cp(s, "pool", Rb, Rb[:, 1, :], R32, R32[:, 1, :])
        s.phase_es = old
    s_sub_barrier(s)
    if cstop < 3:
        s.end_phase()
        return

    with ExitStack() as es3:
        old = s.phase_es
        s.phase_es = es3
        gate_proj_ln(s, cx, dr, xT, HN_res, HN, 2048, w_in[:, 4096:6144], AF.Silu, dr["c_w_out"][j_], h_in, h_out,
                     dr["ln1_g"][l], dr["ln1_b"][l])
        s.phase_es = old
    s.end_phase()


NSLOT = 128
BIGIDX = 4 * 64 * 128


def phase_F(s, cx, dr, l, h_in, h_out):
    import os
    nc = s.nc
    h_in_res, h_in_ap = h_in[0], h_in[1]
    h_out_res, h_out_ap, final = h_out
    wres = dr["wres"]
    xbf_res, xbf = dr["xbf"]
    bt_res, bt = dr["buftok"]
    yb_res, yb = dr["yb"]
    fstop = int(os.environ.get("FSTOP", "9"))

    s.begin_phase()
    OH1 = s.sb([128, NT, 64], F32, "OH1")
    OH2 = s.sb([128, NT, 64], F32, "OH2")
    RK = s.sb([128, NT, 2], F32, "RK")
    GT = s.sb([128, NT, 2], F32, "GT")
    DESTi = s.sb([128, NT, 2], I32, "DESTi")
    IDXW = s.sb([128, NSLOT], I32, "IDXW")
    breg_x = nc.gpsimd.to_reg(S - 1)
    breg_w = nc.gpsimd.to_reg((l + 1) * 64 * 128 - 1)
    breg_y = nc.gpsimd.to_reg(NSLOT * 128 - 1)
    breg_t = nc.gpsimd.to_reg(NSLOT * 128 - 1)

    with ExitStack() as es1:
        old = s.phase_es
        s.phase_es = es1
        Wr = s.sb([128, 8, 72], F32, "Wr")
        ld(s, "sp", Wr, Wr[:, :, 0:8], wres, dr["r_group_w"][l].rearrange("(c p) n -> p c n", p=128))
        ld(s, "sp", Wr, Wr[:, :, 8:72], wres, dr["r_expert_w"][l].rearrange("(c p) n -> p c n", p=128))
        bias = s.sb([128, 72], F32, "rbias")
        ld(s, "sp", bias, bias[:, 0:8], wres, bcast_row(dr["r_group_b"][l], 8))
        ld(s, "sp", bias, bias[:, 8:72], wres, bcast_row(dr["r_expert_b"][l], 64))
        tri_lt = s.sb([128, 128], BF16, "trilt")
        ts(s, "dve", tri_lt, tri_lt[:, :], cx.iof, cx.iof[:, :], 0.0, None, ALU.is_gt)
        toki = s.sb([128, NT], I32, "toki")
        s.op("pool", lambda e: e.iota(toki[:, :], pattern=[[128, NT]], base=0, channel_multiplier=1), writes=[toki])
        hfs = [s.sb([128, 1024], F32, "hfr") for _ in range(2)]
        hbs = [s.sb([128, 1024], BF16, "hbr") for _ in range(2)]
        xTts = [s.sb([128, 8, 128], F32, "xTt") for _ in range(2)]
        pts = [s.ps([128, 512], F32, "ptr") for _ in range(2)]
        psL = s.ps([128, 512], F32, "psL")
        psPC = s.ps([128, 512], F32, "psPC")
        LG = s.sb([128, NT, 72], F32, "LG")
        for i in range(NT):
            hf = hfs[i % 2]
            hb = hbs[i % 2]
            xTt = xTts[i % 2]
            ld(s, "sp", hf, hf[:, :], h_in_res, h_in_ap[i * 128:(i + 1) * 128, :])
            cp(s, "pool", hb, hb[:, :], hf, hf[:, :])
            st(s, "pool", xbf_res, xbf[i * 128:(i + 1) * 128, :], hb, hb[:, :])
            for half in range(2):
                pt = pts[half]
                for c4 in range(4):
                    c = half * 4 + c4
                    s.op("pe", lambda e, pt=pt, c4=c4, c=c, hf=hf: e.transpose(
                        out=pt[:, c4 * 128:(c4 + 1) * 128], in_=hf[:, c * 128:(c + 1) * 128], identity=cx.ident_f[:, :]),
                        reads=[hf, cx.ident_f], writes=[pt])
                cp(s, "act" if half else "dve", xTt, xTt[:, half * 4:(half + 1) * 4, :], pt,
                   pt[:, :].rearrange("p (c t) -> p c t", c=4))
            for c in range(8):
                mm(s, psL, psL[:, 0:72], xTt, xTt[:, c, :], Wr, Wr[:, c, :], c == 0, c == 7)
            tt(s, "dve", LG, LG[:, i, :], psL, psL[:, 0:72], bias, bias[:, :], ALU.add)
        def t2(name, n=NT):
            return s.sb([128, n], F32, name)
        gmax, sumg, pgrp, v1, v2, dv, ex, den, rec = [t2(k) for k in ("gmax", "sumg", "pgrp", "v1", "v2", "dv", "ex", "den", "rec")]
        ohg = s.sb([128, NT, 8], F32, "ohg")
        eg = s.sb([128, NT, 8], F32, "eg")
        pen = s.sb([128, NT, 8], F32, "pen")
        em = s.sb([128, NT, 64], F32, "em")
        em2 = s.sb([128, NT, 64], F32, "em2")
        Mb = s.sb([128, NT, 64], BF16, "Mb")
        G = LG[:, :, 0:8]

        def bc2(t, w):
            return t[:, :].unsqueeze(2).to_broadcast([128, NT, w])
        s.op("dve", lambda e: e.reduce_max(out=gmax[:, :], in_=G, axis=AX.X), reads=[LG], writes=[gmax], cost=400)
        tt(s, "dve", ohg, ohg[:, :, :], LG, G, gmax, bc2(gmax, 8), ALU.is_equal)
        tt(s, "dve", eg, eg[:, :, :], LG, G, gmax, bc2(gmax, 8), ALU.subtract)
        act(s, eg, eg[:, :, :], eg, eg[:, :, :], AF.Exp)
        s.op("dve", lambda e: e.reduce_sum(out=sumg[:, :], in_=eg[:, :, :], axis=AX.X), reads=[eg], writes=[sumg], cost=400)
        s.op("dve", lambda e: e.reciprocal(out=pgrp[:, :], in_=sumg[:, :]), reads=[sumg], writes=[pgrp])
        ts(s, "dve", pen, pen[:, :, :], ohg, ohg[:, :, :], -1.0, 1e30, ALU.add, ALU.mult)
        tt(s, "dve", em, em[:, :, :].rearrange("p i (g j) -> p i g j", g=8), LG, LG[:, :, 8:72].rearrange("p i (g j) -> p i g j", g=8),
           pen, pen[:, :, :].unsqueeze(3).to_broadcast([128, NT, 8, 8]), ALU.add)
        s.op("dve", lambda e: e.reduce_max(out=v1[:, :], in_=em[:, :, :], axis=AX.X), reads=[em], writes=[v1], cost=2000)
        tt(s, "dve", OH1, OH1[:, :, :], em, em[:, :, :], v1, bc2(v1, 64), ALU.is_equal)
        stt(s, "dve", em2, em2[:, :, :], OH1, OH1[:, :, :], -1e30, em, em[:, :, :], ALU.mult, ALU.add)
        s.op("dve", lambda e: e.reduce_max(out=v2[:, :], in_=em2[:, :, :], axis=AX.X), reads=[em2], writes=[v2], cost=2000)
        tt(s, "dve", OH2, OH2[:, :, :], em2, em2[:, :, :], v2, bc2(v2, 64), ALU.is_equal)
        tt(s, "dve", dv, dv[:, :], v2, v2[:, :], v1, v1[:, :], ALU.subtract)
        act(s, ex, ex[:, :], dv, dv[:, :], AF.Exp)
        ts(s, "dve", den, den[:, :], ex, ex[:, :], 1.0, None, ALU.add)
        s.op("dve", lambda e: e.reciprocal(out=rec[:, :], in_=den[:, :]), reads=[den], writes=[rec])
        tt(s, "dve", GT, GT[:, :, 0], pgrp, pgrp[:, :], rec, rec[:, :], ALU.mult)
        tt(s, "dve", GT, GT[:, :, 1], GT, GT[:, :, 0], ex, ex[:, :], ALU.mult)
        tt(s, "dve", Mb, Mb[:, :, :], OH1, OH1[:, :, :], OH2, OH2[:, :, :], ALU.add)
        TOT = s.sb([128, NT, 64], F32, "TOT")
        TOT2 = s.sb([128, NT, 64], F32, "TOT2")
        pos = s.sb([128, NT, 64], F32, "pos")
        psP = [s.ps([128, 512], F32, "psP") for _ in range(2)]
        Mb2 = Mb[:, :, :].rearrange("p i e -> p (i e)")
        for q in range(4):
            pp = psP[q % 2]
            mm(s, pp, pp[:, :], cx.ones_b, cx.ones_b[:, :], Mb, Mb2[:, q * 512:(q + 1) * 512], True, True)
            cp(s, "act", TOT, TOT[:, q * 8:(q + 1) * 8, :], pp, pp[:, :].rearrange("p (i e) -> p i e", i=8))
        cur_, oth_ = TOT, TOT2
        for k in (1, 2, 4, 8, 16):
            cp(s, "pool", oth_, oth_[:, 0:k, :], cur_, cur_[:, 0:k, :])
            tt(s, "dve", oth_, oth_[:, k:NT, :], cur_, cur_[:, k:NT, :], cur_, cur_[:, 0:NT - k, :], ALU.add)
            cur_, oth_ = oth_, cur_
        incl = cur_
        for q in range(4):
            pp = psP[q % 2]
            for i8 in range(8):
                i = q * 8 + i8
                mm(s, pp, pp[:, i8 * 64:(i8 + 1) * 64], tri_lt, tri_lt[:, :], Mb, Mb[:, i, :], True, True)
            tt(s, "dve", pos, pos[:, q * 8:(q + 1) * 8, :], pp, pp[:, :].rearrange("p (i e) -> p i e", i=8),
               incl, incl[:, q * 8:(q + 1) * 8, :], ALU.add)
        own = s.sb([128, NT, 64], F32, "own")
        for q in range(4):
            pp = psP[q % 2]
            mm(s, pp, pp[:, :], cx.ones_b, cx.ones_b[:, :], Mb, Mb2[:, q * 512:(q + 1) * 512], True, True)
            cp(s, "act", own, own[:, q * 8:(q + 1) * 8, :], pp, pp[:, :].rearrange("p (i e) -> p i e", i=8))
        tt(s, "dve", pos, pos[:, :, :], pos, pos[:, :, :], own, own[:, :, :], ALU.subtract)
        cnt = s.sb([128, 64], F32, "cnt")
        cp(s, "dve", cnt, cnt[:, :], incl, incl[:, NT - 1, :])
        bigr = s.sb([128, NT, 64], F32, "bigr")
        for k, OH in enumerate((OH1, OH2)):
            tt(s, "dve", bigr, bigr[:, :, :], OH, OH[:, :, :], pos, pos[:, :, :], ALU.mult)
            s.op("dve", lambda e, k=k: e.reduce_sum(out=RK[:, :, k], in_=bigr[:, :, :], axis=AX.X), reads=[bigr], writes=[RK], cost=2000)
        cnti = s.sb([128, 64], I32, "cnti")
        padf = s.sb([128, 64], F32, "padf")
        pends = s.sb([128, 64], F32, "pends")
        pstart = s.sb([128, 64], F32, "pstart")
        ts(s, "dve", cnti, cnti[:, :], cnt, cnt[:, :], 127.0, None, ALU.add)
        ts(s, "dve", cnti, cnti[:, :], cnti, cnti[:, :], 7, 7, ALU.arith_shift_right, ALU.logical_shift_left)
        cp(s, "dve", padf, padf[:, :], cnti, cnti[:, :])
        s.op("dve", lambda e: e.tensor_tensor_scan(out=pends[:, :], data0=cx.ones_f[:, 0:64], data1=padf[:, :],
                                                   initial=0.0, op0=ALU.mult, op1=ALU.add),
             reads=[cx.ones_f, padf], writes=[pends])
        tt(s, "dve", pstart, pstart[:, :], pends, pends[:, :], padf, padf[:, :], ALU.subtract)
        big = s.sb([128, NT, 64], F32, "big")
        DEST = s.sb([128, NT, 2], F32, "DEST")
        for k, OH in enumerate((OH1, OH2)):
            tt(s, "dve", big, big[:, :, :], OH, OH[:, :, :], pstart, pstart[:, :].unsqueeze(1).to_broadcast([128, NT, 64]), ALU.mult)
            s.op("dve", lambda e, k=k: e.reduce_sum(out=DEST[:, :, k], in_=big[:, :, :], axis=AX.X), reads=[big], writes=[DEST])
        tt(s, "dve", DEST, DEST[:, :, :], DEST, DEST[:, :, :], RK, RK[:, :, :], ALU.add)
        cp(s, "dve", DESTi, DESTi[:, :, :], DEST, DEST[:, :, :])
        fill = s.sb([128, NSLOT], I32, "fill")
        s.op("pool", lambda e: e.iota(fill[:, :], pattern=[[0, NSLOT]], base=S, channel_multiplier=0), writes=[fill])
        st(s, "sp", bt_res, bt.rearrange("(p n) o -> p (n o)", p=128), fill, fill[:, :])
        for i in range(NT):
            for k in range(2):
                s.dma("pool", lambda e, i=i, k=k: e.indirect_dma_start(
                    out=bt, out_offset=bass.IndirectOffsetOnAxis(ap=DESTi[:, i, k:k + 1], axis=0),
                    in_=toki[:, i:i + 1], in_offset=None, bounds_check=breg_t, oob_is_err=False),
                    reads=[DESTi, toki], writes=[bt_res])
        slotoff = s.sb([128, NSLOT], F32, "slotoff")
        sloti = s.sb([128, NSLOT], I32, "sloti")
        s.op("pool", lambda e: e.iota(sloti[:, :], pattern=[[128, NSLOT]], base=0, channel_multiplier=0), writes=[sloti])
        cp(s, "dve", slotoff, slotoff[:, :], sloti, sloti[:, :])
        blk = s.sb([128, NSLOT], F32, "blk")
        cmp_ = s.sb([128, 32, 64], F32, "cmp")
        for q in range(NSLOT // 32):
            tt(s, "dve", cmp_, cmp_[:, :, :], pends, pends[:, :].unsqueeze(1).to_broadcast([128, 32, 64]),
               slotoff, slotoff[:, q * 32:(q + 1) * 32].unsqueeze(2).to_broadcast([128, 32, 64]), ALU.is_le)
            s.op("dve", lambda e, q=q: e.reduce_sum(out=blk[:, q * 32:(q + 1) * 32], in_=cmp_[:, :, :], axis=AX.X),
                 reads=[cmp_], writes=[blk])
        pidx = s.sb([128, 1], F32, "pidx")
        pidi = s.sb([128, 1], I32, "pidi")
        s.op("pool", lambda e: e.iota(pidi[:, :], pattern=[[0, 1]], base=0, channel_multiplier=1), writes=[pidi])
        cp(s, "dve", pidx, pidx[:, :], pidi, pidi[:, :])
        used = s.sb([128, NSLOT], F32, "used")
        ts(s, "dve", used, used[:, :], slotoff, slotoff[:, :], pends[:, 63:64], None, ALU.is_lt, extra_reads=[pends])
        ts(s, "dve", blk, blk[:, :], blk, blk[:, :], 63.0, 128.0, ALU.min, ALU.mult)
        ts(s, "dve", blk, blk[:, :], blk, blk[:, :], pidx[:, 0:1], float(l * 64 * 128 - BIGIDX), ALU.add, ALU.add, extra_reads=[pidx])
        tt(s, "dve", blk, blk[:, :], blk, blk[:, :], used, used[:, :], ALU.mult)
        ts(s, "dve", blk, blk[:, :], blk, blk[:, :], float(BIGIDX), None, ALU.add)
        cp(s, "dve", IDXW, IDXW[:, :], blk, blk[:, :])
        if "dbg" in dr:
            dbg = s.sb([128, 1024], F32, "dbg")
            s.op("dve", lambda e: e.memset(dbg[:, :], 0.0), writes=[dbg])
            cp(s, "dve", dbg, dbg[:, 0:128], IDXW, IDXW[:, :])
            cp(s, "dve", dbg, dbg[:, 128:192], cnt, cnt[:, :])
            cp(s, "dve", dbg, dbg[:, 192:256], pends, pends[:, :])
            cp(s, "dve", dbg, dbg[:, 256:320], DEST, DEST[:, :, :].rearrange("p a b -> p (a b)"))
            cp(s, "dve", dbg, dbg[:, 320:384], GT, GT[:, :, :].rearrange("p a b -> p (a b)"))
            cp(s, "dve", dbg, dbg[:, 384:448], used, used[:, 0:64])
            cp(s, "dve", dbg, dbg[:, 448:512], padf, padf[:, :])
            cp(s, "dve", dbg, dbg[:, 512:576], OH1, OH1[:, 0, :])
            cp(s, "dve", dbg, dbg[:, 576:640], OH2, OH2[:, 0, :])
            cp(s, "dve", dbg, dbg[:, 640:704], RK, RK[:, :, :].rearrange("p a b -> p (a b)"))
            st(s, "sp", wres, dr["dbg"], dbg, dbg[:, :], final=True)
        s.phase_es = old
    s_sub_barrier(s)
    if fstop < 2:
        s.end_phase()
        return

    with ExitStack() as es2:
        old = s.phase_es
        s.phase_es = es2
        wviews = [dr[k].rearrange("l e (p j) n -> (l e p) (j n)", p=128) for k in ("e_w_gate", "e_w_up", "e_w_down")]
        NSTG = 3
        stgs = [[s.sb([128, 2048], F32, f"ws{k}") for k in range(3)] for _ in range(NSTG)]
        wbs = [[s.sb([128, 2048], BF16, f"wb{k}") for k in range(3)] for _ in range(2)]
        idxs = [s.sb([128, 1], I32, "idxb") for _ in range(3)]
        xgs = [s.sb([128, 1024], BF16, "xg") for _ in range(3)]
        for xg in xgs:
            s.op("pool", lambda e, xg=xg: e.memset(xg[:, :], 0.0), writes=[xg])
        xgTs = [s.sb([128, 8, 128], BF16, "xgT") for _ in range(2)]
        sgs = [s.sb([128, 256], F32, "sg") for _ in range(2)]
        aTs = [s.sb([128, 2, 128], BF16, "aT") for _ in range(2)]
        yos = [s.sb([128, 1024], F32, "yo") for _ in range(2)]
        psT = [s.ps([128, 1024], BF16, "psT") for _ in range(2)]
        psG = s.ps([128, 512], F32, "psG")
        psU = s.ps([128, 512], F32, "psU")
        psY = [s.ps([128, 512], F32, "psY") for _ in range(2)]
        nslot = int(os.environ.get("FSLOTS", str(NSLOT)))

        def loads(b):
            ix = idxs[b % 3]
            ld(s, "sp", ix, ix[:, :], bt_res, bt[b * 128:(b + 1) * 128, :])
            xg = xgs[b % 3]
            s.dma("pool", lambda e: e.indirect_dma_start(
                out=xg[:, :], out_offset=None, in_=xbf, in_offset=bass.IndirectOffsetOnAxis(ap=ix[:, 0:1], axis=0),
                bounds_check=breg_x, oob_is_err=False), reads=[ix, xbf_res], writes=[xg])
            for k in range(3):
                stg = stgs[b % NSTG][k]
                s.dma("pool", lambda e, stg=stg, k=k: e.indirect_dma_start(
                    out=stg[:, :], out_offset=None, in_=wviews[k],
                    in_offset=bass.IndirectOffsetOnAxis(ap=IDXW[:, b:b + 1], axis=0),
                    bounds_check=breg_w, oob_is_err=False), reads=[IDXW, wres], writes=[stg], nbytes=1 << 20)

        loads(0)
        loads(1)
        for b in range(nslot):
            if b + 2 < nslot:
                loads(b + 2)
            xg = xgs[b % 3]
            xgT = xgTs[b % 2]
            wg, wu, wd = wbs[b % 2]
            sg = sgs[b % 2]
            aT = aTs[b % 2]
            yo = yos[b % 2]
            pT = psT[b % 2]
            for k, eng in enumerate(("act", "dve", "act")):
                cp(s, eng, wbs[b % 2][k], wbs[b % 2][k][:, :], stgs[b % NSTG][k], stgs[b % NSTG][k][:, :])
            for j in range(8):
                s.op("pe", lambda e, j=j, pT=pT, xg=xg: e.transpose(out=pT[:, j * 128:(j + 1) * 128], in_=xg[:, j:1024:8],
                                                      identity=cx.ident_b[:, :]), reads=[xg, cx.ident_b], writes=[pT])
            cp(s, "dve", xgT, xgT[:, :, :], pT, pT[:, :].rearrange("p (j t) -> p j t", j=8))
            wgv = wg[:, :].rearrange("p (j n) -> p j n", j=8)
            wuv = wu[:, :].rearrange("p (j n) -> p j n", j=8)
            wdv = wd[:, :].rearrange("p (j n) -> p j n", j=2)
            for pS_, wv_ in ((psG, wgv), (psU, wuv)):
                for jh in range(2):
                    for j in range(8):
                        mm(s, pS_, pS_[:, jh * 128:(jh + 1) * 128], wbs[b % 2][0 if pS_ is psG else 1], wv_[:, j, jh:256:2],
                           xgT, xgT[:, j, :], j == 0, j == 7)
            act(s, sg, sg[:, :], psG, psG[:, 0:256], AF.Silu)
            tt(s, "dve", aT, aT[:, :, :].rearrange("p a t -> p (a t)"), sg, sg[:, :], psU, psU[:, 0:256], ALU.mult)
            for half in range(2):
                for jh in range(2):
                    mm(s, psY[half], psY[half][:, :], aT, aT[:, jh, :], wd, wdv[:, jh, half * 512:(half + 1) * 512], jh == 0, jh == 1)
            cp(s, "act", yo, yo[:, 0:512], psY[0], psY[0][:, :])
            cp(s, "dve", yo, yo[:, 512:1024], psY[1], psY[1][:, :])
            st(s, "sp", yb_res, yb[b * 128:(b + 1) * 128, :], yo, yo[:, :])
        s.phase_es = old
    s_sub_barrier(s)
    if fstop < 3:
        s.end_phase()
        return

    with ExitStack() as es3:
        old = s.phase_es
        s.phase_es = es3
        g_bc = s.sb([128, 1024], F32, "gbc")
        b_bc = s.sb([128, 1024], F32, "bbc")
        ld(s, "sp", g_bc, g_bc[:, :], wres, bcast_row(dr["ln2_g"][l], 1024))
        ld(s, "sp", b_bc, b_bc[:, :], wres, bcast_row(dr["ln2_b"][l], 1024))
        y1s = [s.sb([128, 1024], F32, "y1") for _ in range(2)]
        y2s = [s.sb([128, 1024], F32, "y2") for _ in range(2)]
        hfs = [s.sb([128, 1024], F32, "hf3") for _ in range(2)]
        hos = [s.sb([128, 1024], F32, "ho3") for _ in range(2)]
        wk = ln_work(s)
        for i in range(NT):
            y1, y2, hf, ho = y1s[i % 2], y2s[i % 2], hfs[i % 2], hos[i % 2]
            for k, yk in enumerate((y1, y2)):
                s.dma("pool", lambda e, yk=yk, k=k, i=i: e.indirect_dma_start(
                    out=yk[:, :], out_offset=None, in_=yb, in_offset=bass.IndirectOffsetOnAxis(ap=DESTi[:, i, k:k + 1], axis=0),
                    bounds_check=breg_y, oob_is_err=False), reads=[DESTi, yb_res], writes=[yk])
            ld(s, "sp", hf, hf[:, :], h_in_res, h_in_ap[i * 128:(i + 1) * 128, :])
            act(s, y2, y2[:, :], y2, y2[:, :], AF.Copy, scale=GT[:, i, 1:2], extra_reads=[GT])
            stt(s, "dve", y1, y1[:, :], y1, y1[:, :], GT[:, i, 0:1], y2, y2[:, :], ALU.mult, ALU.add, extra_reads=[GT])
            ln_epilogue(s, cx, [(y1, y1[:, 0:512]), (y1, y1[:, 512:1024])], hf, g_bc, b_bc, wk, ho)
            st(s, "pool", h_out_res, h_out_ap[i * 128:(i + 1) * 128, :], ho, ho[:, :], final=final)
        s.phase_es = old
    s.end_phase()


def pack_experts(g, u, d):
    g = np.asarray(g, dtype=np.float32).reshape(4, 64, 128, 2048)
    u = np.asarray(u, dtype=np.float32).reshape(4, 64, 128, 2048)
    d = np.asarray(d, dtype=np.float32).reshape(4, 64, 128, 2048)
    return np.ascontiguousarray(np.stack([g, u, d], axis=3)).reshape(4 * 64 * 128, 3 * 2048)


def rope_consts_np():
    rc = np.zeros((128, 4), np.float32)
    inv_a = 500000.0 ** (-np.arange(0, 16, 2, dtype=np.float32) / 16.0)
    for hh in range(2):
        for jx in range(16):
            rc[hh * 64 + jx, 0] = inv_a[jx % 8]
            rc[hh * 64 + jx, 1] = -1.0 if jx < 8 else 1.0
    inv_c = 10000.0 ** (-np.arange(0, 256, 2, dtype=np.float32) / 256.0)
    rc[:, 2] = inv_c
    return rc


IN_SPECS = [
    ("x", [S, D], F32), ("positions", [S], I32),
    ("ln1_g", [4, D], F32), ("ln1_b", [4, D], F32), ("ln2_g", [4, D], F32), ("ln2_b", [4, D], F32),
    ("a_w_in", [2, D, 3072], F32), ("a_w_out", [2, D, D], F32),
    ("b_w_in", [1, D, 3088], F32), ("b_gate_bias", [1, 16], F32), ("b_conv_w", [1, 4, D], F32),
    ("b_conv_b", [1, D], F32), ("b_norm_g", [1, D], F32), ("b_w_out", [1, D, D], F32),
    ("c_w_in", [1, D, 6144], F32), ("c_norm_g", [1, 2 * D], F32), ("c_w_out", [1, 2 * D, D], F32),
    ("r_group_w", [4, D, 8], F32), ("r_group_b", [4, 8], F32), ("r_expert_w", [4, D, 64], F32),
    ("r_expert_b", [4, 64], F32), ("e_w_gate", [4, 64, D, 256], F32), ("e_w_up", [4, 64, D, 256], F32),
    ("e_w_down", [4, 64, 256, D], F32), ("rope_consts", [128, 4], F32),
]


def build(plan, used_inputs=None):
    nc = bass.Bass("TRN2", target_bir_lowering=False)
    dr = {}
    for name, shape, dt in IN_SPECS:
        if used_inputs is not None and name not in used_inputs:
            continue
        dr[name] = nc.dram_tensor(name, shape, dt, kind="ExternalInput").ap()
    out = nc.dram_tensor("out", [S, D], F32, kind="ExternalOutput").ap()
    import os
    if os.environ.get("KDBG"):
        dr["dbg"] = nc.dram_tensor("dbg", [128, 1024], F32, kind="ExternalOutput").ap()
    dr["wres"] = Res("weights")
    dr["qkT"] = (Res("qkT"), nc.dram_tensor("qkT_s", [2, D, S], BF16).ap())
    dr["vsc"] = (Res("vsc"), nc.dram_tensor("vsc_s", [2, S, 128], BF16).ap())
    dr["attT"] = (Res("attT"), nc.dram_tensor("attT_s", [2 * D, S], BF16).ap())
    dr["hn"] = (Res("hn"), nc.dram_tensor("hn_s", [S, 2 * D], F32).ap())
    dr["xbf"] = (Res("xbf"), nc.dram_tensor("xbf_s", [S, D], BF16).ap())
    dr["buftok"] = (Res("buftok"), nc.dram_tensor("buftok_s", [NSLOT * 128, 1], I32).ap())
    dr["yb"] = (Res("yb"), nc.dram_tensor("yb_s", [NSLOT * 128, D], F32).ap())
    hbuf = [(Res("hA"), nc.dram_tensor("hA_s", [S, D], F32).ap(), False),
            (Res("hB"), nc.dram_tensor("hB_s", [S, D], F32).ap(), False)]
    with ExitStack() as es:
        s = Sched(nc, es)
        cx = Ctx()
        make_consts(s, cx)
        cur = (dr["wres"], dr["x"], False)
        for pi, (ph, l) in enumerate(plan):
            last = pi == len(plan) - 1
            nxt = (Res("out"), out, True) if last else hbuf[pi % 2]
            if ph == "A":
                phase_A(s, cx, dr, l, l // 3, cur, nxt)
            elif ph == "B":
                phase_B(s, cx, dr, l, l // 3, cur, nxt)
            elif ph == "C":
                phase_C(s, cx, dr, l, l // 3, cur, nxt)
            elif ph == "F":
                phase_F(s, cx, dr, l, cur, nxt)
            else:
                raise ValueError(ph)
            cur = nxt
        s.finish()
        print("instructions:", s.ninst, "sems:", s.n_sem)
    return nc


PLAN = [("A", 0), ("F", 0), ("B", 1), ("F", 1), ("C", 2), ("F", 2), ("A", 3), ("F", 3)]
_NC_CACHE = {}


def kernel(**inputs):
    x = np.ascontiguousarray(np.asarray(inputs["x"], dtype=np.float32))
    nb = x.shape[0]
    if "nc" not in _NC_CACHE:
        _NC_CACHE["nc"] = build(PLAN)
    nc = _NC_CACHE["nc"]
    shared = {}
    for name, shape, dt in IN_SPECS:
        if name == "x":
            continue
        if name == "rope_consts":
            shared[name] = rope_consts_np()

        elif name == "positions":
            shared[name] = np.ascontiguousarray(np.asarray(inputs[name]).astype(np.int32))
        else:
            shared[name] = np.ascontiguousarray(np.asarray(inputs[name], dtype=np.float32))
    in_maps = []
    for b in range(nb):
        m = dict(shared)
        m["x"] = x[b]
        in_maps.append(m)
    res = run_bass_kernel_spmd(nc, in_maps, core_ids=list(range(nb)))
    return np.stack([np.asarray(r["out"], dtype=np.float32) for r in res.results], axis=0)
```

```python
import math
import numpy as np
import concourse.bass as bass
import concourse.mybir as mybir
from concourse.bass_utils import run_bass_kernel_spmd
from contextlib import ExitStack

F32 = mybir.dt.float32
BF16 = mybir.dt.bfloat16
I32 = mybir.dt.int32
ALU = mybir.AluOpType
AF = mybir.ActivationFunctionType
AX = mybir.AxisListType

S = 4096
D = 1024
DEPTH = 4
NT = S // 128
ALPHA = (2.0 * DEPTH) ** 0.25
LN_EPS = 1e-5
TWO_PI = 2.0 * math.pi

SEM_WRAP = 30000
ENGS = ("pe", "act", "dve", "pool", "sp")


class Res:
    __slots__ = ("w", "r", "name", "excl")

    def __init__(self, name="", excl=False):
        self.w = None
        self.r = []
        self.name = name
        self.excl = excl


class T:
    __slots__ = ("t", "res")

    def __init__(self, t, res):
        self.t = t
        self.res = res

    def __getitem__(self, k):
        return self.t[k]


class Sched:
    def __init__(self, nc, es):
        self.nc = nc
        self.es = es
        self.sems = {}
        self.cur = {}
        self.waited = {e: {} for e in ENGS}
        self.pending = {e: {} for e in ENGS}
        self.dma_pool = {}
        self.dma_rr = {}
        self.n_sem = 0
        self.n_tiles = 0
        self.ninst = 0
        self.finals = []
        self.eobj = {"pe": nc.tensor, "act": nc.scalar, "dve": nc.vector, "pool": nc.gpsimd, "sp": nc.sync}
        self.phase_es = None
        self.recs = []
        self.ev_of = {}
        self.next_id = 0
        import os
        self.window = int(os.environ.get("KWIN", "48"))

    def _newsem(self, name):
        h = self.es.enter_context(self.nc.semaphore(f"{name}_{self.n_sem}"))
        self.n_sem += 1
        key = self.n_sem
        self.sems[key] = h
        return key

    def sb(self, shape, dtype, name=None):
        self.n_tiles += 1
        name = name or "t"
        es = self.phase_es or self.es
        t = es.enter_context(self.nc.sbuf_tensor(f"{name}_{self.n_tiles}", list(shape), dtype))
        return T(t, Res(name))

    def ps(self, shape, dtype, name=None):
        self.n_tiles += 1
        name = name or "p"
        es = self.phase_es or self.es
        t = es.enter_context(self.nc.psum_tensor(f"{name}_{self.n_tiles}", list(shape), dtype))
        return T(t, Res(name, excl=True))

    def begin_phase(self):
        self.phase_es = ExitStack()
        return self.phase_es

    def end_phase(self):
        self.flush()
        snap = {}
        for e, c in self.cur.items():
            snap[c[0]] = c[1]
        for q, slots in self.dma_pool.items():
            for sl in slots:
                if sl[1] > 0:
                    snap[sl[0]] = sl[1]
        for e in ENGS:
            p = self.pending[e]
            for k, v in snap.items():
                if p.get(k, 0) < v:
                    p[k] = v
        self.phase_es.close()
        self.phase_es = None

    def _deps(self, reads, writes):
        deps = set()
        for r in reads:
            r = r.res if isinstance(r, T) else r
            if r.w is not None:
                deps.add(r.w)
            if r.excl:
                deps.update(r.r)
        for w in writes:
            w = w.res if isinstance(w, T) else w
            if w.w is not None:
                deps.add(w.w)
            deps.update(w.r)
        return deps

    def _commit(self, oid, reads, writes):
        for r in reads:
            r = r.res if isinstance(r, T) else r
            r.r.append(oid)
        for w in writes:
            w = w.res if isinstance(w, T) else w
            w.w = oid
            w.r = []

    def op(self, eng, fn, reads=(), writes=(), cost=150.0):
        oid = self.next_id
        self.next_id += 1
        deps = self._deps(reads, writes)
        self._commit(oid, reads, writes)
        self.recs.append((oid, eng, False, fn, deps, float(cost), 0.0, False))

    def dma(self, q, fn, reads=(), writes=(), final=False, nbytes=65536, issue=None):
        oid = self.next_id
        self.next_id += 1
        deps = self._deps(reads, writes)
        self._commit(oid, reads, writes)
        if issue is None:
            issue = 1200.0 if q == "pool" else 80.0
        self.recs.append((oid, q, True, fn, deps, float(issue), 2000.0 + nbytes / 150.0, final))

    def flush(self):
        recs = self.recs
        self.recs = []
        if not recs:
            return
        fin = {}
        queues = {e: [] for e in ENGS}
        for r in recs:
            queues[r[1]].append(r)
        heads = {e: 0 for e in ENGS}
        placed = set()
        mine = {r[0] for r in recs}
        efree = {e: 0.0 for e in ENGS}
        dma_free = [0.0]
        order = []
        nleft = len(recs)
        W = self.window
        taken = {e: set() for e in ENGS}
        while nleft:
            best = None
            for e in ENGS:
                q = queues[e]
                h = heads[e]
                n = len(q)
                while h < n and q[h][0] in taken[e]:
                    h += 1
                heads[e] = h
                lim = min(n, h + W)
                for k in range(h, lim):
                    r = q[k]
                    if r[0] in taken[e]:
                        continue
                    ok = True
                    st_ = efree[e]
                    for d in r[4]:
                        if d in mine:
                            if d not in placed:
                                ok = False
                                break
                            if fin[d] > st_:
                                st_ = fin[d]
                    if not ok:
                        continue
                    key = (st_, r[0])
                    if best is None or key < best[0]:
                        best = (key, e, r)
                    if st_ <= efree[e]:
                        break
            assert best is not None, "scheduler deadlock"
            (st_, _), e, r = best
            taken[e].add(r[0])
            placed.add(r[0])
            nleft -= 1
            if r[2]:
                iend = st_ + r[5]
                efree[e] = iend
                t0 = max(iend, dma_free[0])
                dma_free[0] = t0 + max(0.0, r[6] - 2000.0)
                fin[r[0]] = dma_free[0] + 2000.0
            else:
                efree[e] = st_ + r[5]
                fin[r[0]] = efree[e] + 60.0
            order.append(r)
        for r in order:
            self._emit_rec(r)

    def _engine_event(self, eng):
        if eng not in self.cur or self.cur[eng][1] >= SEM_WRAP:
            self.cur[eng] = [self._newsem(eng), 0]
        c = self.cur[eng]
        c[1] += 1
        return (c[0], c[1])

    def _dma_event(self, q):
        if q not in self.dma_pool:
            self.dma_pool[q] = [[self._newsem(f"dma{q}"), 0] for _ in range(12)]
            self.dma_rr[q] = 0
        i = self.dma_rr[q]
        self.dma_rr[q] = (i + 1) % len(self.dma_pool[q])
        slot = self.dma_pool[q][i]
        extra = (slot[0], slot[1]) if slot[1] > 0 else None
        if slot[1] + 16 > SEM_WRAP:
            slot[0] = self._newsem(f"dma{q}")
            slot[1] = 0
        slot[1] += 16
        return (slot[0], slot[1]), extra

    def _emit_rec(self, r):
        oid, eng, is_dma, fn, deps, _, _, final = r
        need = self.pending[eng]
        self.pending[eng] = {}
        for d in deps:
            k, v = self.ev_of[d]
            if need.get(k, 0) < v:
                need[k] = v
        if is_dma:
            ev, extra = self._dma_event(eng)
            if extra is not None:
                k, v = extra
                if need.get(k, 0) < v:
                    need[k] = v
            inc = 16
        else:
            ev = self._engine_event(eng)
            inc = 1
        wd = self.waited[eng]
        own = self.cur.get(eng, [None])[0] if eng == "pe" else None
        e = self.eobj[eng]
        for k, v in need.items():
            if k == own:
                continue
            if wd.get(k, 0) < v:
                wd[k] = v
                e.wait_ge(self.sems[k], v)
        ins = fn(e)
        ins.then_inc(self.sems[ev[0]], inc)
        self.ev_of[oid] = ev
        self.ninst += 1
        if final:
            self.finals.append(ev)

    def finish(self):
        self.flush()
        deps = {}
        for k, v in self.finals:
            if deps.get(k, 0) < v:
                deps[k] = v
        for q, slots in self.dma_pool.items():
            for sl in slots:
                if sl[1] > 0 and deps.get(sl[0], 0) < sl[1]:
                    deps[sl[0]] = sl[1]
        for k, v in deps.items():
            self.nc.sync.wait_ge(self.sems[k], v)


def _fsz(ap):
    n = 1
    for d in ap.shape[1:]:
        n *= d
    return n


def mm(s, out_t, out_ap, lhsT_t, lhsT_ap, rhs_t, rhs_ap, start, stop):
    n = _fsz(out_ap)
    c = 30.0 + max(n, 64) * 0.45
    if lhsT_ap.dtype == F32:
        c *= 4
    s.op("pe", lambda e: e.matmul(out_ap, lhsT=lhsT_ap, rhs=rhs_ap, start=start, stop=stop),
         reads=[lhsT_t, rhs_t], writes=[out_t], cost=c)


def _ecost(eng, n):
    if eng == "dve":
        return 70.0 + 0.85 * n
    if eng == "act":
        return 200.0 + 0.65 * n
    return 160.0 + 1.6 * n


def tt(s, eng, out_t, out_ap, a_t, a_ap, b_t, b_ap, op):
    s.op(eng, lambda e: e.tensor_tensor(out=out_ap, in0=a_ap, in1=b_ap, op=op), reads=[a_t, b_t], writes=[out_t],
         cost=_ecost(eng, _fsz(out_ap)))


def ts(s, eng, out_t, out_ap, a_t, a_ap, s1, s2, op0, op1=None, extra_reads=()):
    c = _ecost(eng, _fsz(out_ap))
    if op1 is None:
        s.op(eng, lambda e: e.tensor_scalar(out=out_ap, in0=a_ap, scalar1=s1, scalar2=None, op0=op0),
             reads=[a_t, *extra_reads], writes=[out_t], cost=c)
    else:
        s.op(eng, lambda e: e.tensor_scalar(out=out_ap, in0=a_ap, scalar1=s1, scalar2=s2, op0=op0, op1=op1),
             reads=[a_t, *extra_reads], writes=[out_t], cost=c)


def stt(s, eng, out_t, out_ap, a_t, a_ap, scalar, b_t, b_ap, op0, op1, extra_reads=()):
    s.op(eng, lambda e: e.scalar_tensor_tensor(out=out_ap, in0=a_ap, scalar=scalar, in1=b_ap, op0=op0, op1=op1),
         reads=[a_t, b_t, *extra_reads], writes=[out_t], cost=_ecost(eng, _fsz(out_ap)))


def cp(s, eng, out_t, out_ap, a_t, a_ap):
    n = _fsz(out_ap)
    if eng == "act":
        s.op("act", lambda e: e.activation(out=out_ap, in_=a_ap, func=AF.Copy), reads=[a_t], writes=[out_t], cost=_ecost("act", n))
    else:
        c = _ecost(eng, n) if eng == "dve" else 160.0 + 3.0 * n
        s.op(eng, lambda e: e.tensor_copy(out=out_ap, in_=a_ap), reads=[a_t], writes=[out_t], cost=c)


def act(s, out_t, out_ap, a_t, a_ap, func, scale=1.0, bias=0.0, extra_reads=()):
    s.op("act", lambda e: e.activation(out=out_ap, in_=a_ap, func=func, bias=bias, scale=scale),
         reads=[a_t, *extra_reads], writes=[out_t], cost=_ecost("act", _fsz(out_ap)))


def _nbytes(ap):
    n = 1
    for d in ap.shape:
        n *= d
    return n * (2 if ap.dtype == BF16 else 4)


def ld(s, q, out_t, out_ap, src_res, src_ap):
    s.dma(q, lambda e: e.dma_start(out=out_ap, in_=src_ap), reads=[src_res], writes=[out_t], nbytes=_nbytes(out_ap))


def st(s, q, dst_res, dst_ap, in_t, in_ap, final=False):
    s.dma(q, lambda e: e.dma_start(out=dst_ap, in_=in_ap), reads=[in_t], writes=[dst_res], final=final, nbytes=_nbytes(in_ap))


class Ctx:
    pass


def wload(s, cx, dst_t, dst_ap, wres, src_ap, shape):
    stg = cx.stg[cx.stg_i % 2]
    cx.stg_i += 1
    n = int(np.prod(shape))
    if len(shape) == 1:
        v = stg[:, 0:n]
    else:
        v = stg[:, 0:n].rearrange("p (a b) -> p a b", a=shape[0])
    ld(s, "sp", stg, v, wres, src_ap)
    cp(s, "dve" if cx.stg_i % 2 else "act", dst_t, dst_ap, stg, v)


def make_consts(s, cx):
    nc = s.nc
    iot = s.sb([128, 128], I32, "iot")
    iof = s.sb([128, 128], F32, "iof")
    cx.iof = iof
    cx.ident_f = s.sb([128, 128], F32, "identf")
    cx.ident_b = s.sb([128, 128], BF16, "identb")
    cx.maskA = s.sb([128, 2, 2, 128], BF16, "maskA")
    cx.perm = s.sb([128, 128], BF16, "perm")
    cx.ones_b = s.sb([128, 128], BF16, "onesb")
    cx.ones_f = s.sb([128, 128], F32, "onesf")
    cx.tri_le = s.sb([128, 128], F32, "trile")
    cx.tri_b = s.sb([128, 128], BF16, "trib")
    cx.swlo = s.sb([128, 128], F32, "swlo")
    cx.swhi = s.sb([128, 128], F32, "swhi")
    cx.stg = [s.sb([128, 2048], F32, "stg") for _ in range(2)]
    cx.stg_i = 0
    s.op("pool", lambda e: e.iota(iot[:, :], pattern=[[1, 128]], base=0, channel_multiplier=-1), writes=[iot])
    cp(s, "dve", iof, iof[:, :], iot, iot[:, :])
    ts(s, "dve", cx.ident_f, cx.ident_f[:, :], iof, iof[:, :], 0.0, None, ALU.is_equal)
    cp(s, "dve", cx.ident_b, cx.ident_b[:, :], cx.ident_f, cx.ident_f[:, :])
    ts(s, "dve", cx.tri_le, cx.tri_le[:, :], iof, iof[:, :], 0.0, None, ALU.is_ge)
    cp(s, "dve", cx.tri_b, cx.tri_b[:, :], cx.tri_le, cx.tri_le[:, :])
    ts(s, "dve", cx.swlo, cx.swlo[:, :], iof, iof[:, :], 64.0, None, ALU.is_equal)
    ts(s, "dve", cx.swhi, cx.swhi[:, :], iof, iof[:, :], -64.0, None, ALU.is_equal)
    for hh in range(2):
        ts(s, "dve", cx.maskA, cx.maskA[:, hh, 0, :], iof, iof[:, :], 0.0, None, ALU.is_ge)
        ts(s, "dve", cx.maskA, cx.maskA[:, hh, 1, :], iof, iof[:, :], 0.0, None, ALU.is_le)
    s.op("pool", lambda e: e.memset(cx.perm[:, :], 0.0), writes=[cx.perm])
    s.op("pool", lambda e: e.memset(cx.ones_b[:, :], 1.0), writes=[cx.ones_b])
    s.op("pool", lambda e: e.memset(cx.ones_f[:, :], 1.0), writes=[cx.ones_f])
    for hh in range(2):
        c0 = hh * 64
        ts(s, "dve", cx.perm, cx.perm[:, c0:c0 + 8], iof, iof[:, c0:c0 + 8], -8.0, None, ALU.is_equal)
        ts(s, "dve", cx.perm, cx.perm[:, c0 + 8:c0 + 16], iof, iof[:, c0 + 8:c0 + 16], 8.0, None, ALU.is_equal)


def sin_table(s, out_t, out_ap, x_t, x_ap, shape, sign_ap, sign_t, scratch):
    ki, kf, r = scratch
    C1 = 6.28125
    C2 = TWO_PI - C1
    ts(s, "dve", ki, ki[:, :], x_t, x_ap, 1.0 / TWO_PI, None, ALU.mult)
    cp(s, "dve", kf, kf[:, :], ki, ki[:, :])
    stt(s, "dve", r, r[:, :], kf, kf[:, :], -C1, x_t, x_ap, ALU.mult, ALU.add)
    stt(s, "dve", r, r[:, :], kf, kf[:, :], -C2, r, r[:, :], ALU.mult, ALU.add)
    ts(s, "dve", kf, kf[:, :], r, r[:, :], math.pi, None, ALU.is_gt)
    stt(s, "dve", r, r[:, :], kf, kf[:, :], -TWO_PI, r, r[:, :], ALU.mult, ALU.add)
    ts(s, "dve", kf, kf[:, :], r, r[:, :], -math.pi, None, ALU.is_lt)
    stt(s, "dve", r, r[:, :], kf, kf[:, :], TWO_PI, r, r[:, :], ALU.mult, ALU.add)
    ts(s, "dve", r, r[:, :], r, r[:, :], 3.14159, -3.14159, ALU.min, ALU.max)
    act(s, out_t, out_ap, r, r[:, :], AF.Sin)
    if sign_ap is not None:
        ts(s, "dve", out_t, out_ap, out_t, out_ap, sign_ap, None, ALU.mult, extra_reads=[sign_t])


def load_xT(s, cx, h_res, h_ap, xT):
    old = s.phase_es
    s.phase_es = ExitStack()
    hfs = [s.sb([128, 1024], F32, "hf") for _ in range(3)]
    pts = [s.ps([128, 512], F32, "ptr") for _ in range(2)]
    for i in range(NT):
        hf = hfs[i % 3]
        ld(s, "sp", hf, hf[:, :], h_res, h_ap[i * 128:(i + 1) * 128, :])
        for half in range(2):
            pt = pts[half]
            for c4 in range(4):
                c = half * 4 + c4
                s.op("pe", lambda e, pt=pt, c4=c4, c=c, hf=hf: e.transpose(
                    out=pt[:, c4 * 128:(c4 + 1) * 128], in_=hf[:, c * 128:(c + 1) * 128], identity=cx.ident_f[:, :]),
                    reads=[hf, cx.ident_f], writes=[pt])
            dst = xT[:, half * 4:(half + 1) * 4, i * 128:(i + 1) * 128]
            src = pt[:, :].rearrange("p (c t) -> p c t", c=4)
            cp(s, "act" if half else "dve", xT, dst, pt, src)
    s.end_phase()
    s.phase_es = old


def ln_epilogue(s, cx, y_ps, hf, g_bc, b_bc, wk, out_t):
    r, stats, mv, sd, rstd, nmr, xn = wk
    for half in range(2):
        sl = slice(half * 512, (half + 1) * 512)
        yt, yap = y_ps[half] if isinstance(y_ps[half], tuple) else (y_ps[half], y_ps[half][:, :])
        stt(s, "dve", r, r[:, sl], hf, hf[:, sl], ALPHA, yt, yap, ALU.mult, ALU.add)
        s.op("dve", lambda e, half=half, sl=sl: e.bn_stats(out=stats[:, half, :], in_=r[:, sl]), reads=[r], writes=[stats])
    s.op("dve", lambda e: e.bn_aggr(out=mv[:, :], in_=stats[:, :, :].rearrange("p a b -> p (a b)")), reads=[stats], writes=[mv])
    ts(s, "dve", sd, sd[:, :], mv, mv[:, 1:2], LN_EPS, None, ALU.add)
    act(s, sd, sd[:, :], sd, sd[:, :], AF.Sqrt)
    s.op("dve", lambda e: e.reciprocal(out=rstd[:, :], in_=sd[:, :]), reads=[sd], writes=[rstd])
    stt(s, "dve", nmr, nmr[:, :], mv, mv[:, 0:1], -1.0, rstd, rstd[:, :], ALU.mult, ALU.mult)
    act(s, xn, xn[:, :], r, r[:, :], AF.Identity, scale=rstd[:, 0:1], bias=nmr[:, 0:1], extra_reads=[rstd, nmr])
    tt(s, "pool", xn, xn[:, :], xn, xn[:, :], g_bc, g_bc[:, :], ALU.mult)
    tt(s, "pool", out_t, out_t[:, :], xn, xn[:, :], b_bc, b_bc[:, :], ALU.add)


def ln_work(s):
    return (s.sb([128, 1024], F32, "lnr"), s.sb([128, 2, 6], F32, "lnst"), s.sb([128, 2], F32, "lnmv"),
            s.sb([128, 1], F32, "lnsd"), s.sb([128, 1], F32, "lnrs"), s.sb([128, 1], F32, "lnnm"),
            s.sb([128, 1024], F32, "lnxn"))


def bcast_row(ap_row, n):
    return ap_row.partition_broadcast(128)


def phase_A(s, cx, dr, l, j, h_in, h_out):
    nc = s.nc
    h_in_res, h_in_ap = h_in[0], h_in[1]
    w_in = dr["a_w_in"][j]
    w_out = dr["a_w_out"][j]
    w_in_v = w_in.rearrange("(c p) n -> p c n", p=128)
    qkT_res, qkT = dr["qkT"]
    attT_res, attT = dr["attT"]
    wres = dr["wres"]

    s.begin_phase()
    xT = s.sb([128, 8, S], BF16, "xT")
    load_xT(s, cx, h_in_res, h_in_ap, xT)

    import os
    stop = int(os.environ.get("KSTOP", "9"))
    if stop < 1:
        s.end_phase()
        return
    with ExitStack() as es1:
        old = s.phase_es
        s.phase_es = es1
        wqk = s.sb([128, 8, 2048], BF16, "wqk")
        for c in range(8):
            wload(s, cx, wqk, wqk[:, c, :], wres, w_in[c * 128:(c + 1) * 128, 0:2048], [2048])
        rc = s.sb([128, 4], F32, "ropec")
        ld(s, "sp", rc, rc[:, :], wres, dr["rope_consts"])
        posi = s.sb([128, 512], I32, "posi")
        posf = s.sb([128, 512], F32, "posf")
        ang = s.sb([128, 512], F32, "ang")
        ang2 = s.sb([128, 512], F32, "ang2")
        Ct = s.sb([128, 512], F32, "Ct")
        St = s.sb([128, 512], F32, "St")
        scr = (s.sb([128, 512], I32, "ki"), s.sb([128, 512], F32, "kf"), s.sb([128, 512], F32, "rr"))
        psq = [s.ps([128, 512], F32, "psq") for _ in range(2)]
        psp = [s.ps([128, 512], F32, "psp") for _ in range(2)]
        qbs = [s.sb([128, 512], BF16, "qb") for _ in range(2)]
        t1s = [s.sb([128, 512], F32, "t1") for _ in range(2)]
        t2s = [s.sb([128, 512], F32, "t2") for _ in range(2)]
        obs = [s.sb([128, 512], BF16, "ob") for _ in range(3)]
        it = 0
        ksub = int(os.environ.get("KSUB", "99"))
        for tg in range(int(os.environ.get('KTG', '8')) if ksub > 0 else 0):
            tsl = slice(tg * 512, (tg + 1) * 512)
            ld(s, "sp", posi, posi[:, :], wres, bcast_row(dr["positions"][tg * 512:(tg + 1) * 512], 512))
            cp(s, "dve", posf, posf[:, :], posi, posi[:, :])
            ts(s, "dve", ang, ang[:, :], posf, posf[:, :], rc[:, 0:1], None, ALU.mult, extra_reads=[rc])
            ts(s, "dve", ang2, ang2[:, :], ang, ang[:, :], math.pi / 2, None, ALU.add)
            sin_table(s, St, St[:, :], ang, ang[:, :], None, rc[:, 1:2], rc, scr)
            sin_table(s, Ct, Ct[:, :], ang2, ang2[:, :], None, None, None, scr)
            for hp in range(int(os.environ.get('KHP', '8')) if ksub > 1 else 0):
                for w in range(2):
                    pq = psq[it % 2]
                    pp = psp[it % 2]
                    qb = qbs[it % 2]
                    t1 = t1s[it % 2]
                    t2 = t2s[it % 2]
                    ob = obs[it % 3]
                    it += 1
                    col = w * 1024 + hp * 128
                    for c in range(8):
                        mm(s, pq, pq[:, :], wqk, wqk[:, c, col:col + 128], xT, xT[:, c, tsl], c == 0, c == 7)
                    cp(s, "act", qb, qb[:, :], pq, pq[:, :])
                    if ksub < 3:
                        continue
                    mm(s, pp, pp[:, :], cx.perm, cx.perm[:, :], qb, qb[:, :], True, True)
                    tt(s, "dve", t1, t1[:, :], pq, pq[:, :], Ct, Ct[:, :], ALU.mult)
                    tt(s, "dve", t2, t2[:, :], pp, pp[:, :], St, St[:, :], ALU.mult)
                    tt(s, "pool", ob, ob[:, :], t1, t1[:, :], t2, t2[:, :], ALU.add)
                    if ksub < 4:
                        continue
                    st(s, "pool", qkT_res, qkT[w, hp * 128:(hp + 1) * 128, tsl], ob, ob[:, :])
        s.phase_es = old
    s_sub_barrier(s)
    if stop < 2:
        s.end_phase()
        return

    vsc_res, vsc = dr["vsc"]
    with ExitStack() as es2:
        old = s.phase_es
        s.phase_es = es2
        QT = s.sb([128, S], BF16, "QT")
        KT = s.sb([128, S], BF16, "KT")
        wv = s.sb([128, 8, 128], BF16, "wv")
        Vn = s.sb([128, 32, 128], BF16, "Vn")
        Vaugs = [s.sb([128, 32, 2, 128], BF16, "Vaug") for _ in range(2)]
        for Va in Vaugs:
            s.op("pool", lambda e, Va=Va: e.memset(Va[:, :, 0, 64:128], 1.0), writes=[Va], cost=3000)
            s.op("pool", lambda e, Va=Va: e.memset(Va[:, :, 1, 0:64], 1.0), writes=[Va], cost=3000)
        acc = s.sb([128, 2, S], F32, "acc")
        obuf = s.sb([128, S], BF16, "obuf")
        recs = [s.sb([128, 512], F32, "rec") for _ in range(2)]
        psV = [s.ps([128, 512], F32, "psV") for _ in range(2)]
        psS = [s.ps([128, 512], F32, "psS") for _ in range(4)]
        psO_ = [s.ps([128, 512], F32, "psO") for _ in range(2)]
        psO = [T(p[:, 0:256].rearrange("p (a q) -> p a q", a=2), p.res) for p in psO_]
        pes = [s.sb([128, 512], BF16, "pe") for _ in range(4)]
        PTs = [s.sb([128, 2, 2, 128], BF16, "PT") for _ in range(4)]
        vi = 0
        bi_ctr = 0
        k2 = int(os.environ.get("KSUB2", "99"))
        for hp in range(int(os.environ.get("KHP2", "8"))):
            ld(s, "sp", QT, QT[:, :], qkT_res, qkT[0, hp * 128:(hp + 1) * 128, :])
            ld(s, "sp", KT, KT[:, :], qkT_res, qkT[1, hp * 128:(hp + 1) * 128, :])
            wload(s, cx, wv, wv[:, :, :], wres, w_in_v[:, :, 2048 + hp * 128:2048 + (hp + 1) * 128], [8, 128])
            for b in range(32):
                pv = psV[(b // 4) % 2]
                for c in range(8):
                    mm(s, pv, pv[:, (b % 4) * 128:(b % 4 + 1) * 128], xT, xT[:, c, b * 128:(b + 1) * 128], wv, wv[:, c, :], c == 0, c == 7)
                if b % 4 == 3:
                    cp(s, "act" if (b // 4) % 2 else "dve", Vn, Vn[:, b - 3:b + 1, :], pv,
                       pv[:, :].rearrange("p (b f) -> p b f", b=4))
            vs = vsc[hp % 2]
            st(s, "sp", vsc_res, vs.rearrange("(i p) c -> p i c", p=128), Vn, Vn[:, :, :])
            for di, d in enumerate((1, 4, 16)[:int(os.environ.get("KBR", "3"))] if k2 > 0 else ()):
                nb = 32 // d
                Vaug = Vaugs[vi % 2]
                vi += 1

                def tok(r, n, d=d):
                    return slice(r + d * 128 * n, r + d * 128 * n + 127 * d + 1, d)
                if d > 1:
                    view = vs.rearrange("(n p r) c -> p r n c", p=128, r=d)
                    for r in range(d):
                        s.dma("sp" if r % 2 else "act", lambda e, r=r, nb=nb, view=view: e.dma_start(
                            out=Vn[:, r * nb:(r + 1) * nb, :], in_=view[:, r, :, :]),
                            reads=[vsc_res], writes=[Vn], nbytes=4 * nb * 32768)
                cp(s, "act", Vaug, Vaug[:, :, 0, 0:64], Vn, Vn[:, :, 0:64])
                cp(s, "act", Vaug, Vaug[:, :, 1, 64:128], Vn, Vn[:, :, 64:128])
                for b2 in range(int(os.environ.get("KB", "16")) if k2 > 1 else 0):
                    blocks = (2 * b2, 2 * b2 + 1)
                    n0 = blocks[0] % nb
                    PTh = []
                    for hh in range(2):
                        hs = slice(hh * 64, (hh + 1) * 64)
                        pS = psS[(2 * bi_ctr + hh) % 4]
                        pe_ = pes[(2 * bi_ctr + hh) % 4]
                        PT = PTs[(2 * bi_ctr + hh) % 4]
                        PTh.append(PT)
                        pS4 = pS[:, :].rearrange("p (b c q) -> p b c q", b=2, c=2)
                        pe4 = pe_[:, :].rearrange("p (b c q) -> p b c q", b=2, c=2)
                        for bi, b in enumerate(blocks):
                            r, n = divmod(b, nb)
                            mm(s, pS, pS4[:, bi, 0, :], KT, KT[hs, tok(r, n)], QT, QT[hs, tok(r, n)], True, True)
                            if n > 0:
                                mm(s, pS, pS4[:, bi, 1, :], KT, KT[hs, tok(r, n - 1)], QT, QT[hs, tok(r, n)], True, True)
                        if n0 > 0:
                            act(s, pe_, pe_[:, :], pS, pS[:, :], AF.Exp, scale=0.125)
                            tt(s, "pool", PT, PT[:, :, :, :], pe_, pe4, cx.maskA, cx.maskA[:, :, :, :], ALU.mult)
                        else:
                            act(s, pe_, pe4[:, 0, 0, :], pS, pS4[:, 0, 0, :], AF.Exp, scale=0.125)
                            act(s, pe_, pe4[:, 1, :, :], pS, pS4[:, 1, :, :], AF.Exp, scale=0.125)
                            tt(s, "pool", PT, PT[:, 0, 0, :], pe_, pe4[:, 0, 0, :], cx.maskA, cx.maskA[:, 0, 0, :], ALU.mult)
                            tt(s, "pool", PT, PT[:, 1, :, :], pe_, pe4[:, 1, :, :], cx.maskA, cx.maskA[:, 1, :, :], ALU.mult)
                    bi_ctr += 1
                    if k2 < 3:
                        continue
                    for bi, b in enumerate(blocks):
                        r, n = divmod(b, nb)
                        pO = psO[b % 2]
                        for hh in range(2):
                            PT = PTh[hh]
                            mm(s, pO, pO[:, hh, :], Vaug, Vaug[:, b, hh, :], PT, PT[:, bi, 0, :], True, n == 0)
                            if n > 0:
                                mm(s, pO, pO[:, hh, :], Vaug, Vaug[:, b - 1, hh, :], PT, PT[:, bi, 1, :], False, True)
                        if k2 < 4:
                            continue
                        if di == 0:
                            cp(s, "dve", acc, acc[:, :, tok(r, n)], pO, pO[:, :, :])
                        else:
                            tt(s, "dve", acc, acc[:, :, tok(r, n)], acc, acc[:, :, tok(r, n)], pO, pO[:, :, :], ALU.add)
            if k2 < 5:
                continue
            for tg in range(8):
                tsl = slice(tg * 512, (tg + 1) * 512)
                pd = psV[tg % 2]
                rec = recs[tg % 2]
                mm(s, pd, pd[:, :], cx.swlo, cx.swlo[:, :], acc, acc[:, 1, tsl], True, False)
                mm(s, pd, pd[:, :], cx.swhi, cx.swhi[:, :], acc, acc[:, 0, tsl], False, True)
                s.op("dve", lambda e, rec=rec, pd=pd: e.reciprocal(out=rec[:, :], in_=pd[:, :]), reads=[pd], writes=[rec], cost=500)
                tt(s, "pool", obuf, obuf[0:64, tsl], acc, acc[0:64, 0, tsl], rec, rec[0:64, :], ALU.mult)
                tt(s, "pool", obuf, obuf[64:128, tsl], acc, acc[64:128, 1, tsl], rec, rec[64:128, :], ALU.mult)
            st(s, "sp", attT_res, attT[hp * 128:(hp + 1) * 128, :], obuf, obuf[:, :])
        s.phase_es = old
    s.end_phase()

    if stop < 3:
        return
    s.begin_phase()
    proj_ln(s, cx, dr, attT_res, attT, 8, w_out, h_in, h_out, dr["ln1_g"][l], dr["ln1_b"][l])
    s.end_phase()


def s_sub_barrier(s):
    es = s.phase_es
    s.phase_es = ExitStack()
    s.end_phase()
    s.phase_es = es


def proj_ln(s, cx, dr, aT_res, aT, nchunk, w_out, h_in, h_out, g_row, b_row):
    h_in_res, h_in_ap = h_in[0], h_in[1]
    h_out_res, h_out_ap, final = h_out
    wres = dr["wres"]
    wo = s.sb([128, nchunk, 1024], BF16, "wo")
    for c in range(nchunk):
        wload(s, cx, wo, wo[:, c, :], wres, w_out[c * 128:(c + 1) * 128, :], [1024])
    g_bc = s.sb([128, 1024], F32, "gbc")
    b_bc = s.sb([128, 1024], F32, "bbc")
    ld(s, "sp", g_bc, g_bc[:, :], wres, bcast_row(g_row, 1024))
    ld(s, "sp", b_bc, b_bc[:, :], wres, bcast_row(b_row, 1024))
    ats = [s.sb([128, nchunk, 512], BF16, "at") for _ in range(2)]
    hfs = [s.sb([128, 1024], F32, "hf2") for _ in range(2)]
    hos = [s.sb([128, 1024], F32, "ho") for _ in range(2)]
    psY = [[s.ps([128, 512], F32, "psY") for _ in range(2)] for _ in range(2)]
    wk = ln_work(s)
    aTv = aT[0:nchunk * 128, :].rearrange("(c p) t -> p c t", p=128)
    for tg in range(8):
        at = ats[tg % 2]
        ld(s, "sp", at, at[:, :, :], aT_res, aTv[:, :, tg * 512:(tg + 1) * 512])
        for ti in range(4):
            i = tg * 4 + ti
            hf = hfs[i % 2]
            ho = hos[i % 2]
            py = psY[i % 2]
            ld(s, "sp", hf, hf[:, :], h_in_res, h_in_ap[i * 128:(i + 1) * 128, :])
            for half in range(2):
                for c in range(nchunk):
                    mm(s, py[half], py[half][:, :], at, at[:, c, ti * 128:(ti + 1) * 128], wo,
                       wo[:, c, half * 512:(half + 1) * 512], c == 0, c == nchunk - 1)
            ln_epilogue(s, cx, py, hf, g_bc, b_bc, wk, ho)
            st(s, "pool", h_out_res, h_out_ap[i * 128:(i + 1) * 128, :], ho, ho[:, :], final=final)


def decay_tables(s, cx, LF, ig, nh, kscale_ln, EA, EB, ET, psX, DEC=None):
    n = NT * nh
    Fin = s.sb([128, NT, nh], F32, "Fin")
    tot = s.sb([128, NT, nh], F32, "tot")
    inc = s.sb([128, NT, nh], F32, "inc")
    LF2 = LF[:, :, :].rearrange("p a b -> p (a b)")
    mm(s, psX, psX[:, 0:n], cx.tri_le, cx.tri_le[:, :], LF, LF2, True, True)
    cp(s, "dve", Fin, Fin[:, :, :].rearrange("p a b -> p (a b)"), psX, psX[:, 0:n])
    mm(s, psX, psX[:, 0:n], cx.ones_f, cx.ones_f[:, :], LF, LF2, True, True)
    cp(s, "dve", tot, tot[:, :, :].rearrange("p a b -> p (a b)"), psX, psX[:, 0:n])
    for h in range(nh):
        s.op("dve", lambda e, h=h: e.tensor_tensor_scan(out=inc[:, :, h], data0=cx.ones_f[:, 0:NT], data1=tot[:, :, h],
                                                        initial=0.0, op0=ALU.mult, op1=ALU.add),
             reads=[cx.ones_f, tot], writes=[inc])
    tt(s, "dve", EB, EB[:, :, :], inc, inc[:, :, :], tot, tot[:, :, :], ALU.subtract)
    tt(s, "dve", EA, EA[:, :, :], Fin, Fin[:, :, :], EB, EB[:, :, :], ALU.add)
    if ig is not None:
        tt(s, "dve", EA, EA[:, :, :], ig[0], ig[1], EA, EA[:, :, :], ALU.subtract)
        ts(s, "dve", EA, EA[:, :, :], EA, EA[:, :, :], kscale_ln, None, ALU.add)
    else:
        ts(s, "dve", EA, EA[:, :, :], EA, EA[:, :, :], -1.0, kscale_ln, ALU.mult, ALU.add)
    act(s, ET, ET[:, :, :], Fin, Fin[:, :, :], AF.Exp)
    if DEC is not None:
        act(s, DEC, DEC[:, :, :], tot, tot[:, :, :], AF.Exp)


def make_Ediag(s, Ediag, Enext, EA, EB, h, tmp2):
    tt(s, "dve", tmp2, tmp2[:, :], EA, EA[:, :, h], EB, EB[:, :, h], ALU.add)
    ts(s, "dve", tmp2, tmp2[:, :], tmp2, tmp2[:, :], 80.0, None, ALU.min)
    act(s, Ediag, Ediag[:, :], tmp2, tmp2[:, :], AF.Exp)
    tt(s, "dve", tmp2, tmp2[:, 0:NT - 1], EA, EA[:, 0:NT - 1, h], EB, EB[:, 1:NT, h], ALU.add)
    ts(s, "dve", tmp2, tmp2[:, 0:NT - 1], tmp2, tmp2[:, 0:NT - 1], 80.0, None, ALU.min)
    act(s, Enext, Enext[:, 0:NT - 1], tmp2, tmp2[:, 0:NT - 1], AF.Exp)


def make_E(s, E, EA, EB, h, tmpE):
    tt(s, "pool", tmpE, tmpE[:, :, :], EA, EA[:, :, h].unsqueeze(2).to_broadcast([128, NT, NT]),
       EB, EB[:, :, h].unsqueeze(1).to_broadcast([128, NT, NT]), ALU.add)
    ts(s, "pool", tmpE, tmpE[:, :, :], tmpE, tmpE[:, :, :], 80.0, None, ALU.min)
    act(s, E, E[:, :, :], tmpE, tmpE[:, :, :], AF.Exp)


def decay_main(s, cx, heads, psS, psAcc, PTs, fin_cb, jmax=NT):
    cnt = [0] * len(heads)
    for j in range(jmax):
        for hi, hd in enumerate(heads):
            acc = psAcc[hi]
            dvw = hd["dvw"]
            nk = len(hd["kparts"])
            for i0 in range(0, j + 1, 4):
                i1 = min(i0 + 4, j + 1)
                n = i1 - i0
                pS = psS[hi][cnt[hi] % 2]
                PT = PTs[hi][cnt[hi] % 2]
                cnt[hi] += 1
                for ii in range(n):
                    i = i0 + ii
                    for kc, (Kt, Kap, Qt, Qap) in enumerate(hd["kparts"]):
                        mm(s, pS, pS[:, ii * 128:(ii + 1) * 128], Kt, Kap(i), Qt, Qap(j), kc == 0, kc == nk - 1)
                E = hd["E"]
                tt(s, "dve", PT, PT[:, 0:n, :], pS, pS[:, 0:n * 128].rearrange("p (a t) -> p a t", a=n),
                   E, E[:, i0:i1, j].unsqueeze(2).to_broadcast([128, n, 128]), ALU.mult)
                if i1 - 1 == j:
                    tt(s, "pool", PT, PT[:, n - 1, :], PT, PT[:, n - 1, :], cx.tri_b, cx.tri_b[:, :], ALU.mult)
                for ii in range(n):
                    i = i0 + ii
                    mm(s, acc, acc[:, 0:dvw], PT, PT[:, ii, :], hd["V"], hd["Vap"](i), i == 0, i == j)
            fin_cb(hi, hd, j, acc)


def small_norm(s, x_t, x_ap, width, wk, out_t, out_ap, g_t, g_ap):
    stats, mv, sd, rstd, nmr, xn = wk
    s.op("dve", lambda e: e.bn_stats(out=stats[:, :], in_=x_ap), reads=[x_t], writes=[stats])
    s.op("dve", lambda e: e.bn_aggr(out=mv[:, :], in_=stats[:, :]), reads=[stats], writes=[mv])
    ts(s, "dve", sd, sd[:, :], mv, mv[:, 1:2], LN_EPS, None, ALU.add)
    act(s, sd, sd[:, :], sd, sd[:, :], AF.Sqrt)
    s.op("dve", lambda e: e.reciprocal(out=rstd[:, :], in_=sd[:, :]), reads=[sd], writes=[rstd])
    stt(s, "dve", nmr, nmr[:, :], mv, mv[:, 0:1], -1.0, rstd, rstd[:, :], ALU.mult, ALU.mult)
    act(s, xn, xn[:, 0:width], x_t, x_ap, AF.Identity, scale=rstd[:, 0:1], bias=nmr[:, 0:1], extra_reads=[rstd, nmr])
    tt(s, "pool", out_t, out_ap, xn, xn[:, 0:width], g_t, g_ap, ALU.mult)


def small_norm_work(s, width):
    return (s.sb([128, 6], F32, "snst"), s.sb([128, 2], F32, "snmv"), s.sb([128, 1], F32, "snsd"),
            s.sb([128, 1], F32, "snrs"), s.sb([128, 1], F32, "snnm"), s.sb([128, width], F32, "snxn"))


def gate_proj_ln(s, cx, dr, xT, HN_res, HN, F, w_gate_ap, gfunc, w_out, h_in, h_out, g_row, b_row):
    h_in_res, h_in_ap = h_in[0], h_in[1]
    h_out_res, h_out_ap, final = h_out
    wres = dr["wres"]
    nch = F // 128
    nb = F // 512
    wg = s.sb([128, 8, F], BF16, "wgate")
    for c in range(8):
        for q in range(F // 1024):
            wload(s, cx, wg, wg[:, c, q * 1024:(q + 1) * 1024], wres, w_gate_ap[c * 128:(c + 1) * 128, q * 1024:(q + 1) * 1024], [1024])
    wo = s.sb([128, nch, 1024], BF16, "wo")
    for c in range(nch):
        wload(s, cx, wo, wo[:, c, :], wres, w_out[c * 128:(c + 1) * 128, :], [1024])
    g_bc = s.sb([128, 1024], F32, "gbc")
    b_bc = s.sb([128, 1024], F32, "bbc")
    ld(s, "sp", g_bc, g_bc[:, :], wres, bcast_row(g_row, 1024))
    ld(s, "sp", b_bc, b_bc[:, :], wres, bcast_row(b_row, 1024))
    nbuf = 1 if F > 1024 else 2
    hns = [s.sb([128, F], F32, "hn") for _ in range(nbuf)]
    sig = s.sb([128, F], F32, "sig")
    Gb = s.sb([128, F], BF16, "Gb")
    GT_ = s.sb([128, nch, 128], BF16, "GTt")
    hfs = [s.sb([128, 1024], F32, "hf2") for _ in range(nbuf)]
    hos = [s.sb([128, 1024], F32, "ho") for _ in range(nbuf)]
    psg = [s.ps([128, 512], F32, "psg") for _ in range(2)]
    pst = [s.ps([128, 1024], BF16, "pst") for _ in range(2)]
    psY = [s.ps([128, 512], F32, "psY") for _ in range(2)]
    wk = ln_work(s)
    for i in range(NT):
        hn = hns[i % nbuf]
        hf = hfs[i % nbuf]
        ho = hos[i % nbuf]
        ld(s, "sp", hn, hn[:, :], HN_res, HN[i * 128:(i + 1) * 128, 0:F])
        ld(s, "sp", hf, hf[:, :], h_in_res, h_in_ap[i * 128:(i + 1) * 128, :])
        for q in range(nb):
            pg = psg[q % 2]
            for c in range(8):
                mm(s, pg, pg[:, :], xT, xT[:, c, i * 128:(i + 1) * 128], wg, wg[:, c, q * 512:(q + 1) * 512], c == 0, c == 7)
            act(s, sig, sig[:, q * 512:(q + 1) * 512], pg, pg[:, :], gfunc)
        tt(s, "pool", Gb, Gb[:, :], hn, hn[:, :], sig, sig[:, :], ALU.mult)
        for q in range(nch // 8):
            pt = pst[q % 2]
            for c8 in range(8):
                c = q * 8 + c8
                s.op("pe", lambda e, pt=pt, c8=c8, c=c: e.transpose(out=pt[:, c8 * 128:(c8 + 1) * 128], in_=Gb[:, c * 128:(c + 1) * 128],
                                                                   identity=cx.ident_b[:, :]), reads=[Gb, cx.ident_b], writes=[pt])
            cp(s, "act" if q else "dve", GT_, GT_[:, q * 8:(q + 1) * 8, :], pt, pt[:, :].rearrange("p (c t) -> p c t", c=8))
        for half in range(2):
            for c in range(nch):
                mm(s, psY[half], psY[half][:, :], GT_, GT_[:, c, :], wo, wo[:, c, half * 512:(half + 1) * 512], c == 0, c == nch - 1)
        ln_epilogue(s, cx, psY, hf, g_bc, b_bc, wk, ho)
        st(s, "pool", h_out_res, h_out_ap[i * 128:(i + 1) * 128, :], ho, ho[:, :], final=final)


def phase_B(s, cx, dr, l, j_, h_in, h_out):
    import os
    nc = s.nc
    h_in_res, h_in_ap = h_in[0], h_in[1]
    wres = dr["wres"]
    w_in = dr["b_w_in"][j_]
    w_in_v = w_in.rearrange("(c p) n -> p c n", p=128)
    HN_res, HN = dr["hn"]
    bstop = int(os.environ.get("BSTOP", "9"))
    jmax = int(os.environ.get("BJMAX", str(NT)))
    npair = int(os.environ.get("BPAIRS", "4"))

    s.begin_phase()
    xT = s.sb([128, 8, S], BF16, "xT")
    GATES = s.sb([128, NT, 16], F32, "GATES")
    with ExitStack() as es0:
        old = s.phase_es
        s.phase_es = es0
        Wg32 = s.sb([128, 8, 16], F32, "Wg32")
        ld(s, "sp", Wg32, Wg32[:, :, :], wres, w_in_v[:, :, 3072:3088])
        gb = s.sb([128, 16], F32, "gbias")
        ld(s, "sp", gb, gb[:, :], wres, bcast_row(dr["b_gate_bias"][j_], 16))
        hfs = [s.sb([128, 1024], F32, "hf") for _ in range(3)]
        xTts = [s.sb([128, 8, 128], F32, "xTt") for _ in range(2)]
        pts = [s.ps([128, 512], F32, "ptr") for _ in range(2)]
        psg = s.ps([128, 512], F32, "psgate")
        for i in range(NT):
            hf = hfs[i % 3]
            xTt = xTts[i % 2]
            ld(s, "sp", hf, hf[:, :], h_in_res, h_in_ap[i * 128:(i + 1) * 128, :])
            for half in range(2):
                pt = pts[half]
                for c4 in range(4):
                    c = half * 4 + c4
                    s.op("pe", lambda e, pt=pt, c4=c4, c=c, hf=hf: e.transpose(
                        out=pt[:, c4 * 128:(c4 + 1) * 128], in_=hf[:, c * 128:(c + 1) * 128], identity=cx.ident_f[:, :]),
                        reads=[hf, cx.ident_f], writes=[pt])
                src = pt[:, :].rearrange("p (c t) -> p c t", c=4)
                cp(s, "act", xT, xT[:, half * 4:(half + 1) * 4, i * 128:(i + 1) * 128], pt, src)
                cp(s, "dve", xTt, xTt[:, half * 4:(half + 1) * 4, :], pt, src)
            for c in range(8):
                mm(s, psg, psg[:, 0:16], xTt, xTt[:, c, :], Wg32, Wg32[:, c, :], c == 0, c == 7)
            tt(s, "dve", GATES, GATES[:, i, :], psg, psg[:, 0:16], gb, gb[:, :], ALU.add)
        s.phase_es = old
    s_sub_barrier(s)

    EA = s.sb([128, NT, 8], F32, "EA")
    EB = s.sb([128, NT, 8], F32, "EB")
    ET = s.sb([128, NT, 8], F32, "ET")
    DEC = s.sb([128, NT, 8], F32, "DEC")
    with ExitStack() as es1:
        old = s.phase_es
        s.phase_es = es1
        LF = s.sb([128, NT, 8], F32, "LF")
        psX = s.ps([128, 512], F32, "psX")
        act(s, LF, LF[:, :, :], GATES, GATES[:, :, 8:16], AF.Exp, scale=-1.0)
        ts(s, "dve", LF, LF[:, :, :], LF, LF[:, :, :], 1.0, None, ALU.add)
        act(s, LF, LF[:, :, :], LF, LF[:, :, :], AF.Ln)
        ts(s, "dve", LF, LF[:, :, :], LF, LF[:, :, :], -1.0, None, ALU.mult)
        decay_tables(s, cx, LF, (GATES, GATES[:, :, 0:8]), 8, math.log(0.125), EA, EB, ET, psX, DEC)
        s.phase_es = old
    s_sub_barrier(s)
    if bstop < 2:
        s.end_phase()
        return

    with ExitStack() as es2:
        old = s.phase_es
        s.phase_es = es2
        ng_bc = s.sb([128, 1024], F32, "ngbc")
        ld(s, "sp", ng_bc, ng_bc[:, :], wres, bcast_row(dr["b_norm_g"][j_], 1024))
        wq = s.sb([128, 8, 128], BF16, "wq")
        wkk = s.sb([128, 8, 128], BF16, "wk")
        wv = s.sb([128, 8, 256], BF16, "wv")
        cw = [s.sb([128, 4], F32, "cw") for _ in range(2)]
        cb = [s.sb([128, 1], F32, "cb") for _ in range(2)]
        UT = s.sb([128, S + 3], F32, "UT")
        s.op("pool", lambda e: e.memset(UT[:, 0:3], 0.0), writes=[UT])
        cacc = s.sb([128, S], F32, "cacc")
        QT = s.sb([128, S], BF16, "QT")
        KT = s.sb([128, S], BF16, "KT")
        Vaug = s.sb([128, NT, 2, 129], BF16, "Vaug")
        s.op("pool", lambda e: e.memset(Vaug[:, :, :, 128:129], 1.0), writes=[Vaug])
        Edg = [s.sb([128, NT], F32, "Edg") for _ in range(2)]
        Enx = [s.sb([128, NT], F32, "Enx") for _ in range(2)]
        tmp2 = s.sb([128, NT], F32, "tmp2")
        Dp = s.sb([128, NT], F32, "Dp")
        R32 = s.sb([128, 129], F32, "R32")
        Rb = s.sb([128, 129], BF16, "Rb")
        Ktss = [s.sb([128, 128], BF16, "Kts") for _ in range(2)]
        psX = [s.ps([128, 512], F32, "psX") for _ in range(2)]
        psS = [s.ps([128, 512], F32, "psS") for _ in range(2)]
        psAcc = [s.ps([128, 512], F32, "psAcc") for _ in range(2)]
        psT = s.ps([128, 1024], BF16, "psTk")
        psKV = s.ps([128, 512], F32, "psKV")
        PTs = [[s.sb([128, 1, 128], BF16, "PT") for _ in range(2)] for _ in range(2)]
        snw = small_norm_work(s, 128)
        sm = {k: s.sb([128, 1], F32, k) for k in ("ed", "ad", "rec", "fac")}
        hnr = s.sb([128, 128], F32, "hnr")
        HNt = [s.sb([128, 256], F32, "HNt") for _ in range(2)]
        for hp in range(npair):
            for w, (wt, dst) in enumerate(((wq, QT), (wkk, KT))):
                col = w * 512 + hp * 128
                wload(s, cx, wt, wt[:, :, :], wres, w_in_v[:, :, col:col + 128], [8, 128])
                for jj in range(4):
                    ld(s, "sp", cw[w], cw[w][:, jj:jj + 1], wres, dr["b_conv_w"][j_][jj, col:col + 128].rearrange("(c o) -> c o", o=1))
                ld(s, "sp", cb[w], cb[w][:, :], wres, dr["b_conv_b"][j_][col:col + 128].rearrange("(c o) -> c o", o=1))
                for tg in range(8):
                    px = psX[tg % 2]
                    for c in range(8):
                        mm(s, px, px[:, :], wt, wt[:, c, :], xT, xT[:, c, tg * 512:(tg + 1) * 512], c == 0, c == 7)
                    cp(s, "act", UT, UT[:, 3 + tg * 512:3 + (tg + 1) * 512], px, px[:, :])
                ts(s, "dve", cacc, cacc[:, :], UT, UT[:, 0:S], cw[w][:, 0:1], None, ALU.mult, extra_reads=[cw[w]])
                for jj in range(1, 4):
                    stt(s, "dve", cacc, cacc[:, :], UT, UT[:, jj:S + jj], cw[w][:, jj:jj + 1], cacc, cacc[:, :], ALU.mult, ALU.add,
                        extra_reads=[cw[w]])
                act(s, dst, dst[:, :], cacc, cacc[:, :], AF.Silu, scale=1.0, bias=cb[w][:, 0:1], extra_reads=[cb[w]])
            wload(s, cx, wv, wv[:, :, :], wres, w_in_v[:, :, 1024 + hp * 256:1024 + (hp + 1) * 256], [8, 256])
            for i in range(NT):
                px = psX[i % 2]
                for c in range(8):
                    mm(s, px, px[:, 0:256], xT, xT[:, c, i * 128:(i + 1) * 128], wv, wv[:, c, :], c == 0, c == 7)
                cp(s, "act", Vaug, Vaug[:, i, :, 0:128], px, px[:, 0:256].rearrange("p (a d) -> p a d", a=2))
            heads = []
            for hh in range(2):
                h = hp * 2 + hh
                make_Ediag(s, Edg[hh], Enx[hh], EA, EB, h, tmp2)
                hs = slice(hh * 64, (hh + 1) * 64)
                cp(s, "pool", Dp, Dp[hs, :], DEC, DEC[hs, :, h])
                heads.append(dict(h=h, hh=hh))
            s.op("pool", lambda e: e.memset(R32[:, :], 0.0), writes=[R32])
            s.op("pool", lambda e: e.memset(Rb[:, :], 0.0), writes=[Rb])

            def fin(hi, hd, j, acc):
                h, hh = hd["h"], hd["hh"]
                tt(s, "dve", sm["ed"], sm["ed"][:, :], acc, acc[:, 128:129], ET, ET[:, j, h:h + 1], ALU.mult)
                stt(s, "dve", sm["ad"], sm["ad"][:, :], sm["ed"], sm["ed"][:, :], -1.0, sm["ed"], sm["ed"][:, :], ALU.mult, ALU.max)
                ts(s, "dve", sm["ad"], sm["ad"][:, :], sm["ad"], sm["ad"][:, :], 1.0, None, ALU.max)
                s.op("dve", lambda e: e.reciprocal(out=sm["rec"][:, :], in_=sm["ad"][:, :]), reads=[sm["ad"]], writes=[sm["rec"]])
                tt(s, "dve", sm["fac"], sm["fac"][:, :], sm["rec"], sm["rec"][:, :], ET, ET[:, j, h:h + 1], ALU.mult)
                act(s, hnr, hnr[:, :], acc, acc[:, 0:128], AF.Copy, scale=sm["fac"][:, 0:1], extra_reads=[sm["fac"]])
                ht = HNt[j % 2]
                small_norm(s, hnr, hnr[:, :], 128, snw, ht, ht[:, hh * 128:(hh + 1) * 128], ng_bc, ng_bc[:, h * 128:(h + 1) * 128])
                if hh == 1:
                    st(s, "pool", HN_res, HN[j * 128:(j + 1) * 128, hp * 256:(hp + 1) * 256], ht, ht[:, :])
            for j in range(jmax):
                tj = slice(j * 128, (j + 1) * 128)
                for hh in range(2):
                    hs = slice(hh * 64, (hh + 1) * 64)
                    pS = psS[hh]
                    PT = PTs[hh][j % 2]
                    acc = psAcc[hh]
                    mm(s, pS, pS[:, 0:128], KT, KT[hs, tj], QT, QT[hs, tj], True, True)
                    ts(s, "dve", PT, PT[:, 0, :], pS, pS[:, 0:128], Edg[hh][:, j:j + 1], None, ALU.mult, extra_reads=[Edg[hh]])
                    tt(s, "pool", PT, PT[:, 0, :], PT, PT[:, 0, :], cx.tri_b, cx.tri_b[:, :], ALU.mult)
                    mm(s, acc, acc[:, 0:129], PT, PT[:, 0, :], Vaug, Vaug[:, j, hh, :], True, j == 0)
                    if j > 0:
                        mm(s, acc, acc[:, 0:129], QT, QT[hs, tj], Rb, Rb[hs, :], False, True)
                    fin(hh, heads[hh], j, acc)
                if j < jmax - 1:
                    s.op("pe", lambda e, tj=tj: e.transpose(out=psT[:, 0:128], in_=KT[:, tj], identity=cx.ident_b[:, :]),
                         reads=[KT, cx.ident_b], writes=[psT], cost=150)
                    Kts = Ktss[j % 2]
                    for hh in range(2):
                        hs = slice(hh * 64, (hh + 1) * 64)
                        act(s, Kts, Kts[:, hs], psT, psT[:, hs], AF.Copy, scale=Enx[hh][:, j:j + 1], extra_reads=[Enx[hh]])
                    for hh in range(2):
                        hs = slice(hh * 64, (hh + 1) * 64)
                        mm(s, psKV, psKV[hs, 0:129], Kts, Kts[:, hs], Vaug, Vaug[:, j, hh, :], True, True)
                    stt(s, "dve", R32, R32[:, :], R32, R32[:, :], Dp[:, j:j + 1], psKV, psKV[:, 0:129], ALU.mult, ALU.add, extra_reads=[Dp])
                    cp(s, "act", Rb, Rb[:, :], R32, R32[:, :])
        s.phase_es = old
    s_sub_barrier(s)
    if bstop < 3:
        s.end_phase()
        return

    with ExitStack() as es3:
        old = s.phase_es
        s.phase_es = es3
        gate_proj_ln(s, cx, dr, xT, HN_res, HN, 1024, w_in[:, 2048:3072], AF.Sigmoid, dr["b_w_out"][j_], h_in, h_out,
                     dr["ln1_g"][l], dr["ln1_b"][l])
        s.phase_es = old
    s.end_phase()


def phase_C(s, cx, dr, l, j_, h_in, h_out):
    import os
    nc = s.nc
    h_in_res, h_in_ap = h_in[0], h_in[1]
    wres = dr["wres"]
    w_in = dr["c_w_in"][j_]
    w_in_v = w_in.rearrange("(c p) n -> p c n", p=128)
    HN_res, HN = dr["hn"]
    qkT_res, qkT = dr["qkT"]
    cstop = int(os.environ.get("CSTOP", "9"))
    jmax = int(os.environ.get("CJMAX", str(NT)))
    nheads = int(os.environ.get("CHEADS", "4"))

    s.begin_phase()
    xT = s.sb([128, 8, S], BF16, "xT")
    load_xT(s, cx, h_in_res, h_in_ap, xT)
    EA = s.sb([128, NT, 4], F32, "EA")
    EB = s.sb([128, NT, 4], F32, "EB")
    ET = s.sb([128, NT, 4], F32, "ET")
    DEC = s.sb([128, NT, 4], F32, "DEC")
    with ExitStack() as es1:
        old = s.phase_es
        s.phase_es = es1
        LF = s.sb([128, NT, 4], F32, "LF")
        psX = s.ps([128, 512], F32, "psX")
        for h in range(4):
            s.op("pool", lambda e, h=h: e.memset(LF[:, :, h:h + 1], math.log(1.0 - 2.0 ** (-5.0 - h))), writes=[LF])
        decay_tables(s, cx, LF, None, 4, math.log(1.0 / 16.0), EA, EB, ET, psX, DEC)
        s.phase_es = old
    s_sub_barrier(s)

    with ExitStack() as es1:
        old = s.phase_es
        s.phase_es = es1
        wqk = s.sb([128, 8, 2048], BF16, "wqk")
        for c in range(8):
            wload(s, cx, wqk, wqk[:, c, :], wres, w_in[c * 128:(c + 1) * 128, 0:2048], [2048])
        rc = s.sb([128, 4], F32, "ropec")
        ld(s, "sp", rc, rc[:, :], wres, dr["rope_consts"])
        posi = s.sb([128, 512], I32, "posi")
        posf = s.sb([128, 512], F32, "posf")
        ang = s.sb([128, 512], F32, "ang")
        ang2 = s.sb([128, 512], F32, "ang2")
        Ct = s.sb([128, 512], F32, "Ct")
        St = s.sb([128, 512], F32, "St")
        scr = (s.sb([128, 512], I32, "ki"), s.sb([128, 512], F32, "kf"), s.sb([128, 512], F32, "rr"))
        psa = [s.ps([128, 512], F32, "psa") for _ in range(2)]
        psb = [s.ps([128, 512], F32, "psb") for _ in range(2)]
        t1s = [s.sb([128, 512], F32, "t1") for _ in range(2)]
        t2s = [s.sb([128, 512], F32, "t2") for _ in range(2)]
        oas = [s.sb([128, 512], BF16, "oa") for _ in range(2)]
        obs = [s.sb([128, 512], BF16, "ob") for _ in range(2)]
        it = 0
        for tg in range(8):
            tsl = slice(tg * 512, (tg + 1) * 512)
            ld(s, "sp", posi, posi[:, :], wres, bcast_row(dr["positions"][tg * 512:(tg + 1) * 512], 512))
            cp(s, "dve", posf, posf[:, :], posi, posi[:, :])
            ts(s, "dve", ang, ang[:, :], posf, posf[:, :], rc[:, 2:3], None, ALU.mult, extra_reads=[rc])
            ts(s, "dve", ang2, ang2[:, :], ang, ang[:, :], math.pi / 2, None, ALU.add)
            sin_table(s, St, St[:, :], ang, ang[:, :], None, None, None, scr)
            sin_table(s, Ct, Ct[:, :], ang2, ang2[:, :], None, None, None, scr)
            for h in range(4):
                for w in range(2):
                    pa, pb = psa[it % 2], psb[it % 2]
                    t1, t2, oa, ob = t1s[it % 2], t2s[it % 2], oas[it % 2], obs[it % 2]
                    it += 1
                    col = w * 1024 + h * 256
                    for c in range(8):
                        mm(s, pa, pa[:, :], wqk, wqk[:, c, col:col + 128], xT, xT[:, c, tsl], c == 0, c == 7)
                    for c in range(8):
                        mm(s, pb, pb[:, :], wqk, wqk[:, c, col + 128:col + 256], xT, xT[:, c, tsl], c == 0, c == 7)
                    tt(s, "dve", t1, t1[:, :], pa, pa[:, :], Ct, Ct[:, :], ALU.mult)
                    tt(s, "dve", t2, t2[:, :], pb, pb[:, :], St, St[:, :], ALU.mult)
                    tt(s, "pool", oa, oa[:, :], t1, t1[:, :], t2, t2[:, :], ALU.subtract)
                    st(s, "pool", qkT_res, qkT[w, h * 256:h * 256 + 128, tsl], oa, oa[:, :])
                    tt(s, "dve", t1, t1[:, :], pb, pb[:, :], Ct, Ct[:, :], ALU.mult)
                    tt(s, "dve", t2, t2[:, :], pa, pa[:, :], St, St[:, :], ALU.mult)
                    tt(s, "pool", ob, ob[:, :], t1, t1[:, :], t2, t2[:, :], ALU.add)
                    st(s, "pool", qkT_res, qkT[w, h * 256 + 128:h * 256 + 256, tsl], ob, ob[:, :])
        s.phase_es = old
    s_sub_barrier(s)
    if cstop < 2:
        s.end_phase()
        return

    with ExitStack() as es2:
        old = s.phase_es
        s.phase_es = es2
        ng_bc = s.sb([128, 2048], F32, "ngbc")
        ld(s, "sp", ng_bc, ng_bc[:, :], wres, bcast_row(dr["c_norm_g"][j_], 2048))
        QA = s.sb([128, S], BF16, "QA")
        QB = s.sb([128, S], BF16, "QB")
        KA = s.sb([128, S], BF16, "KA")
        KB = s.sb([128, S], BF16, "KB")
        wv = s.sb([128, 8, 512], BF16, "wv")
        Vh = s.sb([128, NT, 512], BF16, "Vh")
        Edg = s.sb([128, NT], F32, "Edg")
        Enx = s.sb([128, NT], F32, "Enx")
        tmp2 = s.sb([128, NT], F32, "tmp2")
        R32 = s.sb([128, 2, 512], F32, "R32")
        Rb = s.sb([128, 2, 512], BF16, "Rb")
        Ktss = [s.sb([128, 256], BF16, "Kts") for _ in range(2)]
        psX = [s.ps([128, 512], F32, "psX") for _ in range(2)]
        psS = [s.ps([128, 512], F32, "psS") for _ in range(2)]
        psAcc = s.ps([128, 512], F32, "psAcc")
        psT = s.ps([128, 1024], BF16, "psTk")
        psKV = [s.ps([128, 512], F32, "psKV") for _ in range(2)]
        PTs = [s.sb([128, 128], BF16, "PT") for _ in range(2)]
        snw = small_norm_work(s, 512)
        onr = s.sb([128, 512], F32, "onr")
        HNt = [s.sb([128, 512], F32, "HNt") for _ in range(2)]
        for h in range(nheads):
            for w, (ta, tb) in enumerate(((QA, QB), (KA, KB))):
                ld(s, "sp", ta, ta[:, :], qkT_res, qkT[w, h * 256:h * 256 + 128, :])
                ld(s, "sp", tb, tb[:, :], qkT_res, qkT[w, h * 256 + 128:h * 256 + 256, :])
            wload(s, cx, wv, wv[:, 0:4, :], wres, w_in_v[:, 0:4, 2048 + h * 512:2048 + (h + 1) * 512], [4, 512])
            wload(s, cx, wv, wv[:, 4:8, :], wres, w_in_v[:, 4:8, 2048 + h * 512:2048 + (h + 1) * 512], [4, 512])
            for i in range(NT):
                px = psX[i % 2]
                for c in range(8):
                    mm(s, px, px[:, :], xT, xT[:, c, i * 128:(i + 1) * 128], wv, wv[:, c, :], c == 0, c == 7)
                cp(s, "act", Vh, Vh[:, i, :], px, px[:, :])
            make_Ediag(s, Edg, Enx, EA, EB, h, tmp2)
            s.op("pool", lambda e: e.memset(R32[:, :, :], 0.0), writes=[R32], cost=2000)
            s.op("pool", lambda e: e.memset(Rb[:, :, :], 0.0), writes=[Rb], cost=2000)

            def fin(hi, hd, j, acc, h=h):
                act(s, onr, onr[:, :], acc, acc[:, 0:512], AF.Copy, scale=ET[:, j, h:h + 1], extra_reads=[ET])
                ht = HNt[j % 2]
                small_norm(s, onr, onr[:, :], 512, snw, ht, ht[:, :], ng_bc, ng_bc[:, h * 512:(h + 1) * 512])
                st(s, "pool", HN_res, HN[j * 128:(j + 1) * 128, h * 512:(h + 1) * 512], ht, ht[:, :])
            for j in range(jmax):
                tj = slice(j * 128, (j + 1) * 128)
                pS = psS[j % 2]
                PT = PTs[j % 2]
                acc = psAcc
                mm(s, pS, pS[:, 0:128], KA, KA[:, tj], QA, QA[:, tj], True, False)
                mm(s, pS, pS[:, 0:128], KB, KB[:, tj], QB, QB[:, tj], False, True)
                ts(s, "dve", PT, PT[:, :], pS, pS[:, 0:128], Edg[:, j:j + 1], None, ALU.mult, extra_reads=[Edg])
                tt(s, "pool", PT, PT[:, :], PT, PT[:, :], cx.tri_b, cx.tri_b[:, :], ALU.mult)
                mm(s, acc, acc[:, :], PT, PT[:, :], Vh, Vh[:, j, :], True, j == 0)
                if j > 0:
                    mm(s, acc, acc[:, :], QA, QA[:, tj], Rb, Rb[:, 0, :], False, False)
                    mm(s, acc, acc[:, :], QB, QB[:, tj], Rb, Rb[:, 1, :], False, True)
                fin(0, None, j, acc)
                if j < jmax - 1:
                    s.op("pe", lambda e, tj=tj: e.transpose(out=psT[:, 0:128], in_=KA[:, tj], identity=cx.ident_b[:, :]),
                         reads=[KA, cx.ident_b], writes=[psT], cost=150)
                    s.op("pe", lambda e, tj=tj: e.transpose(out=psT[:, 128:256], in_=KB[:, tj], identity=cx.ident_b[:, :]),
                         reads=[KB, cx.ident_b], writes=[psT], cost=150)
                    Kts = Ktss[j % 2]
                    act(s, Kts, Kts[:, :], psT, psT[:, 0:256], AF.Copy, scale=Enx[:, j:j + 1], extra_reads=[Enx])
                    for ck in range(2):
                        mm(s, psKV[ck], psKV[ck][:, :], Kts, Kts[:, ck * 128:(ck + 1) * 128], Vh, Vh[:, j, :], True, True)
                        stt(s, "dve", R32, R32[:, ck, :], R32, R32[:, ck, :], DEC[:, j, h:h + 1], psKV[ck], psKV[ck][:, :],
                            ALU.mult, ALU.add, extra_reads=[DEC])
                    cp(s, "act", Rb, Rb[:, 0, :], R32, R32[:, 0, :])
                    cp(s, "pool", Rb, Rb[:, 1, :], R32, R32[:, 1, :])
        s.phase_es = old
    s_sub_barrier(s)
    if cstop < 3:
        s.end_phase()
        return

    with ExitStack() as es3:
        old = s.phase_es
        s.phase_es = es3
        gate_proj_ln(s, cx, dr, xT, HN_res, HN, 2048, w_in[:, 4096:6144], AF.Silu, dr["c_w_out"][j_], h_in, h_out,
                     dr["ln1_g"][l], dr["ln1_b"][l])
        s.phase_es = old
    s.end_phase()


NSLOT = 128
BIGIDX = 4 * 64 * 128


def phase_F(s, cx, dr, l, h_in, h_out):
    import os
    nc = s.nc
    h_in_res, h_in_ap = h_in[0], h_in[1]
    h_out_res, h_out_ap, final = h_out
    wres = dr["wres"]
    xbf_res, xbf = dr["xbf"]
    bt_res, bt = dr["buftok"]
    yb_res, yb = dr["yb"]
    fstop = int(os.environ.get("FSTOP", "9"))

    s.begin_phase()
    OH1 = s.sb([128, NT, 64], F32, "OH1")
    OH2 = s.sb([128, NT, 64], F32, "OH2")
    RK = s.sb([128, NT, 2], F32, "RK")
    GT = s.sb([128, NT, 2], F32, "GT")
    DESTi = s.sb([128, NT, 2], I32, "DESTi")
    IDXW = s.sb([128, NSLOT], I32, "IDXW")
    breg_x = nc.gpsimd.to_reg(S - 1)
    breg_w = nc.gpsimd.to_reg((l + 1) * 64 * 128 - 1)
    breg_y = nc.gpsimd.to_reg(NSLOT * 128 - 1)
    breg_t = nc.gpsimd.to_reg(NSLOT * 128 - 1)

    with ExitStack() as es1:
        old = s.phase_es
        s.phase_es = es1
        Wr = s.sb([128, 8, 72], F32, "Wr")
        ld(s, "sp", Wr, Wr[:, :, 0:8], wres, dr["r_group_w"][l].rearrange("(c p) n -> p c n", p=128))
        ld(s, "sp", Wr, Wr[:, :, 8:72], wres, dr["r_expert_w"][l].rearrange("(c p) n -> p c n", p=128))
        bias = s.sb([128, 72], F32, "rbias")
        ld(s, "sp", bias, bias[:, 0:8], wres, bcast_row(dr["r_group_b"][l], 8))
        ld(s, "sp", bias, bias[:, 8:72], wres, bcast_row(dr["r_expert_b"][l], 64))
        tri_lt = s.sb([128, 128], BF16, "trilt")
        ts(s, "dve", tri_lt, tri_lt[:, :], cx.iof, cx.iof[:, :], 0.0, None, ALU.is_gt)
        toki = s.sb([128, NT], I32, "toki")
        s.op("pool", lambda e: e.iota(toki[:, :], pattern=[[128, NT]], base=0, channel_multiplier=1), writes=[toki])
        hfs = [s.sb([128, 1024], F32, "hfr") for _ in range(2)]
        hbs = [s.sb([128, 1024], BF16, "hbr") for _ in range(2)]
        xTts = [s.sb([128, 8, 128], F32, "xTt") for _ in range(2)]
        pts = [s.ps([128, 512], F32, "ptr") for _ in range(2)]
        psL = s.ps([128, 512], F32, "psL")
        psPC = s.ps([128, 512], F32, "psPC")
        LG = s.sb([128, NT, 72], F32, "LG")
        for i in range(NT):
            hf = hfs[i % 2]
            hb = hbs[i % 2]
            xTt = xTts[i % 2]
            ld(s, "sp", hf, hf[:, :], h_in_res, h_in_ap[i * 128:(i + 1) * 128, :])
            cp(s, "pool", hb, hb[:, :], hf, hf[:, :])
            st(s, "pool", xbf_res, xbf[i * 128:(i + 1) * 128, :], hb, hb[:, :])
            for half in range(2):
                pt = pts[half]
                for c4 in range(4):
                    c = half * 4 + c4
                    s.op("pe", lambda e, pt=pt, c4=c4, c=c, hf=hf: e.transpose(
                        out=pt[:, c4 * 128:(c4 + 1) * 128], in_=hf[:, c * 128:(c + 1) * 128], identity=cx.ident_f[:, :]),
                        reads=[hf, cx.ident_f], writes=[pt])
                cp(s, "act" if half else "dve", xTt, xTt[:, half * 4:(half + 1) * 4, :], pt,
                   pt[:, :].rearrange("p (c t) -> p c t", c=4))
            for c in range(8):
                mm(s, psL, psL[:, 0:72], xTt, xTt[:, c, :], Wr, Wr[:, c, :], c == 0, c == 7)
            tt(s, "dve", LG, LG[:, i, :], psL, psL[:, 0:72], bias, bias[:, :], ALU.add)
        def t2(name, n=NT):
            return s.sb([128, n], F32, name)
        gmax, sumg, pgrp, v1, v2, dv, ex, den, rec = [t2(k) for k in ("gmax", "sumg", "pgrp", "v1", "v2", "dv", "ex", "den", "rec")]
        ohg = s.sb([128, NT, 8], F32, "ohg")
        eg = s.sb([128, NT, 8], F32, "eg")
        pen = s.sb([128, NT, 8], F32, "pen")
        em = s.sb([128, NT, 64], F32, "em")
        em2 = s.sb([128, NT, 64], F32, "em2")
        Mb = s.sb([128, NT, 64], BF16, "Mb")
        G = LG[:, :, 0:8]

        def bc2(t, w):
            return t[:, :].unsqueeze(2).to_broadcast([128, NT, w])
        s.op("dve", lambda e: e.reduce_max(out=gmax[:, :], in_=G, axis=AX.X), reads=[LG], writes=[gmax], cost=400)
        tt(s, "dve", ohg, ohg[:, :, :], LG, G, gmax, bc2(gmax, 8), ALU.is_equal)
        tt(s, "dve", eg, eg[:, :, :], LG, G, gmax, bc2(gmax, 8), ALU.subtract)
        act(s, eg, eg[:, :, :], eg, eg[:, :, :], AF.Exp)
        s.op("dve", lambda e: e.reduce_sum(out=sumg[:, :], in_=eg[:, :, :], axis=AX.X), reads=[eg], writes=[sumg], cost=400)
        s.op("dve", lambda e: e.reciprocal(out=pgrp[:, :], in_=sumg[:, :]), reads=[sumg], writes=[pgrp])
        ts(s, "dve", pen, pen[:, :, :], ohg, ohg[:, :, :], -1.0, 1e30, ALU.add, ALU.mult)
        tt(s, "dve", em, em[:, :, :].rearrange("p i (g j) -> p i g j", g=8), LG, LG[:, :, 8:72].rearrange("p i (g j) -> p i g j", g=8),
           pen, pen[:, :, :].unsqueeze(3).to_broadcast([128, NT, 8, 8]), ALU.add)
        s.op("dve", lambda e: e.reduce_max(out=v1[:, :], in_=em[:, :, :], axis=AX.X), reads=[em], writes=[v1], cost=2000)
        tt(s, "dve", OH1, OH1[:, :, :], em, em[:, :, :], v1, bc2(v1, 64), ALU.is_equal)
        stt(s, "dve", em2, em2[:, :, :], OH1, OH1[:, :, :], -1e30, em, em[:, :, :], ALU.mult, ALU.add)
        s.op("dve", lambda e: e.reduce_max(out=v2[:, :], in_=em2[:, :, :], axis=AX.X), reads=[em2], writes=[v2], cost=2000)
        tt(s, "dve", OH2, OH2[:, :, :], em2, em2[:, :, :], v2, bc2(v2, 64), ALU.is_equal)
        tt(s, "dve", dv, dv[:, :], v2, v2[:, :], v1, v1[:, :], ALU.subtract)
        act(s, ex, ex[:, :], dv, dv[:, :], AF.Exp)
        ts(s, "dve", den, den[:, :], ex, ex[:, :], 1.0, None, ALU.add)
        s.op("dve", lambda e: e.reciprocal(out=rec[:, :], in_=den[:, :]), reads=[den], writes=[rec])
        tt(s, "dve", GT, GT[:, :, 0], pgrp, pgrp[:, :], rec, rec[:, :], ALU.mult)
        tt(s, "dve", GT, GT[:, :, 1], GT, GT[:, :, 0], ex, ex[:, :], ALU.mult)
        tt(s, "dve", Mb, Mb[:, :, :], OH1, OH1[:, :, :], OH2, OH2[:, :, :], ALU.add)
        TOT = s.sb([128, NT, 64], F32, "TOT")
        TOT2 = s.sb([128, NT, 64], F32, "TOT2")
        pos = s.sb([128, NT, 64], F32, "pos")
        psP = [s.ps([128, 512], F32, "psP") for _ in range(2)]
        Mb2 = Mb[:, :, :].rearrange("p i e -> p (i e)")
        for q in range(4):
            pp = psP[q % 2]
            mm(s, pp, pp[:, :], cx.ones_b, cx.ones_b[:, :], Mb, Mb2[:, q * 512:(q + 1) * 512], True, True)
            cp(s, "act", TOT, TOT[:, q * 8:(q + 1) * 8, :], pp, pp[:, :].rearrange("p (i e) -> p i e", i=8))
        cur_, oth_ = TOT, TOT2
        for k in (1, 2, 4, 8, 16):
            cp(s, "pool", oth_, oth_[:, 0:k, :], cur_, cur_[:, 0:k, :])
            tt(s, "dve", oth_, oth_[:, k:NT, :], cur_, cur_[:, k:NT, :], cur_, cur_[:, 0:NT - k, :], ALU.add)
            cur_, oth_ = oth_, cur_
        incl = cur_
        for q in range(4):
            pp = psP[q % 2]
            for i8 in range(8):
                i = q * 8 + i8
                mm(s, pp, pp[:, i8 * 64:(i8 + 1) * 64], tri_lt, tri_lt[:, :], Mb, Mb[:, i, :], True, True)
            tt(s, "dve", pos, pos[:, q * 8:(q + 1) * 8, :], pp, pp[:, :].rearrange("p (i e) -> p i e", i=8),
               incl, incl[:, q * 8:(q + 1) * 8, :], ALU.add)
        own = s.sb([128, NT, 64], F32, "own")
        for q in range(4):
            pp = psP[q % 2]
            mm(s, pp, pp[:, :], cx.ones_b, cx.ones_b[:, :], Mb, Mb2[:, q * 512:(q + 1) * 512], True, True)
            cp(s, "act", own, own[:, q * 8:(q + 1) * 8, :], pp, pp[:, :].rearrange("p (i e) -> p i e", i=8))
        tt(s, "dve", pos, pos[:, :, :], pos, pos[:, :, :], own, own[:, :, :], ALU.subtract)
        cnt = s.sb([128, 64], F32, "cnt")
        cp(s, "dve", cnt, cnt[:, :], incl, incl[:, NT - 1, :])
        bigr = s.sb([128, NT, 64], F32, "bigr")
        for k, OH in enumerate((OH1, OH2)):
            tt(s, "dve", bigr, bigr[:, :, :], OH, OH[:, :, :], pos, pos[:, :, :], ALU.mult)
            s.op("dve", lambda e, k=k: e.reduce_sum(out=RK[:, :, k], in_=bigr[:, :, :], axis=AX.X), reads=[bigr], writes=[RK], cost=2000)
        cnti = s.sb([128, 64], I32, "cnti")
        padf = s.sb([128, 64], F32, "padf")
        pends = s.sb([128, 64], F32, "pends")
        pstart = s.sb([128, 64], F32, "pstart")
        ts(s, "dve", cnti, cnti[:, :], cnt, cnt[:, :], 127.0, None, ALU.add)
        ts(s, "dve", cnti, cnti[:, :], cnti, cnti[:, :], 7, 7, ALU.arith_shift_right, ALU.logical_shift_left)
        cp(s, "dve", padf, padf[:, :], cnti, cnti[:, :])
        s.op("dve", lambda e: e.tensor_tensor_scan(out=pends[:, :], data0=cx.ones_f[:, 0:64], data1=padf[:, :],
                                                   initial=0.0, op0=ALU.mult, op1=ALU.add),
             reads=[cx.ones_f, padf], writes=[pends])
        tt(s, "dve", pstart, pstart[:, :], pends, pends[:, :], padf, padf[:, :], ALU.subtract)
        big = s.sb([128, NT, 64], F32, "big")
        DEST = s.sb([128, NT, 2], F32, "DEST")
        for k, OH in enumerate((OH1, OH2)):
            tt(s, "dve", big, big[:, :, :], OH, OH[:, :, :], pstart, pstart[:, :].unsqueeze(1).to_broadcast([128, NT, 64]), ALU.mult)
            s.op("dve", lambda e, k=k: e.reduce_sum(out=DEST[:, :, k], in_=big[:, :, :], axis=AX.X), reads=[big], writes=[DEST])
        tt(s, "dve", DEST, DEST[:, :, :], DEST, DEST[:, :, :], RK, RK[:, :, :], ALU.add)
        cp(s, "dve", DESTi, DESTi[:, :, :], DEST, DEST[:, :, :])
        fill = s.sb([128, NSLOT], I32, "fill")
        s.op("pool", lambda e: e.iota(fill[:, :], pattern=[[0, NSLOT]], base=S, channel_multiplier=0), writes=[fill])
        btfill = Res("btfill")
        s.dma("sp", lambda e: e.dma_start(out=bt.rearrange("(p n) o -> p (n o)", p=128), in_=fill[:, :]),
              reads=[fill, bt_res], writes=[btfill], nbytes=65536)
        for i in range(NT):
            for k in range(2):
                s.dma("pool", lambda e, i=i, k=k: e.indirect_dma_start(
                    out=bt, out_offset=bass.IndirectOffsetOnAxis(ap=DESTi[:, i, k:k + 1], axis=0),
                    in_=toki[:, i:i + 1], in_offset=None, bounds_check=breg_t, oob_is_err=False),
                    reads=[DESTi, toki, btfill], writes=[], nbytes=4096)
        joinr = s.sb([128, 1], F32, "joinr")
        s.op("pool", lambda e: e.memset(joinr[:, :], 0.0), reads=[], writes=[btfill, joinr, bt_res])
        slotoff = s.sb([128, NSLOT], F32, "slotoff")
        sloti = s.sb([128, NSLOT], I32, "sloti")
        s.op("pool", lambda e: e.iota(sloti[:, :], pattern=[[128, NSLOT]], base=0, channel_multiplier=0), writes=[sloti])
        cp(s, "dve", slotoff, slotoff[:, :], sloti, sloti[:, :])
        blk = s.sb([128, NSLOT], F32, "blk")
        cmp_ = s.sb([128, 32, 64], F32, "cmp")
        for q in range(NSLOT // 32):
            tt(s, "dve", cmp_, cmp_[:, :, :], pends, pends[:, :].unsqueeze(1).to_broadcast([128, 32, 64]),
               slotoff, slotoff[:, q * 32:(q + 1) * 32].unsqueeze(2).to_broadcast([128, 32, 64]), ALU.is_le)
            s.op("dve", lambda e, q=q: e.reduce_sum(out=blk[:, q * 32:(q + 1) * 32], in_=cmp_[:, :, :], axis=AX.X),
                 reads=[cmp_], writes=[blk])
        pidx = s.sb([128, 1], F32, "pidx")
        pidi = s.sb([128, 1], I32, "pidi")
        s.op("pool", lambda e: e.iota(pidi[:, :], pattern=[[0, 1]], base=0, channel_multiplier=1), writes=[pidi])
        cp(s, "dve", pidx, pidx[:, :], pidi, pidi[:, :])
        used = s.sb([128, NSLOT], F32, "used")
        ts(s, "dve", used, used[:, :], slotoff, slotoff[:, :], pends[:, 63:64], None, ALU.is_lt, extra_reads=[pends])
        ts(s, "dve", blk, blk[:, :], blk, blk[:, :], 63.0, 128.0, ALU.min, ALU.mult)
        ts(s, "dve", blk, blk[:, :], blk, blk[:, :], pidx[:, 0:1], float(l * 64 * 128 - BIGIDX), ALU.add, ALU.add, extra_reads=[pidx])
        tt(s, "dve", blk, blk[:, :], blk, blk[:, :], used, used[:, :], ALU.mult)
        ts(s, "dve", blk, blk[:, :], blk, blk[:, :], float(BIGIDX), None, ALU.add)
        cp(s, "dve", IDXW, IDXW[:, :], blk, blk[:, :])
        if "dbg" in dr:
            dbg = s.sb([128, 1024], F32, "dbg")
            s.op("dve", lambda e: e.memset(dbg[:, :], 0.0), writes=[dbg])
            cp(s, "dve", dbg, dbg[:, 0:128], IDXW, IDXW[:, :])
            cp(s, "dve", dbg, dbg[:, 128:192], cnt, cnt[:, :])
            cp(s, "dve", dbg, dbg[:, 192:256], pends, pends[:, :])
            cp(s, "dve", dbg, dbg[:, 256:320], DEST, DEST[:, :, :].rearrange("p a b -> p (a b)"))
            cp(s, "dve", dbg, dbg[:, 320:384], GT, GT[:, :, :].rearrange("p a b -> p (a b)"))
            cp(s, "dve", dbg, dbg[:, 384:448], used, used[:, 0:64])
            cp(s, "dve", dbg, dbg[:, 448:512], padf, padf[:, :])
            cp(s, "dve", dbg, dbg[:, 512:576], OH1, OH1[:, 0, :])
            cp(s, "dve", dbg, dbg[:, 576:640], OH2, OH2[:, 0, :])
            cp(s, "dve", dbg, dbg[:, 640:704], RK, RK[:, :, :].rearrange("p a b -> p (a b)"))
            st(s, "sp", wres, dr["dbg"], dbg, dbg[:, :], final=True)
        s.phase_es = old
    s_sub_barrier(s)
    if fstop < 2:
        s.end_phase()
        return

    with ExitStack() as es2:
        old = s.phase_es
        s.phase_es = es2
        wviews = [dr[k].rearrange("l e (p j) n -> (l e p) (j n)", p=128) for k in ("e_w_gate", "e_w_up", "e_w_down")]
        NSTG = 3
        stgs = [[s.sb([128, 2048], F32, f"ws{k}") for k in range(3)] for _ in range(NSTG)]
        wbs = [[s.sb([128, 2048], BF16, f"wb{k}") for k in range(3)] for _ in range(2)]
        idxs = [s.sb([128, 1], I32, "idxb") for _ in range(3)]
        xgs = [s.sb([128, 1024], BF16, "xg") for _ in range(3)]
        for xg in xgs:
            s.op("pool", lambda e, xg=xg: e.memset(xg[:, :], 0.0), writes=[xg])
        xgTs = [s.sb([128, 8, 128], BF16, "xgT") for _ in range(2)]
        sgs = [s.sb([128, 256], F32, "sg") for _ in range(2)]
        aTs = [s.sb([128, 2, 128], BF16, "aT") for _ in range(2)]
        yos = [s.sb([128, 1024], F32, "yo") for _ in range(2)]
        psT = [s.ps([128, 1024], BF16, "psT") for _ in range(2)]
        psGs = [s.ps([128, 512], F32, "psG") for _ in range(2)]
        psUs = [s.ps([128, 512], F32, "psU") for _ in range(2)]
        psY = [s.ps([128, 512], F32, "psY") for _ in range(2)]
        nslot = int(os.environ.get("FSLOTS", str(NSLOT)))

        def loads(b):
            ix = idxs[b % 3]
            ld(s, "sp", ix, ix[:, :], bt_res, bt[b * 128:(b + 1) * 128, :])
            xg = xgs[b % 3]
            s.dma("pool", lambda e: e.indirect_dma_start(
                out=xg[:, :], out_offset=None, in_=xbf, in_offset=bass.IndirectOffsetOnAxis(ap=ix[:, 0:1], axis=0),
                bounds_check=breg_x, oob_is_err=False), reads=[ix, xbf_res], writes=[xg])
            for k in range(3):
                stg = stgs[b % NSTG][k]
                s.dma("pool", lambda e, stg=stg, k=k: e.indirect_dma_start(
                    out=stg[:, :], out_offset=None, in_=wviews[k],
                    in_offset=bass.IndirectOffsetOnAxis(ap=IDXW[:, b:b + 1], axis=0),
                    bounds_check=breg_w, oob_is_err=False), reads=[IDXW, wres], writes=[stg], nbytes=1 << 20)

        loads(0)
        loads(1)
        for b in range(nslot):
            if b + 2 < nslot:
                loads(b + 2)
            xg = xgs[b % 3]
            xgT = xgTs[b % 2]
            wg, wu, wd = wbs[b % 2]
            sg = sgs[b % 2]
            aT = aTs[b % 2]
            yo = yos[b % 2]
            pT = psT[b % 2]
            psG = psGs[b % 2]
            psU = psUs[b % 2]
            for k, eng in enumerate(("act", "dve", "act")):
                cp(s, eng, wbs[b % 2][k], wbs[b % 2][k][:, :], stgs[b % NSTG][k], stgs[b % NSTG][k][:, :])
            for j in range(8):
                s.op("pe", lambda e, j=j, pT=pT, xg=xg: e.transpose(out=pT[:, j * 128:(j + 1) * 128], in_=xg[:, j:1024:8],
                                                      identity=cx.ident_b[:, :]), reads=[xg, cx.ident_b], writes=[pT])
            cp(s, "dve", xgT, xgT[:, :, :], pT, pT[:, :].rearrange("p (j t) -> p j t", j=8))
            wgv = wg[:, :].rearrange("p (j n) -> p j n", j=8)
            wuv = wu[:, :].rearrange("p (j n) -> p j n", j=8)
            wdv = wd[:, :].rearrange("p (j n) -> p j n", j=2)
            for pS_, wv_ in ((psG, wgv), (psU, wuv)):
                for jh in range(2):
                    for j in range(8):
                        mm(s, pS_, pS_[:, jh * 128:(jh + 1) * 128], wbs[b % 2][0 if pS_ is psG else 1], wv_[:, j, jh:256:2],
                           xgT, xgT[:, j, :], j == 0, j == 7)
            act(s, sg, sg[:, :], psG, psG[:, 0:256], AF.Silu)
            tt(s, "dve", aT, aT[:, :, :].rearrange("p a t -> p (a t)"), sg, sg[:, :], psU, psU[:, 0:256], ALU.mult)
            for half in range(2):
                for jh in range(2):
                    mm(s, psY[half], psY[half][:, :], aT, aT[:, jh, :], wd, wdv[:, jh, half * 512:(half + 1) * 512], jh == 0, jh == 1)
            cp(s, "act", yo, yo[:, 0:512], psY[0], psY[0][:, :])
            cp(s, "dve", yo, yo[:, 512:1024], psY[1], psY[1][:, :])
            st(s, "sp", yb_res, yb[b * 128:(b + 1) * 128, :], yo, yo[:, :])
        s.phase_es = old
    s_sub_barrier(s)
    if fstop < 3:
        s.end_phase()
        return

    with ExitStack() as es3:
        old = s.phase_es
        s.phase_es = es3
        g_bc = s.sb([128, 1024], F32, "gbc")
        b_bc = s.sb([128, 1024], F32, "bbc")
        ld(s, "sp", g_bc, g_bc[:, :], wres, bcast_row(dr["ln2_g"][l], 1024))
        ld(s, "sp", b_bc, b_bc[:, :], wres, bcast_row(dr["ln2_b"][l], 1024))
        y1s = [s.sb([128, 1024], F32, "y1") for _ in range(2)]
        y2s = [s.sb([128, 1024], F32, "y2") for _ in range(2)]
        hfs = [s.sb([128, 1024], F32, "hf3") for _ in range(2)]
        hos = [s.sb([128, 1024], F32, "ho3") for _ in range(2)]
        wk = ln_work(s)
        for i in range(NT):
            y1, y2, hf, ho = y1s[i % 2], y2s[i % 2], hfs[i % 2], hos[i % 2]
            for k, yk in enumerate((y1, y2)):
                s.dma("pool", lambda e, yk=yk, k=k, i=i: e.indirect_dma_start(
                    out=yk[:, :], out_offset=None, in_=yb, in_offset=bass.IndirectOffsetOnAxis(ap=DESTi[:, i, k:k + 1], axis=0),
                    bounds_check=breg_y, oob_is_err=False), reads=[DESTi, yb_res], writes=[yk])
            ld(s, "sp", hf, hf[:, :], h_in_res, h_in_ap[i * 128:(i + 1) * 128, :])
            act(s, y2, y2[:, :], y2, y2[:, :], AF.Copy, scale=GT[:, i, 1:2], extra_reads=[GT])
            stt(s, "dve", y1, y1[:, :], y1, y1[:, :], GT[:, i, 0:1], y2, y2[:, :], ALU.mult, ALU.add, extra_reads=[GT])
            ln_epilogue(s, cx, [(y1, y1[:, 0:512]), (y1, y1[:, 512:1024])], hf, g_bc, b_bc, wk, ho)
            st(s, "pool", h_out_res, h_out_ap[i * 128:(i + 1) * 128, :], ho, ho[:, :], final=final)
        s.phase_es = old
    s.end_phase()


def pack_experts(g, u, d):
    g = np.asarray(g, dtype=np.float32).reshape(4, 64, 128, 2048)
    u = np.asarray(u, dtype=np.float32).reshape(4, 64, 128, 2048)
    d = np.asarray(d, dtype=np.float32).reshape(4, 64, 128, 2048)
    return np.ascontiguousarray(np.stack([g, u, d], axis=3)).reshape(4 * 64 * 128, 3 * 2048)


def rope_consts_np():
    rc = np.zeros((128, 4), np.float32)
    inv_a = 500000.0 ** (-np.arange(0, 16, 2, dtype=np.float32) / 16.0)
    for hh in range(2):
        for jx in range(16):
            rc[hh * 64 + jx, 0] = inv_a[jx % 8]
            rc[hh * 64 + jx, 1] = -1.0 if jx < 8 else 1.0
    inv_c = 10000.0 ** (-np.arange(0, 256, 2, dtype=np.float32) / 256.0)
    rc[:, 2] = inv_c
    return rc


IN_SPECS = [
    ("x", [S, D], F32), ("positions", [S], I32),
    ("ln1_g", [4, D], F32), ("ln1_b", [4, D], F32), ("ln2_g", [4, D], F32), ("ln2_b", [4, D], F32),
    ("a_w_in", [2, D, 3072], F32), ("a_w_out", [2, D, D], F32),
    ("b_w_in", [1, D, 3088], F32), ("b_gate_bias", [1, 16], F32), ("b_conv_w", [1, 4, D], F32),
    ("b_conv_b", [1, D], F32), ("b_norm_g", [1, D], F32), ("b_w_out", [1, D, D], F32),
    ("c_w_in", [1, D, 6144], F32), ("c_norm_g", [1, 2 * D], F32), ("c_w_out", [1, 2 * D, D], F32),
    ("r_group_w", [4, D, 8], F32), ("r_group_b", [4, 8], F32), ("r_expert_w", [4, D, 64], F32),
    ("r_expert_b", [4, 64], F32), ("e_w_gate", [4, 64, D, 256], F32), ("e_w_up", [4, 64, D, 256], F32),
    ("e_w_down", [4, 64, 256, D], F32), ("rope_consts", [128, 4], F32),
]


def build(plan, used_inputs=None):
    nc = bass.Bass("TRN2", target_bir_lowering=False)
    dr = {}
    for name, shape, dt in IN_SPECS:
        if used_inputs is not None and name not in used_inputs:
            continue
        dr[name] = nc.dram_tensor(name, shape, dt, kind="ExternalInput").ap()
    out = nc.dram_tensor("out", [S, D], F32, kind="ExternalOutput").ap()
    import os
    if os.environ.get("KDBG"):
        dr["dbg"] = nc.dram_tensor("dbg", [128, 1024], F32, kind="ExternalOutput").ap()
    dr["wres"] = Res("weights")
    dr["qkT"] = (Res("qkT"), nc.dram_tensor("qkT_s", [2, D, S], BF16).ap())
    dr["vsc"] = (Res("vsc"), nc.dram_tensor("vsc_s", [2, S, 128], BF16).ap())
    dr["attT"] = (Res("attT"), nc.dram_tensor("attT_s", [2 * D, S], BF16).ap())
    dr["hn"] = (Res("hn"), nc.dram_tensor("hn_s", [S, 2 * D], F32).ap())
    dr["xbf"] = (Res("xbf"), nc.dram_tensor("xbf_s", [S, D], BF16).ap())
    dr["buftok"] = (Res("buftok"), nc.dram_tensor("buftok_s", [NSLOT * 128, 1], I32).ap())
    dr["yb"] = (Res("yb"), nc.dram_tensor("yb_s", [NSLOT * 128, D], F32).ap())
    hbuf = [(Res("hA"), nc.dram_tensor("hA_s", [S, D], F32).ap(), False),
            (Res("hB"), nc.dram_tensor("hB_s", [S, D], F32).ap(), False)]
    with ExitStack() as es:
        s = Sched(nc, es)
        cx = Ctx()
        make_consts(s, cx)
        cur = (dr["wres"], dr["x"], False)
        for pi, (ph, l) in enumerate(plan):
            last = pi == len(plan) - 1
            nxt = (Res("out"), out, True) if last else hbuf[pi % 2]
            if ph == "A":
                phase_A(s, cx, dr, l, l // 3, cur, nxt)
            elif ph == "B":
                phase_B(s, cx, dr, l, l // 3, cur, nxt)
            elif ph == "C":
                phase_C(s, cx, dr, l, l // 3, cur, nxt)
            elif ph == "F":
                phase_F(s, cx, dr, l, cur, nxt)
            else:
                raise ValueError(ph)
            cur = nxt
        s.finish()
        print("instructions:", s.ninst, "sems:", s.n_sem)
    return nc


PLAN = [("A", 0), ("F", 0), ("B", 1), ("F", 1), ("C", 2), ("F", 2), ("A", 3), ("F", 3)]
_NC_CACHE = {}


def kernel(**inputs):
    x = np.ascontiguousarray(np.asarray(inputs["x"], dtype=np.float32))
    nb = x.shape[0]
    if "nc" not in _NC_CACHE:
        _NC_CACHE["nc"] = build(PLAN)
    nc = _NC_CACHE["nc"]
    shared = {}
    for name, shape, dt in IN_SPECS:
        if name == "x":
            continue
        if name == "rope_consts":
            shared[name] = rope_consts_np()

        elif name == "positions":
            shared[name] = np.ascontiguousarray(np.asarray(inputs[name]).astype(np.int32))
        else:
            shared[name] = np.ascontiguousarray(np.asarray(inputs[name], dtype=np.float32))
    in_maps = []
    for b in range(nb):
        m = dict(shared)
        m["x"] = x[b]
        in_maps.append(m)
    res = run_bass_kernel_spmd(nc, in_maps, core_ids=list(range(nb)))
    return np.stack([np.asarray(r["out"], dtype=np.float32) for r in res.results], axis=0)
```

```python
import math
import numpy as np
import concourse.bass as bass
import concourse.mybir as mybir
from concourse.bass_utils import run_bass_kernel_spmd
from contextlib import ExitStack

F32 = mybir.dt.float32
BF16 = mybir.dt.bfloat16
I32 = mybir.dt.int32
ALU = mybir.AluOpType
AF = mybir.ActivationFunctionType
AX = mybir.AxisListType

S = 4096
D = 1024
DEPTH = 4
NT = S // 128
ALPHA = (2.0 * DEPTH) ** 0.25
LN_EPS = 1e-5
TWO_PI = 2.0 * math.pi

SEM_WRAP = 30000
ENGS = ("pe", "act", "dve", "pool", "sp")


class Res:
    __slots__ = ("w", "r", "name", "excl")

    def __init__(self, name="", excl=False):
        self.w = None
        self.r = []
        self.name = name
        self.excl = excl


class T:
    __slots__ = ("t", "res")

    def __init__(self, t, res):
        self.t = t
        self.res = res

    def __getitem__(self, k):
        return self.t[k]


class Sched:
    def __init__(self, nc, es):
        self.nc = nc
        self.es = es
        self.sems = {}
        self.cur = {}
        self.waited = {e: {} for e in ENGS}
        self.pending = {e: {} for e in ENGS}
        self.dma_pool = {}
        self.dma_rr = {}
        self.n_sem = 0
        self.n_tiles = 0
        self.ninst = 0
        self.finals = []
        self.eobj = {"pe": nc.tensor, "act": nc.scalar, "dve": nc.vector, "pool": nc.gpsimd, "sp": nc.sync}
        self.phase_es = None
        self.recs = []
        self.ev_of = {}
        self.next_id = 0
        import os
        self.window = int(os.environ.get("KWIN", "48"))
        self.sparse_pe_inc = os.environ.get("KSPARSE", "1") == "1"
        self.pe_noinc = set()
        self.fill = None
        self.nfill = int(os.environ.get("KFILL", "0"))

    def _newsem(self, name):
        h = self.es.enter_context(self.nc.semaphore(f"{name}_{self.n_sem}"))
        self.n_sem += 1
        key = self.n_sem
        self.sems[key] = h
        return key

    def sb(self, shape, dtype, name=None):
        self.n_tiles += 1
        name = name or "t"
        es = self.phase_es or self.es
        t = es.enter_context(self.nc.sbuf_tensor(f"{name}_{self.n_tiles}", list(shape), dtype))
        return T(t, Res(name))

    def ps(self, shape, dtype, name=None):
        self.n_tiles += 1
        name = name or "p"
        es = self.phase_es or self.es
        t = es.enter_context(self.nc.psum_tensor(f"{name}_{self.n_tiles}", list(shape), dtype))
        return T(t, Res(name, excl=True))

    def begin_phase(self):
        self.phase_es = ExitStack()
        return self.phase_es

    def end_phase(self):
        self.flush()
        snap = {}
        for e, c in self.cur.items():
            snap[c[0]] = c[1]
        for q, slots in self.dma_pool.items():
            for sl in slots:
                if sl[1] > 0:
                    snap[sl[0]] = sl[1]
        for e in ENGS:
            p = self.pending[e]
            for k, v in snap.items():
                if p.get(k, 0) < v:
                    p[k] = v
        self.phase_es.close()
        self.phase_es = None

    def _deps(self, reads, writes):
        deps = set()
        for r in reads:
            r = r.res if isinstance(r, T) else r
            if r.w is not None:
                deps.add(r.w)
            if r.excl:
                deps.update(r.r)
        for w in writes:
            w = w.res if isinstance(w, T) else w
            if w.w is not None:
                deps.add(w.w)
            deps.update(w.r)
        return deps

    def _commit(self, oid, reads, writes):
        for r in reads:
            r = r.res if isinstance(r, T) else r
            r.r.append(oid)
        for w in writes:
            w = w.res if isinstance(w, T) else w
            w.w = oid
            w.r = []

    def op(self, eng, fn, reads=(), writes=(), cost=150.0):
        oid = self.next_id
        self.next_id += 1
        deps = self._deps(reads, writes)
        self._commit(oid, reads, writes)
        self.recs.append((oid, eng, False, fn, deps, float(cost), 0.0, False))

    def dma(self, q, fn, reads=(), writes=(), final=False, nbytes=65536, issue=None):
        oid = self.next_id
        self.next_id += 1
        deps = self._deps(reads, writes)
        self._commit(oid, reads, writes)
        if issue is None:
            issue = 1200.0 if q == "pool" else 80.0
        self.recs.append((oid, q, True, fn, deps, float(issue), 2000.0 + nbytes / 150.0, final))

    def flush(self):
        recs = self.recs
        self.recs = []
        if not recs:
            return
        fin = {}
        queues = {e: [] for e in ENGS}
        for r in recs:
            queues[r[1]].append(r)
        heads = {e: 0 for e in ENGS}
        placed = set()
        mine = {r[0] for r in recs}
        efree = {e: 0.0 for e in ENGS}
        dma_free = [0.0]
        order = []
        nleft = len(recs)
        W = self.window
        taken = {e: set() for e in ENGS}
        while nleft:
            best = None
            for e in ENGS:
                q = queues[e]
                h = heads[e]
                n = len(q)
                while h < n and q[h][0] in taken[e]:
                    h += 1
                heads[e] = h
                lim = min(n, h + W)
                for k in range(h, lim):
                    r = q[k]
                    if r[0] in taken[e]:
                        continue
                    ok = True
                    st_ = efree[e]
                    for d in r[4]:
                        if d in mine:
                            if d not in placed:
                                ok = False
                                break
                            if fin[d] > st_:
                                st_ = fin[d]
                    if not ok:
                        continue
                    key = (st_, r[0])
                    if best is None or key < best[0]:
                        best = (key, e, r)
                    if st_ <= efree[e]:
                        break
            assert best is not None, "scheduler deadlock"
            (st_, _), e, r = best
            taken[e].add(r[0])
            placed.add(r[0])
            nleft -= 1
            if r[2]:
                iend = st_ + r[5]
                efree[e] = iend
                t0 = max(iend, dma_free[0])
                dma_free[0] = t0 + max(0.0, r[6] - 2000.0)
                fin[r[0]] = dma_free[0] + 2000.0
            else:
                efree[e] = st_ + r[5]
                fin[r[0]] = efree[e] + 60.0
            order.append(r)
        eng_of = {r[0]: r[1] for r in recs}
        xdep = set()
        for r in recs:
            if r[1] != "pe":
                for d in r[4]:
                    if eng_of.get(d) == "pe":
                        xdep.add(d)
        pe_list = [r for r in order if r[1] == "pe"]
        self.pe_noinc = set()
        if self.sparse_pe_inc:
            for k, r in enumerate(pe_list):
                if r[0] not in xdep and k != len(pe_list) - 1 and not r[7]:
                    self.pe_noinc.add(r[0])
        for r in order:
            self._emit_rec(r)

    def _engine_event(self, eng):
        if eng not in self.cur or self.cur[eng][1] >= SEM_WRAP:
            self.cur[eng] = [self._newsem(eng), 0]
        c = self.cur[eng]
        c[1] += 1
        return (c[0], c[1])

    def _peek_next_event(self, eng):
        if eng not in self.cur or self.cur[eng][1] >= SEM_WRAP:
            self.cur[eng] = [self._newsem(eng), 0]
        c = self.cur[eng]
        return (c[0], c[1] + 1)

    def _dma_event(self, q):
        if q not in self.dma_pool:
            self.dma_pool[q] = [[self._newsem(f"dma{q}"), 0] for _ in range(12)]
            self.dma_rr[q] = 0
        i = self.dma_rr[q]
        self.dma_rr[q] = (i + 1) % len(self.dma_pool[q])
        slot = self.dma_pool[q][i]
        extra = (slot[0], slot[1]) if slot[1] > 0 else None
        if slot[1] + 16 > SEM_WRAP:
            slot[0] = self._newsem(f"dma{q}")
            slot[1] = 0
        slot[1] += 16
        return (slot[0], slot[1]), extra

    def _emit_rec(self, r):
        oid, eng, is_dma, fn, deps, _, _, final = r
        need = self.pending[eng]
        self.pending[eng] = {}
        for d in deps:
            k, v = self.ev_of[d]
            if need.get(k, 0) < v:
                need[k] = v
        if is_dma:
            ev, extra = self._dma_event(eng)
            if extra is not None:
                k, v = extra
                if need.get(k, 0) < v:
                    need[k] = v
            inc = 16
        elif oid in self.pe_noinc:
            ev = self._peek_next_event(eng)
            inc = 0
        else:
            ev = self._engine_event(eng)
            inc = 1
        wd = self.waited[eng]
        own = self.cur.get(eng, [None])[0] if eng == "pe" else None
        e = self.eobj[eng]
        wl = []
        for k, v in need.items():
            if k == own:
                continue
            if wd.get(k, 0) < v:
                wd[k] = v
                wl.append((k, v))
        if wl and eng == "pe" and self.fill is not None and self.nfill > 0:
            fo, fl, fr = self.fill
            for _ in range(self.nfill):
                e.matmul(fo, lhsT=fl, rhs=fr, start=True, stop=True)
        for k, v in wl:
            e.wait_ge(self.sems[k], v)
        ins = fn(e)
        if inc:
            ins.then_inc(self.sems[ev[0]], inc)
        self.ev_of[oid] = ev
        self.ninst += 1
        if final:
            self.finals.append(ev)

    def finish(self):
        self.flush()
        deps = {}
        for k, v in self.finals:
            if deps.get(k, 0) < v:
                deps[k] = v
        for q, slots in self.dma_pool.items():
            for sl in slots:
                if sl[1] > 0 and deps.get(sl[0], 0) < sl[1]:
                    deps[sl[0]] = sl[1]
        for k, v in deps.items():
            self.nc.sync.wait_ge(self.sems[k], v)


def _fsz(ap):
    n = 1
    for d in ap.shape[1:]:
        n *= d
    return n


def mm(s, out_t, out_ap, lhsT_t, lhsT_ap, rhs_t, rhs_ap, start, stop):
    n = _fsz(out_ap)
    c = 30.0 + max(n, 64) * 0.45
    if lhsT_ap.dtype == F32:
        c *= 4
    s.op("pe", lambda e: e.matmul(out_ap, lhsT=lhsT_ap, rhs=rhs_ap, start=start, stop=stop),
         reads=[lhsT_t, rhs_t], writes=[out_t], cost=c)


def _ecost(eng, n):
    if eng == "dve":
        return 70.0 + 0.85 * n
    if eng == "act":
        return 200.0 + 0.65 * n
    return 160.0 + 1.6 * n


def tt(s, eng, out_t, out_ap, a_t, a_ap, b_t, b_ap, op):
    s.op(eng, lambda e: e.tensor_tensor(out=out_ap, in0=a_ap, in1=b_ap, op=op), reads=[a_t, b_t], writes=[out_t],
         cost=_ecost(eng, _fsz(out_ap)))


def ts(s, eng, out_t, out_ap, a_t, a_ap, s1, s2, op0, op1=None, extra_reads=()):
    c = _ecost(eng, _fsz(out_ap))
    if op1 is None:
        s.op(eng, lambda e: e.tensor_scalar(out=out_ap, in0=a_ap, scalar1=s1, scalar2=None, op0=op0),
             reads=[a_t, *extra_reads], writes=[out_t], cost=c)
    else:
        s.op(eng, lambda e: e.tensor_scalar(out=out_ap, in0=a_ap, scalar1=s1, scalar2=s2, op0=op0, op1=op1),
             reads=[a_t, *extra_reads], writes=[out_t], cost=c)


def stt(s, eng, out_t, out_ap, a_t, a_ap, scalar, b_t, b_ap, op0, op1, extra_reads=()):
    s.op(eng, lambda e: e.scalar_tensor_tensor(out=out_ap, in0=a_ap, scalar=scalar, in1=b_ap, op0=op0, op1=op1),
         reads=[a_t, b_t, *extra_reads], writes=[out_t], cost=_ecost(eng, _fsz(out_ap)))


def cp(s, eng, out_t, out_ap, a_t, a_ap):
    n = _fsz(out_ap)
    if eng == "act":
        s.op("act", lambda e: e.activation(out=out_ap, in_=a_ap, func=AF.Copy), reads=[a_t], writes=[out_t], cost=_ecost("act", n))
    else:
        c = _ecost(eng, n) if eng == "dve" else 160.0 + 3.0 * n
        s.op(eng, lambda e: e.tensor_copy(out=out_ap, in_=a_ap), reads=[a_t], writes=[out_t], cost=c)


def act(s, out_t, out_ap, a_t, a_ap, func, scale=1.0, bias=0.0, extra_reads=()):
    s.op("act", lambda e: e.activation(out=out_ap, in_=a_ap, func=func, bias=bias, scale=scale),
         reads=[a_t, *extra_reads], writes=[out_t], cost=_ecost("act", _fsz(out_ap)))


def _nbytes(ap):
    n = 1
    for d in ap.shape:
        n *= d
    return n * (2 if ap.dtype == BF16 else 4)


def ld(s, q, out_t, out_ap, src_res, src_ap):
    s.dma(q, lambda e: e.dma_start(out=out_ap, in_=src_ap), reads=[src_res], writes=[out_t], nbytes=_nbytes(out_ap))


def st(s, q, dst_res, dst_ap, in_t, in_ap, final=False):
    s.dma(q, lambda e: e.dma_start(out=dst_ap, in_=in_ap), reads=[in_t], writes=[dst_res], final=final, nbytes=_nbytes(in_ap))


class Ctx:
    pass


def wload(s, cx, dst_t, dst_ap, wres, src_ap, shape):
    stg = cx.stg[cx.stg_i % 2]
    cx.stg_i += 1
    n = int(np.prod(shape))
    if len(shape) == 1:
        v = stg[:, 0:n]
    else:
        v = stg[:, 0:n].rearrange("p (a b) -> p a b", a=shape[0])
    ld(s, "sp", stg, v, wres, src_ap)
    cp(s, "dve" if cx.stg_i % 2 else "act", dst_t, dst_ap, stg, v)


def make_consts(s, cx):
    nc = s.nc
    iot = s.sb([128, 128], I32, "iot")
    iof = s.sb([128, 128], F32, "iof")
    cx.iof = iof
    cx.ident_f = s.sb([128, 128], F32, "identf")
    cx.ident_b = s.sb([128, 128], BF16, "identb")
    cx.maskA = s.sb([128, 2, 2, 128], BF16, "maskA")
    cx.perm = s.sb([128, 128], BF16, "perm")
    cx.ones_b = s.sb([128, 128], BF16, "onesb")
    cx.ones_f = s.sb([128, 128], F32, "onesf")
    cx.tri_le = s.sb([128, 128], F32, "trile")
    cx.tri_b = s.sb([128, 128], BF16, "trib")
    cx.swlo = s.sb([128, 128], F32, "swlo")
    cx.swhi = s.sb([128, 128], F32, "swhi")
    cx.stg = [s.sb([128, 2048], F32, "stg") for _ in range(2)]
    cx.stg_i = 0
    s.op("pool", lambda e: e.iota(iot[:, :], pattern=[[1, 128]], base=0, channel_multiplier=-1), writes=[iot])
    cp(s, "dve", iof, iof[:, :], iot, iot[:, :])
    ts(s, "dve", cx.ident_f, cx.ident_f[:, :], iof, iof[:, :], 0.0, None, ALU.is_equal)
    cp(s, "dve", cx.ident_b, cx.ident_b[:, :], cx.ident_f, cx.ident_f[:, :])
    ts(s, "dve", cx.tri_le, cx.tri_le[:, :], iof, iof[:, :], 0.0, None, ALU.is_ge)
    cp(s, "dve", cx.tri_b, cx.tri_b[:, :], cx.tri_le, cx.tri_le[:, :])
    ts(s, "dve", cx.swlo, cx.swlo[:, :], iof, iof[:, :], 64.0, None, ALU.is_equal)
    ts(s, "dve", cx.swhi, cx.swhi[:, :], iof, iof[:, :], -64.0, None, ALU.is_equal)
    for hh in range(2):
        ts(s, "dve", cx.maskA, cx.maskA[:, hh, 0, :], iof, iof[:, :], 0.0, None, ALU.is_ge)
        ts(s, "dve", cx.maskA, cx.maskA[:, hh, 1, :], iof, iof[:, :], 0.0, None, ALU.is_le)
    s.op("pool", lambda e: e.memset(cx.perm[:, :], 0.0), writes=[cx.perm])
    s.op("pool", lambda e: e.memset(cx.ones_b[:, :], 1.0), writes=[cx.ones_b])
    s.op("pool", lambda e: e.memset(cx.ones_f[:, :], 1.0), writes=[cx.ones_f])
    for hh in range(2):
        c0 = hh * 64
        ts(s, "dve", cx.perm, cx.perm[:, c0:c0 + 8], iof, iof[:, c0:c0 + 8], -8.0, None, ALU.is_equal)
        ts(s, "dve", cx.perm, cx.perm[:, c0 + 8:c0 + 16], iof, iof[:, c0 + 8:c0 + 16], 8.0, None, ALU.is_equal)


def sin_table(s, out_t, out_ap, x_t, x_ap, shape, sign_ap, sign_t, scratch):
    ki, kf, r = scratch
    C1 = 6.28125
    C2 = TWO_PI - C1
    ts(s, "dve", ki, ki[:, :], x_t, x_ap, 1.0 / TWO_PI, None, ALU.mult)
    cp(s, "dve", kf, kf[:, :], ki, ki[:, :])
    stt(s, "dve", r, r[:, :], kf, kf[:, :], -C1, x_t, x_ap, ALU.mult, ALU.add)
    stt(s, "dve", r, r[:, :], kf, kf[:, :], -C2, r, r[:, :], ALU.mult, ALU.add)
    ts(s, "dve", kf, kf[:, :], r, r[:, :], math.pi, None, ALU.is_gt)
    stt(s, "dve", r, r[:, :], kf, kf[:, :], -TWO_PI, r, r[:, :], ALU.mult, ALU.add)
    ts(s, "dve", kf, kf[:, :], r, r[:, :], -math.pi, None, ALU.is_lt)
    stt(s, "dve", r, r[:, :], kf, kf[:, :], TWO_PI, r, r[:, :], ALU.mult, ALU.add)
    ts(s, "dve", r, r[:, :], r, r[:, :], 3.14159, -3.14159, ALU.min, ALU.max)
    act(s, out_t, out_ap, r, r[:, :], AF.Sin)
    if sign_ap is not None:
        ts(s, "dve", out_t, out_ap, out_t, out_ap, sign_ap, None, ALU.mult, extra_reads=[sign_t])


def load_xT(s, cx, h_res, h_ap, xT):
    old = s.phase_es
    s.phase_es = ExitStack()
    hfs = [s.sb([128, 1024], F32, "hf") for _ in range(3)]
    pts = [s.ps([128, 512], F32, "ptr") for _ in range(2)]
    for i in range(NT):
        hf = hfs[i % 3]
        ld(s, "sp", hf, hf[:, :], h_res, h_ap[i * 128:(i + 1) * 128, :])
        for half in range(2):
            pt = pts[half]
            for c4 in range(4):
                c = half * 4 + c4
                s.op("pe", lambda e, pt=pt, c4=c4, c=c, hf=hf: e.transpose(
                    out=pt[:, c4 * 128:(c4 + 1) * 128], in_=hf[:, c * 128:(c + 1) * 128], identity=cx.ident_f[:, :]),
                    reads=[hf, cx.ident_f], writes=[pt])
            dst = xT[:, half * 4:(half + 1) * 4, i * 128:(i + 1) * 128]
            src = pt[:, :].rearrange("p (c t) -> p c t", c=4)
            cp(s, "act" if half else "dve", xT, dst, pt, src)
    s.end_phase()
    s.phase_es = old


def ln_epilogue(s, cx, y_ps, hf, g_bc, b_bc, wk, out_t):
    r, stats, mv, sd, rstd, nmr, xn = wk
    for half in range(2):
        sl = slice(half * 512, (half + 1) * 512)
        yt, yap = y_ps[half] if isinstance(y_ps[half], tuple) else (y_ps[half], y_ps[half][:, :])
        stt(s, "dve", r, r[:, sl], hf, hf[:, sl], ALPHA, yt, yap, ALU.mult, ALU.add)
        s.op("dve", lambda e, half=half, sl=sl: e.bn_stats(out=stats[:, half, :], in_=r[:, sl]), reads=[r], writes=[stats])
    s.op("dve", lambda e: e.bn_aggr(out=mv[:, :], in_=stats[:, :, :].rearrange("p a b -> p (a b)")), reads=[stats], writes=[mv])
    ts(s, "dve", sd, sd[:, :], mv, mv[:, 1:2], LN_EPS, None, ALU.add)
    act(s, sd, sd[:, :], sd, sd[:, :], AF.Sqrt)
    s.op("dve", lambda e: e.reciprocal(out=rstd[:, :], in_=sd[:, :]), reads=[sd], writes=[rstd])
    stt(s, "dve", nmr, nmr[:, :], mv, mv[:, 0:1], -1.0, rstd, rstd[:, :], ALU.mult, ALU.mult)
    act(s, xn, xn[:, :], r, r[:, :], AF.Identity, scale=rstd[:, 0:1], bias=nmr[:, 0:1], extra_reads=[rstd, nmr])
    tt(s, "pool", xn, xn[:, :], xn, xn[:, :], g_bc, g_bc[:, :], ALU.mult)
    tt(s, "pool", out_t, out_t[:, :], xn, xn[:, :], b_bc, b_bc[:, :], ALU.add)


def ln_work(s):
    return (s.sb([128, 1024], F32, "lnr"), s.sb([128, 2, 6], F32, "lnst"), s.sb([128, 2], F32, "lnmv"),
            s.sb([128, 1], F32, "lnsd"), s.sb([128, 1], F32, "lnrs"), s.sb([128, 1], F32, "lnnm"),
            s.sb([128, 1024], F32, "lnxn"))


def bcast_row(ap_row, n):
    return ap_row.partition_broadcast(128)


def phase_A(s, cx, dr, l, j, h_in, h_out):
    nc = s.nc
    h_in_res, h_in_ap = h_in[0], h_in[1]
    w_in = dr["a_w_in"][j]
    w_out = dr["a_w_out"][j]
    w_in_v = w_in.rearrange("(c p) n -> p c n", p=128)
    qkT_res, qkT = dr["qkT"]
    attT_res, attT = dr["attT"]
    wres = dr["wres"]

    s.begin_phase()
    xT = s.sb([128, 8, S], BF16, "xT")
    load_xT(s, cx, h_in_res, h_in_ap, xT)

    import os
    stop = int(os.environ.get("KSTOP", "9"))
    if stop < 1:
        s.end_phase()
        return
    with ExitStack() as es1:
        old = s.phase_es
        s.phase_es = es1
        wqk = s.sb([128, 8, 2048], BF16, "wqk")
        for c in range(8):
            wload(s, cx, wqk, wqk[:, c, :], wres, w_in[c * 128:(c + 1) * 128, 0:2048], [2048])
        rc = s.sb([128, 4], F32, "ropec")
        ld(s, "sp", rc, rc[:, :], wres, dr["rope_consts"])
        posi = s.sb([128, 512], I32, "posi")
        posf = s.sb([128, 512], F32, "posf")
        ang = s.sb([128, 512], F32, "ang")
        ang2 = s.sb([128, 512], F32, "ang2")
        Ct = s.sb([128, 512], F32, "Ct")
        St = s.sb([128, 512], F32, "St")
        scr = (s.sb([128, 512], I32, "ki"), s.sb([128, 512], F32, "kf"), s.sb([128, 512], F32, "rr"))
        psq = [s.ps([128, 512], F32, "psq") for _ in range(2)]
        psp = [s.ps([128, 512], F32, "psp") for _ in range(2)]
        qbs = [s.sb([128, 512], BF16, "qb") for _ in range(2)]
        t1s = [s.sb([128, 512], F32, "t1") for _ in range(2)]
        t2s = [s.sb([128, 512], F32, "t2") for _ in range(2)]
        obs = [s.sb([128, 512], BF16, "ob") for _ in range(3)]
        it = 0
        ksub = int(os.environ.get("KSUB", "99"))
        for tg in range(int(os.environ.get('KTG', '8')) if ksub > 0 else 0):
            tsl = slice(tg * 512, (tg + 1) * 512)
            ld(s, "sp", posi, posi[:, :], wres, bcast_row(dr["positions"][tg * 512:(tg + 1) * 512], 512))
            cp(s, "dve", posf, posf[:, :], posi, posi[:, :])
            ts(s, "dve", ang, ang[:, :], posf, posf[:, :], rc[:, 0:1], None, ALU.mult, extra_reads=[rc])
            ts(s, "dve", ang2, ang2[:, :], ang, ang[:, :], math.pi / 2, None, ALU.add)
            sin_table(s, St, St[:, :], ang, ang[:, :], None, rc[:, 1:2], rc, scr)
            sin_table(s, Ct, Ct[:, :], ang2, ang2[:, :], None, None, None, scr)
            for hp in range(int(os.environ.get('KHP', '8')) if ksub > 1 else 0):
                for w in range(2):
                    pq = psq[it % 2]
                    pp = psp[it % 2]
                    qb = qbs[it % 2]
                    t1 = t1s[it % 2]
                    t2 = t2s[it % 2]
                    ob = obs[it % 3]
                    it += 1
                    col = w * 1024 + hp * 128
                    for c in range(8):
                        mm(s, pq, pq[:, :], wqk, wqk[:, c, col:col + 128], xT, xT[:, c, tsl], c == 0, c == 7)
                    cp(s, "act", qb, qb[:, :], pq, pq[:, :])
                    if ksub < 3:
                        continue
                    mm(s, pp, pp[:, :], cx.perm, cx.perm[:, :], qb, qb[:, :], True, True)
                    tt(s, "dve", t1, t1[:, :], pq, pq[:, :], Ct, Ct[:, :], ALU.mult)
                    tt(s, "dve", t2, t2[:, :], pp, pp[:, :], St, St[:, :], ALU.mult)
                    tt(s, "pool", ob, ob[:, :], t1, t1[:, :], t2, t2[:, :], ALU.add)
                    if ksub < 4:
                        continue
                    st(s, "pool", qkT_res, qkT[w, hp * 128:(hp + 1) * 128, tsl], ob, ob[:, :])
        s.phase_es = old
    s_sub_barrier(s)
    if stop < 2:
        s.end_phase()
        return

    vsc_res, vsc = dr["vsc"]
    with ExitStack() as es2:
        old = s.phase_es
        s.phase_es = es2
        QT = s.sb([128, S], BF16, "QT")
        KT = s.sb([128, S], BF16, "KT")
        wv = s.sb([128, 8, 128], BF16, "wv")
        Vn = s.sb([128, 32, 128], BF16, "Vn")
        Vaugs = [s.sb([128, 32, 2, 128], BF16, "Vaug") for _ in range(2)]
        for Va in Vaugs:
            s.op("pool", lambda e, Va=Va: e.memset(Va[:, :, 0, 64:128], 1.0), writes=[Va], cost=3000)
            s.op("pool", lambda e, Va=Va: e.memset(Va[:, :, 1, 0:64], 1.0), writes=[Va], cost=3000)
        acc = s.sb([128, 2, S], F32, "acc")
        obuf = s.sb([128, S], BF16, "obuf")
        recs = [s.sb([128, 512], F32, "rec") for _ in range(2)]
        psV = [s.ps([128, 512], F32, "psV") for _ in range(2)]
        psS = [s.ps([128, 512], F32, "psS") for _ in range(4)]
        psO_ = [s.ps([128, 512], F32, "psO") for _ in range(2)]
        psO = [T(p[:, 0:256].rearrange("p (a q) -> p a q", a=2), p.res) for p in psO_]
        pes = [s.sb([128, 512], BF16, "pe") for _ in range(4)]
        PTs = [s.sb([128, 2, 2, 128], BF16, "PT") for _ in range(4)]
        vi = 0
        bi_ctr = 0
        k2 = int(os.environ.get("KSUB2", "99"))
        for hp in range(int(os.environ.get("KHP2", "8"))):
            ld(s, "sp", QT, QT[:, :], qkT_res, qkT[0, hp * 128:(hp + 1) * 128, :])
            ld(s, "sp", KT, KT[:, :], qkT_res, qkT[1, hp * 128:(hp + 1) * 128, :])
            wload(s, cx, wv, wv[:, :, :], wres, w_in_v[:, :, 2048 + hp * 128:2048 + (hp + 1) * 128], [8, 128])
            for b in range(32):
                pv = psV[(b // 4) % 2]
                for c in range(8):
                    mm(s, pv, pv[:, (b % 4) * 128:(b % 4 + 1) * 128], xT, xT[:, c, b * 128:(b + 1) * 128], wv, wv[:, c, :], c == 0, c == 7)
                if b % 4 == 3:
                    cp(s, "act" if (b // 4) % 2 else "dve", Vn, Vn[:, b - 3:b + 1, :], pv,
                       pv[:, :].rearrange("p (b f) -> p b f", b=4))
            vs = vsc[hp % 2]
            st(s, "sp", vsc_res, vs.rearrange("(i p) c -> p i c", p=128), Vn, Vn[:, :, :])
            for di, d in enumerate((1, 4, 16)[:int(os.environ.get("KBR", "3"))] if k2 > 0 else ()):
                nb = 32 // d
                Vaug = Vaugs[vi % 2]
                vi += 1

                def tok(r, n, d=d):
                    return slice(r + d * 128 * n, r + d * 128 * n + 127 * d + 1, d)
                if d > 1:
                    view = vs.rearrange("(n p r) c -> p r n c", p=128, r=d)
                    for r in range(d):
                        s.dma("sp" if r % 2 else "act", lambda e, r=r, nb=nb, view=view: e.dma_start(
                            out=Vn[:, r * nb:(r + 1) * nb, :], in_=view[:, r, :, :]),
                            reads=[vsc_res], writes=[Vn], nbytes=4 * nb * 32768)
                cp(s, "act", Vaug, Vaug[:, :, 0, 0:64], Vn, Vn[:, :, 0:64])
                cp(s, "act", Vaug, Vaug[:, :, 1, 64:128], Vn, Vn[:, :, 64:128])
                for b2 in range(int(os.environ.get("KB", "16")) if k2 > 1 else 0):
                    blocks = (2 * b2, 2 * b2 + 1)
                    n0 = blocks[0] % nb
                    PTh = []
                    for hh in range(2):
                        hs = slice(hh * 64, (hh + 1) * 64)
                        pS = psS[(2 * bi_ctr + hh) % 4]
                        pe_ = pes[(2 * bi_ctr + hh) % 4]
                        PT = PTs[(2 * bi_ctr + hh) % 4]
                        PTh.append(PT)
                        pS4 = pS[:, :].rearrange("p (b c q) -> p b c q", b=2, c=2)
                        pe4 = pe_[:, :].rearrange("p (b c q) -> p b c q", b=2, c=2)
                        for bi, b in enumerate(blocks):
                            r, n = divmod(b, nb)
                            mm(s, pS, pS4[:, bi, 0, :], KT, KT[hs, tok(r, n)], QT, QT[hs, tok(r, n)], True, True)
                            if n > 0:
                                mm(s, pS, pS4[:, bi, 1, :], KT, KT[hs, tok(r, n - 1)], QT, QT[hs, tok(r, n)], True, True)
                        if n0 > 0:
                            act(s, pe_, pe_[:, :], pS, pS[:, :], AF.Exp, scale=0.125)
                            tt(s, "pool", PT, PT[:, :, :, :], pe_, pe4, cx.maskA, cx.maskA[:, :, :, :], ALU.mult)
                        else:
                            act(s, pe_, pe4[:, 0, 0, :], pS, pS4[:, 0, 0, :], AF.Exp, scale=0.125)
                            act(s, pe_, pe4[:, 1, :, :], pS, pS4[:, 1, :, :], AF.Exp, scale=0.125)
                            tt(s, "pool", PT, PT[:, 0, 0, :], pe_, pe4[:, 0, 0, :], cx.maskA, cx.maskA[:, 0, 0, :], ALU.mult)
                            tt(s, "pool", PT, PT[:, 1, :, :], pe_, pe4[:, 1, :, :], cx.maskA, cx.maskA[:, 1, :, :], ALU.mult)
                    bi_ctr += 1
                    if k2 < 3:
                        continue
                    for bi, b in enumerate(blocks):
                        r, n = divmod(b, nb)
                        pO = psO[b % 2]
                        for hh in range(2):
                            PT = PTh[hh]
                            mm(s, pO, pO[:, hh, :], Vaug, Vaug[:, b, hh, :], PT, PT[:, bi, 0, :], True, n == 0)
                            if n > 0:
                                mm(s, pO, pO[:, hh, :], Vaug, Vaug[:, b - 1, hh, :], PT, PT[:, bi, 1, :], False, True)
                        if k2 < 4:
                            continue
                        if di == 0:
                            cp(s, "dve", acc, acc[:, :, tok(r, n)], pO, pO[:, :, :])
                        else:
                            tt(s, "dve", acc, acc[:, :, tok(r, n)], acc, acc[:, :, tok(r, n)], pO, pO[:, :, :], ALU.add)
            if k2 < 5:
                continue
            for tg in range(8):
                tsl = slice(tg * 512, (tg + 1) * 512)
                pd = psV[tg % 2]
                rec = recs[tg % 2]
                mm(s, pd, pd[:, :], cx.swlo, cx.swlo[:, :], acc, acc[:, 1, tsl], True, False)
                mm(s, pd, pd[:, :], cx.swhi, cx.swhi[:, :], acc, acc[:, 0, tsl], False, True)
                s.op("dve", lambda e, rec=rec, pd=pd: e.reciprocal(out=rec[:, :], in_=pd[:, :]), reads=[pd], writes=[rec], cost=500)
                tt(s, "pool", obuf, obuf[0:64, tsl], acc, acc[0:64, 0, tsl], rec, rec[0:64, :], ALU.mult)
                tt(s, "pool", obuf, obuf[64:128, tsl], acc, acc[64:128, 1, tsl], rec, rec[64:128, :], ALU.mult)
            st(s, "sp", attT_res, attT[hp * 128:(hp + 1) * 128, :], obuf, obuf[:, :])
        s.phase_es = old
    s.end_phase()

    if stop < 3:
        return
    s.begin_phase()
    proj_ln(s, cx, dr, attT_res, attT, 8, w_out, h_in, h_out, dr["ln1_g"][l], dr["ln1_b"][l])
    s.end_phase()


def s_sub_barrier(s):
    es = s.phase_es
    s.phase_es = ExitStack()
    s.end_phase()
    s.phase_es = es


def proj_ln(s, cx, dr, aT_res, aT, nchunk, w_out, h_in, h_out, g_row, b_row):
    h_in_res, h_in_ap = h_in[0], h_in[1]
    h_out_res, h_out_ap, final = h_out
    wres = dr["wres"]
    wo = s.sb([128, nchunk, 1024], BF16, "wo")
    for c in range(nchunk):
        wload(s, cx, wo, wo[:, c, :], wres, w_out[c * 128:(c + 1) * 128, :], [1024])
    g_bc = s.sb([128, 1024], F32, "gbc")
    b_bc = s.sb([128, 1024], F32, "bbc")
    ld(s, "sp", g_bc, g_bc[:, :], wres, bcast_row(g_row, 1024))
    ld(s, "sp", b_bc, b_bc[:, :], wres, bcast_row(b_row, 1024))
    ats = [s.sb([128, nchunk, 512], BF16, "at") for _ in range(2)]
    hfs = [s.sb([128, 1024], F32, "hf2") for _ in range(2)]
    hos = [s.sb([128, 1024], F32, "ho") for _ in range(2)]
    psY = [[s.ps([128, 512], F32, "psY") for _ in range(2)] for _ in range(2)]
    wk = ln_work(s)
    aTv = aT[0:nchunk * 128, :].rearrange("(c p) t -> p c t", p=128)
    for tg in range(8):
        at = ats[tg % 2]
        ld(s, "sp", at, at[:, :, :], aT_res, aTv[:, :, tg * 512:(tg + 1) * 512])
        for ti in range(4):
            i = tg * 4 + ti
            hf = hfs[i % 2]
            ho = hos[i % 2]
            py = psY[i % 2]
            ld(s, "sp", hf, hf[:, :], h_in_res, h_in_ap[i * 128:(i + 1) * 128, :])
            for half in range(2):
                for c in range(nchunk):
                    mm(s, py[half], py[half][:, :], at, at[:, c, ti * 128:(ti + 1) * 128], wo,
                       wo[:, c, half * 512:(half + 1) * 512], c == 0, c == nchunk - 1)
            ln_epilogue(s, cx, py, hf, g_bc, b_bc, wk, ho)
            st(s, "pool", h_out_res, h_out_ap[i * 128:(i + 1) * 128, :], ho, ho[:, :], final=final)


def decay_tables(s, cx, LF, ig, nh, kscale_ln, EA, EB, ET, psX, DEC=None):
    n = NT * nh
    Fin = s.sb([128, NT, nh], F32, "Fin")
    tot = s.sb([128, NT, nh], F32, "tot")
    inc = s.sb([128, NT, nh], F32, "inc")
    LF2 = LF[:, :, :].rearrange("p a b -> p (a b)")
    mm(s, psX, psX[:, 0:n], cx.tri_le, cx.tri_le[:, :], LF, LF2, True, True)
    cp(s, "dve", Fin, Fin[:, :, :].rearrange("p a b -> p (a b)"), psX, psX[:, 0:n])
    mm(s, psX, psX[:, 0:n], cx.ones_f, cx.ones_f[:, :], LF, LF2, True, True)
    cp(s, "dve", tot, tot[:, :, :].rearrange("p a b -> p (a b)"), psX, psX[:, 0:n])
    for h in range(nh):
        s.op("dve", lambda e, h=h: e.tensor_tensor_scan(out=inc[:, :, h], data0=cx.ones_f[:, 0:NT], data1=tot[:, :, h],
                                                        initial=0.0, op0=ALU.mult, op1=ALU.add),
             reads=[cx.ones_f, tot], writes=[inc])
    tt(s, "dve", EB, EB[:, :, :], inc, inc[:, :, :], tot, tot[:, :, :], ALU.subtract)
    tt(s, "dve", EA, EA[:, :, :], Fin, Fin[:, :, :], EB, EB[:, :, :], ALU.add)
    if ig is not None:
        tt(s, "dve", EA, EA[:, :, :], ig[0], ig[1], EA, EA[:, :, :], ALU.subtract)
        ts(s, "dve", EA, EA[:, :, :], EA, EA[:, :, :], kscale_ln, None, ALU.add)
    else:
        ts(s, "dve", EA, EA[:, :, :], EA, EA[:, :, :], -1.0, kscale_ln, ALU.mult, ALU.add)
    act(s, ET, ET[:, :, :], Fin, Fin[:, :, :], AF.Exp)
    if DEC is not None:
        act(s, DEC, DEC[:, :, :], tot, tot[:, :, :], AF.Exp)


def make_Ediag(s, Ediag, Enext, EA, EB, h, tmp2):
    tt(s, "dve", tmp2, tmp2[:, :], EA, EA[:, :, h], EB, EB[:, :, h], ALU.add)
    ts(s, "dve", tmp2, tmp2[:, :], tmp2, tmp2[:, :], 80.0, None, ALU.min)
    act(s, Ediag, Ediag[:, :], tmp2, tmp2[:, :], AF.Exp)
    tt(s, "dve", tmp2, tmp2[:, 0:NT - 1], EA, EA[:, 0:NT - 1, h], EB, EB[:, 1:NT, h], ALU.add)
    ts(s, "dve", tmp2, tmp2[:, 0:NT - 1], tmp2, tmp2[:, 0:NT - 1], 80.0, None, ALU.min)
    act(s, Enext, Enext[:, 0:NT - 1], tmp2, tmp2[:, 0:NT - 1], AF.Exp)


def make_E(s, E, EA, EB, h, tmpE):
    tt(s, "pool", tmpE, tmpE[:, :, :], EA, EA[:, :, h].unsqueeze(2).to_broadcast([128, NT, NT]),
       EB, EB[:, :, h].unsqueeze(1).to_broadcast([128, NT, NT]), ALU.add)
    ts(s, "pool", tmpE, tmpE[:, :, :], tmpE, tmpE[:, :, :], 80.0, None, ALU.min)
    act(s, E, E[:, :, :], tmpE, tmpE[:, :, :], AF.Exp)


def decay_main(s, cx, heads, psS, psAcc, PTs, fin_cb, jmax=NT):
    cnt = [0] * len(heads)
    for j in range(jmax):
        for hi, hd in enumerate(heads):
            acc = psAcc[hi]
            dvw = hd["dvw"]
            nk = len(hd["kparts"])
            for i0 in range(0, j + 1, 4):
                i1 = min(i0 + 4, j + 1)
                n = i1 - i0
                pS = psS[hi][cnt[hi] % 2]
                PT = PTs[hi][cnt[hi] % 2]
                cnt[hi] += 1
                for ii in range(n):
                    i = i0 + ii
                    for kc, (Kt, Kap, Qt, Qap) in enumerate(hd["kparts"]):
                        mm(s, pS, pS[:, ii * 128:(ii + 1) * 128], Kt, Kap(i), Qt, Qap(j), kc == 0, kc == nk - 1)
                E = hd["E"]
                tt(s, "dve", PT, PT[:, 0:n, :], pS, pS[:, 0:n * 128].rearrange("p (a t) -> p a t", a=n),
                   E, E[:, i0:i1, j].unsqueeze(2).to_broadcast([128, n, 128]), ALU.mult)
                if i1 - 1 == j:
                    tt(s, "pool", PT, PT[:, n - 1, :], PT, PT[:, n - 1, :], cx.tri_b, cx.tri_b[:, :], ALU.mult)
                for ii in range(n):
                    i = i0 + ii
                    mm(s, acc, acc[:, 0:dvw], PT, PT[:, ii, :], hd["V"], hd["Vap"](i), i == 0, i == j)
            fin_cb(hi, hd, j, acc)


def small_norm(s, x_t, x_ap, width, wk, out_t, out_ap, g_t, g_ap):
    stats, mv, sd, rstd, nmr, xn = wk
    s.op("dve", lambda e: e.bn_stats(out=stats[:, :], in_=x_ap), reads=[x_t], writes=[stats])
    s.op("dve", lambda e: e.bn_aggr(out=mv[:, :], in_=stats[:, :]), reads=[stats], writes=[mv])
    ts(s, "dve", sd, sd[:, :], mv, mv[:, 1:2], LN_EPS, None, ALU.add)
    act(s, sd, sd[:, :], sd, sd[:, :], AF.Sqrt)
    s.op("dve", lambda e: e.reciprocal(out=rstd[:, :], in_=sd[:, :]), reads=[sd], writes=[rstd])
    stt(s, "dve", nmr, nmr[:, :], mv, mv[:, 0:1], -1.0, rstd, rstd[:, :], ALU.mult, ALU.mult)
    act(s, xn, xn[:, 0:width], x_t, x_ap, AF.Identity, scale=rstd[:, 0:1], bias=nmr[:, 0:1], extra_reads=[rstd, nmr])
    tt(s, "pool", out_t, out_ap, xn, xn[:, 0:width], g_t, g_ap, ALU.mult)


def small_norm_work(s, width):
    return (s.sb([128, 6], F32, "snst"), s.sb([128, 2], F32, "snmv"), s.sb([128, 1], F32, "snsd"),
            s.sb([128, 1], F32, "snrs"), s.sb([128, 1], F32, "snnm"), s.sb([128, width], F32, "snxn"))


def gate_proj_ln(s, cx, dr, xT, HN_res, HN, F, w_gate_ap, gfunc, w_out, h_in, h_out, g_row, b_row):
    h_in_res, h_in_ap = h_in[0], h_in[1]
    h_out_res, h_out_ap, final = h_out
    wres = dr["wres"]
    nch = F // 128
    nb = F // 512
    wg = s.sb([128, 8, F], BF16, "wgate")
    for c in range(8):
        for q in range(F // 1024):
            wload(s, cx, wg, wg[:, c, q * 1024:(q + 1) * 1024], wres, w_gate_ap[c * 128:(c + 1) * 128, q * 1024:(q + 1) * 1024], [1024])
    wo = s.sb([128, nch, 1024], BF16, "wo")
    for c in range(nch):
        wload(s, cx, wo, wo[:, c, :], wres, w_out[c * 128:(c + 1) * 128, :], [1024])
    g_bc = s.sb([128, 1024], F32, "gbc")
    b_bc = s.sb([128, 1024], F32, "bbc")
    ld(s, "sp", g_bc, g_bc[:, :], wres, bcast_row(g_row, 1024))
    ld(s, "sp", b_bc, b_bc[:, :], wres, bcast_row(b_row, 1024))
    nbuf = 1 if F > 1024 else 2
    hns = [s.sb([128, F], F32, "hn") for _ in range(nbuf)]
    sig = s.sb([128, F], F32, "sig")
    Gb = s.sb([128, F], BF16, "Gb")
    GT_ = s.sb([128, nch, 128], BF16, "GTt")
    hfs = [s.sb([128, 1024], F32, "hf2") for _ in range(nbuf)]
    hos = [s.sb([128, 1024], F32, "ho") for _ in range(nbuf)]
    psg = [s.ps([128, 512], F32, "psg") for _ in range(2)]
    pst = [s.ps([128, 1024], BF16, "pst") for _ in range(2)]
    psY = [s.ps([128, 512], F32, "psY") for _ in range(2)]
    if s.nfill > 0:
        pfill = s.ps([128, 512], F32, "pfill")
        s.fill = (pfill[:, 0:256], cx.ident_b[:, :], cx.ones_b[:, 0:128].to_broadcast([128, 256]) if False else cx.maskA[:, 0, :, :].rearrange("p a q -> p (a q)"))
    wk = ln_work(s)
    for i in range(NT):
        hn = hns[i % nbuf]
        hf = hfs[i % nbuf]
        ho = hos[i % nbuf]
        ld(s, "sp", hn, hn[:, :], HN_res, HN[i * 128:(i + 1) * 128, 0:F])
        ld(s, "sp", hf, hf[:, :], h_in_res, h_in_ap[i * 128:(i + 1) * 128, :])
        for q in range(nb):
            pg = psg[q % 2]
            for c in range(8):
                mm(s, pg, pg[:, :], xT, xT[:, c, i * 128:(i + 1) * 128], wg, wg[:, c, q * 512:(q + 1) * 512], c == 0, c == 7)
            act(s, sig, sig[:, q * 512:(q + 1) * 512], pg, pg[:, :], gfunc)
        tt(s, "pool", Gb, Gb[:, :], hn, hn[:, :], sig, sig[:, :], ALU.mult)
        for q in range(nch // 8):
            pt = pst[q % 2]
            for c8 in range(8):
                c = q * 8 + c8
                s.op("pe", lambda e, pt=pt, c8=c8, c=c: e.transpose(out=pt[:, c8 * 128:(c8 + 1) * 128], in_=Gb[:, c * 128:(c + 1) * 128],
                                                                   identity=cx.ident_b[:, :]), reads=[Gb, cx.ident_b], writes=[pt])
            cp(s, "act" if q else "dve", GT_, GT_[:, q * 8:(q + 1) * 8, :], pt, pt[:, :].rearrange("p (c t) -> p c t", c=8))
        for half in range(2):
            for c in range(nch):
                mm(s, psY[half], psY[half][:, :], GT_, GT_[:, c, :], wo, wo[:, c, half * 512:(half + 1) * 512], c == 0, c == nch - 1)
        ln_epilogue(s, cx, psY, hf, g_bc, b_bc, wk, ho)
        st(s, "pool", h_out_res, h_out_ap[i * 128:(i + 1) * 128, :], ho, ho[:, :], final=final)


def phase_B(s, cx, dr, l, j_, h_in, h_out):
    import os
    nc = s.nc
    h_in_res, h_in_ap = h_in[0], h_in[1]
    wres = dr["wres"]
    w_in = dr["b_w_in"][j_]
    w_in_v = w_in.rearrange("(c p) n -> p c n", p=128)
    HN_res, HN = dr["hn"]
    bstop = int(os.environ.get("BSTOP", "9"))
    jmax = int(os.environ.get("BJMAX", str(NT)))
    npair = int(os.environ.get("BPAIRS", "4"))

    s.begin_phase()
    xT = s.sb([128, 8, S], BF16, "xT")
    GATES = s.sb([128, NT, 16], F32, "GATES")
    with ExitStack() as es0:
        old = s.phase_es
        s.phase_es = es0
        Wg32 = s.sb([128, 8, 16], F32, "Wg32")
        ld(s, "sp", Wg32, Wg32[:, :, :], wres, w_in_v[:, :, 3072:3088])
        gb = s.sb([128, 16], F32, "gbias")
        ld(s, "sp", gb, gb[:, :], wres, bcast_row(dr["b_gate_bias"][j_], 16))
        hfs = [s.sb([128, 1024], F32, "hf") for _ in range(3)]
        xTts = [s.sb([128, 8, 128], F32, "xTt") for _ in range(2)]
        pts = [s.ps([128, 512], F32, "ptr") for _ in range(2)]
        psg = s.ps([128, 512], F32, "psgate")
        for i in range(NT):
            hf = hfs[i % 3]
            xTt = xTts[i % 2]
            ld(s, "sp", hf, hf[:, :], h_in_res, h_in_ap[i * 128:(i + 1) * 128, :])
            for half in range(2):
                pt = pts[half]
                for c4 in range(4):
                    c = half * 4 + c4
                    s.op("pe", lambda e, pt=pt, c4=c4, c=c, hf=hf: e.transpose(
                        out=pt[:, c4 * 128:(c4 + 1) * 128], in_=hf[:, c * 128:(c + 1) * 128], identity=cx.ident_f[:, :]),
                        reads=[hf, cx.ident_f], writes=[pt])
                src = pt[:, :].rearrange("p (c t) -> p c t", c=4)
                cp(s, "act", xT, xT[:, half * 4:(half + 1) * 4, i * 128:(i + 1) * 128], pt, src)
                cp(s, "dve", xTt, xTt[:, half * 4:(half + 1) * 4, :], pt, src)
            for c in range(8):
                mm(s, psg, psg[:, 0:16], xTt, xTt[:, c, :], Wg32, Wg32[:, c, :], c == 0, c == 7)
            tt(s, "dve", GATES, GATES[:, i, :], psg, psg[:, 0:16], gb, gb[:, :], ALU.add)
        s.phase_es = old
    s_sub_barrier(s)

    EA = s.sb([128, NT, 8], F32, "EA")
    EB = s.sb([128, NT, 8], F32, "EB")
    ET = s.sb([128, NT, 8], F32, "ET")
    DEC = s.sb([128, NT, 8], F32, "DEC")
    with ExitStack() as es1:
        old = s.phase_es
        s.phase_es = es1
        LF = s.sb([128, NT, 8], F32, "LF")
        psX = s.ps([128, 512], F32, "psX")
        act(s, LF, LF[:, :, :], GATES, GATES[:, :, 8:16], AF.Exp, scale=-1.0)
        ts(s, "dve", LF, LF[:, :, :], LF, LF[:, :, :], 1.0, None, ALU.add)
        act(s, LF, LF[:, :, :], LF, LF[:, :, :], AF.Ln)
        ts(s, "dve", LF, LF[:, :, :], LF, LF[:, :, :], -1.0, None, ALU.mult)
        decay_tables(s, cx, LF, (GATES, GATES[:, :, 0:8]), 8, math.log(0.125), EA, EB, ET, psX, DEC)
        s.phase_es = old
    s_sub_barrier(s)
    if bstop < 2:
        s.end_phase()
        return

    with ExitStack() as es2:
        old = s.phase_es
        s.phase_es = es2
        ng_bc = s.sb([128, 1024], F32, "ngbc")
        ld(s, "sp", ng_bc, ng_bc[:, :], wres, bcast_row(dr["b_norm_g"][j_], 1024))
        wq = s.sb([128, 8, 128], BF16, "wq")
        wkk = s.sb([128, 8, 128], BF16, "wk")
        wv = s.sb([128, 8, 256], BF16, "wv")
        cw = [s.sb([128, 4], F32, "cw") for _ in range(2)]
        cb = [s.sb([128, 1], F32, "cb") for _ in range(2)]
        UT = s.sb([128, S + 3], F32, "UT")
        s.op("pool", lambda e: e.memset(UT[:, 0:3], 0.0), writes=[UT])
        cacc = s.sb([128, S], F32, "cacc")
        QT = s.sb([128, S], BF16, "QT")
        KT = s.sb([128, S], BF16, "KT")
        Vaug = s.sb([128, NT, 2, 129], BF16, "Vaug")
        s.op("pool", lambda e: e.memset(Vaug[:, :, :, 128:129], 1.0), writes=[Vaug])
        Edg = [s.sb([128, NT], F32, "Edg") for _ in range(2)]
        Enx = [s.sb([128, NT], F32, "Enx") for _ in range(2)]
        tmp2 = s.sb([128, NT], F32, "tmp2")
        Dp = s.sb([128, NT], F32, "Dp")
        R32 = s.sb([128, 129], F32, "R32")
        Rb = s.sb([128, 129], BF16, "Rb")
        Ktss = [s.sb([128, 128], BF16, "Kts") for _ in range(2)]
        psX = [s.ps([128, 512], F32, "psX") for _ in range(2)]
        psS = [s.ps([128, 512], F32, "psS") for _ in range(2)]
        psAcc = [s.ps([128, 512], F32, "psAcc") for _ in range(2)]
        psT = s.ps([128, 1024], BF16, "psTk")
        psKV = s.ps([128, 512], F32, "psKV")
        PTs = [[s.sb([128, 1, 128], BF16, "PT") for _ in range(2)] for _ in range(2)]
        snw = small_norm_work(s, 128)
        sm = {k: s.sb([128, 1], F32, k) for k in ("ed", "ad", "rec", "fac")}
        hnr = s.sb([128, 128], F32, "hnr")
        HNt = [s.sb([128, 256], F32, "HNt") for _ in range(2)]
        for hp in range(npair):
            for w, (wt, dst) in enumerate(((wq, QT), (wkk, KT))):
                col = w * 512 + hp * 128
                wload(s, cx, wt, wt[:, :, :], wres, w_in_v[:, :, col:col + 128], [8, 128])
                for jj in range(4):
                    ld(s, "sp", cw[w], cw[w][:, jj:jj + 1], wres, dr["b_conv_w"][j_][jj, col:col + 128].rearrange("(c o) -> c o", o=1))
                ld(s, "sp", cb[w], cb[w][:, :], wres, dr["b_conv_b"][j_][col:col + 128].rearrange("(c o) -> c o", o=1))
                for tg in range(8):
                    px = psX[tg % 2]
                    for c in range(8):
                        mm(s, px, px[:, :], wt, wt[:, c, :], xT, xT[:, c, tg * 512:(tg + 1) * 512], c == 0, c == 7)
                    cp(s, "act", UT, UT[:, 3 + tg * 512:3 + (tg + 1) * 512], px, px[:, :])
                ts(s, "dve", cacc, cacc[:, :], UT, UT[:, 0:S], cw[w][:, 0:1], None, ALU.mult, extra_reads=[cw[w]])
                for jj in range(1, 4):
                    stt(s, "dve", cacc, cacc[:, :], UT, UT[:, jj:S + jj], cw[w][:, jj:jj + 1], cacc, cacc[:, :], ALU.mult, ALU.add,
                        extra_reads=[cw[w]])
                act(s, dst, dst[:, :], cacc, cacc[:, :], AF.Silu, scale=1.0, bias=cb[w][:, 0:1], extra_reads=[cb[w]])
            wload(s, cx, wv, wv[:, :, :], wres, w_in_v[:, :, 1024 + hp * 256:1024 + (hp + 1) * 256], [8, 256])
            for i in range(NT):
                px = psX[i % 2]
                for c in range(8):
                    mm(s, px, px[:, 0:256], xT, xT[:, c, i * 128:(i + 1) * 128], wv, wv[:, c, :], c == 0, c == 7)
                cp(s, "act", Vaug, Vaug[:, i, :, 0:128], px, px[:, 0:256].rearrange("p (a d) -> p a d", a=2))
            heads = []
            for hh in range(2):
                h = hp * 2 + hh
                make_Ediag(s, Edg[hh], Enx[hh], EA, EB, h, tmp2)
                hs = slice(hh * 64, (hh + 1) * 64)
                cp(s, "pool", Dp, Dp[hs, :], DEC, DEC[hs, :, h])
                heads.append(dict(h=h, hh=hh))
            s.op("pool", lambda e: e.memset(R32[:, :], 0.0), writes=[R32])
            s.op("pool", lambda e: e.memset(Rb[:, :], 0.0), writes=[Rb])

            def fin(hi, hd, j, acc):
                h, hh = hd["h"], hd["hh"]
                tt(s, "dve", sm["ed"], sm["ed"][:, :], acc, acc[:, 128:129], ET, ET[:, j, h:h + 1], ALU.mult)
                stt(s, "dve", sm["ad"], sm["ad"][:, :], sm["ed"], sm["ed"][:, :], -1.0, sm["ed"], sm["ed"][:, :], ALU.mult, ALU.max)
                ts(s, "dve", sm["ad"], sm["ad"][:, :], sm["ad"], sm["ad"][:, :], 1.0, None, ALU.max)
                s.op("dve", lambda e: e.reciprocal(out=sm["rec"][:, :], in_=sm["ad"][:, :]), reads=[sm["ad"]], writes=[sm["rec"]])
                tt(s, "dve", sm["fac"], sm["fac"][:, :], sm["rec"], sm["rec"][:, :], ET, ET[:, j, h:h + 1], ALU.mult)
                act(s, hnr, hnr[:, :], acc, acc[:, 0:128], AF.Copy, scale=sm["fac"][:, 0:1], extra_reads=[sm["fac"]])
                ht = HNt[j % 2]
                small_norm(s, hnr, hnr[:, :], 128, snw, ht, ht[:, hh * 128:(hh + 1) * 128], ng_bc, ng_bc[:, h * 128:(h + 1) * 128])
                if hh == 1:
                    st(s, "pool", HN_res, HN[j * 128:(j + 1) * 128, hp * 256:(hp + 1) * 256], ht, ht[:, :])
            for j in range(jmax):
                tj = slice(j * 128, (j + 1) * 128)
                for hh in range(2):
                    hs = slice(hh * 64, (hh + 1) * 64)
                    pS = psS[hh]
                    PT = PTs[hh][j % 2]
                    acc = psAcc[hh]
                    mm(s, pS, pS[:, 0:128], KT, KT[hs, tj], QT, QT[hs, tj], True, True)
                    ts(s, "dve", PT, PT[:, 0, :], pS, pS[:, 0:128], Edg[hh][:, j:j + 1], None, ALU.mult, extra_reads=[Edg[hh]])
                    tt(s, "pool", PT, PT[:, 0, :], PT, PT[:, 0, :], cx.tri_b, cx.tri_b[:, :], ALU.mult)
                    mm(s, acc, acc[:, 0:129], PT, PT[:, 0, :], Vaug, Vaug[:, j, hh, :], True, j == 0)
                    if j > 0:
                        mm(s, acc, acc[:, 0:129], QT, QT[hs, tj], Rb, Rb[hs, :], False, True)
                    fin(hh, heads[hh], j, acc)
                if j < jmax - 1:
                    s.op("pe", lambda e, tj=tj: e.transpose(out=psT[:, 0:128], in_=KT[:, tj], identity=cx.ident_b[:, :]),
                         reads=[KT, cx.ident_b], writes=[psT], cost=150)
                    Kts = Ktss[j % 2]
                    for hh in range(2):
                        hs = slice(hh * 64, (hh + 1) * 64)
                        act(s, Kts, Kts[:, hs], psT, psT[:, hs], AF.Copy, scale=Enx[hh][:, j:j + 1], extra_reads=[Enx[hh]])
                    for hh in range(2):
                        hs = slice(hh * 64, (hh + 1) * 64)
                        mm(s, psKV, psKV[hs, 0:129], Kts, Kts[:, hs], Vaug, Vaug[:, j, hh, :], True, True)
                    stt(s, "dve", R32, R32[:, :], R32, R32[:, :], Dp[:, j:j + 1], psKV, psKV[:, 0:129], ALU.mult, ALU.add, extra_reads=[Dp])
                    cp(s, "act", Rb, Rb[:, :], R32, R32[:, :])
        s.phase_es = old
    s_sub_barrier(s)
    if bstop < 3:
        s.end_phase()
        return

    with ExitStack() as es3:
        old = s.phase_es
        s.phase_es = es3
        gate_proj_ln(s, cx, dr, xT, HN_res, HN, 1024, w_in[:, 2048:3072], AF.Sigmoid, dr["b_w_out"][j_], h_in, h_out,
                     dr["ln1_g"][l], dr["ln1_b"][l])
        s.phase_es = old
    s.end_phase()


def phase_C(s, cx, dr, l, j_, h_in, h_out):
    import os
    nc = s.nc
    h_in_res, h_in_ap = h_in[0], h_in[1]
    wres = dr["wres"]
    w_in = dr["c_w_in"][j_]
    w_in_v = w_in.rearrange("(c p) n -> p c n", p=128)
    HN_res, HN = dr["hn"]
    qkT_res, qkT = dr["qkT"]
    cstop = int(os.environ.get("CSTOP", "9"))
    jmax = int(os.environ.get("CJMAX", str(NT)))
    nheads = int(os.environ.get("CHEADS", "4"))

    s.begin_phase()
    xT = s.sb([128, 8, S], BF16, "xT")
    load_xT(s, cx, h_in_res, h_in_ap, xT)
    EA = s.sb([128, NT, 4], F32, "EA")
    EB = s.sb([128, NT, 4], F32, "EB")
    ET = s.sb([128, NT, 4], F32, "ET")
    DEC = s.sb([128, NT, 4], F32, "DEC")
    with ExitStack() as es1:
        old = s.phase_es
        s.phase_es = es1
        LF = s.sb([128, NT, 4], F32, "LF")
        psX = s.ps([128, 512], F32, "psX")
        for h in range(4):
            s.op("pool", lambda e, h=h: e.memset(LF[:, :, h:h + 1], math.log(1.0 - 2.0 ** (-5.0 - h))), writes=[LF])
        decay_tables(s, cx, LF, None, 4, math.log(1.0 / 16.0), EA, EB, ET, psX, DEC)
        s.phase_es = old
    s_sub_barrier(s)

    with ExitStack() as es1:
        old = s.phase_es
        s.phase_es = es1
        wqk = s.sb([128, 8, 2048], BF16, "wqk")
        for c in range(8):
            wload(s, cx, wqk, wqk[:, c, :], wres, w_in[c * 128:(c + 1) * 128, 0:2048], [2048])
        rc = s.sb([128, 4], F32, "ropec")
        ld(s, "sp", rc, rc[:, :], wres, dr["rope_consts"])
        posi = s.sb([128, 512], I32, "posi")
        posf = s.sb([128, 512], F32, "posf")
        ang = s.sb([128, 512], F32, "ang")
        ang2 = s.sb([128, 512], F32, "ang2")
        Ct = s.sb([128, 512], F32, "Ct")
        St = s.sb([128, 512], F32, "St")
        scr = (s.sb([128, 512], I32, "ki"), s.sb([128, 512], F32, "kf"), s.sb([128, 512], F32, "rr"))
        psa = [s.ps([128, 512], F32, "psa") for _ in range(2)]
        psb = [s.ps([128, 512], F32, "psb") for _ in range(2)]
        t1s = [s.sb([128, 512], F32, "t1") for _ in range(2)]
        t2s = [s.sb([128, 512], F32, "t2") for _ in range(2)]
        oas = [s.sb([128, 512], BF16, "oa") for _ in range(2)]
        obs = [s.sb([128, 512], BF16, "ob") for _ in range(2)]
        it = 0
        for tg in range(8):
            tsl = slice(tg * 512, (tg + 1) * 512)
            ld(s, "sp", posi, posi[:, :], wres, bcast_row(dr["positions"][tg * 512:(tg + 1) * 512], 512))
            cp(s, "dve", posf, posf[:, :], posi, posi[:, :])
            ts(s, "dve", ang, ang[:, :], posf, posf[:, :], rc[:, 2:3], None, ALU.mult, extra_reads=[rc])
            ts(s, "dve", ang2, ang2[:, :], ang, ang[:, :], math.pi / 2, None, ALU.add)
            sin_table(s, St, St[:, :], ang, ang[:, :], None, None, None, scr)
            sin_table(s, Ct, Ct[:, :], ang2, ang2[:, :], None, None, None, scr)
            for h in range(4):
                for w in range(2):
                    pa, pb = psa[it % 2], psb[it % 2]
                    t1, t2, oa, ob = t1s[it % 2], t2s[it % 2], oas[it % 2], obs[it % 2]
                    it += 1
                    col = w * 1024 + h * 256
                    for c in range(8):
                        mm(s, pa, pa[:, :], wqk, wqk[:, c, col:col + 128], xT, xT[:, c, tsl], c == 0, c == 7)
                    for c in range(8):
                        mm(s, pb, pb[:, :], wqk, wqk[:, c, col + 128:col + 256], xT, xT[:, c, tsl], c == 0, c == 7)
                    tt(s, "dve", t1, t1[:, :], pa, pa[:, :], Ct, Ct[:, :], ALU.mult)
                    tt(s, "dve", t2, t2[:, :], pb, pb[:, :], St, St[:, :], ALU.mult)
                    tt(s, "pool", oa, oa[:, :], t1, t1[:, :], t2, t2[:, :], ALU.subtract)
                    st(s, "pool", qkT_res, qkT[w, h * 256:h * 256 + 128, tsl], oa, oa[:, :])
                    tt(s, "dve", t1, t1[:, :], pb, pb[:, :], Ct, Ct[:, :], ALU.mult)
                    tt(s, "dve", t2, t2[:, :], pa, pa[:, :], St, St[:, :], ALU.mult)
                    tt(s, "pool", ob, ob[:, :], t1, t1[:, :], t2, t2[:, :], ALU.add)
                    st(s, "pool", qkT_res, qkT[w, h * 256 + 128:h * 256 + 256, tsl], ob, ob[:, :])
        s.phase_es = old
    s_sub_barrier(s)
    if cstop < 2:
        s.end_phase()
        return

    with ExitStack() as es2:
        old = s.phase_es
        s.phase_es = es2
        ng_bc = s.sb([128, 2048], F32, "ngbc")
        ld(s, "sp", ng_bc, ng_bc[:, :], wres, bcast_row(dr["c_norm_g"][j_], 2048))
        QA = s.sb([128, S], BF16, "QA")
        QB = s.sb([128, S], BF16, "QB")
        KA = s.sb([128, S], BF16, "KA")
        KB = s.sb([128, S], BF16, "KB")
        wv = s.sb([128, 8, 512], BF16, "wv")
        Vh = s.sb([128, NT, 512], BF16, "Vh")
        Edg = s.sb([128, NT], F32, "Edg")
        Enx = s.sb([128, NT], F32, "Enx")
        tmp2 = s.sb([128, NT], F32, "tmp2")
        R32 = s.sb([128, 2, 512], F32, "R32")
        Rb = s.sb([128, 2, 512], BF16, "Rb")
        Ktss = [s.sb([128, 256], BF16, "Kts") for _ in range(2)]
        psX = [s.ps([128, 512], F32, "psX") for _ in range(2)]
        psS = [s.ps([128, 512], F32, "psS") for _ in range(2)]
        psAcc = s.ps([128, 512], F32, "psAcc")
        psT = s.ps([128, 1024], BF16, "psTk")
        psKV = [s.ps([128, 512], F32, "psKV") for _ in range(2)]
        PTs = [s.sb([128, 128], BF16, "PT") for _ in range(2)]
        snw = small_norm_work(s, 512)
        onr = s.sb([128, 512], F32, "onr")
        HNt = [s.sb([128, 512], F32, "HNt") for _ in range(2)]
        for h in range(nheads):
            for w, (ta, tb) in enumerate(((QA, QB), (KA, KB))):
                ld(s, "sp", ta, ta[:, :], qkT_res, qkT[w, h * 256:h * 256 + 128, :])
                ld(s, "sp", tb, tb[:, :], qkT_res, qkT[w, h * 256 + 128:h * 256 + 256, :])
            wload(s, cx, wv, wv[:, 0:4, :], wres, w_in_v[:, 0:4, 2048 + h * 512:2048 + (h + 1) * 512], [4, 512])
            wload(s, cx, wv, wv[:, 4:8, :], wres, w_in_v[:, 4:8, 2048 + h * 512:2048 + (h + 1) * 512], [4, 512])
            for i in range(NT):
                px = psX[i % 2]
                for c in range(8):
                    mm(s, px, px[:, :], xT, xT[:, c, i * 128:(i + 1) * 128], wv, wv[:, c, :], c == 0, c == 7)
                cp(s, "act", Vh, Vh[:, i, :], px, px[:, :])
            make_Ediag(s, Edg, Enx, EA, EB, h, tmp2)
            s.op("pool", lambda e: e.memset(R32[:, :, :], 0.0), writes=[R32], cost=2000)
            s.op("pool", lambda e: e.memset(Rb[:, :, :], 0.0), writes=[Rb], cost=2000)

            def fin(hi, hd, j, acc, h=h):
                act(s, onr, onr[:, :], acc, acc[:, 0:512], AF.Copy, scale=ET[:, j, h:h + 1], extra_reads=[ET])
                ht = HNt[j % 2]
                small_norm(s, onr, onr[:, :], 512, snw, ht, ht[:, :], ng_bc, ng_bc[:, h * 512:(h + 1) * 512])
                st(s, "pool", HN_res, HN[j * 128:(j + 1) * 128, h * 512:(h + 1) * 512], ht, ht[:, :])
            for j in range(jmax):
                tj = slice(j * 128, (j + 1) * 128)
                pS = psS[j % 2]
                PT = PTs[j % 2]
                acc = psAcc
                mm(s, pS, pS[:, 0:128], KA, KA[:, tj], QA, QA[:, tj], True, False)
                mm(s, pS, pS[:, 0:128], KB, KB[:, tj], QB, QB[:, tj], False, True)
                ts(s, "dve", PT, PT[:, :], pS, pS[:, 0:128], Edg[:, j:j + 1], None, ALU.mult, extra_reads=[Edg])
                tt(s, "pool", PT, PT[:, :], PT, PT[:, :], cx.tri_b, cx.tri_b[:, :], ALU.mult)
                mm(s, acc, acc[:, :], PT, PT[:, :], Vh, Vh[:, j, :], True, j == 0)
                if j > 0:
                    mm(s, acc, acc[:, :], QA, QA[:, tj], Rb, Rb[:, 0, :], False, False)
                    mm(s, acc, acc[:, :], QB, QB[:, tj], Rb, Rb[:, 1, :], False, True)
                fin(0, None, j, acc)
                if j < jmax - 1:
                    s.op("pe", lambda e, tj=tj: e.transpose(out=psT[:, 0:128], in_=KA[:, tj], identity=cx.ident_b[:, :]),
                         reads=[KA, cx.ident_b], writes=[psT], cost=150)
                    s.op("pe", lambda e, tj=tj: e.transpose(out=psT[:, 128:256], in_=KB[:, tj], identity=cx.ident_b[:, :]),
                         reads=[KB, cx.ident_b], writes=[psT], cost=150)
                    Kts = Ktss[j % 2]
                    act(s, Kts, Kts[:, :], psT, psT[:, 0:256], AF.Copy, scale=Enx[:, j:j + 1], extra_reads=[Enx])
                    for ck in range(2):
                        mm(s, psKV[ck], psKV[ck][:, :], Kts, Kts[:, ck * 128:(ck + 1) * 128], Vh, Vh[:, j, :], True, True)
                        stt(s, "dve", R32, R32[:, ck, :], R32, R32[:, ck, :], DEC[:, j, h:h + 1], psKV[ck], psKV[ck][:, :],
                            ALU.mult, ALU.add, extra_reads=[DEC])
                    cp(s, "act", Rb, Rb[:, 0, :], R32, R32[:, 0, :])
                    cp(s, "pool", Rb, Rb[:, 1, :], R32, R32[:, 1, :])
        s.phase_es = old
    s_sub_barrier(s)
    if cstop < 3:
        s.end_phase()
        return

    with ExitStack() as es3:
        old = s.phase_es
        s.phase_es = es3
        gate_proj_ln(s, cx, dr, xT, HN_res, HN, 2048, w_in[:, 4096:6144], AF.Silu, dr["c_w_out"][j_], h_in, h_out,
                     dr["ln1_g"][l], dr["ln1_b"][l])
        s.phase_es = old
    s.end_phase()


NSLOT = 128
BIGIDX = 4 * 64 * 128


def phase_F(s, cx, dr, l, h_in, h_out):
    import os
    nc = s.nc
    h_in_res, h_in_ap = h_in[0], h_in[1]
    h_out_res, h_out_ap, final = h_out
    wres = dr["wres"]
    xbf_res, xbf = dr["xbf"]
    bt_res, bt = dr["buftok"]
    yb_res, yb = dr["yb"]
    fstop = int(os.environ.get("FSTOP", "9"))

    s.begin_phase()
    OH1 = s.sb([128, NT, 64], F32, "OH1")
    OH2 = s.sb([128, NT, 64], F32, "OH2")
    RK = s.sb([128, NT, 2], F32, "RK")
    GT = s.sb([128, NT, 2], F32, "GT")
    DESTi = s.sb([128, NT, 2], I32, "DESTi")
    IDXW = s.sb([128, NSLOT], I32, "IDXW")
    breg_x = nc.gpsimd.to_reg(S - 1)
    breg_w = nc.gpsimd.to_reg((l + 1) * 64 * 128 - 1)
    breg_y = nc.gpsimd.to_reg(NSLOT * 128 - 1)
    breg_t = nc.gpsimd.to_reg(NSLOT * 128 - 1)

    with ExitStack() as es1:
        old = s.phase_es
        s.phase_es = es1
        Wr = s.sb([128, 8, 72], F32, "Wr")
        ld(s, "sp", Wr, Wr[:, :, 0:8], wres, dr["r_group_w"][l].rearrange("(c p) n -> p c n", p=128))
        ld(s, "sp", Wr, Wr[:, :, 8:72], wres, dr["r_expert_w"][l].rearrange("(c p) n -> p c n", p=128))
        bias = s.sb([128, 72], F32, "rbias")
        ld(s, "sp", bias, bias[:, 0:8], wres, bcast_row(dr["r_group_b"][l], 8))
        ld(s, "sp", bias, bias[:, 8:72], wres, bcast_row(dr["r_expert_b"][l], 64))
        tri_lt = s.sb([128, 128], BF16, "trilt")
        ts(s, "dve", tri_lt, tri_lt[:, :], cx.iof, cx.iof[:, :], 0.0, None, ALU.is_gt)
        toki = s.sb([128, NT], I32, "toki")
        s.op("pool", lambda e: e.iota(toki[:, :], pattern=[[128, NT]], base=0, channel_multiplier=1), writes=[toki])
        hfs = [s.sb([128, 1024], F32, "hfr") for _ in range(2)]
        hbs = [s.sb([128, 1024], BF16, "hbr") for _ in range(2)]
        xTts = [s.sb([128, 8, 128], F32, "xTt") for _ in range(2)]
        pts = [s.ps([128, 512], F32, "ptr") for _ in range(2)]
        psL = s.ps([128, 512], F32, "psL")
        psPC = s.ps([128, 512], F32, "psPC")
        LG = s.sb([128, NT, 72], F32, "LG")
        for i in range(NT):
            hf = hfs[i % 2]
            hb = hbs[i % 2]
            xTt = xTts[i % 2]
            ld(s, "sp", hf, hf[:, :], h_in_res, h_in_ap[i * 128:(i + 1) * 128, :])
            cp(s, "pool", hb, hb[:, :], hf, hf[:, :])
            st(s, "pool", xbf_res, xbf[i * 128:(i + 1) * 128, :], hb, hb[:, :])
            for half in range(2):
                pt = pts[half]
                for c4 in range(4):
                    c = half * 4 + c4
                    s.op("pe", lambda e, pt=pt, c4=c4, c=c, hf=hf: e.transpose(
                        out=pt[:, c4 * 128:(c4 + 1) * 128], in_=hf[:, c * 128:(c + 1) * 128], identity=cx.ident_f[:, :]),
                        reads=[hf, cx.ident_f], writes=[pt])
                cp(s, "act" if half else "dve", xTt, xTt[:, half * 4:(half + 1) * 4, :], pt,
                   pt[:, :].rearrange("p (c t) -> p c t", c=4))
            for c in range(8):
                mm(s, psL, psL[:, 0:72], xTt, xTt[:, c, :], Wr, Wr[:, c, :], c == 0, c == 7)
            tt(s, "dve", LG, LG[:, i, :], psL, psL[:, 0:72], bias, bias[:, :], ALU.add)
        def t2(name, n=NT):
            return s.sb([128, n], F32, name)
        gmax, sumg, pgrp, v1, v2, dv, ex, den, rec = [t2(k) for k in ("gmax", "sumg", "pgrp", "v1", "v2", "dv", "ex", "den", "rec")]
        ohg = s.sb([128, NT, 8], F32, "ohg")
        eg = s.sb([128, NT, 8], F32, "eg")
        pen = s.sb([128, NT, 8], F32, "pen")
        em = s.sb([128, NT, 64], F32, "em")
        em2 = s.sb([128, NT, 64], F32, "em2")
        Mb = s.sb([128, NT, 64], BF16, "Mb")
        G = LG[:, :, 0:8]

        def bc2(t, w):
            return t[:, :].unsqueeze(2).to_broadcast([128, NT, w])
        s.op("dve", lambda e: e.reduce_max(out=gmax[:, :], in_=G, axis=AX.X), reads=[LG], writes=[gmax], cost=400)
        tt(s, "dve", ohg, ohg[:, :, :], LG, G, gmax, bc2(gmax, 8), ALU.is_equal)
        tt(s, "dve", eg, eg[:, :, :], LG, G, gmax, bc2(gmax, 8), ALU.subtract)
        act(s, eg, eg[:, :, :], eg, eg[:, :, :], AF.Exp)
        s.op("dve", lambda e: e.reduce_sum(out=sumg[:, :], in_=eg[:, :, :], axis=AX.X), reads=[eg], writes=[sumg], cost=400)
        s.op("dve", lambda e: e.reciprocal(out=pgrp[:, :], in_=sumg[:, :]), reads=[sumg], writes=[pgrp])
        ts(s, "dve", pen, pen[:, :, :], ohg, ohg[:, :, :], -1.0, 1e30, ALU.add, ALU.mult)
        tt(s, "dve", em, em[:, :, :].rearrange("p i (g j) -> p i g j", g=8), LG, LG[:, :, 8:72].rearrange("p i (g j) -> p i g j", g=8),
           pen, pen[:, :, :].unsqueeze(3).to_broadcast([128, NT, 8, 8]), ALU.add)
        s.op("dve", lambda e: e.reduce_max(out=v1[:, :], in_=em[:, :, :], axis=AX.X), reads=[em], writes=[v1], cost=2000)
        tt(s, "dve", OH1, OH1[:, :, :], em, em[:, :, :], v1, bc2(v1, 64), ALU.is_equal)
        stt(s, "dve", em2, em2[:, :, :], OH1, OH1[:, :, :], -1e30, em, em[:, :, :], ALU.mult, ALU.add)
        s.op("dve", lambda e: e.reduce_max(out=v2[:, :], in_=em2[:, :, :], axis=AX.X), reads=[em2], writes=[v2], cost=2000)
        tt(s, "dve", OH2, OH2[:, :, :], em2, em2[:, :, :], v2, bc2(v2, 64), ALU.is_equal)
        tt(s, "dve", dv, dv[:, :], v2, v2[:, :], v1, v1[:, :], ALU.subtract)
        act(s, ex, ex[:, :], dv, dv[:, :], AF.Exp)
        ts(s, "dve", den, den[:, :], ex, ex[:, :], 1.0, None, ALU.add)
        s.op("dve", lambda e: e.reciprocal(out=rec[:, :], in_=den[:, :]), reads=[den], writes=[rec])
        tt(s, "dve", GT, GT[:, :, 0], pgrp, pgrp[:, :], rec, rec[:, :], ALU.mult)
        tt(s, "dve", GT, GT[:, :, 1], GT, GT[:, :, 0], ex, ex[:, :], ALU.mult)
        tt(s, "dve", Mb, Mb[:, :, :], OH1, OH1[:, :, :], OH2, OH2[:, :, :], ALU.add)
        TOT = s.sb([128, NT, 64], F32, "TOT")
        TOT2 = s.sb([128, NT, 64], F32, "TOT2")
        pos = s.sb([128, NT, 64], F32, "pos")
        psP = [s.ps([128, 512], F32, "psP") for _ in range(2)]
        Mb2 = Mb[:, :, :].rearrange("p i e -> p (i e)")
        for q in range(4):
            pp = psP[q % 2]
            mm(s, pp, pp[:, :], cx.ones_b, cx.ones_b[:, :], Mb, Mb2[:, q * 512:(q + 1) * 512], True, True)
            cp(s, "act", TOT, TOT[:, q * 8:(q + 1) * 8, :], pp, pp[:, :].rearrange("p (i e) -> p i e", i=8))
        cur_, oth_ = TOT, TOT2
        for k in (1, 2, 4, 8, 16):
            cp(s, "pool", oth_, oth_[:, 0:k, :], cur_, cur_[:, 0:k, :])
            tt(s, "dve", oth_, oth_[:, k:NT, :], cur_, cur_[:, k:NT, :], cur_, cur_[:, 0:NT - k, :], ALU.add)
            cur_, oth_ = oth_, cur_
        incl = cur_
        for q in range(4):
            pp = psP[q % 2]
            for i8 in range(8):
                i = q * 8 + i8
                mm(s, pp, pp[:, i8 * 64:(i8 + 1) * 64], tri_lt, tri_lt[:, :], Mb, Mb[:, i, :], True, True)
            tt(s, "dve", pos, pos[:, q * 8:(q + 1) * 8, :], pp, pp[:, :].rearrange("p (i e) -> p i e", i=8),
               incl, incl[:, q * 8:(q + 1) * 8, :], ALU.add)
        own = s.sb([128, NT, 64], F32, "own")
        for q in range(4):
            pp = psP[q % 2]
            mm(s, pp, pp[:, :], cx.ones_b, cx.ones_b[:, :], Mb, Mb2[:, q * 512:(q + 1) * 512], True, True)
            cp(s, "act", own, own[:, q * 8:(q + 1) * 8, :], pp, pp[:, :].rearrange("p (i e) -> p i e", i=8))
        tt(s, "dve", pos, pos[:, :, :], pos, pos[:, :, :], own, own[:, :, :], ALU.subtract)
        cnt = s.sb([128, 64], F32, "cnt")
        cp(s, "dve", cnt, cnt[:, :], incl, incl[:, NT - 1, :])
        bigr = s.sb([128, NT, 64], F32, "bigr")
        for k, OH in enumerate((OH1, OH2)):
            tt(s, "dve", bigr, bigr[:, :, :], OH, OH[:, :, :], pos, pos[:, :, :], ALU.mult)
            s.op("dve", lambda e, k=k: e.reduce_sum(out=RK[:, :, k], in_=bigr[:, :, :], axis=AX.X), reads=[bigr], writes=[RK], cost=2000)
        cnti = s.sb([128, 64], I32, "cnti")
        padf = s.sb([128, 64], F32, "padf")
        pends = s.sb([128, 64], F32, "pends")
        pstart = s.sb([128, 64], F32, "pstart")
        ts(s, "dve", cnti, cnti[:, :], cnt, cnt[:, :], 127.0, None, ALU.add)
        ts(s, "dve", cnti, cnti[:, :], cnti, cnti[:, :], 7, 7, ALU.arith_shift_right, ALU.logical_shift_left)
        cp(s, "dve", padf, padf[:, :], cnti, cnti[:, :])
        s.op("dve", lambda e: e.tensor_tensor_scan(out=pends[:, :], data0=cx.ones_f[:, 0:64], data1=padf[:, :],
                                                   initial=0.0, op0=ALU.mult, op1=ALU.add),
             reads=[cx.ones_f, padf], writes=[pends])
        tt(s, "dve", pstart, pstart[:, :], pends, pends[:, :], padf, padf[:, :], ALU.subtract)
        big = s.sb([128, NT, 64], F32, "big")
        DEST = s.sb([128, NT, 2], F32, "DEST")
        for k, OH in enumerate((OH1, OH2)):
            tt(s, "dve", big, big[:, :, :], OH, OH[:, :, :], pstart, pstart[:, :].unsqueeze(1).to_broadcast([128, NT, 64]), ALU.mult)
            s.op("dve", lambda e, k=k: e.reduce_sum(out=DEST[:, :, k], in_=big[:, :, :], axis=AX.X), reads=[big], writes=[DEST])
        tt(s, "dve", DEST, DEST[:, :, :], DEST, DEST[:, :, :], RK, RK[:, :, :], ALU.add)
        cp(s, "dve", DESTi, DESTi[:, :, :], DEST, DEST[:, :, :])
        fill = s.sb([128, NSLOT], I32, "fill")
        s.op("pool", lambda e: e.iota(fill[:, :], pattern=[[0, NSLOT]], base=S, channel_multiplier=0), writes=[fill])
        btfill = Res("btfill")
        s.dma("sp", lambda e: e.dma_start(out=bt.rearrange("(p n) o -> p (n o)", p=128), in_=fill[:, :]),
              reads=[fill, bt_res], writes=[btfill], nbytes=65536)
        for i in range(NT):
            for k in range(2):
                s.dma("pool", lambda e, i=i, k=k: e.indirect_dma_start(
                    out=bt, out_offset=bass.IndirectOffsetOnAxis(ap=DESTi[:, i, k:k + 1], axis=0),
                    in_=toki[:, i:i + 1], in_offset=None, bounds_check=breg_t, oob_is_err=False),
                    reads=[DESTi, toki, btfill], writes=[], nbytes=4096)
        joinr = s.sb([128, 1], F32, "joinr")
        s.op("pool", lambda e: e.memset(joinr[:, :], 0.0), reads=[], writes=[btfill, joinr, bt_res])
        slotoff = s.sb([128, NSLOT], F32, "slotoff")
        sloti = s.sb([128, NSLOT], I32, "sloti")
        s.op("pool", lambda e: e.iota(sloti[:, :], pattern=[[128, NSLOT]], base=0, channel_multiplier=0), writes=[sloti])
        cp(s, "dve", slotoff, slotoff[:, :], sloti, sloti[:, :])
        blk = s.sb([128, NSLOT], F32, "blk")
        cmp_ = s.sb([128, 32, 64], F32, "cmp")
        for q in range(NSLOT // 32):
            tt(s, "dve", cmp_, cmp_[:, :, :], pends, pends[:, :].unsqueeze(1).to_broadcast([128, 32, 64]),
               slotoff, slotoff[:, q * 32:(q + 1) * 32].unsqueeze(2).to_broadcast([128, 32, 64]), ALU.is_le)
            s.op("dve", lambda e, q=q: e.reduce_sum(out=blk[:, q * 32:(q + 1) * 32], in_=cmp_[:, :, :], axis=AX.X),
                 reads=[cmp_], writes=[blk])
        pidx = s.sb([128, 1], F32, "pidx")
        pidi = s.sb([128, 1], I32, "pidi")
        s.op("pool", lambda e: e.iota(pidi[:, :], pattern=[[0, 1]], base=0, channel_multiplier=1), writes=[pidi])
        cp(s, "dve", pidx, pidx[:, :], pidi, pidi[:, :])
        used = s.sb([128, NSLOT], F32, "used")
        ts(s, "dve", used, used[:, :], slotoff, slotoff[:, :], pends[:, 63:64], None, ALU.is_lt, extra_reads=[pends])
        ts(s, "dve", blk, blk[:, :], blk, blk[:, :], 63.0, 128.0, ALU.min, ALU.mult)
        ts(s, "dve", blk, blk[:, :], blk, blk[:, :], pidx[:, 0:1], float(l * 64 * 128 - BIGIDX), ALU.add, ALU.add, extra_reads=[pidx])
        tt(s, "dve", blk, blk[:, :], blk, blk[:, :], used, used[:, :], ALU.mult)
        ts(s, "dve", blk, blk[:, :], blk, blk[:, :], float(BIGIDX), None, ALU.add)
        cp(s, "dve", IDXW, IDXW[:, :], blk, blk[:, :])
        if "dbg" in dr:
            dbg = s.sb([128, 1024], F32, "dbg")
            s.op("dve", lambda e: e.memset(dbg[:, :], 0.0), writes=[dbg])
            cp(s, "dve", dbg, dbg[:, 0:128], IDXW, IDXW[:, :])
            cp(s, "dve", dbg, dbg[:, 128:192], cnt, cnt[:, :])
            cp(s, "dve", dbg, dbg[:, 192:256], pends, pends[:, :])
            cp(s, "dve", dbg, dbg[:, 256:320], DEST, DEST[:, :, :].rearrange("p a b -> p (a b)"))
            cp(s, "dve", dbg, dbg[:, 320:384], GT, GT[:, :, :].rearrange("p a b -> p (a b)"))
            cp(s, "dve", dbg, dbg[:, 384:448], used, used[:, 0:64])
            cp(s, "dve", dbg, dbg[:, 448:512], padf, padf[:, :])
            cp(s, "dve", dbg, dbg[:, 512:576], OH1, OH1[:, 0, :])
            cp(s, "dve", dbg, dbg[:, 576:640], OH2, OH2[:, 0, :])
            cp(s, "dve", dbg, dbg[:, 640:704], RK, RK[:, :, :].rearrange("p a b -> p (a b)"))
            st(s, "sp", wres, dr["dbg"], dbg, dbg[:, :], final=True)
        s.phase_es = old
    s_sub_barrier(s)
    if fstop < 2:
        s.end_phase()
        return

    with ExitStack() as es2:
        old = s.phase_es
        s.phase_es = es2
        wviews = [dr[k].rearrange("l e (p j) n -> (l e p) (j n)", p=128) for k in ("e_w_gate", "e_w_up", "e_w_down")]
        NSTG = 3
        stgs = [[s.sb([128, 2048], F32, f"ws{k}") for k in range(3)] for _ in range(NSTG)]
        NB3 = 3
        wbs = [[s.sb([128, 2048], BF16, f"wb{k}") for k in range(3)] for _ in range(NB3)]
        idxs = [s.sb([128, 1], I32, "idxb") for _ in range(NB3)]
        xgs = [s.sb([128, 1024], BF16, "xg") for _ in range(NB3)]
        for xg in xgs:
            s.op("pool", lambda e, xg=xg: e.memset(xg[:, :], 0.0), writes=[xg])
        xgTs = [s.sb([128, 8, 128], BF16, "xgT") for _ in range(NB3)]
        sgs = [s.sb([128, 256], F32, "sg") for _ in range(NB3)]
        aTs = [s.sb([128, 2, 128], BF16, "aT") for _ in range(NB3)]
        yos = [s.sb([128, 1024], F32, "yo") for _ in range(NB3)]
        psT = [s.ps([128, 1024], BF16, "psT") for _ in range(3)]
        psGUs = [s.ps([128, 512], F32, "psGU") for _ in range(3)]
        psY = [s.ps([128, 512], F32, "psY") for _ in range(2)]
        nslot = int(os.environ.get("FSLOTS", str(NSLOT)))

        def loads(b):
            ix = idxs[b % NB3]
            ld(s, "sp", ix, ix[:, :], bt_res, bt[b * 128:(b + 1) * 128, :])
            xg = xgs[b % NB3]
            s.dma("pool", lambda e: e.indirect_dma_start(
                out=xg[:, :], out_offset=None, in_=xbf, in_offset=bass.IndirectOffsetOnAxis(ap=ix[:, 0:1], axis=0),
                bounds_check=breg_x, oob_is_err=False), reads=[ix, xbf_res], writes=[xg], nbytes=262144)
            for k in range(3):
                stg = stgs[b % NSTG][k]
                s.dma("pool", lambda e, stg=stg, k=k: e.indirect_dma_start(
                    out=stg[:, :], out_offset=None, in_=wviews[k],
                    in_offset=bass.IndirectOffsetOnAxis(ap=IDXW[:, b:b + 1], axis=0),
                    bounds_check=breg_w, oob_is_err=False), reads=[IDXW, wres], writes=[stg], nbytes=1 << 20)

        loads(0)
        loads(1)
        for b in range(nslot):
            if b + 2 < nslot:
                loads(b + 2)
            xg = xgs[b % NB3]
            xgT = xgTs[b % NB3]
            wg, wu, wd = wbs[b % NB3]
            sg = sgs[b % NB3]
            aT = aTs[b % NB3]
            yo = yos[b % NB3]
            pT = psT[b % 3]
            pGU = psGUs[b % 3]
            for k, eng in enumerate(("act", "dve", "act")):
                cp(s, eng, wbs[b % NB3][k], wbs[b % NB3][k][:, :], stgs[b % NSTG][k], stgs[b % NSTG][k][:, :])
            for j in range(8):
                s.op("pe", lambda e, j=j, pT=pT, xg=xg: e.transpose(out=pT[:, j * 128:(j + 1) * 128], in_=xg[:, j:1024:8],
                                                                  identity=cx.ident_b[:, :]), reads=[xg, cx.ident_b], writes=[pT], cost=150)
            cp(s, "dve", xgT, xgT[:, :, :], pT, pT[:, :].rearrange("p (j t) -> p j t", j=8))
            wgv = wg[:, :].rearrange("p (j n) -> p j n", j=8)
            wuv = wu[:, :].rearrange("p (j n) -> p j n", j=8)
            wdv = wd[:, :].rearrange("p (j n) -> p j n", j=2)
            for off, wt_, wv_ in ((0, wg, wgv), (256, wu, wuv)):
                for jh in range(2):
                    for j in range(8):
                        mm(s, pGU, pGU[:, off + jh * 128:off + (jh + 1) * 128], wt_, wv_[:, j, jh:256:2],
                           xgT, xgT[:, j, :], j == 0, j == 7)
            act(s, sg, sg[:, :], pGU, pGU[:, 0:256], AF.Silu)
            tt(s, "dve", aT, aT[:, :, :].rearrange("p a t -> p (a t)"), sg, sg[:, :], pGU, pGU[:, 256:512], ALU.mult)
            for half in range(2):
                for jh in range(2):
                    mm(s, psY[half], psY[half][:, :], aT, aT[:, jh, :], wd, wdv[:, jh, half * 512:(half + 1) * 512], jh == 0, jh == 1)
            cp(s, "act", yo, yo[:, 0:512], psY[0], psY[0][:, :])
            cp(s, "dve", yo, yo[:, 512:1024], psY[1], psY[1][:, :])
            st(s, "sp", yb_res, yb[b * 128:(b + 1) * 128, :], yo, yo[:, :])
        s.phase_es = old
    s_sub_barrier(s)
    if fstop < 3:
        s.end_phase()
        return

    with ExitStack() as es3:
        old = s.phase_es
        s.phase_es = es3
        g_bc = s.sb([128, 1024], F32, "gbc")
        b_bc = s.sb([128, 1024], F32, "bbc")
        ld(s, "sp", g_bc, g_bc[:, :], wres, bcast_row(dr["ln2_g"][l], 1024))
        ld(s, "sp", b_bc, b_bc[:, :], wres, bcast_row(dr["ln2_b"][l], 1024))
        y1s = [s.sb([128, 1024], F32, "y1") for _ in range(2)]
        y2s = [s.sb([128, 1024], F32, "y2") for _ in range(2)]
        hfs = [s.sb([128, 1024], F32, "hf3") for _ in range(2)]
        hos = [s.sb([128, 1024], F32, "ho3") for _ in range(2)]
        wk = ln_work(s)
        for i in range(NT):
            y1, y2, hf, ho = y1s[i % 2], y2s[i % 2], hfs[i % 2], hos[i % 2]
            for k, yk in enumerate((y1, y2)):
                s.dma("pool", lambda e, yk=yk, k=k, i=i: e.indirect_dma_start(
                    out=yk[:, :], out_offset=None, in_=yb, in_offset=bass.IndirectOffsetOnAxis(ap=DESTi[:, i, k:k + 1], axis=0),
                    bounds_check=breg_y, oob_is_err=False), reads=[DESTi, yb_res], writes=[yk])
            ld(s, "sp", hf, hf[:, :], h_in_res, h_in_ap[i * 128:(i + 1) * 128, :])
            act(s, y2, y2[:, :], y2, y2[:, :], AF.Copy, scale=GT[:, i, 1:2], extra_reads=[GT])
            stt(s, "dve", y1, y1[:, :], y1, y1[:, :], GT[:, i, 0:1], y2, y2[:, :], ALU.mult, ALU.add, extra_reads=[GT])
            ln_epilogue(s, cx, [(y1, y1[:, 0:512]), (y1, y1[:, 512:1024])], hf, g_bc, b_bc, wk, ho)
            st(s, "pool", h_out_res, h_out_ap[i * 128:(i + 1) * 128, :], ho, ho[:, :], final=final)
        s.phase_es = old
    s.end_phase()


def pack_experts(g, u, d):
    g = np.asarray(g, dtype=np.float32).reshape(4, 64, 128, 2048)
    u = np.asarray(u, dtype=np.float32).reshape(4, 64, 128, 2048)
    d = np.asarray(d, dtype=np.float32).reshape(4, 64, 128, 2048)
    return np.ascontiguousarray(np.stack([g, u, d], axis=3)).reshape(4 * 64 * 128, 3 * 2048)


def rope_consts_np():
    rc = np.zeros((128, 4), np.float32)
    inv_a = 500000.0 ** (-np.arange(0, 16, 2, dtype=np.float32) / 16.0)
    for hh in range(2):
        for jx in range(16):
            rc[hh * 64 + jx, 0] = inv_a[jx % 8]
            rc[hh * 64 + jx, 1] = -1.0 if jx < 8 else 1.0
    inv_c = 10000.0 ** (-np.arange(0, 256, 2, dtype=np.float32) / 256.0)
    rc[:, 2] = inv_c
    return rc


IN_SPECS = [
    ("x", [S, D], F32), ("positions", [S], I32),
    ("ln1_g", [4, D], F32), ("ln1_b", [4, D], F32), ("ln2_g", [4, D], F32), ("ln2_b", [4, D], F32),
    ("a_w_in", [2, D, 3072], F32), ("a_w_out", [2, D, D], F32),
    ("b_w_in", [1, D, 3088], F32), ("b_gate_bias", [1, 16], F32), ("b_conv_w", [1, 4, D], F32),
    ("b_conv_b", [1, D], F32), ("b_norm_g", [1, D], F32), ("b_w_out", [1, D, D], F32),
    ("c_w_in", [1, D, 6144], F32), ("c_norm_g", [1, 2 * D], F32), ("c_w_out", [1, 2 * D, D], F32),
    ("r_group_w", [4, D, 8], F32), ("r_group_b", [4, 8], F32), ("r_expert_w", [4, D, 64], F32),
    ("r_expert_b", [4, 64], F32), ("e_w_gate", [4, 64, D, 256], F32), ("e_w_up", [4, 64, D, 256], F32),
    ("e_w_down", [4, 64, 256, D], F32), ("rope_consts", [128, 4], F32),
]


def build(plan, used_inputs=None):
    nc = bass.Bass("TRN2", target_bir_lowering=False)
    dr = {}
    for name, shape, dt in IN_SPECS:
        if used_inputs is not None and name not in used_inputs:
            continue
        dr[name] = nc.dram_tensor(name, shape, dt, kind="ExternalInput").ap()
    out = nc.dram_tensor("out", [S, D], F32, kind="ExternalOutput").ap()
    import os
    if os.environ.get("KDBG"):
        dr["dbg"] = nc.dram_tensor("dbg", [128, 1024], F32, kind="ExternalOutput").ap()
    dr["wres"] = Res("weights")
    dr["qkT"] = (Res("qkT"), nc.dram_tensor("qkT_s", [2, D, S], BF16).ap())
    dr["vsc"] = (Res("vsc"), nc.dram_tensor("vsc_s", [2, S, 128], BF16).ap())
    dr["attT"] = (Res("attT"), nc.dram_tensor("attT_s", [2 * D, S], BF16).ap())
    dr["hn"] = (Res("hn"), nc.dram_tensor("hn_s", [S, 2 * D], F32).ap())
    dr["xbf"] = (Res("xbf"), nc.dram_tensor("xbf_s", [S, D], BF16).ap())
    dr["buftok"] = (Res("buftok"), nc.dram_tensor("buftok_s", [NSLOT * 128, 1], I32).ap())
    dr["yb"] = (Res("yb"), nc.dram_tensor("yb_s", [NSLOT * 128, D], F32).ap())
    hbuf = [(Res("hA"), nc.dram_tensor("hA_s", [S, D], F32).ap(), False),
            (Res("hB"), nc.dram_tensor("hB_s", [S, D], F32).ap(), False)]
    with ExitStack() as es:
        s = Sched(nc, es)
        cx = Ctx()
        make_consts(s, cx)
        cur = (dr["wres"], dr["x"], False)
        for pi, (ph, l) in enumerate(plan):
            last = pi == len(plan) - 1
            nxt = (Res("out"), out, True) if last else hbuf[pi % 2]
            if ph == "A":
                phase_A(s, cx, dr, l, l // 3, cur, nxt)
            elif ph == "B":
                phase_B(s, cx, dr, l, l // 3, cur, nxt)
            elif ph == "C":
                phase_C(s, cx, dr, l, l // 3, cur, nxt)
            elif ph == "F":
                phase_F(s, cx, dr, l, cur, nxt)
            else:
                raise ValueError(ph)
            cur = nxt
        s.finish()
        print("instructions:", s.ninst, "sems:", s.n_sem)
    return nc


PLAN = [("A", 0), ("F", 0), ("B", 1), ("F", 1), ("C", 2), ("F", 2), ("A", 3), ("F", 3)]
_NC_CACHE = {}


def kernel(**inputs):
    x = np.ascontiguousarray(np.asarray(inputs["x"], dtype=np.float32))
    nb = x.shape[0]
    if "nc" not in _NC_CACHE:
        _NC_CACHE["nc"] = build(PLAN)
    nc = _NC_CACHE["nc"]
    shared = {}
    for name, shape, dt in IN_SPECS:
        if name == "x":
            continue
        if name == "rope_consts":
            shared[name] = rope_consts_np()

        elif name == "positions":
            shared[name] = np.ascontiguousarray(np.asarray(inputs[name]).astype(np.int32))
        else:
            shared[name] = np.ascontiguousarray(np.asarray(inputs[name], dtype=np.float32))
    in_maps = []
    for b in range(nb):
        m = dict(shared)
        m["x"] = x[b]
        in_maps.append(m)
    res = run_bass_kernel_spmd(nc, in_maps, core_ids=list(range(nb)))
    return np.stack([np.asarray(r["out"], dtype=np.float32) for r in res.results], axis=0)
```

```python
import math
import numpy as np
import concourse.bass as bass
import concourse.mybir as mybir
from concourse.bass_utils import run_bass_kernel_spmd
from contextlib import ExitStack

F32 = mybir.dt.float32
BF16 = mybir.dt.bfloat16
I32 = mybir.dt.int32
ALU = mybir.AluOpType
AF = mybir.ActivationFunctionType
AX = mybir.AxisListType

S = 4096
D = 1024
DEPTH = 4
NT = S // 128
ALPHA = (2.0 * DEPTH) ** 0.25
LN_EPS = 1e-5
TWO_PI = 2.0 * math.pi

SEM_WRAP = 30000
ENGS = ("pe", "act", "dve", "pool", "sp")


class Res:
    __slots__ = ("w", "r", "name", "excl")

    def __init__(self, name="", excl=False):
        self.w = None
        self.r = []
        self.name = name
        self.excl = excl


class T:
    __slots__ = ("t", "res")

    def __init__(self, t, res):
        self.t = t
        self.res = res

    def __getitem__(self, k):
        return self.t[k]


class Sched:
    def __init__(self, nc, es):
        self.nc = nc
        self.es = es
        self.sems = {}
        self.cur = {}
        self.waited = {e: {} for e in ENGS}
        self.pending = {e: {} for e in ENGS}
        self.dma_pool = {}
        self.dma_rr = {}
        self.n_sem = 0
        self.n_tiles = 0
        self.ninst = 0
        self.finals = []
        self.eobj = {"pe": nc.tensor, "act": nc.scalar, "dve": nc.vector, "pool": nc.gpsimd, "sp": nc.sync}
        self.phase_es = None
        self.recs = []
        self.ev_of = {}
        self.next_id = 0
        import os
        self.window = int(os.environ.get("KWIN", "200"))

    def _newsem(self, name):
        h = self.es.enter_context(self.nc.semaphore(f"{name}_{self.n_sem}"))
        self.n_sem += 1
        key = self.n_sem
        self.sems[key] = h
        return key

    def sb(self, shape, dtype, name=None):
        self.n_tiles += 1
        name = name or "t"
        es = self.phase_es or self.es
        t = es.enter_context(self.nc.sbuf_tensor(f"{name}_{self.n_tiles}", list(shape), dtype))
        return T(t, Res(name))

    def ps(self, shape, dtype, name=None):
        self.n_tiles += 1
        name = name or "p"
        es = self.phase_es or self.es
        t = es.enter_context(self.nc.psum_tensor(f"{name}_{self.n_tiles}", list(shape), dtype))
        return T(t, Res(name, excl=True))

    def begin_phase(self):
        self.phase_es = ExitStack()
        return self.phase_es

    def end_phase(self):
        self.flush()
        snap = {}
        for e, c in self.cur.items():
            snap[c[0]] = c[1]
        for q, slots in self.dma_pool.items():
            for sl in slots:
                if sl[1] > 0:
                    snap[sl[0]] = sl[1]
        for e in ENGS:
            p = self.pending[e]
            for k, v in snap.items():
                if p.get(k, 0) < v:
                    p[k] = v
        self.phase_es.close()
        self.phase_es = None

    def _deps(self, reads, writes):
        deps = set()
        for r in reads:
            r = r.res if isinstance(r, T) else r
            if r.w is not None:
                deps.add(r.w)
            if r.excl:
                deps.update(r.r)
        for w in writes:
            w = w.res if isinstance(w, T) else w
            if w.w is not None:
                deps.add(w.w)
            deps.update(w.r)
        return deps

    def _commit(self, oid, reads, writes):
        for r in reads:
            r = r.res if isinstance(r, T) else r
            r.r.append(oid)
        for w in writes:
            w = w.res if isinstance(w, T) else w
            w.w = oid
            w.r = []

    def op(self, eng, fn, reads=(), writes=(), cost=150.0):
        oid = self.next_id
        self.next_id += 1
        deps = self._deps(reads, writes)
        self._commit(oid, reads, writes)
        self.recs.append((oid, eng, False, fn, deps, float(cost), 0.0, False))

    def dma(self, q, fn, reads=(), writes=(), final=False, nbytes=65536, issue=None):
        oid = self.next_id
        self.next_id += 1
        deps = self._deps(reads, writes)
        self._commit(oid, reads, writes)
        if issue is None:
            issue = 1200.0 if q == "pool" else 80.0
        self.recs.append((oid, q, True, fn, deps, float(issue), 2000.0 + nbytes / 150.0, final))

    def flush(self):
        recs = self.recs
        self.recs = []
        if not recs:
            return
        fin = {}
        queues = {e: [] for e in ENGS}
        for r in recs:
            queues[r[1]].append(r)
        heads = {e: 0 for e in ENGS}
        placed = set()
        mine = {r[0] for r in recs}
        efree = {e: 0.0 for e in ENGS}
        dma_free = [0.0]
        order = []
        nleft = len(recs)
        W = self.window
        taken = {e: set() for e in ENGS}
        while nleft:
            best = None
            for e in ENGS:
                q = queues[e]
                h = heads[e]
                n = len(q)
                while h < n and q[h][0] in taken[e]:
                    h += 1
                heads[e] = h
                lim = min(n, h + W)
                for k in range(h, lim):
                    r = q[k]
                    if r[0] in taken[e]:
                        continue
                    ok = True
                    st_ = efree[e]
                    for d in r[4]:
                        if d in mine:
                            if d not in placed:
                                ok = False
                                break
                            if fin[d] > st_:
                                st_ = fin[d]
                    if not ok:
                        continue
                    key = (st_, r[0])
                    if best is None or key < best[0]:
                        best = (key, e, r)
                    if st_ <= efree[e]:
                        break
            assert best is not None, "scheduler deadlock"
            (st_, _), e, r = best
            taken[e].add(r[0])
            placed.add(r[0])
            nleft -= 1
            if r[2]:
                iend = st_ + r[5]
                efree[e] = iend
                t0 = max(iend, dma_free[0])
                dma_free[0] = t0 + max(0.0, r[6] - 2000.0)
                fin[r[0]] = dma_free[0] + 2000.0
            else:
                efree[e] = st_ + r[5]
                fin[r[0]] = efree[e] + 60.0
            order.append(r)
        for r in order:
            self._emit_rec(r)

    def _engine_event(self, eng):
        if eng not in self.cur or self.cur[eng][1] >= SEM_WRAP:
            self.cur[eng] = [self._newsem(eng), 0]
        c = self.cur[eng]
        c[1] += 1
        return (c[0], c[1])

    def _dma_event(self, q):
        if q not in self.dma_pool:
            self.dma_pool[q] = [[self._newsem(f"dma{q}"), 0] for _ in range(12)]
            self.dma_rr[q] = 0
        i = self.dma_rr[q]
        self.dma_rr[q] = (i + 1) % len(self.dma_pool[q])
        slot = self.dma_pool[q][i]
        extra = (slot[0], slot[1]) if slot[1] > 0 else None
        if slot[1] + 16 > SEM_WRAP:
            slot[0] = self._newsem(f"dma{q}")
            slot[1] = 0
        slot[1] += 16
        return (slot[0], slot[1]), extra

    def _emit_rec(self, r):
        oid, eng, is_dma, fn, deps, _, _, final = r
        need = self.pending[eng]
        self.pending[eng] = {}
        for d in deps:
            k, v = self.ev_of[d]
            if need.get(k, 0) < v:
                need[k] = v
        if is_dma:
            ev, extra = self._dma_event(eng)
            if extra is not None:
                k, v = extra
                if need.get(k, 0) < v:
                    need[k] = v
            inc = 16
        else:
            ev = self._engine_event(eng)
            inc = 1
        wd = self.waited[eng]
        own = self.cur.get(eng, [None])[0] if eng == "pe" else None
        e = self.eobj[eng]
        for k, v in need.items():
            if k == own:
                continue
            if wd.get(k, 0) < v:
                wd[k] = v
                e.wait_ge(self.sems[k], v)
        ins = fn(e)
        ins.then_inc(self.sems[ev[0]], inc)
        self.ev_of[oid] = ev
        self.ninst += 1
        if final:
            self.finals.append(ev)

    def finish(self):
        self.flush()
        deps = {}
        for k, v in self.finals:
            if deps.get(k, 0) < v:
                deps[k] = v
        for q, slots in self.dma_pool.items():
            for sl in slots:
                if sl[1] > 0 and deps.get(sl[0], 0) < sl[1]:
                    deps[sl[0]] = sl[1]
        for k, v in deps.items():
            self.nc.sync.wait_ge(self.sems[k], v)


def _fsz(ap):
    n = 1
    for d in ap.shape[1:]:
        n *= d
    return n


def mm(s, out_t, out_ap, lhsT_t, lhsT_ap, rhs_t, rhs_ap, start, stop):
    n = _fsz(out_ap)
    c = 30.0 + max(n, 64) * 0.45
    if lhsT_ap.dtype == F32:
        c *= 4
    s.op("pe", lambda e: e.matmul(out_ap, lhsT=lhsT_ap, rhs=rhs_ap, start=start, stop=stop),
         reads=[lhsT_t, rhs_t], writes=[out_t], cost=c)


def _ecost(eng, n):
    if eng == "dve":
        return 70.0 + 0.85 * n
    if eng == "act":
        return 200.0 + 0.65 * n
    return 160.0 + 1.6 * n


def tt(s, eng, out_t, out_ap, a_t, a_ap, b_t, b_ap, op):
    s.op(eng, lambda e: e.tensor_tensor(out=out_ap, in0=a_ap, in1=b_ap, op=op), reads=[a_t, b_t], writes=[out_t],
         cost=_ecost(eng, _fsz(out_ap)))


def ts(s, eng, out_t, out_ap, a_t, a_ap, s1, s2, op0, op1=None, extra_reads=()):
    c = _ecost(eng, _fsz(out_ap))
    if op1 is None:
        s.op(eng, lambda e: e.tensor_scalar(out=out_ap, in0=a_ap, scalar1=s1, scalar2=None, op0=op0),
             reads=[a_t, *extra_reads], writes=[out_t], cost=c)
    else:
        s.op(eng, lambda e: e.tensor_scalar(out=out_ap, in0=a_ap, scalar1=s1, scalar2=s2, op0=op0, op1=op1),
             reads=[a_t, *extra_reads], writes=[out_t], cost=c)


def stt(s, eng, out_t, out_ap, a_t, a_ap, scalar, b_t, b_ap, op0, op1, extra_reads=()):
    s.op(eng, lambda e: e.scalar_tensor_tensor(out=out_ap, in0=a_ap, scalar=scalar, in1=b_ap, op0=op0, op1=op1),
         reads=[a_t, b_t, *extra_reads], writes=[out_t], cost=_ecost(eng, _fsz(out_ap)))


def cp(s, eng, out_t, out_ap, a_t, a_ap):
    n = _fsz(out_ap)
    if eng == "act":
        s.op("act", lambda e: e.activation(out=out_ap, in_=a_ap, func=AF.Copy), reads=[a_t], writes=[out_t], cost=_ecost("act", n))
    else:
        c = _ecost(eng, n) if eng == "dve" else 160.0 + 3.0 * n
        s.op(eng, lambda e: e.tensor_copy(out=out_ap, in_=a_ap), reads=[a_t], writes=[out_t], cost=c)


def act(s, out_t, out_ap, a_t, a_ap, func, scale=1.0, bias=0.0, extra_reads=()):
    s.op("act", lambda e: e.activation(out=out_ap, in_=a_ap, func=func, bias=bias, scale=scale),
         reads=[a_t, *extra_reads], writes=[out_t], cost=_ecost("act", _fsz(out_ap)))


def _nbytes(ap):
    n = 1
    for d in ap.shape:
        n *= d
    return n * (2 if ap.dtype == BF16 else 4)


def ld(s, q, out_t, out_ap, src_res, src_ap):
    s.dma(q, lambda e: e.dma_start(out=out_ap, in_=src_ap), reads=[src_res], writes=[out_t], nbytes=_nbytes(out_ap))


def st(s, q, dst_res, dst_ap, in_t, in_ap, final=False):
    s.dma(q, lambda e: e.dma_start(out=dst_ap, in_=in_ap), reads=[in_t], writes=[dst_res], final=final, nbytes=_nbytes(in_ap))


class Ctx:
    pass


def wload(s, cx, dst_t, dst_ap, wres, src_ap, shape):
    stg = cx.stg[cx.stg_i % 2]
    cx.stg_i += 1
    n = int(np.prod(shape))
    if len(shape) == 1:
        v = stg[:, 0:n]
    else:
        v = stg[:, 0:n].rearrange("p (a b) -> p a b", a=shape[0])
    ld(s, "sp", stg, v, wres, src_ap)
    cp(s, "dve" if cx.stg_i % 2 else "act", dst_t, dst_ap, stg, v)


def make_consts(s, cx):
    nc = s.nc
    iot = s.sb([128, 128], I32, "iot")
    iof = s.sb([128, 128], F32, "iof")
    cx.iof = iof
    cx.ident_f = s.sb([128, 128], F32, "identf")
    cx.ident_b = s.sb([128, 128], BF16, "identb")
    cx.maskA = s.sb([128, 2, 2, 128], BF16, "maskA")
    cx.perm = s.sb([128, 128], BF16, "perm")
    cx.ones_b = s.sb([128, 128], BF16, "onesb")
    cx.ones_f = s.sb([128, 128], F32, "onesf")
    cx.tri_le = s.sb([128, 128], F32, "trile")
    cx.tri_b = s.sb([128, 128], BF16, "trib")
    cx.swlo = s.sb([128, 128], F32, "swlo")
    cx.swhi = s.sb([128, 128], F32, "swhi")
    cx.stg = [s.sb([128, 2048], F32, "stg") for _ in range(2)]
    cx.stg_i = 0
    s.op("pool", lambda e: e.iota(iot[:, :], pattern=[[1, 128]], base=0, channel_multiplier=-1), writes=[iot])
    cp(s, "dve", iof, iof[:, :], iot, iot[:, :])
    ts(s, "dve", cx.ident_f, cx.ident_f[:, :], iof, iof[:, :], 0.0, None, ALU.is_equal)
    cp(s, "dve", cx.ident_b, cx.ident_b[:, :], cx.ident_f, cx.ident_f[:, :])
    ts(s, "dve", cx.tri_le, cx.tri_le[:, :], iof, iof[:, :], 0.0, None, ALU.is_ge)
    cp(s, "dve", cx.tri_b, cx.tri_b[:, :], cx.tri_le, cx.tri_le[:, :])
    ts(s, "dve", cx.swlo, cx.swlo[:, :], iof, iof[:, :], 64.0, None, ALU.is_equal)
    ts(s, "dve", cx.swhi, cx.swhi[:, :], iof, iof[:, :], -64.0, None, ALU.is_equal)
    for hh in range(2):
        ts(s, "dve", cx.maskA, cx.maskA[:, hh, 0, :], iof, iof[:, :], 0.0, None, ALU.is_ge)
        ts(s, "dve", cx.maskA, cx.maskA[:, hh, 1, :], iof, iof[:, :], 0.0, None, ALU.is_le)
    s.op("pool", lambda e: e.memset(cx.perm[:, :], 0.0), writes=[cx.perm])
    s.op("pool", lambda e: e.memset(cx.ones_b[:, :], 1.0), writes=[cx.ones_b])
    s.op("pool", lambda e: e.memset(cx.ones_f[:, :], 1.0), writes=[cx.ones_f])
    for hh in range(2):
        c0 = hh * 64
        ts(s, "dve", cx.perm, cx.perm[:, c0:c0 + 8], iof, iof[:, c0:c0 + 8], -8.0, None, ALU.is_equal)
        ts(s, "dve", cx.perm, cx.perm[:, c0 + 8:c0 + 16], iof, iof[:, c0 + 8:c0 + 16], 8.0, None, ALU.is_equal)


def sin_table(s, out_t, out_ap, x_t, x_ap, shape, sign_ap, sign_t, scratch):
    ki, kf, r = scratch
    C1 = 6.28125
    C2 = TWO_PI - C1
    ts(s, "dve", ki, ki[:, :], x_t, x_ap, 1.0 / TWO_PI, None, ALU.mult)
    cp(s, "dve", kf, kf[:, :], ki, ki[:, :])
    stt(s, "dve", r, r[:, :], kf, kf[:, :], -C1, x_t, x_ap, ALU.mult, ALU.add)
    stt(s, "dve", r, r[:, :], kf, kf[:, :], -C2, r, r[:, :], ALU.mult, ALU.add)
    ts(s, "dve", kf, kf[:, :], r, r[:, :], math.pi, None, ALU.is_gt)
    stt(s, "dve", r, r[:, :], kf, kf[:, :], -TWO_PI, r, r[:, :], ALU.mult, ALU.add)
    ts(s, "dve", kf, kf[:, :], r, r[:, :], -math.pi, None, ALU.is_lt)
    stt(s, "dve", r, r[:, :], kf, kf[:, :], TWO_PI, r, r[:, :], ALU.mult, ALU.add)
    ts(s, "dve", r, r[:, :], r, r[:, :], 3.14159, -3.14159, ALU.min, ALU.max)
    act(s, out_t, out_ap, r, r[:, :], AF.Sin)
    if sign_ap is not None:
        ts(s, "dve", out_t, out_ap, out_t, out_ap, sign_ap, None, ALU.mult, extra_reads=[sign_t])


def load_xT(s, cx, h_res, h_ap, xT):
    old = s.phase_es
    s.phase_es = ExitStack()
    hfs = [s.sb([128, 1024], F32, "hf") for _ in range(3)]
    pts = [s.ps([128, 512], F32, "ptr") for _ in range(2)]
    for i in range(NT):
        hf = hfs[i % 3]
        ld(s, "sp", hf, hf[:, :], h_res, h_ap[i * 128:(i + 1) * 128, :])
        for half in range(2):
            pt = pts[half]
            for c4 in range(4):
                c = half * 4 + c4
                s.op("pe", lambda e, pt=pt, c4=c4, c=c, hf=hf: e.transpose(
                    out=pt[:, c4 * 128:(c4 + 1) * 128], in_=hf[:, c * 128:(c + 1) * 128], identity=cx.ident_f[:, :]),
                    reads=[hf, cx.ident_f], writes=[pt])
            dst = xT[:, half * 4:(half + 1) * 4, i * 128:(i + 1) * 128]
            src = pt[:, :].rearrange("p (c t) -> p c t", c=4)
            cp(s, "act" if half else "dve", xT, dst, pt, src)
    s.end_phase()
    s.phase_es = old


def ln_epilogue(s, cx, y_ps, hf, g_bc, b_bc, wk, out_t):
    r, stats, mv, sd, rstd, nmr, xn = wk
    for half in range(2):
        sl = slice(half * 512, (half + 1) * 512)
        yt, yap = y_ps[half] if isinstance(y_ps[half], tuple) else (y_ps[half], y_ps[half][:, :])
        stt(s, "dve", r, r[:, sl], hf, hf[:, sl], ALPHA, yt, yap, ALU.mult, ALU.add)
        s.op("dve", lambda e, half=half, sl=sl: e.bn_stats(out=stats[:, half, :], in_=r[:, sl]), reads=[r], writes=[stats])
    s.op("dve", lambda e: e.bn_aggr(out=mv[:, :], in_=stats[:, :, :].rearrange("p a b -> p (a b)")), reads=[stats], writes=[mv])
    ts(s, "dve", sd, sd[:, :], mv, mv[:, 1:2], LN_EPS, None, ALU.add)
    act(s, sd, sd[:, :], sd, sd[:, :], AF.Sqrt)
    s.op("dve", lambda e: e.reciprocal(out=rstd[:, :], in_=sd[:, :]), reads=[sd], writes=[rstd])
    stt(s, "dve", nmr, nmr[:, :], mv, mv[:, 0:1], -1.0, rstd, rstd[:, :], ALU.mult, ALU.mult)
    act(s, xn, xn[:, :], r, r[:, :], AF.Identity, scale=rstd[:, 0:1], bias=nmr[:, 0:1], extra_reads=[rstd, nmr])
    tt(s, "pool", xn, xn[:, :], xn, xn[:, :], g_bc, g_bc[:, :], ALU.mult)
    tt(s, "pool", out_t, out_t[:, :], xn, xn[:, :], b_bc, b_bc[:, :], ALU.add)


def ln_work(s):
    return (s.sb([128, 1024], F32, "lnr"), s.sb([128, 2, 6], F32, "lnst"), s.sb([128, 2], F32, "lnmv"),
            s.sb([128, 1], F32, "lnsd"), s.sb([128, 1], F32, "lnrs"), s.sb([128, 1], F32, "lnnm"),
            s.sb([128, 1024], F32, "lnxn"))


def bcast_row(ap_row, n):
    return ap_row.partition_broadcast(128)


def phase_A(s, cx, dr, l, j, h_in, h_out):
    nc = s.nc
    h_in_res, h_in_ap = h_in[0], h_in[1]
    w_in = dr["a_w_in"][j]
    w_out = dr["a_w_out"][j]
    w_in_v = w_in.rearrange("(c p) n -> p c n", p=128)
    qkT_res, qkT = dr["qkT"]
    attT_res, attT = dr["attT"]
    wres = dr["wres"]

    s.begin_phase()
    xT = s.sb([128, 8, S], BF16, "xT")
    load_xT(s, cx, h_in_res, h_in_ap, xT)

    import os
    stop = int(os.environ.get("KSTOP", "9"))
    if stop < 1:
        s.end_phase()
        return
    with ExitStack() as es1:
        old = s.phase_es
        s.phase_es = es1
        wqk = s.sb([128, 8, 2048], BF16, "wqk")
        for c in range(8):
            wload(s, cx, wqk, wqk[:, c, :], wres, w_in[c * 128:(c + 1) * 128, 0:2048], [2048])
        rc = s.sb([128, 4], F32, "ropec")
        ld(s, "sp", rc, rc[:, :], wres, dr["rope_consts"])
        posi = s.sb([128, 512], I32, "posi")
        posf = s.sb([128, 512], F32, "posf")
        ang = s.sb([128, 512], F32, "ang")
        ang2 = s.sb([128, 512], F32, "ang2")
        Ct = s.sb([128, 512], F32, "Ct")
        St = s.sb([128, 512], F32, "St")
        scr = (s.sb([128, 512], I32, "ki"), s.sb([128, 512], F32, "kf"), s.sb([128, 512], F32, "rr"))
        psq = [s.ps([128, 512], F32, "psq") for _ in range(2)]
        psp = [s.ps([128, 512], F32, "psp") for _ in range(2)]
        qbs = [s.sb([128, 512], BF16, "qb") for _ in range(2)]
        t1s = [s.sb([128, 512], F32, "t1") for _ in range(2)]
        t2s = [s.sb([128, 512], F32, "t2") for _ in range(2)]
        obs = [s.sb([128, 512], BF16, "ob") for _ in range(3)]
        it = 0
        ksub = int(os.environ.get("KSUB", "99"))
        for tg in range(int(os.environ.get('KTG', '8')) if ksub > 0 else 0):
            tsl = slice(tg * 512, (tg + 1) * 512)
            ld(s, "sp", posi, posi[:, :], wres, bcast_row(dr["positions"][tg * 512:(tg + 1) * 512], 512))
            cp(s, "dve", posf, posf[:, :], posi, posi[:, :])
            ts(s, "dve", ang, ang[:, :], posf, posf[:, :], rc[:, 0:1], None, ALU.mult, extra_reads=[rc])
            ts(s, "dve", ang2, ang2[:, :], ang, ang[:, :], math.pi / 2, None, ALU.add)
            sin_table(s, St, St[:, :], ang, ang[:, :], None, rc[:, 1:2], rc, scr)
            sin_table(s, Ct, Ct[:, :], ang2, ang2[:, :], None, None, None, scr)
            for hp in range(int(os.environ.get('KHP', '8')) if ksub > 1 else 0):
                for w in range(2):
                    pq = psq[it % 2]
                    pp = psp[it % 2]
                    qb = qbs[it % 2]
                    t1 = t1s[it % 2]
                    t2 = t2s[it % 2]
                    ob = obs[it % 3]
                    it += 1
                    col = w * 1024 + hp * 128
                    for c in range(8):
                        mm(s, pq, pq[:, :], wqk, wqk[:, c, col:col + 128], xT, xT[:, c, tsl], c == 0, c == 7)
                    cp(s, "act", qb, qb[:, :], pq, pq[:, :])
                    if ksub < 3:
                        continue
                    mm(s, pp, pp[:, :], cx.perm, cx.perm[:, :], qb, qb[:, :], True, True)
                    tt(s, "dve", t1, t1[:, :], pq, pq[:, :], Ct, Ct[:, :], ALU.mult)
                    tt(s, "dve", t2, t2[:, :], pp, pp[:, :], St, St[:, :], ALU.mult)
                    tt(s, "pool", ob, ob[:, :], t1, t1[:, :], t2, t2[:, :], ALU.add)
                    if ksub < 4:
                        continue
                    st(s, "pool", qkT_res, qkT[w, hp * 128:(hp + 1) * 128, tsl], ob, ob[:, :])
        s.phase_es = old
    s_sub_barrier(s)
    if stop < 2:
        s.end_phase()
        return

    vsc_res, vsc = dr["vsc"]
    with ExitStack() as es2:
        old = s.phase_es
        s.phase_es = es2
        QT = s.sb([128, S], BF16, "QT")
        KT = s.sb([128, S], BF16, "KT")
        wv = s.sb([128, 8, 128], BF16, "wv")
        Vn = s.sb([128, 32, 128], BF16, "Vn")
        Vaugs = [s.sb([128, 32, 2, 128], BF16, "Vaug") for _ in range(2)]
        for Va in Vaugs:
            s.op("pool", lambda e, Va=Va: e.memset(Va[:, :, 0, 64:128], 1.0), writes=[Va], cost=3000)
            s.op("pool", lambda e, Va=Va: e.memset(Va[:, :, 1, 0:64], 1.0), writes=[Va], cost=3000)
        acc = s.sb([128, 2, S], F32, "acc")
        obuf = s.sb([128, S], BF16, "obuf")
        recs = [s.sb([128, 512], F32, "rec") for _ in range(2)]
        psV = [s.ps([128, 512], F32, "psV") for _ in range(2)]
        psS = [s.ps([128, 512], F32, "psS") for _ in range(4)]
        psO_ = [s.ps([128, 512], F32, "psO") for _ in range(2)]
        psO = [T(p[:, 0:256].rearrange("p (a q) -> p a q", a=2), p.res) for p in psO_]
        pes = [s.sb([128, 512], BF16, "pe") for _ in range(4)]
        PTs = [s.sb([128, 2, 2, 128], BF16, "PT") for _ in range(4)]
        vi = 0
        bi_ctr = 0
        k2 = int(os.environ.get("KSUB2", "99"))
        for hp in range(int(os.environ.get("KHP2", "8"))):
            ld(s, "sp", QT, QT[:, :], qkT_res, qkT[0, hp * 128:(hp + 1) * 128, :])
            ld(s, "sp", KT, KT[:, :], qkT_res, qkT[1, hp * 128:(hp + 1) * 128, :])
            wload(s, cx, wv, wv[:, :, :], wres, w_in_v[:, :, 2048 + hp * 128:2048 + (hp + 1) * 128], [8, 128])
            for b in range(32):
                pv = psV[(b // 4) % 2]
                for c in range(8):
                    mm(s, pv, pv[:, (b % 4) * 128:(b % 4 + 1) * 128], xT, xT[:, c, b * 128:(b + 1) * 128], wv, wv[:, c, :], c == 0, c == 7)
                if b % 4 == 3:
                    cp(s, "act" if (b // 4) % 2 else "dve", Vn, Vn[:, b - 3:b + 1, :], pv,
                       pv[:, :].rearrange("p (b f) -> p b f", b=4))
            vs = vsc[hp % 2]
            st(s, "sp", vsc_res, vs.rearrange("(i p) c -> p i c", p=128), Vn, Vn[:, :, :])
            for di, d in enumerate((1, 4, 16)[:int(os.environ.get("KBR", "3"))] if k2 > 0 else ()):
                nb = 32 // d
                Vaug = Vaugs[vi % 2]
                vi += 1

                def tok(r, n, d=d):
                    return slice(r + d * 128 * n, r + d * 128 * n + 127 * d + 1, d)
                if d > 1:
                    view = vs.rearrange("(n p r) c -> p r n c", p=128, r=d)
                    for r in range(d):
                        s.dma("sp" if r % 2 else "act", lambda e, r=r, nb=nb, view=view: e.dma_start(
                            out=Vn[:, r * nb:(r + 1) * nb, :], in_=view[:, r, :, :]),
                            reads=[vsc_res], writes=[Vn], nbytes=4 * nb * 32768)
                cp(s, "act", Vaug, Vaug[:, :, 0, 0:64], Vn, Vn[:, :, 0:64])
                cp(s, "act", Vaug, Vaug[:, :, 1, 64:128], Vn, Vn[:, :, 64:128])
                for b2 in range(int(os.environ.get("KB", "16")) if k2 > 1 else 0):
                    blocks = (2 * b2, 2 * b2 + 1)
                    n0 = blocks[0] % nb
                    PTh = []
                    for hh in range(2):
                        hs = slice(hh * 64, (hh + 1) * 64)
                        pS = psS[(2 * bi_ctr + hh) % 4]
                        pe_ = pes[(2 * bi_ctr + hh) % 4]
                        PT = PTs[(2 * bi_ctr + hh) % 4]
                        PTh.append(PT)
                        pS4 = pS[:, :].rearrange("p (b c q) -> p b c q", b=2, c=2)
                        pe4 = pe_[:, :].rearrange("p (b c q) -> p b c q", b=2, c=2)
                        for bi, b in enumerate(blocks):
                            r, n = divmod(b, nb)
                            mm(s, pS, pS4[:, bi, 0, :], KT, KT[hs, tok(r, n)], QT, QT[hs, tok(r, n)], True, True)
                            if n > 0:
                                mm(s, pS, pS4[:, bi, 1, :], KT, KT[hs, tok(r, n - 1)], QT, QT[hs, tok(r, n)], True, True)
                        if n0 > 0:
                            act(s, pe_, pe_[:, :], pS, pS[:, :], AF.Exp, scale=0.125)
                            tt(s, "pool", PT, PT[:, :, :, :], pe_, pe4, cx.maskA, cx.maskA[:, :, :, :], ALU.mult)
                        else:
                            act(s, pe_, pe4[:, 0, 0, :], pS, pS4[:, 0, 0, :], AF.Exp, scale=0.125)
                            act(s, pe_, pe4[:, 1, :, :], pS, pS4[:, 1, :, :], AF.Exp, scale=0.125)
                            tt(s, "pool", PT, PT[:, 0, 0, :], pe_, pe4[:, 0, 0, :], cx.maskA, cx.maskA[:, 0, 0, :], ALU.mult)
                            tt(s, "pool", PT, PT[:, 1, :, :], pe_, pe4[:, 1, :, :], cx.maskA, cx.maskA[:, 1, :, :], ALU.mult)
                    bi_ctr += 1
                    if k2 < 3:
                        continue
                    for bi, b in enumerate(blocks):
                        r, n = divmod(b, nb)
                        pO = psO[b % 2]
                        for hh in range(2):
                            PT = PTh[hh]
                            mm(s, pO, pO[:, hh, :], Vaug, Vaug[:, b, hh, :], PT, PT[:, bi, 0, :], True, n == 0)
                            if n > 0:
                                mm(s, pO, pO[:, hh, :], Vaug, Vaug[:, b - 1, hh, :], PT, PT[:, bi, 1, :], False, True)
                        if k2 < 4:
                            continue
                        if di == 0:
                            cp(s, "dve", acc, acc[:, :, tok(r, n)], pO, pO[:, :, :])
                        else:
                            tt(s, "dve", acc, acc[:, :, tok(r, n)], acc, acc[:, :, tok(r, n)], pO, pO[:, :, :], ALU.add)
            if k2 < 5:
                continue
            for tg in range(8):
                tsl = slice(tg * 512, (tg + 1) * 512)
                pd = psV[tg % 2]
                rec = recs[tg % 2]
                mm(s, pd, pd[:, :], cx.swlo, cx.swlo[:, :], acc, acc[:, 1, tsl], True, False)
                mm(s, pd, pd[:, :], cx.swhi, cx.swhi[:, :], acc, acc[:, 0, tsl], False, True)
                s.op("dve", lambda e, rec=rec, pd=pd: e.reciprocal(out=rec[:, :], in_=pd[:, :]), reads=[pd], writes=[rec], cost=500)
                tt(s, "pool", obuf, obuf[0:64, tsl], acc, acc[0:64, 0, tsl], rec, rec[0:64, :], ALU.mult)
                tt(s, "pool", obuf, obuf[64:128, tsl], acc, acc[64:128, 1, tsl], rec, rec[64:128, :], ALU.mult)
            st(s, "sp", attT_res, attT[hp * 128:(hp + 1) * 128, :], obuf, obuf[:, :])
        s.phase_es = old
    s.end_phase()

    if stop < 3:
        return
    s.begin_phase()
    proj_ln(s, cx, dr, attT_res, attT, 8, w_out, h_in, h_out, dr["ln1_g"][l], dr["ln1_b"][l])
    s.end_phase()


def s_sub_barrier(s):
    es = s.phase_es
    s.phase_es = ExitStack()
    s.end_phase()
    s.phase_es = es


def proj_ln(s, cx, dr, aT_res, aT, nchunk, w_out, h_in, h_out, g_row, b_row):
    h_in_res, h_in_ap = h_in[0], h_in[1]
    h_out_res, h_out_ap, final = h_out
    wres = dr["wres"]
    wo = s.sb([128, nchunk, 1024], BF16, "wo")
    for c in range(nchunk):
        wload(s, cx, wo, wo[:, c, :], wres, w_out[c * 128:(c + 1) * 128, :], [1024])
    g_bc = s.sb([128, 1024], F32, "gbc")
    b_bc = s.sb([128, 1024], F32, "bbc")
    ld(s, "sp", g_bc, g_bc[:, :], wres, bcast_row(g_row, 1024))
    ld(s, "sp", b_bc, b_bc[:, :], wres, bcast_row(b_row, 1024))
    ats = [s.sb([128, nchunk, 512], BF16, "at") for _ in range(2)]
    hfs = [s.sb([128, 1024], F32, "hf2") for _ in range(2)]
    hos = [s.sb([128, 1024], F32, "ho") for _ in range(2)]
    psY = [[s.ps([128, 512], F32, "psY") for _ in range(2)] for _ in range(2)]
    wk = ln_work(s)
    aTv = aT[0:nchunk * 128, :].rearrange("(c p) t -> p c t", p=128)
    for tg in range(8):
        at = ats[tg % 2]
        ld(s, "sp", at, at[:, :, :], aT_res, aTv[:, :, tg * 512:(tg + 1) * 512])
        for ti in range(4):
            i = tg * 4 + ti
            hf = hfs[i % 2]
            ho = hos[i % 2]
            py = psY[i % 2]
            ld(s, "sp", hf, hf[:, :], h_in_res, h_in_ap[i * 128:(i + 1) * 128, :])
            for half in range(2):
                for c in range(nchunk):
                    mm(s, py[half], py[half][:, :], at, at[:, c, ti * 128:(ti + 1) * 128], wo,
                       wo[:, c, half * 512:(half + 1) * 512], c == 0, c == nchunk - 1)
            ln_epilogue(s, cx, py, hf, g_bc, b_bc, wk, ho)
            st(s, "pool", h_out_res, h_out_ap[i * 128:(i + 1) * 128, :], ho, ho[:, :], final=final)


def decay_tables(s, cx, LF, ig, nh, kscale_ln, EA, EB, ET, psX, DEC=None):
    n = NT * nh
    Fin = s.sb([128, NT, nh], F32, "Fin")
    tot = s.sb([128, NT, nh], F32, "tot")
    inc = s.sb([128, NT, nh], F32, "inc")
    LF2 = LF[:, :, :].rearrange("p a b -> p (a b)")
    mm(s, psX, psX[:, 0:n], cx.tri_le, cx.tri_le[:, :], LF, LF2, True, True)
    cp(s, "dve", Fin, Fin[:, :, :].rearrange("p a b -> p (a b)"), psX, psX[:, 0:n])
    mm(s, psX, psX[:, 0:n], cx.ones_f, cx.ones_f[:, :], LF, LF2, True, True)
    cp(s, "dve", tot, tot[:, :, :].rearrange("p a b -> p (a b)"), psX, psX[:, 0:n])
    for h in range(nh):
        s.op("dve", lambda e, h=h: e.tensor_tensor_scan(out=inc[:, :, h], data0=cx.ones_f[:, 0:NT], data1=tot[:, :, h],
                                                        initial=0.0, op0=ALU.mult, op1=ALU.add),
             reads=[cx.ones_f, tot], writes=[inc])
    tt(s, "dve", EB, EB[:, :, :], inc, inc[:, :, :], tot, tot[:, :, :], ALU.subtract)
    tt(s, "dve", EA, EA[:, :, :], Fin, Fin[:, :, :], EB, EB[:, :, :], ALU.add)
    if ig is not None:
        tt(s, "dve", EA, EA[:, :, :], ig[0], ig[1], EA, EA[:, :, :], ALU.subtract)
        ts(s, "dve", EA, EA[:, :, :], EA, EA[:, :, :], kscale_ln, None, ALU.add)
    else:
        ts(s, "dve", EA, EA[:, :, :], EA, EA[:, :, :], -1.0, kscale_ln, ALU.mult, ALU.add)
    act(s, ET, ET[:, :, :], Fin, Fin[:, :, :], AF.Exp)
    if DEC is not None:
        act(s, DEC, DEC[:, :, :], tot, tot[:, :, :], AF.Exp)


def make_Ediag(s, Ediag, Enext, EA, EB, h, tmp2):
    tt(s, "dve", tmp2, tmp2[:, :], EA, EA[:, :, h], EB, EB[:, :, h], ALU.add)
    ts(s, "dve", tmp2, tmp2[:, :], tmp2, tmp2[:, :], 80.0, None, ALU.min)
    act(s, Ediag, Ediag[:, :], tmp2, tmp2[:, :], AF.Exp)
    tt(s, "dve", tmp2, tmp2[:, 0:NT - 1], EA, EA[:, 0:NT - 1, h], EB, EB[:, 1:NT, h], ALU.add)
    ts(s, "dve", tmp2, tmp2[:, 0:NT - 1], tmp2, tmp2[:, 0:NT - 1], 80.0, None, ALU.min)
    act(s, Enext, Enext[:, 0:NT - 1], tmp2, tmp2[:, 0:NT - 1], AF.Exp)


def make_E(s, E, EA, EB, h, tmpE):
    tt(s, "pool", tmpE, tmpE[:, :, :], EA, EA[:, :, h].unsqueeze(2).to_broadcast([128, NT, NT]),
       EB, EB[:, :, h].unsqueeze(1).to_broadcast([128, NT, NT]), ALU.add)
    ts(s, "pool", tmpE, tmpE[:, :, :], tmpE, tmpE[:, :, :], 80.0, None, ALU.min)
    act(s, E, E[:, :, :], tmpE, tmpE[:, :, :], AF.Exp)


def decay_main(s, cx, heads, psS, psAcc, PTs, fin_cb, jmax=NT):
    cnt = [0] * len(heads)
    for j in range(jmax):
        for hi, hd in enumerate(heads):
            acc = psAcc[hi]
            dvw = hd["dvw"]
            nk = len(hd["kparts"])
            for i0 in range(0, j + 1, 4):
                i1 = min(i0 + 4, j + 1)
                n = i1 - i0
                pS = psS[hi][cnt[hi] % 2]
                PT = PTs[hi][cnt[hi] % 2]
                cnt[hi] += 1
                for ii in range(n):
                    i = i0 + ii
                    for kc, (Kt, Kap, Qt, Qap) in enumerate(hd["kparts"]):
                        mm(s, pS, pS[:, ii * 128:(ii + 1) * 128], Kt, Kap(i), Qt, Qap(j), kc == 0, kc == nk - 1)
                E = hd["E"]
                tt(s, "dve", PT, PT[:, 0:n, :], pS, pS[:, 0:n * 128].rearrange("p (a t) -> p a t", a=n),
                   E, E[:, i0:i1, j].unsqueeze(2).to_broadcast([128, n, 128]), ALU.mult)
                if i1 - 1 == j:
                    tt(s, "pool", PT, PT[:, n - 1, :], PT, PT[:, n - 1, :], cx.tri_b, cx.tri_b[:, :], ALU.mult)
                for ii in range(n):
                    i = i0 + ii
                    mm(s, acc, acc[:, 0:dvw], PT, PT[:, ii, :], hd["V"], hd["Vap"](i), i == 0, i == j)
            fin_cb(hi, hd, j, acc)


def small_norm(s, x_t, x_ap, width, wk, out_t, out_ap, g_t, g_ap):
    stats, mv, sd, rstd, nmr, xn = wk
    s.op("dve", lambda e: e.bn_stats(out=stats[:, :], in_=x_ap), reads=[x_t], writes=[stats])
    s.op("dve", lambda e: e.bn_aggr(out=mv[:, :], in_=stats[:, :]), reads=[stats], writes=[mv])
    ts(s, "dve", sd, sd[:, :], mv, mv[:, 1:2], LN_EPS, None, ALU.add)
    act(s, sd, sd[:, :], sd, sd[:, :], AF.Sqrt)
    s.op("dve", lambda e: e.reciprocal(out=rstd[:, :], in_=sd[:, :]), reads=[sd], writes=[rstd])
    stt(s, "dve", nmr, nmr[:, :], mv, mv[:, 0:1], -1.0, rstd, rstd[:, :], ALU.mult, ALU.mult)
    act(s, xn, xn[:, 0:width], x_t, x_ap, AF.Identity, scale=rstd[:, 0:1], bias=nmr[:, 0:1], extra_reads=[rstd, nmr])
    tt(s, "pool", out_t, out_ap, xn, xn[:, 0:width], g_t, g_ap, ALU.mult)


def small_norm_work(s, width):
    return (s.sb([128, 6], F32, "snst"), s.sb([128, 2], F32, "snmv"), s.sb([128, 1], F32, "snsd"),
            s.sb([128, 1], F32, "snrs"), s.sb([128, 1], F32, "snnm"), s.sb([128, width], F32, "snxn"))


def gate_proj_ln(s, cx, dr, xT, HN_res, HN, F, w_gate_ap, gfunc, w_out, h_in, h_out, g_row, b_row):
    h_in_res, h_in_ap = h_in[0], h_in[1]
    h_out_res, h_out_ap, final = h_out
    wres = dr["wres"]
    nch = F // 128
    nb = F // 512
    wg = s.sb([128, 8, F], BF16, "wgate")
    for c in range(8):
        for q in range(F // 1024):
            wload(s, cx, wg, wg[:, c, q * 1024:(q + 1) * 1024], wres, w_gate_ap[c * 128:(c + 1) * 128, q * 1024:(q + 1) * 1024], [1024])
    wo = s.sb([128, nch, 1024], BF16, "wo")
    for c in range(nch):
        wload(s, cx, wo, wo[:, c, :], wres, w_out[c * 128:(c + 1) * 128, :], [1024])
    g_bc = s.sb([128, 1024], F32, "gbc")
    b_bc = s.sb([128, 1024], F32, "bbc")
    ld(s, "sp", g_bc, g_bc[:, :], wres, bcast_row(g_row, 1024))
    ld(s, "sp", b_bc, b_bc[:, :], wres, bcast_row(b_row, 1024))
    nbuf = 1 if F > 1024 else 2
    hns = [s.sb([128, F], F32, "hn") for _ in range(nbuf)]
    sig = s.sb([128, F], F32, "sig")
    Gb = s.sb([128, F], BF16, "Gb")
    GT_ = s.sb([128, nch, 128], BF16, "GTt")
    hfs = [s.sb([128, 1024], F32, "hf2") for _ in range(nbuf)]
    hos = [s.sb([128, 1024], F32, "ho") for _ in range(nbuf)]
    psg = [s.ps([128, 512], F32, "psg") for _ in range(2)]
    pst = [s.ps([128, 1024], BF16, "pst") for _ in range(2)]
    psY = [s.ps([128, 512], F32, "psY") for _ in range(2)]
    wk = ln_work(s)
    for i in range(NT):
        hn = hns[i % nbuf]
        hf = hfs[i % nbuf]
        ho = hos[i % nbuf]
        ld(s, "sp", hn, hn[:, :], HN_res, HN[i * 128:(i + 1) * 128, 0:F])
        ld(s, "sp", hf, hf[:, :], h_in_res, h_in_ap[i * 128:(i + 1) * 128, :])
        for q in range(nb):
            pg = psg[q % 2]
            for c in range(8):
                mm(s, pg, pg[:, :], xT, xT[:, c, i * 128:(i + 1) * 128], wg, wg[:, c, q * 512:(q + 1) * 512], c == 0, c == 7)
            act(s, sig, sig[:, q * 512:(q + 1) * 512], pg, pg[:, :], gfunc)
        tt(s, "pool", Gb, Gb[:, :], hn, hn[:, :], sig, sig[:, :], ALU.mult)
        for q in range(nch // 8):
            pt = pst[q % 2]
            for c8 in range(8):
                c = q * 8 + c8
                s.op("pe", lambda e, pt=pt, c8=c8, c=c: e.transpose(out=pt[:, c8 * 128:(c8 + 1) * 128], in_=Gb[:, c * 128:(c + 1) * 128],
                                                                   identity=cx.ident_b[:, :]), reads=[Gb, cx.ident_b], writes=[pt])
            cp(s, "act" if q else "dve", GT_, GT_[:, q * 8:(q + 1) * 8, :], pt, pt[:, :].rearrange("p (c t) -> p c t", c=8))
        for half in range(2):
            for c in range(nch):
                mm(s, psY[half], psY[half][:, :], GT_, GT_[:, c, :], wo, wo[:, c, half * 512:(half + 1) * 512], c == 0, c == nch - 1)
        ln_epilogue(s, cx, psY, hf, g_bc, b_bc, wk, ho)
        st(s, "pool", h_out_res, h_out_ap[i * 128:(i + 1) * 128, :], ho, ho[:, :], final=final)


def phase_B(s, cx, dr, l, j_, h_in, h_out):
    import os
    nc = s.nc
    h_in_res, h_in_ap = h_in[0], h_in[1]
    wres = dr["wres"]
    w_in = dr["b_w_in"][j_]
    w_in_v = w_in.rearrange("(c p) n -> p c n", p=128)
    HN_res, HN = dr["hn"]
    bstop = int(os.environ.get("BSTOP", "9"))
    jmax = int(os.environ.get("BJMAX", str(NT)))
    npair = int(os.environ.get("BPAIRS", "4"))

    s.begin_phase()
    xT = s.sb([128, 8, S], BF16, "xT")
    GATES = s.sb([128, NT, 16], F32, "GATES")
    with ExitStack() as es0:
        old = s.phase_es
        s.phase_es = es0
        Wg32 = s.sb([128, 8, 16], F32, "Wg32")
        ld(s, "sp", Wg32, Wg32[:, :, :], wres, w_in_v[:, :, 3072:3088])
        gb = s.sb([128, 16], F32, "gbias")
        ld(s, "sp", gb, gb[:, :], wres, bcast_row(dr["b_gate_bias"][j_], 16))
        hfs = [s.sb([128, 1024], F32, "hf") for _ in range(3)]
        xTts = [s.sb([128, 8, 128], F32, "xTt") for _ in range(2)]
        pts = [s.ps([128, 512], F32, "ptr") for _ in range(2)]
        psg = s.ps([128, 512], F32, "psgate")
        for i in range(NT):
            hf = hfs[i % 3]
            xTt = xTts[i % 2]
            ld(s, "sp", hf, hf[:, :], h_in_res, h_in_ap[i * 128:(i + 1) * 128, :])
            for half in range(2):
                pt = pts[half]
                for c4 in range(4):
                    c = half * 4 + c4
                    s.op("pe", lambda e, pt=pt, c4=c4, c=c, hf=hf: e.transpose(
                        out=pt[:, c4 * 128:(c4 + 1) * 128], in_=hf[:, c * 128:(c + 1) * 128], identity=cx.ident_f[:, :]),
                        reads=[hf, cx.ident_f], writes=[pt])
                src = pt[:, :].rearrange("p (c t) -> p c t", c=4)
                cp(s, "act", xT, xT[:, half * 4:(half + 1) * 4, i * 128:(i + 1) * 128], pt, src)
                cp(s, "dve", xTt, xTt[:, half * 4:(half + 1) * 4, :], pt, src)
            for c in range(8):
                mm(s, psg, psg[:, 0:16], xTt, xTt[:, c, :], Wg32, Wg32[:, c, :], c == 0, c == 7)
            tt(s, "dve", GATES, GATES[:, i, :], psg, psg[:, 0:16], gb, gb[:, :], ALU.add)
        s.phase_es = old
    s_sub_barrier(s)

    EA = s.sb([128, NT, 8], F32, "EA")
    EB = s.sb([128, NT, 8], F32, "EB")
    ET = s.sb([128, NT, 8], F32, "ET")
    DEC = s.sb([128, NT, 8], F32, "DEC")
    with ExitStack() as es1:
        old = s.phase_es
        s.phase_es = es1
        LF = s.sb([128, NT, 8], F32, "LF")
        psX = s.ps([128, 512], F32, "psX")
        act(s, LF, LF[:, :, :], GATES, GATES[:, :, 8:16], AF.Exp, scale=-1.0)
        ts(s, "dve", LF, LF[:, :, :], LF, LF[:, :, :], 1.0, None, ALU.add)
        act(s, LF, LF[:, :, :], LF, LF[:, :, :], AF.Ln)
        ts(s, "dve", LF, LF[:, :, :], LF, LF[:, :, :], -1.0, None, ALU.mult)
        decay_tables(s, cx, LF, (GATES, GATES[:, :, 0:8]), 8, math.log(0.125), EA, EB, ET, psX, DEC)
        s.phase_es = old
    s_sub_barrier(s)
    if bstop < 2:
        s.end_phase()
        return

    with ExitStack() as es2:
        old = s.phase_es
        s.phase_es = es2
        ng_bc = s.sb([128, 1024], F32, "ngbc")
        ld(s, "sp", ng_bc, ng_bc[:, :], wres, bcast_row(dr["b_norm_g"][j_], 1024))
        wq = s.sb([128, 8, 128], BF16, "wq")
        wkk = s.sb([128, 8, 128], BF16, "wk")
        wv = s.sb([128, 8, 256], BF16, "wv")
        cw = [s.sb([128, 4], F32, "cw") for _ in range(2)]
        cb = [s.sb([128, 1], F32, "cb") for _ in range(2)]
        UT = s.sb([128, S + 3], F32, "UT")
        s.op("pool", lambda e: e.memset(UT[:, 0:3], 0.0), writes=[UT])
        cacc = s.sb([128, S], F32, "cacc")
        QT = s.sb([128, S], BF16, "QT")
        KT = s.sb([128, S], BF16, "KT")
        Vaug = s.sb([128, NT, 2, 129], BF16, "Vaug")
        s.op("pool", lambda e: e.memset(Vaug[:, :, :, 128:129], 1.0), writes=[Vaug])
        Edg = [s.sb([128, NT], F32, "Edg") for _ in range(2)]
        Enx = [s.sb([128, NT], F32, "Enx") for _ in range(2)]
        tmp2 = s.sb([128, NT], F32, "tmp2")
        Dp = s.sb([128, NT], F32, "Dp")
        R32 = s.sb([128, 129], F32, "R32")
        Rb = s.sb([128, 129], BF16, "Rb")
        Ktss = [s.sb([128, 128], BF16, "Kts") for _ in range(2)]
        psX = [s.ps([128, 512], F32, "psX") for _ in range(2)]
        psS = [s.ps([128, 512], F32, "psS") for _ in range(2)]
        psAcc = [s.ps([128, 512], F32, "psAcc") for _ in range(2)]
        psT = s.ps([128, 1024], BF16, "psTk")
        psKV = s.ps([128, 512], F32, "psKV")
        PTs = [[s.sb([128, 1, 128], BF16, "PT") for _ in range(2)] for _ in range(2)]
        snw = small_norm_work(s, 128)
        sm = {k: s.sb([128, 1], F32, k) for k in ("ed", "ad", "rec", "fac")}
        hnr = s.sb([128, 128], F32, "hnr")
        HNt = [s.sb([128, 256], F32, "HNt") for _ in range(2)]
        for hp in range(npair):
            for w, (wt, dst) in enumerate(((wq, QT), (wkk, KT))):
                col = w * 512 + hp * 128
                wload(s, cx, wt, wt[:, :, :], wres, w_in_v[:, :, col:col + 128], [8, 128])
                for jj in range(4):
                    ld(s, "sp", cw[w], cw[w][:, jj:jj + 1], wres, dr["b_conv_w"][j_][jj, col:col + 128].rearrange("(c o) -> c o", o=1))
                ld(s, "sp", cb[w], cb[w][:, :], wres, dr["b_conv_b"][j_][col:col + 128].rearrange("(c o) -> c o", o=1))
                for tg in range(8):
                    px = psX[tg % 2]
                    for c in range(8):
                        mm(s, px, px[:, :], wt, wt[:, c, :], xT, xT[:, c, tg * 512:(tg + 1) * 512], c == 0, c == 7)
                    cp(s, "act", UT, UT[:, 3 + tg * 512:3 + (tg + 1) * 512], px, px[:, :])
                ts(s, "dve", cacc, cacc[:, :], UT, UT[:, 0:S], cw[w][:, 0:1], None, ALU.mult, extra_reads=[cw[w]])
                for jj in range(1, 4):
                    stt(s, "dve", cacc, cacc[:, :], UT, UT[:, jj:S + jj], cw[w][:, jj:jj + 1], cacc, cacc[:, :], ALU.mult, ALU.add,
                        extra_reads=[cw[w]])
                act(s, dst, dst[:, :], cacc, cacc[:, :], AF.Silu, scale=1.0, bias=cb[w][:, 0:1], extra_reads=[cb[w]])
            wload(s, cx, wv, wv[:, :, :], wres, w_in_v[:, :, 1024 + hp * 256:1024 + (hp + 1) * 256], [8, 256])
            for i in range(NT):
                px = psX[i % 2]
                for c in range(8):
                    mm(s, px, px[:, 0:256], xT, xT[:, c, i * 128:(i + 1) * 128], wv, wv[:, c, :], c == 0, c == 7)
                cp(s, "act", Vaug, Vaug[:, i, :, 0:128], px, px[:, 0:256].rearrange("p (a d) -> p a d", a=2))
            heads = []
            for hh in range(2):
                h = hp * 2 + hh
                make_Ediag(s, Edg[hh], Enx[hh], EA, EB, h, tmp2)
                hs = slice(hh * 64, (hh + 1) * 64)
                cp(s, "pool", Dp, Dp[hs, :], DEC, DEC[hs, :, h])
                heads.append(dict(h=h, hh=hh))
            s.op("pool", lambda e: e.memset(R32[:, :], 0.0), writes=[R32])
            s.op("pool", lambda e: e.memset(Rb[:, :], 0.0), writes=[Rb])

            def fin(hi, hd, j, acc):
                h, hh = hd["h"], hd["hh"]
                tt(s, "dve", sm["ed"], sm["ed"][:, :], acc, acc[:, 128:129], ET, ET[:, j, h:h + 1], ALU.mult)
                stt(s, "dve", sm["ad"], sm["ad"][:, :], sm["ed"], sm["ed"][:, :], -1.0, sm["ed"], sm["ed"][:, :], ALU.mult, ALU.max)
                ts(s, "dve", sm["ad"], sm["ad"][:, :], sm["ad"], sm["ad"][:, :], 1.0, None, ALU.max)
                s.op("dve", lambda e: e.reciprocal(out=sm["rec"][:, :], in_=sm["ad"][:, :]), reads=[sm["ad"]], writes=[sm["rec"]])
                tt(s, "dve", sm["fac"], sm["fac"][:, :], sm["rec"], sm["rec"][:, :], ET, ET[:, j, h:h + 1], ALU.mult)
                act(s, hnr, hnr[:, :], acc, acc[:, 0:128], AF.Copy, scale=sm["fac"][:, 0:1], extra_reads=[sm["fac"]])
                ht = HNt[j % 2]
                small_norm(s, hnr, hnr[:, :], 128, snw, ht, ht[:, hh * 128:(hh + 1) * 128], ng_bc, ng_bc[:, h * 128:(h + 1) * 128])
                if hh == 1:
                    st(s, "pool", HN_res, HN[j * 128:(j + 1) * 128, hp * 256:(hp + 1) * 256], ht, ht[:, :])
            for j in range(jmax):
                tj = slice(j * 128, (j + 1) * 128)
                for hh in range(2):
                    hs = slice(hh * 64, (hh + 1) * 64)
                    pS = psS[hh]
                    PT = PTs[hh][j % 2]
                    acc = psAcc[hh]
                    mm(s, pS, pS[:, 0:128], KT, KT[hs, tj], QT, QT[hs, tj], True, True)
                    ts(s, "dve", PT, PT[:, 0, :], pS, pS[:, 0:128], Edg[hh][:, j:j + 1], None, ALU.mult, extra_reads=[Edg[hh]])
                    tt(s, "pool", PT, PT[:, 0, :], PT, PT[:, 0, :], cx.tri_b, cx.tri_b[:, :], ALU.mult)
                    mm(s, acc, acc[:, 0:129], PT, PT[:, 0, :], Vaug, Vaug[:, j, hh, :], True, j == 0)
                    if j > 0:
                        mm(s, acc, acc[:, 0:129], QT, QT[hs, tj], Rb, Rb[hs, :], False, True)
                    fin(hh, heads[hh], j, acc)
                if j < jmax - 1:
                    s.op("pe", lambda e, tj=tj: e.transpose(out=psT[:, 0:128], in_=KT[:, tj], identity=cx.ident_b[:, :]),
                         reads=[KT, cx.ident_b], writes=[psT], cost=150)
                    Kts = Ktss[j % 2]
                    for hh in range(2):
                        hs = slice(hh * 64, (hh + 1) * 64)
                        act(s, Kts, Kts[:, hs], psT, psT[:, hs], AF.Copy, scale=Enx[hh][:, j:j + 1], extra_reads=[Enx[hh]])
                    for hh in range(2):
                        hs = slice(hh * 64, (hh + 1) * 64)
                        mm(s, psKV, psKV[hs, 0:129], Kts, Kts[:, hs], Vaug, Vaug[:, j, hh, :], True, True)
                    stt(s, "dve", R32, R32[:, :], R32, R32[:, :], Dp[:, j:j + 1], psKV, psKV[:, 0:129], ALU.mult, ALU.add, extra_reads=[Dp])
                    cp(s, "act", Rb, Rb[:, :], R32, R32[:, :])
        s.phase_es = old
    s_sub_barrier(s)
    if bstop < 3:
        s.end_phase()
        return

    with ExitStack() as es3:
        old = s.phase_es
        s.phase_es = es3
        gate_proj_ln(s, cx, dr, xT, HN_res, HN, 1024, w_in[:, 2048:3072], AF.Sigmoid, dr["b_w_out"][j_], h_in, h_out,
                     dr["ln1_g"][l], dr["ln1_b"][l])
        s.phase_es = old
    s.end_phase()


def phase_C(s, cx, dr, l, j_, h_in, h_out):
    import os
    nc = s.nc
    h_in_res, h_in_ap = h_in[0], h_in[1]
    wres = dr["wres"]
    w_in = dr["c_w_in"][j_]
    w_in_v = w_in.rearrange("(c p) n -> p c n", p=128)
    HN_res, HN = dr["hn"]
    qkT_res, qkT = dr["qkT"]
    cstop = int(os.environ.get("CSTOP", "9"))
    jmax = int(os.environ.get("CJMAX", str(NT)))
    nheads = int(os.environ.get("CHEADS", "4"))

    s.begin_phase()
    xT = s.sb([128, 8, S], BF16, "xT")
    load_xT(s, cx, h_in_res, h_in_ap, xT)
    EA = s.sb([128, NT, 4], F32, "EA")
    EB = s.sb([128, NT, 4], F32, "EB")
    ET = s.sb([128, NT, 4], F32, "ET")
    DEC = s.sb([128, NT, 4], F32, "DEC")
    with ExitStack() as es1:
        old = s.phase_es
        s.phase_es = es1
        LF = s.sb([128, NT, 4], F32, "LF")
        psX = s.ps([128, 512], F32, "psX")
        for h in range(4):
            s.op("pool", lambda e, h=h: e.memset(LF[:, :, h:h + 1], math.log(1.0 - 2.0 ** (-5.0 - h))), writes=[LF])
        decay_tables(s, cx, LF, None, 4, math.log(1.0 / 16.0), EA, EB, ET, psX, DEC)
        s.phase_es = old
    s_sub_barrier(s)

    with ExitStack() as es1:
        old = s.phase_es
        s.phase_es = es1
        wqk = s.sb([128, 8, 2048], BF16, "wqk")
        for c in range(8):
            wload(s, cx, wqk, wqk[:, c, :], wres, w_in[c * 128:(c + 1) * 128, 0:2048], [2048])
        rc = s.sb([128, 4], F32, "ropec")
        ld(s, "sp", rc, rc[:, :], wres, dr["rope_consts"])
        posi = s.sb([128, 512], I32, "posi")
        posf = s.sb([128, 512], F32, "posf")
        ang = s.sb([128, 512], F32, "ang")
        ang2 = s.sb([128, 512], F32, "ang2")
        Ct = s.sb([128, 512], F32, "Ct")
        St = s.sb([128, 512], F32, "St")
        scr = (s.sb([128, 512], I32, "ki"), s.sb([128, 512], F32, "kf"), s.sb([128, 512], F32, "rr"))
        psa = [s.ps([128, 512], F32, "psa") for _ in range(2)]
        psb = [s.ps([128, 512], F32, "psb") for _ in range(2)]
        t1s = [s.sb([128, 512], F32, "t1") for _ in range(2)]
        t2s = [s.sb([128, 512], F32, "t2") for _ in range(2)]
        oas = [s.sb([128, 512], BF16, "oa") for _ in range(2)]
        obs = [s.sb([128, 512], BF16, "ob") for _ in range(2)]
        it = 0
        for tg in range(8):
            tsl = slice(tg * 512, (tg + 1) * 512)
            ld(s, "sp", posi, posi[:, :], wres, bcast_row(dr["positions"][tg * 512:(tg + 1) * 512], 512))
            cp(s, "dve", posf, posf[:, :], posi, posi[:, :])
            ts(s, "dve", ang, ang[:, :], posf, posf[:, :], rc[:, 2:3], None, ALU.mult, extra_reads=[rc])
            ts(s, "dve", ang2, ang2[:, :], ang, ang[:, :], math.pi / 2, None, ALU.add)
            sin_table(s, St, St[:, :], ang, ang[:, :], None, None, None, scr)
            sin_table(s, Ct, Ct[:, :], ang2, ang2[:, :], None, None, None, scr)
            for h in range(4):
                for w in range(2):
                    pa, pb = psa[it % 2], psb[it % 2]
                    t1, t2, oa, ob = t1s[it % 2], t2s[it % 2], oas[it % 2], obs[it % 2]
                    it += 1
                    col = w * 1024 + h * 256
                    for c in range(8):
                        mm(s, pa, pa[:, :], wqk, wqk[:, c, col:col + 128], xT, xT[:, c, tsl], c == 0, c == 7)
                    for c in range(8):
                        mm(s, pb, pb[:, :], wqk, wqk[:, c, col + 128:col + 256], xT, xT[:, c, tsl], c == 0, c == 7)
                    tt(s, "dve", t1, t1[:, :], pa, pa[:, :], Ct, Ct[:, :], ALU.mult)
                    tt(s, "dve", t2, t2[:, :], pb, pb[:, :], St, St[:, :], ALU.mult)
                    tt(s, "pool", oa, oa[:, :], t1, t1[:, :], t2, t2[:, :], ALU.subtract)
                    st(s, "pool", qkT_res, qkT[w, h * 256:h * 256 + 128, tsl], oa, oa[:, :])
                    tt(s, "dve", t1, t1[:, :], pb, pb[:, :], Ct, Ct[:, :], ALU.mult)
                    tt(s, "dve", t2, t2[:, :], pa, pa[:, :], St, St[:, :], ALU.mult)
                    tt(s, "pool", ob, ob[:, :], t1, t1[:, :], t2, t2[:, :], ALU.add)
                    st(s, "pool", qkT_res, qkT[w, h * 256 + 128:h * 256 + 256, tsl], ob, ob[:, :])
        s.phase_es = old
    s_sub_barrier(s)
    if cstop < 2:
        s.end_phase()
        return

    with ExitStack() as es2:
        old = s.phase_es
        s.phase_es = es2
        ng_bc = s.sb([128, 2048], F32, "ngbc")
        ld(s, "sp", ng_bc, ng_bc[:, :], wres, bcast_row(dr["c_norm_g"][j_], 2048))
        QA = s.sb([128, S], BF16, "QA")
        QB = s.sb([128, S], BF16, "QB")
        KA = s.sb([128, S], BF16, "KA")
        KB = s.sb([128, S], BF16, "KB")
        wv = s.sb([128, 8, 512], BF16, "wv")
        Vh = s.sb([128, NT, 512], BF16, "Vh")
        Edg = s.sb([128, NT], F32, "Edg")
        Enx = s.sb([128, NT], F32, "Enx")
        tmp2 = s.sb([128, NT], F32, "tmp2")
        R32 = s.sb([128, 2, 512], F32, "R32")
        Rb = s.sb([128, 2, 512], BF16, "Rb")
        Ktss = [s.sb([128, 256], BF16, "Kts") for _ in range(2)]
        psX = [s.ps([128, 512], F32, "psX") for _ in range(2)]
        psS = [s.ps([128, 512], F32, "psS") for _ in range(2)]
        psAcc = s.ps([128, 512], F32, "psAcc")
        psT = s.ps([128, 1024], BF16, "psTk")
        psKV = [s.ps([128, 512], F32, "psKV") for _ in range(2)]
        PTs = [s.sb([128, 128], BF16, "PT") for _ in range(2)]
        snw = small_norm_work(s, 512)
        onr = s.sb([128, 512], F32, "onr")
        HNt = [s.sb([128, 512], F32, "HNt") for _ in range(2)]
        for h in range(nheads):
            for w, (ta, tb) in enumerate(((QA, QB), (KA, KB))):
                ld(s, "sp", ta, ta[:, :], qkT_res, qkT[w, h * 256:h * 256 + 128, :])
                ld(s, "sp", tb, tb[:, :], qkT_res, qkT[w, h * 256 + 128:h * 256 + 256, :])
            wload(s, cx, wv, wv[:, 0:4, :], wres, w_in_v[:, 0:4, 2048 + h * 512:2048 + (h + 1) * 512], [4, 512])
            wload(s, cx, wv, wv[:, 4:8, :], wres, w_in_v[:, 4:8, 2048 + h * 512:2048 + (h + 1) * 512], [4, 512])
            for i in range(NT):
                px = psX[i % 2]
                for c in range(8):
                    mm(s, px, px[:, :], xT, xT[:, c, i * 128:(i + 1) * 128], wv, wv[:, c, :], c == 0, c == 7)
                cp(s, "act", Vh, Vh[:, i, :], px, px[:, :])
            make_Ediag(s, Edg, Enx, EA, EB, h, tmp2)
            s.op("pool", lambda e: e.memset(R32[:, :, :], 0.0), writes=[R32], cost=2000)
            s.op("pool", lambda e: e.memset(Rb[:, :, :], 0.0), writes=[Rb], cost=2000)

            def fin(hi, hd, j, acc, h=h):
                act(s, onr, onr[:, :], acc, acc[:, 0:512], AF.Copy, scale=ET[:, j, h:h + 1], extra_reads=[ET])
                ht = HNt[j % 2]
                small_norm(s, onr, onr[:, :], 512, snw, ht, ht[:, :], ng_bc, ng_bc[:, h * 512:(h + 1) * 512])
                st(s, "pool", HN_res, HN[j * 128:(j + 1) * 128, h * 512:(h + 1) * 512], ht, ht[:, :])
            for j in range(jmax):
                tj = slice(j * 128, (j + 1) * 128)
                pS = psS[j % 2]
                PT = PTs[j % 2]
                acc = psAcc
                mm(s, pS, pS[:, 0:128], KA, KA[:, tj], QA, QA[:, tj], True, False)
                mm(s, pS, pS[:, 0:128], KB, KB[:, tj], QB, QB[:, tj], False, True)
                ts(s, "dve", PT, PT[:, :], pS, pS[:, 0:128], Edg[:, j:j + 1], None, ALU.mult, extra_reads=[Edg])
                tt(s, "pool", PT, PT[:, :], PT, PT[:, :], cx.tri_b, cx.tri_b[:, :], ALU.mult)
                mm(s, acc, acc[:, :], PT, PT[:, :], Vh, Vh[:, j, :], True, j == 0)
                if j > 0:
                    mm(s, acc, acc[:, :], QA, QA[:, tj], Rb, Rb[:, 0, :], False, False)
                    mm(s, acc, acc[:, :], QB, QB[:, tj], Rb, Rb[:, 1, :], False, True)
                fin(0, None, j, acc)
                if j < jmax - 1:
                    s.op("pe", lambda e, tj=tj: e.transpose(out=psT[:, 0:128], in_=KA[:, tj], identity=cx.ident_b[:, :]),
                         reads=[KA, cx.ident_b], writes=[psT], cost=150)
                    s.op("pe", lambda e, tj=tj: e.transpose(out=psT[:, 128:256], in_=KB[:, tj], identity=cx.ident_b[:, :]),
                         reads=[KB, cx.ident_b], writes=[psT], cost=150)
                    Kts = Ktss[j % 2]
                    act(s, Kts, Kts[:, :], psT, psT[:, 0:256], AF.Copy, scale=Enx[:, j:j + 1], extra_reads=[Enx])
                    for ck in range(2):
                        mm(s, psKV[ck], psKV[ck][:, :], Kts, Kts[:, ck * 128:(ck + 1) * 128], Vh, Vh[:, j, :], True, True)
                        stt(s, "dve", R32, R32[:, ck, :], R32, R32[:, ck, :], DEC[:, j, h:h + 1], psKV[ck], psKV[ck][:, :],
                            ALU.mult, ALU.add, extra_reads=[DEC])
                    cp(s, "act", Rb, Rb[:, 0, :], R32, R32[:, 0, :])
                    cp(s, "pool", Rb, Rb[:, 1, :], R32, R32[:, 1, :])
        s.phase_es = old
    s_sub_barrier(s)
    if cstop < 3:
        s.end_phase()
        return

    with ExitStack() as es3:
        old = s.phase_es
        s.phase_es = es3
        gate_proj_ln(s, cx, dr, xT, HN_res, HN, 2048, w_in[:, 4096:6144], AF.Silu, dr["c_w_out"][j_], h_in, h_out,
                     dr["ln1_g"][l], dr["ln1_b"][l])
        s.phase_es = old
    s.end_phase()


NSLOT = 128
BIGIDX = 4 * 64 * 128


def phase_F(s, cx, dr, l, h_in, h_out):
    import os
    nc = s.nc
    h_in_res, h_in_ap = h_in[0], h_in[1]
    h_out_res, h_out_ap, final = h_out
    wres = dr["wres"]
    xbf_res, xbf = dr["xbf"]
    bt_res, bt = dr["buftok"]
    yb_res, yb = dr["yb"]
    fstop = int(os.environ.get("FSTOP", "9"))

    s.begin_phase()
    OH1 = s.sb([128, NT, 64], F32, "OH1")
    OH2 = s.sb([128, NT, 64], F32, "OH2")
    RK = s.sb([128, NT, 2], F32, "RK")
    GT = s.sb([128, NT, 2], F32, "GT")
    DESTi = s.sb([128, NT, 2], I32, "DESTi")
    IDXW = s.sb([128, NSLOT], I32, "IDXW")
    breg_x = nc.gpsimd.to_reg(S - 1)
    breg_w = nc.gpsimd.to_reg((l + 1) * 64 * 128 - 1)
    breg_y = nc.gpsimd.to_reg(NSLOT * 128 - 1)
    breg_t = nc.gpsimd.to_reg(NSLOT * 128 - 1)

    with ExitStack() as es1:
        old = s.phase_es
        s.phase_es = es1
        Wr = s.sb([128, 8, 72], F32, "Wr")
        ld(s, "sp", Wr, Wr[:, :, 0:8], wres, dr["r_group_w"][l].rearrange("(c p) n -> p c n", p=128))
        ld(s, "sp", Wr, Wr[:, :, 8:72], wres, dr["r_expert_w"][l].rearrange("(c p) n -> p c n", p=128))
        bias = s.sb([128, 72], F32, "rbias")
        ld(s, "sp", bias, bias[:, 0:8], wres, bcast_row(dr["r_group_b"][l], 8))
        ld(s, "sp", bias, bias[:, 8:72], wres, bcast_row(dr["r_expert_b"][l], 64))
        tri_lt = s.sb([128, 128], BF16, "trilt")
        ts(s, "dve", tri_lt, tri_lt[:, :], cx.iof, cx.iof[:, :], 0.0, None, ALU.is_gt)
        toki = s.sb([128, NT], I32, "toki")
        s.op("pool", lambda e: e.iota(toki[:, :], pattern=[[128, NT]], base=0, channel_multiplier=1), writes=[toki])
        hfs = [s.sb([128, 1024], F32, "hfr") for _ in range(2)]
        hbs = [s.sb([128, 1024], BF16, "hbr") for _ in range(2)]
        xTts = [s.sb([128, 8, 128], F32, "xTt") for _ in range(2)]
        pts = [s.ps([128, 512], F32, "ptr") for _ in range(2)]
        psL = s.ps([128, 512], F32, "psL")
        psPC = s.ps([128, 512], F32, "psPC")
        LG = s.sb([128, NT, 72], F32, "LG")
        for i in range(NT):
            hf = hfs[i % 2]
            hb = hbs[i % 2]
            xTt = xTts[i % 2]
            ld(s, "sp", hf, hf[:, :], h_in_res, h_in_ap[i * 128:(i + 1) * 128, :])
            cp(s, "pool", hb, hb[:, :], hf, hf[:, :])
            st(s, "pool", xbf_res, xbf[i * 128:(i + 1) * 128, :], hb, hb[:, :])
            for half in range(2):
                pt = pts[half]
                for c4 in range(4):
                    c = half * 4 + c4
                    s.op("pe", lambda e, pt=pt, c4=c4, c=c, hf=hf: e.transpose(
                        out=pt[:, c4 * 128:(c4 + 1) * 128], in_=hf[:, c * 128:(c + 1) * 128], identity=cx.ident_f[:, :]),
                        reads=[hf, cx.ident_f], writes=[pt])
                cp(s, "act" if half else "dve", xTt, xTt[:, half * 4:(half + 1) * 4, :], pt,
                   pt[:, :].rearrange("p (c t) -> p c t", c=4))
            for c in range(8):
                mm(s, psL, psL[:, 0:72], xTt, xTt[:, c, :], Wr, Wr[:, c, :], c == 0, c == 7)
            tt(s, "dve", LG, LG[:, i, :], psL, psL[:, 0:72], bias, bias[:, :], ALU.add)
        def t2(name, n=NT):
            return s.sb([128, n], F32, name)
        gmax, sumg, pgrp, v1, v2, dv, ex, den, rec = [t2(k) for k in ("gmax", "sumg", "pgrp", "v1", "v2", "dv", "ex", "den", "rec")]
        ohg = s.sb([128, NT, 8], F32, "ohg")
        eg = s.sb([128, NT, 8], F32, "eg")
        pen = s.sb([128, NT, 8], F32, "pen")
        em = s.sb([128, NT, 64], F32, "em")
        em2 = s.sb([128, NT, 64], F32, "em2")
        Mb = s.sb([128, NT, 64], BF16, "Mb")
        G = LG[:, :, 0:8]

        def bc2(t, w):
            return t[:, :].unsqueeze(2).to_broadcast([128, NT, w])
        s.op("dve", lambda e: e.reduce_max(out=gmax[:, :], in_=G, axis=AX.X), reads=[LG], writes=[gmax], cost=400)
        tt(s, "dve", ohg, ohg[:, :, :], LG, G, gmax, bc2(gmax, 8), ALU.is_equal)
        tt(s, "dve", eg, eg[:, :, :], LG, G, gmax, bc2(gmax, 8), ALU.subtract)
        act(s, eg, eg[:, :, :], eg, eg[:, :, :], AF.Exp)
        s.op("dve", lambda e: e.reduce_sum(out=sumg[:, :], in_=eg[:, :, :], axis=AX.X), reads=[eg], writes=[sumg], cost=400)
        s.op("dve", lambda e: e.reciprocal(out=pgrp[:, :], in_=sumg[:, :]), reads=[sumg], writes=[pgrp])
        ts(s, "dve", pen, pen[:, :, :], ohg, ohg[:, :, :], -1.0, 1e30, ALU.add, ALU.mult)
        tt(s, "dve", em, em[:, :, :].rearrange("p i (g j) -> p i g j", g=8), LG, LG[:, :, 8:72].rearrange("p i (g j) -> p i g j", g=8),
           pen, pen[:, :, :].unsqueeze(3).to_broadcast([128, NT, 8, 8]), ALU.add)
        s.op("dve", lambda e: e.reduce_max(out=v1[:, :], in_=em[:, :, :], axis=AX.X), reads=[em], writes=[v1], cost=2000)
        tt(s, "dve", OH1, OH1[:, :, :], em, em[:, :, :], v1, bc2(v1, 64), ALU.is_equal)
        stt(s, "dve", em2, em2[:, :, :], OH1, OH1[:, :, :], -1e30, em, em[:, :, :], ALU.mult, ALU.add)
        s.op("dve", lambda e: e.reduce_max(out=v2[:, :], in_=em2[:, :, :], axis=AX.X), reads=[em2], writes=[v2], cost=2000)
        tt(s, "dve", OH2, OH2[:, :, :], em2, em2[:, :, :], v2, bc2(v2, 64), ALU.is_equal)
        tt(s, "dve", dv, dv[:, :], v2, v2[:, :], v1, v1[:, :], ALU.subtract)
        act(s, ex, ex[:, :], dv, dv[:, :], AF.Exp)
        ts(s, "dve", den, den[:, :], ex, ex[:, :], 1.0, None, ALU.add)
        s.op("dve", lambda e: e.reciprocal(out=rec[:, :], in_=den[:, :]), reads=[den], writes=[rec])
        tt(s, "dve", GT, GT[:, :, 0], pgrp, pgrp[:, :], rec, rec[:, :], ALU.mult)
        tt(s, "dve", GT, GT[:, :, 1], GT, GT[:, :, 0], ex, ex[:, :], ALU.mult)
        tt(s, "dve", Mb, Mb[:, :, :], OH1, OH1[:, :, :], OH2, OH2[:, :, :], ALU.add)
        TOT = s.sb([128, NT, 64], F32, "TOT")
        TOT2 = s.sb([128, NT, 64], F32, "TOT2")
        pos = s.sb([128, NT, 64], F32, "pos")
        psP = [s.ps([128, 512], F32, "psP") for _ in range(2)]
        Mb2 = Mb[:, :, :].rearrange("p i e -> p (i e)")
        for q in range(4):
            pp = psP[q % 2]
            mm(s, pp, pp[:, :], cx.ones_b, cx.ones_b[:, :], Mb, Mb2[:, q * 512:(q + 1) * 512], True, True)
            cp(s, "act", TOT, TOT[:, q * 8:(q + 1) * 8, :], pp, pp[:, :].rearrange("p (i e) -> p i e", i=8))
        cur_, oth_ = TOT, TOT2
        for k in (1, 2, 4, 8, 16):
            cp(s, "pool", oth_, oth_[:, 0:k, :], cur_, cur_[:, 0:k, :])
            tt(s, "dve", oth_, oth_[:, k:NT, :], cur_, cur_[:, k:NT, :], cur_, cur_[:, 0:NT - k, :], ALU.add)
            cur_, oth_ = oth_, cur_
        incl = cur_
        for q in range(4):
            pp = psP[q % 2]
            for i8 in range(8):
                i = q * 8 + i8
                mm(s, pp, pp[:, i8 * 64:(i8 + 1) * 64], tri_lt, tri_lt[:, :], Mb, Mb[:, i, :], True, True)
            tt(s, "dve", pos, pos[:, q * 8:(q + 1) * 8, :], pp, pp[:, :].rearrange("p (i e) -> p i e", i=8),
               incl, incl[:, q * 8:(q + 1) * 8, :], ALU.add)
        own = s.sb([128, NT, 64], F32, "own")
        for q in range(4):
            pp = psP[q % 2]
            mm(s, pp, pp[:, :], cx.ones_b, cx.ones_b[:, :], Mb, Mb2[:, q * 512:(q + 1) * 512], True, True)
            cp(s, "act", own, own[:, q * 8:(q + 1) * 8, :], pp, pp[:, :].rearrange("p (i e) -> p i e", i=8))
        tt(s, "dve", pos, pos[:, :, :], pos, pos[:, :, :], own, own[:, :, :], ALU.subtract)
        cnt = s.sb([128, 64], F32, "cnt")
        cp(s, "dve", cnt, cnt[:, :], incl, incl[:, NT - 1, :])
        bigr = s.sb([128, NT, 64], F32, "bigr")
        for k, OH in enumerate((OH1, OH2)):
            tt(s, "dve", bigr, bigr[:, :, :], OH, OH[:, :, :], pos, pos[:, :, :], ALU.mult)
            s.op("dve", lambda e, k=k: e.reduce_sum(out=RK[:, :, k], in_=bigr[:, :, :], axis=AX.X), reads=[bigr], writes=[RK], cost=2000)
        cnti = s.sb([128, 64], I32, "cnti")
        padf = s.sb([128, 64], F32, "padf")
        pends = s.sb([128, 64], F32, "pends")
        pstart = s.sb([128, 64], F32, "pstart")
        ts(s, "dve", cnti, cnti[:, :], cnt, cnt[:, :], 127.0, None, ALU.add)
        ts(s, "dve", cnti, cnti[:, :], cnti, cnti[:, :], 7, 7, ALU.arith_shift_right, ALU.logical_shift_left)
        cp(s, "dve", padf, padf[:, :], cnti, cnti[:, :])
        s.op("dve", lambda e: e.tensor_tensor_scan(out=pends[:, :], data0=cx.ones_f[:, 0:64], data1=padf[:, :],
                                                   initial=0.0, op0=ALU.mult, op1=ALU.add),
             reads=[cx.ones_f, padf], writes=[pends])
        tt(s, "dve", pstart, pstart[:, :], pends, pends[:, :], padf, padf[:, :], ALU.subtract)
        big = s.sb([128, NT, 64], F32, "big")
        DEST = s.sb([128, NT, 2], F32, "DEST")
        for k, OH in enumerate((OH1, OH2)):
            tt(s, "dve", big, big[:, :, :], OH, OH[:, :, :], pstart, pstart[:, :].unsqueeze(1).to_broadcast([128, NT, 64]), ALU.mult)
            s.op("dve", lambda e, k=k: e.reduce_sum(out=DEST[:, :, k], in_=big[:, :, :], axis=AX.X), reads=[big], writes=[DEST])
        tt(s, "dve", DEST, DEST[:, :, :], DEST, DEST[:, :, :], RK, RK[:, :, :], ALU.add)
        cp(s, "dve", DESTi, DESTi[:, :, :], DEST, DEST[:, :, :])
        fill = s.sb([128, NSLOT], I32, "fill")
        s.op("pool", lambda e: e.iota(fill[:, :], pattern=[[0, NSLOT]], base=S, channel_multiplier=0), writes=[fill])
        btfill = Res("btfill")
        s.dma("sp", lambda e: e.dma_start(out=bt.rearrange("(p n) o -> p (n o)", p=128), in_=fill[:, :]),
              reads=[fill, bt_res], writes=[btfill], nbytes=65536)
        for i in range(NT):
            for k in range(2):
                s.dma("pool", lambda e, i=i, k=k: e.indirect_dma_start(
                    out=bt, out_offset=bass.IndirectOffsetOnAxis(ap=DESTi[:, i, k:k + 1], axis=0),
                    in_=toki[:, i:i + 1], in_offset=None, bounds_check=breg_t, oob_is_err=False),
                    reads=[DESTi, toki, btfill], writes=[], nbytes=4096)
        joinr = s.sb([128, 1], F32, "joinr")
        s.op("pool", lambda e: e.memset(joinr[:, :], 0.0), reads=[], writes=[btfill, joinr, bt_res])
        slotoff = s.sb([128, NSLOT], F32, "slotoff")
        sloti = s.sb([128, NSLOT], I32, "sloti")
        s.op("pool", lambda e: e.iota(sloti[:, :], pattern=[[128, NSLOT]], base=0, channel_multiplier=0), writes=[sloti])
        cp(s, "dve", slotoff, slotoff[:, :], sloti, sloti[:, :])
        blk = s.sb([128, NSLOT], F32, "blk")
        cmp_ = s.sb([128, 32, 64], F32, "cmp")
        for q in range(NSLOT // 32):
            tt(s, "dve", cmp_, cmp_[:, :, :], pends, pends[:, :].unsqueeze(1).to_broadcast([128, 32, 64]),
               slotoff, slotoff[:, q * 32:(q + 1) * 32].unsqueeze(2).to_broadcast([128, 32, 64]), ALU.is_le)
            s.op("dve", lambda e, q=q: e.reduce_sum(out=blk[:, q * 32:(q + 1) * 32], in_=cmp_[:, :, :], axis=AX.X),
                 reads=[cmp_], writes=[blk])
        pidx = s.sb([128, 1], F32, "pidx")
        pidi = s.sb([128, 1], I32, "pidi")
        s.op("pool", lambda e: e.iota(pidi[:, :], pattern=[[0, 1]], base=0, channel_multiplier=1), writes=[pidi])
        cp(s, "dve", pidx, pidx[:, :], pidi, pidi[:, :])
        used = s.sb([128, NSLOT], F32, "used")
        ts(s, "dve", used, used[:, :], slotoff, slotoff[:, :], pends[:, 63:64], None, ALU.is_lt, extra_reads=[pends])
        ts(s, "dve", blk, blk[:, :], blk, blk[:, :], 63.0, 128.0, ALU.min, ALU.mult)
        ts(s, "dve", blk, blk[:, :], blk, blk[:, :], pidx[:, 0:1], float(l * 64 * 128 - BIGIDX), ALU.add, ALU.add, extra_reads=[pidx])
        tt(s, "dve", blk, blk[:, :], blk, blk[:, :], used, used[:, :], ALU.mult)
        ts(s, "dve", blk, blk[:, :], blk, blk[:, :], float(BIGIDX), None, ALU.add)
        cp(s, "dve", IDXW, IDXW[:, :], blk, blk[:, :])
        if "dbg" in dr:
            dbg = s.sb([128, 1024], F32, "dbg")
            s.op("dve", lambda e: e.memset(dbg[:, :], 0.0), writes=[dbg])
            cp(s, "dve", dbg, dbg[:, 0:128], IDXW, IDXW[:, :])
            cp(s, "dve", dbg, dbg[:, 128:192], cnt, cnt[:, :])
            cp(s, "dve", dbg, dbg[:, 192:256], pends, pends[:, :])
            cp(s, "dve", dbg, dbg[:, 256:320], DEST, DEST[:, :, :].rearrange("p a b -> p (a b)"))
            cp(s, "dve", dbg, dbg[:, 320:384], GT, GT[:, :, :].rearrange("p a b -> p (a b)"))
            cp(s, "dve", dbg, dbg[:, 384:448], used, used[:, 0:64])
            cp(s, "dve", dbg, dbg[:, 448:512], padf, padf[:, :])
            cp(s, "dve", dbg, dbg[:, 512:576], OH1, OH1[:, 0, :])
            cp(s, "dve", dbg, dbg[:, 576:640], OH2, OH2[:, 0, :])
            cp(s, "dve", dbg, dbg[:, 640:704], RK, RK[:, :, :].rearrange("p a b -> p (a b)"))
            st(s, "sp", wres, dr["dbg"], dbg, dbg[:, :], final=True)
        s.phase_es = old
    s_sub_barrier(s)
    if fstop < 2:
        s.end_phase()
        return

    with ExitStack() as es2:
        old = s.phase_es
        s.phase_es = es2
        wviews = [dr[k].rearrange("l e (p j) n -> (l e p) (j n)", p=128) for k in ("e_w_gate", "e_w_up", "e_w_down")]
        NSTG = 3
        stgs = [[s.sb([128, 2048], F32, f"ws{k}") for k in range(3)] for _ in range(NSTG)]
        wbs = [[s.sb([128, 2048], BF16, f"wb{k}") for k in range(3)] for _ in range(2)]
        idxs = [s.sb([128, 1], I32, "idxb") for _ in range(3)]
        xgs = [s.sb([128, 1024], BF16, "xg") for _ in range(3)]
        for xg in xgs:
            s.op("pool", lambda e, xg=xg: e.memset(xg[:, :], 0.0), writes=[xg])
        xgTs = [s.sb([128, 8, 128], BF16, "xgT") for _ in range(2)]
        sgs = [s.sb([128, 256], F32, "sg") for _ in range(2)]
        aTs = [s.sb([128, 2, 128], BF16, "aT") for _ in range(2)]
        yos = [s.sb([128, 1024], F32, "yo") for _ in range(2)]
        psT = [s.ps([128, 1024], BF16, "psT") for _ in range(2)]
        psGs = [s.ps([128, 512], F32, "psG") for _ in range(2)]
        psUs = [s.ps([128, 512], F32, "psU") for _ in range(2)]
        psY = [s.ps([128, 512], F32, "psY") for _ in range(2)]
        nslot = int(os.environ.get("FSLOTS", str(NSLOT)))

        def loads(b):
            ix = idxs[b % 3]
            ld(s, "sp", ix, ix[:, :], bt_res, bt[b * 128:(b + 1) * 128, :])
            xg = xgs[b % 3]
            s.dma("pool", lambda e: e.indirect_dma_start(
                out=xg[:, :], out_offset=None, in_=xbf, in_offset=bass.IndirectOffsetOnAxis(ap=ix[:, 0:1], axis=0),
                bounds_check=breg_x, oob_is_err=False), reads=[ix, xbf_res], writes=[xg])
            for k in range(3):
                stg = stgs[b % NSTG][k]
                s.dma("pool", lambda e, stg=stg, k=k: e.indirect_dma_start(
                    out=stg[:, :], out_offset=None, in_=wviews[k],
                    in_offset=bass.IndirectOffsetOnAxis(ap=IDXW[:, b:b + 1], axis=0),
                    bounds_check=breg_w, oob_is_err=False), reads=[IDXW, wres], writes=[stg], nbytes=1 << 20)

        loads(0)
        loads(1)
        for b in range(nslot):
            if b + 2 < nslot:
                loads(b + 2)
            xg = xgs[b % 3]
            xgT = xgTs[b % 2]
            wg, wu, wd = wbs[b % 2]
            sg = sgs[b % 2]
            aT = aTs[b % 2]
            yo = yos[b % 2]
            pT = psT[b % 2]
            psG = psGs[b % 2]
            psU = psUs[b % 2]
            for k, eng in enumerate(("act", "dve", "act")):
                cp(s, eng, wbs[b % 2][k], wbs[b % 2][k][:, :], stgs[b % NSTG][k], stgs[b % NSTG][k][:, :])
            for j in range(8):
                s.op("pe", lambda e, j=j, pT=pT, xg=xg: e.transpose(out=pT[:, j * 128:(j + 1) * 128], in_=xg[:, j:1024:8],
                                                      identity=cx.ident_b[:, :]), reads=[xg, cx.ident_b], writes=[pT])
            cp(s, "dve", xgT, xgT[:, :, :], pT, pT[:, :].rearrange("p (j t) -> p j t", j=8))
            wgv = wg[:, :].rearrange("p (j n) -> p j n", j=8)
            wuv = wu[:, :].rearrange("p (j n) -> p j n", j=8)
            wdv = wd[:, :].rearrange("p (j n) -> p j n", j=2)
            for pS_, wv_ in ((psG, wgv), (psU, wuv)):
                for jh in range(2):
                    for j in range(8):
                        mm(s, pS_, pS_[:, jh * 128:(jh + 1) * 128], wbs[b % 2][0 if pS_ is psG else 1], wv_[:, j, jh:256:2],
                           xgT, xgT[:, j, :], j == 0, j == 7)
            act(s, sg, sg[:, :], psG, psG[:, 0:256], AF.Silu)
            tt(s, "dve", aT, aT[:, :, :].rearrange("p a t -> p (a t)"), sg, sg[:, :], psU, psU[:, 0:256], ALU.mult)
            for half in range(2):
                for jh in range(2):
                    mm(s, psY[half], psY[half][:, :], aT, aT[:, jh, :], wd, wdv[:, jh, half * 512:(half + 1) * 512], jh == 0, jh == 1)
            cp(s, "act", yo, yo[:, 0:512], psY[0], psY[0][:, :])
            cp(s, "dve", yo, yo[:, 512:1024], psY[1], psY[1][:, :])
            st(s, "sp", yb_res, yb[b * 128:(b + 1) * 128, :], yo, yo[:, :])
        s.phase_es = old
    s_sub_barrier(s)
    if fstop < 3:
        s.end_phase()
        return

    with ExitStack() as es3:
        old = s.phase_es
        s.phase_es = es3
        g_bc = s.sb([128, 1024], F32, "gbc")
        b_bc = s.sb([128, 1024], F32, "bbc")
        ld(s, "sp", g_bc, g_bc[:, :], wres, bcast_row(dr["ln2_g"][l], 1024))
        ld(s, "sp", b_bc, b_bc[:, :], wres, bcast_row(dr["ln2_b"][l], 1024))
        y1s = [s.sb([128, 1024], F32, "y1") for _ in range(2)]
        y2s = [s.sb([128, 1024], F32, "y2") for _ in range(2)]
        hfs = [s.sb([128, 1024], F32, "hf3") for _ in range(2)]
        hos = [s.sb([128, 1024], F32, "ho3") for _ in range(2)]
        wk = ln_work(s)
        for i in range(NT):
            y1, y2, hf, ho = y1s[i % 2], y2s[i % 2], hfs[i % 2], hos[i % 2]
            for k, yk in enumerate((y1, y2)):
                s.dma("pool", lambda e, yk=yk, k=k, i=i: e.indirect_dma_start(
                    out=yk[:, :], out_offset=None, in_=yb, in_offset=bass.IndirectOffsetOnAxis(ap=DESTi[:, i, k:k + 1], axis=0),
                    bounds_check=breg_y, oob_is_err=False), reads=[DESTi, yb_res], writes=[yk])
            ld(s, "sp", hf, hf[:, :], h_in_res, h_in_ap[i * 128:(i + 1) * 128, :])
            act(s, y2, y2[:, :], y2, y2[:, :], AF.Copy, scale=GT[:, i, 1:2], extra_reads=[GT])
            stt(s, "dve", y1, y1[:, :], y1, y1[:, :], GT[:, i, 0:1], y2, y2[:, :], ALU.mult, ALU.add, extra_reads=[GT])
            ln_epilogue(s, cx, [(y1, y1[:, 0:512]), (y1, y1[:, 512:1024])], hf, g_bc, b_bc, wk, ho)
            st(s, "pool", h_out_res, h_out_ap[i * 128:(i + 1) * 128, :], ho, ho[:, :], final=final)
        s.phase_es = old
    s.end_phase()


def pack_experts(g, u, d):
    g = np.asarray(g, dtype=np.float32).reshape(4, 64, 128, 2048)
    u = np.asarray(u, dtype=np.float32).reshape(4, 64, 128, 2048)
    d = np.asarray(d, dtype=np.float32).reshape(4, 64, 128, 2048)
    return np.ascontiguousarray(np.stack([g, u, d], axis=3)).reshape(4 * 64 * 128, 3 * 2048)


def rope_consts_np():
    rc = np.zeros((128, 4), np.float32)
    inv_a = 500000.0 ** (-np.arange(0, 16, 2, dtype=np.float32) / 16.0)
    for hh in range(2):
        for jx in range(16):
            rc[hh * 64 + jx, 0] = inv_a[jx % 8]
            rc[hh * 64 + jx, 1] = -1.0 if jx < 8 else 1.0
    inv_c = 10000.0 ** (-np.arange(0, 256, 2, dtype=np.float32) / 256.0)
    rc[:, 2] = inv_c
    return rc


IN_SPECS = [
    ("x", [S, D], F32), ("positions", [S], I32),
    ("ln1_g", [4, D], F32), ("ln1_b", [4, D], F32), ("ln2_g", [4, D], F32), ("ln2_b", [4, D], F32),
    ("a_w_in", [2, D, 3072], F32), ("a_w_out", [2, D, D], F32),
    ("b_w_in", [1, D, 3088], F32), ("b_gate_bias", [1, 16], F32), ("b_conv_w", [1, 4, D], F32),
    ("b_conv_b", [1, D], F32), ("b_norm_g", [1, D], F32), ("b_w_out", [1, D, D], F32),
    ("c_w_in", [1, D, 6144], F32), ("c_norm_g", [1, 2 * D], F32), ("c_w_out", [1, 2 * D, D], F32),
    ("r_group_w", [4, D, 8], F32), ("r_group_b", [4, 8], F32), ("r_expert_w", [4, D, 64], F32),
    ("r_expert_b", [4, 64], F32), ("e_w_gate", [4, 64, D, 256], F32), ("e_w_up", [4, 64, D, 256], F32),
    ("e_w_down", [4, 64, 256, D], F32), ("rope_consts", [128, 4], F32),
]


def build(plan, used_inputs=None):
    nc = bass.Bass("TRN2", target_bir_lowering=False)
    dr = {}
    for name, shape, dt in IN_SPECS:
        if used_inputs is not None and name not in used_inputs:
            continue
        dr[name] = nc.dram_tensor(name, shape, dt, kind="ExternalInput").ap()
    out = nc.dram_tensor("out", [S, D], F32, kind="ExternalOutput").ap()
    import os
    if os.environ.get("KDBG"):
        dr["dbg"] = nc.dram_tensor("dbg", [128, 1024], F32, kind="ExternalOutput").ap()
    dr["wres"] = Res("weights")
    dr["qkT"] = (Res("qkT"), nc.dram_tensor("qkT_s", [2, D, S], BF16).ap())
    dr["vsc"] = (Res("vsc"), nc.dram_tensor("vsc_s", [2, S, 128], BF16).ap())
    dr["attT"] = (Res("attT"), nc.dram_tensor("attT_s", [2 * D, S], BF16).ap())
    dr["hn"] = (Res("hn"), nc.dram_tensor("hn_s", [S, 2 * D], F32).ap())
    dr["xbf"] = (Res("xbf"), nc.dram_tensor("xbf_s", [S, D], BF16).ap())
    dr["buftok"] = (Res("buftok"), nc.dram_tensor("buftok_s", [NSLOT * 128, 1], I32).ap())
    dr["yb"] = (Res("yb"), nc.dram_tensor("yb_s", [NSLOT * 128, D], F32).ap())
    hbuf = [(Res("hA"), nc.dram_tensor("hA_s", [S, D], F32).ap(), False),
            (Res("hB"), nc.dram_tensor("hB_s", [S, D], F32).ap(), False)]
    with ExitStack() as es:
        s = Sched(nc, es)
        cx = Ctx()
        make_consts(s, cx)
        cur = (dr["wres"], dr["x"], False)
        for pi, (ph, l) in enumerate(plan):
            last = pi == len(plan) - 1
            nxt = (Res("out"), out, True) if last else hbuf[pi % 2]
            if ph == "A":
                wsave = s.window
                s.window = min(48, wsave)
                phase_A(s, cx, dr, l, l // 3, cur, nxt)
                s.window = wsave
            elif ph == "B":
                phase_B(s, cx, dr, l, l // 3, cur, nxt)
            elif ph == "C":
                phase_C(s, cx, dr, l, l // 3, cur, nxt)
            elif ph == "F":
                phase_F(s, cx, dr, l, cur, nxt)
            else:
                raise ValueError(ph)
            cur = nxt
        s.finish()
        print("instructions:", s.ninst, "sems:", s.n_sem)
    return nc


PLAN = [("A", 0), ("F", 0), ("B", 1), ("F", 1), ("C", 2), ("F", 2), ("A", 3), ("F", 3)]
_NC_CACHE = {}


def kernel(**inputs):
    x = np.ascontiguousarray(np.asarray(inputs["x"], dtype=np.float32))
    nb = x.shape[0]
    if "nc" not in _NC_CACHE:
        _NC_CACHE["nc"] = build(PLAN)
    nc = _NC_CACHE["nc"]
    shared = {}
    for name, shape, dt in IN_SPECS:
        if name == "x":
            continue
        if name == "rope_consts":
            shared[name] = rope_consts_np()

        elif name == "positions":
            shared[name] = np.ascontiguousarray(np.asarray(inputs[name]).astype(np.int32))
        else:
            shared[name] = np.ascontiguousarray(np.asarray(inputs[name], dtype=np.float32))
    in_maps = []
    for b in range(nb):
        m = dict(shared)
        m["x"] = x[b]
        in_maps.append(m)
    res = run_bass_kernel_spmd(nc, in_maps, core_ids=list(range(nb)))
    return np.stack([np.asarray(r["out"], dtype=np.float32) for r in res.results], axis=0)
```
